# Optimizing a Trainium2 kernel written in Bass

```python
import jax, jax.numpy as jnp
from jax import lax
import numpy as np

D_MODEL = 1024
BATCH = 16
SEQ = 2048
DEPTH = 2

CTX_LEN = 256
GRID_W = 64
F32 = jnp.float32
N_REC = (DEPTH + 1) // 2
N_ATT = DEPTH // 2
N_MOD = 6
NORM_EPS = 1e-6
NEG_INF = -1e30

A_HEADS = 8
A_HD = 64
A_W = A_HEADS * A_HD
DECAY_LORA = 64
AAA_LORA = 64
GATE_LORA = 128
RWKV_COLS = 3 * A_W + DECAY_LORA + AAA_LORA + GATE_LORA
RWKV_SPLITS = (A_W, 2 * A_W, 3 * A_W, 3 * A_W + DECAY_LORA, 3 * A_W + DECAY_LORA + AAA_LORA)
GN_EPS = 64e-5

B_HEADS = 4
B_DK = 128
B_DV = 128
B_W = B_HEADS * B_DK
HGRN_COLS = 5 * B_W
HGRN_CHUNK = 64

REC_IN_COLS = RWKV_COLS + HGRN_COLS
D_MIX = A_W + B_HEADS * B_DV

HQ = 16
HKV = 4
GQ = HQ // HKV
HD = 64
WINDOW = 128
ATT_BLOCK = 128
AX_DIM = HD // 2
ROPE_BASE = 10000.0
ATT_COLS = HQ * HD + 2 * HKV * HD

D_FF = 2816
N_EXPERTS = 8
TOP_K = 2
D_FF_E = 2816
MOE_BLOCK = 128

kernel_name = "hybrid_rwkv7_hgrn2_swa_moe_diffusion_trunk"


def rmsnorm(x, g):
    xf = x.astype(F32)
    y = xf * lax.rsqrt(jnp.mean(xf * xf, axis=-1, keepdims=True) + NORM_EPS)
    return (y * g.astype(F32)).astype(x.dtype)


def modulate(x, g, shift, scale):
    return rmsnorm(x, g) * (1 + scale) + shift


def adaln(cvec, w, b):
    return jnp.split(jax.nn.silu(cvec) @ w + b, N_MOD, axis=-1)


def centred_shift(t):
    tp = jnp.pad(t, ((0, 0), (1, 1), (0, 0)))
    return 0.5 * (tp[:, :-2] + tp[:, 2:])


def swiglu(h, wg, wu, wd):
    return (jax.nn.silu(h @ wg) * (h @ wu)) @ wd


def axial_rope(T):
    rows = T // GRID_W
    row = jnp.repeat(jnp.arange(rows, dtype=F32), GRID_W)
    col = jnp.tile(jnp.arange(GRID_W, dtype=F32), rows)
    inv = ROPE_BASE ** (-jnp.arange(0, AX_DIM, 2, dtype=F32) / AX_DIM)
    ang = jnp.stack([row[:, None] * inv, col[:, None] * inv], axis=1)
    return jnp.cos(ang), jnp.sin(ang)


def apply_rope(t, cos, sin):
    B, T, H, _ = t.shape
    tr = t.astype(F32).reshape(B, T, H, 2, 2, AX_DIM // 2)
    t1, t2 = tr[..., 0, :], tr[..., 1, :]
    c, s = cos[None, :, None], sin[None, :, None]
    out = jnp.stack([t1 * c - t2 * s, t2 * c + t1 * s], axis=-2)
    return out.reshape(B, T, H, HD).astype(t.dtype)


def heads_a(t):
    return t.reshape(t.shape[:-1] + (A_HEADS, A_HD))


def rwkv_features(p, mu, w0, w_up, a0, a_up, g_up, k_k, k_a):
    p = p + mu * (centred_shift(p) - p)
    r, k, v, wd, ad, gd = jnp.split(p, RWKV_SPLITS, axis=-1)
    w_log = -jax.nn.softplus(-(w0[:, None, None, :] + jnp.einsum('btr,drc->dbtc', jnp.tanh(wd), w_up))) - 0.5
    decay = heads_a(jnp.exp(-jnp.exp(w_log)))
    a = jax.nn.sigmoid(a0 + ad @ a_up)
    g = jax.nn.sigmoid(gd) @ g_up
    kk = heads_a(k * k_k)
    kk = kk / jnp.maximum(jnp.sqrt(jnp.sum(kk * kk, axis=-1, keepdims=True)), 1e-12)
    k = k * (1 + (a - 1) * k_a)
    return heads_a(r), heads_a(k), heads_a(v), kk, heads_a(a), decay, g


def rwkv_scan(feats, direction, s0, reverse):
    r, k, v, kk, a, decay, _ = feats
    xs = tuple(jnp.moveaxis(t, 1, 0) for t in (r, decay[direction], k, v, kk, kk * a))

    def step(S, inp):
        r_t, w_t, k_t, v_t, kk_t, b_t = inp
        S = (S * w_t[:, :, None, :]
             - jnp.einsum('bhvk,bhk->bhv', S, kk_t)[..., None] * b_t[:, :, None, :]
             + v_t[..., None] * k_t[:, :, None, :])
        return S, jnp.einsum('bhvk,bhk->bhv', S, r_t)

    s_fin, o = lax.scan(step, s0, xs, reverse=reverse)
    return jnp.moveaxis(o, 0, 1), s_fin


def rwkv_post(o, feats, r_k, ln_w, ln_b):
    r, k, v, _, _, _, g = feats
    mean = jnp.mean(o, axis=-1, keepdims=True)
    var = jnp.mean(jnp.square(o - mean), axis=-1, keepdims=True)
    on = (o - mean) * lax.rsqrt(var + GN_EPS)
    bonus = jnp.sum(r * k * r_k, axis=-1, keepdims=True) * v
    B, T = o.shape[:2]
    return (on.reshape(B, T, A_W) * ln_w + ln_b + bonus.reshape(B, T, A_W)) * g


def hgrn_features(p, lb):
    q, i, f_raw, gate = jnp.split(p, (B_W, 2 * B_W, 4 * B_W), axis=-1)
    B, T = p.shape[:2]
    f_raw = jnp.moveaxis(f_raw.reshape(B, T, 2, B_W), 2, 0)
    lbb = lb[:, None, None, :]
    f = lbb + (1 - lbb) * jax.nn.sigmoid(f_raw)
    hk = lambda t: t.reshape(t.shape[:-1] + (B_HEADS, B_DK))
    return hk(jax.nn.silu(q)), i.reshape(B, T, B_HEADS, B_DV), hk(1 - f), hk(jnp.log(f)), gate


def hgrn2_chunked(q, k, v, logf, s0):
    B, T, H, _ = q.shape
    L = HGRN_CHUNK
    n = T // L
    ch = lambda t: t.reshape(B, n, L, H, t.shape[-1])
    q, k, v, logf = ch(q), ch(k), ch(v), ch(logf)
    G = jnp.cumsum(logf, axis=2)
    Gref = G[:, :, L // 2 - 1:L // 2]
    att = jnp.einsum('bnthk,bnshk->bnhts', q * jnp.exp(G - Gref), k * jnp.exp(Gref - G))
    att = jnp.where(jnp.tril(jnp.ones((L, L), bool)), att, 0.0)
    o_intra = jnp.einsum('bnhts,bnshv->bnthv', att, v)
    G_end = G[:, :, -1]
    kv = jnp.einsum('bnshk,bnshv->bnhkv', k * jnp.exp(G_end[:, :, None] - G), v)

    def step(S, inp):
        dec, kv_c = inp
        return dec[..., None] * S + kv_c, S

    s_fin, s_start = lax.scan(step, s0, (jnp.exp(G_end).swapaxes(0, 1), kv.swapaxes(0, 1)))
    o_inter = jnp.einsum('bnthk,nbhkv->bnthv', q * jnp.exp(G), s_start)
    return (o_intra + o_inter).reshape(B, T, H, v.shape[-1]), s_fin


def hgrn_post(o, gate, g_norm):
    B, T = o.shape[:2]
    return rmsnorm(o, g_norm).reshape(B, T, B_HEADS * B_DV) * jax.nn.silu(gate)


def flip(t):
    return t[:, ::-1]


def rec_mixer(h_ctx, h_lat, w_in, w_out, mu, w0, w_up, a0, a_up, g_up, k_k, k_a, r_k, ln_w, ln_b, lb, hg_norm):
    B = h_lat.shape[0]
    pc = (h_ctx @ w_in).astype(F32)
    pl = (h_lat @ w_in).astype(F32)
    fa_c = rwkv_features(pc[..., :RWKV_COLS], mu, w0, w_up, a0, a_up, g_up, k_k, k_a)
    fa_l = rwkv_features(pl[..., :RWKV_COLS], mu, w0, w_up, a0, a_up, g_up, k_k, k_a)
    s0a = jnp.zeros((B, A_HEADS, A_HD, A_HD), F32)
    oac_f, sac_f = rwkv_scan(fa_c, 0, s0a, False)
    oac_b, sac_b = rwkv_scan(fa_c, 1, s0a, True)
    oal_f, _ = rwkv_scan(fa_l, 0, sac_f, False)
    oal_b, _ = rwkv_scan(fa_l, 1, sac_b, True)
    ya_c = rwkv_post(oac_f + oac_b, fa_c, r_k, ln_w, ln_b)
    ya_l = rwkv_post(oal_f + oal_b, fa_l, r_k, ln_w, ln_b)
    qc, ic, kc, lfc, gc = hgrn_features(pc[..., RWKV_COLS:], lb)
    ql, il, kl, lfl, gl = hgrn_features(pl[..., RWKV_COLS:], lb)
    s0b = jnp.zeros((B, B_HEADS, B_DK, B_DV), F32)
    obc_f, sbc_f = hgrn2_chunked(qc, kc[0], ic, lfc[0], s0b)
    obc_b, sbc_b = hgrn2_chunked(flip(qc), flip(kc[1]), flip(ic), flip(lfc[1]), s0b)
    obl_f, _ = hgrn2_chunked(ql, kl[0], il, lfl[0], sbc_f)
    obl_b, _ = hgrn2_chunked(flip(ql), flip(kl[1]), flip(il), flip(lfl[1]), sbc_b)
    yb_c = hgrn_post(obc_f + flip(obc_b), gc, hg_norm)
    yb_l = hgrn_post(obl_f + flip(obl_b), gl, hg_norm)
    y_c = jnp.concatenate([ya_c, yb_c], axis=-1).astype(h_ctx.dtype) @ w_out
    y_l = jnp.concatenate([ya_l, yb_l], axis=-1).astype(h_lat.dtype) @ w_out
    return y_c, y_l


def window_attn_mixer(h_ctx, h_lat, w_in, w_out, sink, cos, sin):
    B, T, _ = h_lat.shape
    q, k, v = jnp.split(h_lat @ w_in, (HQ * HD, HQ * HD + HKV * HD), axis=-1)
    q = apply_rope(q.reshape(B, T, HQ, HD), cos, sin)
    k = apply_rope(k.reshape(B, T, HKV, HD), cos, sin)
    v = v.reshape(B, T, HKV, HD)
    kc, vc = jnp.split(h_ctx @ w_in[:, HQ * HD:], 2, axis=-1)
    C = h_ctx.shape[1]
    kc = kc.reshape(B, C, HKV, HD)
    vc = vc.reshape(B, C, HKV, HD)
    nb = T // ATT_BLOCK
    qb = q.reshape(B, nb, ATT_BLOCK, HKV, GQ, HD).swapaxes(0, 1)

    def band(t):
        tp = jnp.pad(t, ((0, 0), (ATT_BLOCK, ATT_BLOCK), (0, 0), (0, 0))).reshape(B, nb + 2, ATT_BLOCK, HKV, HD)
        return jnp.concatenate([tp[:, :-2], tp[:, 1:-1], tp[:, 2:]], axis=2).swapaxes(0, 1)

    kb, vb = band(k), band(v)
    qpos = jnp.arange(ATT_BLOCK)
    kpos = jnp.arange(3 * ATT_BLOCK) - ATT_BLOCK
    rel_ok = jnp.abs(qpos[:, None] - kpos[None, :]) <= WINDOW
    sink_l = sink.astype(F32).reshape(HKV, GQ)[None, :, :, None, None]
    scale = HD ** -0.5

    def block(args):
        i, qi, ki, vi = args
        kabs = i * ATT_BLOCK + kpos
        valid = rel_ok & ((kabs >= 0) & (kabs < T))[None, :]
        s_lat = jnp.einsum('bqhgd,bkhd->bhgqk', qi, ki).astype(F32) * scale
        s_lat = jnp.where(valid, s_lat, NEG_INF)
        s_ctx = jnp.einsum('bqhgd,bkhd->bhgqk', qi, kc).astype(F32) * scale
        sk = jnp.broadcast_to(sink_l, s_lat.shape[:-1] + (1,))
        pr = jax.nn.softmax(jnp.concatenate([s_ctx, s_lat, sk], axis=-1), axis=-1)
        return (jnp.einsum('bhgqk,bkhd->bqhgd', pr[..., :C].astype(vc.dtype), vc)
                + jnp.einsum('bhgqk,bkhd->bqhgd', pr[..., C:C + 3 * ATT_BLOCK].astype(vi.dtype), vi))

    o = lax.map(block, (jnp.arange(nb), qb, kb, vb))
    return o.swapaxes(0, 1).reshape(B, T, HQ * HD) @ w_out


def moe_swiglu(h, router_w, router_b, w_gate, w_up, w_down):
    B, T, D = h.shape
    hf = h.reshape(B * T, D)
    m = hf.shape[0] * TOP_K
    probs = jax.nn.softmax((hf @ router_w + router_b).astype(F32), axis=-1)
    top_p, top_e = lax.top_k(probs, TOP_K)
    top_p = top_p / jnp.sum(top_p, axis=-1, keepdims=True)
    eid = top_e.reshape(m)
    order = jnp.argsort(eid)
    e_sorted = eid[order]
    tok = order // TOP_K
    wt = top_p.reshape(m)[order]
    counts = jnp.bincount(eid, length=N_EXPERTS)
    padded = (counts + MOE_BLOCK - 1) // MOE_BLOCK * MOE_BLOCK
    pad_end = jnp.cumsum(padded)
    pad_start = pad_end - padded
    start = jnp.cumsum(counts) - counts
    dest = pad_start[e_sorted] + (jnp.arange(m) - start[e_sorted])
    n_blocks = m // MOE_BLOCK + N_EXPERTS
    buf = jnp.zeros((n_blocks * MOE_BLOCK, D), h.dtype).at[dest].set(hf[tok])
    block_e = jnp.minimum(jnp.searchsorted(pad_end, jnp.arange(n_blocks) * MOE_BLOCK, side='right'), N_EXPERTS - 1)

    def expert_block(args):
        e, xb = args
        return (jax.nn.silu(xb @ w_gate[e]) * (xb @ w_up[e])) @ w_down[e]

    yb = lax.map(expert_block, (block_e, buf.reshape(n_blocks, MOE_BLOCK, D)))
    y = yb.reshape(-1, D)[dest] * wt[:, None].astype(h.dtype)
    return jnp.zeros_like(hf).at[tok].add(y).reshape(B, T, D)


def setup_inputs(seed: int = 0) -> dict:
    key = jax.random.key(seed)
    ks = iter(jax.random.split(key, 40))
    D = D_MODEL

    def nrm(shape, scale):
        return jax.random.normal(next(ks), shape, F32) * scale

    def gain(shape):
        return 1.0 + nrm(shape, 0.02)

    inp = {}
    inp["x"] = nrm((BATCH, SEQ, D), 1.0)
    inp["c"] = nrm((BATCH, D), 1.0)
    inp["ctx"] = nrm((BATCH, CTX_LEN, D), 1.0)
    inp["c_ctx"] = nrm((D,), 1.0)
    inp["mod_w"] = nrm((DEPTH, D, N_MOD * D), 0.5 * D ** -0.5)
    inp["mod_b"] = nrm((DEPTH, N_MOD * D), 0.01)
    inp["norm_mix"] = gain((DEPTH, D))
    inp["norm_ffn"] = gain((DEPTH, D))
    inp["norm_final"] = gain((D,))
    inp["rec_w_in"] = nrm((N_REC, D, REC_IN_COLS), D ** -0.5)
    inp["rec_w_out"] = nrm((N_REC, D_MIX, D), D_MIX ** -0.5)
    inp["rwkv_mu"] = jax.random.uniform(next(ks), (N_REC, RWKV_COLS), F32)
    inp["rwkv_w0"] = jax.random.uniform(next(ks), (N_REC, 2, A_W), F32, minval=-6.0, maxval=-1.0)
    inp["rwkv_w_up"] = nrm((N_REC, 2, DECAY_LORA, A_W), 0.5 * DECAY_LORA ** -0.5)
    inp["rwkv_a0"] = nrm((N_REC, A_W), 0.1)
    inp["rwkv_a_up"] = nrm((N_REC, AAA_LORA, A_W), 0.5 * AAA_LORA ** -0.5)
    inp["rwkv_g_up"] = nrm((N_REC, GATE_LORA, A_W), GATE_LORA ** -0.5)
    inp["rwkv_k_k"] = 0.85 + nrm((N_REC, A_W), 0.02)
    inp["rwkv_k_a"] = gain((N_REC, A_W))
    inp["rwkv_r_k"] = nrm((N_REC, A_HEADS, A_HD), 0.1)
    inp["rwkv_ln_w"] = gain((N_REC, A_W))
    inp["rwkv_ln_b"] = nrm((N_REC, A_W), 0.01)
    inp["hgrn_lb"] = nrm((2, N_REC + 1, B_W), 0.1)
    inp["hgrn_norm"] = gain((N_REC, B_DV))
    inp["ffn_w_gate"] = nrm((N_REC, D, D_FF), D ** -0.5)
    inp["ffn_w_up"] = nrm((N_REC, D, D_FF), D ** -0.5)
    inp["ffn_w_down"] = nrm((N_REC, D_FF, D), D_FF ** -0.5)
    inp["att_w_in"] = nrm((N_ATT, D, ATT_COLS), D ** -0.5)
    inp["att_w_out"] = nrm((N_ATT, HQ * HD, D), (HQ * HD) ** -0.5)
    inp["att_sink"] = nrm((N_ATT, HQ), 0.5)
    inp["moe_router"] = nrm((N_ATT, D, N_EXPERTS), D ** -0.5)
    inp["moe_router_b"] = nrm((N_ATT, N_EXPERTS), 0.01)
    inp["moe_w_gate"] = nrm((N_ATT, N_EXPERTS, D, D_FF_E), D ** -0.5)
    inp["moe_w_up"] = nrm((N_ATT, N_EXPERTS, D, D_FF_E), D ** -0.5)
    inp["moe_w_down"] = nrm((N_ATT, N_EXPERTS, D_FF_E, D), D_FF_E ** -0.5)
    return inp


def reference(x, c, ctx, c_ctx, mod_w, mod_b, norm_mix, norm_ffn, norm_final, rec_w_in, rec_w_out,
              rwkv_mu, rwkv_w0, rwkv_w_up, rwkv_a0, rwkv_a_up, rwkv_g_up, rwkv_k_k, rwkv_k_a, rwkv_r_k,
              rwkv_ln_w, rwkv_ln_b, hgrn_lb, hgrn_norm, ffn_w_gate, ffn_w_up, ffn_w_down,
              att_w_in, att_w_out, att_sink, moe_router, moe_router_b, moe_w_gate, moe_w_up, moe_w_down):
    T = x.shape[1]
    cos, sin = axial_rope(T)
    lb_all = jnp.cumsum(jax.nn.softmax(hgrn_lb.astype(F32), axis=1), axis=1)
    x_lat, x_ctx = x, ctx
    for l in range(DEPTH):
        j = l // 2
        m_lat = [m[:, None, :] for m in adaln(c, mod_w[l], mod_b[l])]
        m_ctx = adaln(c_ctx, mod_w[l], mod_b[l])
        h_lat = modulate(x_lat, norm_mix[l], m_lat[0], m_lat[1])
        h_ctx = modulate(x_ctx, norm_mix[l], m_ctx[0], m_ctx[1])
        if l % 2 == 0:
            y_ctx, y_lat = rec_mixer(h_ctx, h_lat, rec_w_in[j], rec_w_out[j], rwkv_mu[j], rwkv_w0[j], rwkv_w_up[j],
                                     rwkv_a0[j], rwkv_a_up[j], rwkv_g_up[j], rwkv_k_k[j], rwkv_k_a[j], rwkv_r_k[j],
                                     rwkv_ln_w[j], rwkv_ln_b[j], lb_all[:, j], hgrn_norm[j])
            x_lat = x_lat + m_lat[2] * y_lat
            x_ctx = x_ctx + m_ctx[2] * y_ctx
            f_lat = modulate(x_lat, norm_ffn[l], m_lat[3], m_lat[4])
            f_ctx = modulate(x_ctx, norm_ffn[l], m_ctx[3], m_ctx[4])
            x_lat = x_lat + m_lat[5] * swiglu(f_lat, ffn_w_gate[j], ffn_w_up[j], ffn_w_down[j])
            x_ctx = x_ctx + m_ctx[5] * swiglu(f_ctx, ffn_w_gate[j], ffn_w_up[j], ffn_w_down[j])
        else:
            y_lat = window_attn_mixer(h_ctx, h_lat, att_w_in[j], att_w_out[j], att_sink[j], cos, sin)
            x_lat = x_lat + m_lat[2] * y_lat
            f_lat = modulate(x_lat, norm_ffn[l], m_lat[3], m_lat[4])
            x_lat = x_lat + m_lat[5] * moe_swiglu(f_lat, moe_router[j], moe_router_b[j],
                                                  moe_w_gate[j], moe_w_up[j], moe_w_down[j])
    return rmsnorm(x_lat, norm_final)
```

```python
import contextlib
import os
import numpy as np
import concourse.bass as bass
import concourse.mybir as mybir
from concourse.bass_utils import run_bass_kernel_spmd

F32 = mybir.dt.float32
BF16 = mybir.dt.bfloat16
AF = mybir.ActivationFunctionType
ALU = mybir.AluOpType
AX = mybir.AxisListType

NB = 2
TC = 256
TL = 2048
TT = TC + TL
D = 1024
W = TT + 4
C0 = float(np.exp(-0.5))


def pcol(t):
    return 1 + t if t < TC else 3 + t


class Tk:
    __slots__ = ("h", "name", "w", "r", "sem", "cnt", "psum")

    def __init__(self, h, name, psum=False):
        self.h = h
        self.name = name
        self.psum = psum
        self.w = {}
        self.r = {}
        self.sem = None
        self.cnt = 0

    def __getitem__(self, idx):
        return self.h[idx]


class Prog:
    ENGS = ("pe", "act", "dve", "pool", "sp")

    def __init__(self, nc):
        self.nc = nc
        self.q = {e: [] for e in self.ENGS}
        self.seq = {e: 0 for e in self.ENGS}
        self.sem = {e: nc.alloc_semaphore("s_" + e) for e in self.ENGS}
        self.waited = {e: {} for e in self.ENGS}
        self.n = 0
        self.uid = 0
        self._stack = None
        self._scope_tiles = []
        self.free_dsems = {}
        self.dtiles = {}

    def _nm(self, name):
        self.uid += 1
        return f"{name}_{self.uid}"

    def sb(self, name, shape, dt=F32):
        nm = self._nm(name)
        if self._stack is not None:
            h = self._stack.enter_context(self.nc.sbuf_tensor(nm, list(shape), dt))
        else:
            h = self.nc.alloc_sbuf_tensor(nm, list(shape), dt)
        t = Tk(h, nm)
        if self._scope_tiles:
            self._scope_tiles[-1].append(t)
        return t

    def ps(self, name, shape, dt=F32):
        nm = self._nm(name)
        return Tk(self.nc.alloc_psum_tensor(nm, list(shape), dt), nm, psum=True)

    def view(self, ap, name):
        return Tk(ap, self._nm(name))

    def dram(self, name, shape, dt=F32, kind="Internal"):
        return Tk(self.nc.dram_tensor(name, list(shape), dt, kind=kind), name)

    @contextlib.contextmanager
    def scope(self):
        st = contextlib.ExitStack()
        prev = self._stack
        self._stack = st
        self._scope_tiles.append([])
        try:
            yield
        finally:
            self.barrier()
            for t in self._scope_tiles.pop():
                if t.sem:
                    for e, sc in t.sem.items():
                        self.free_dsems.setdefault(e, []).append(sc)
                    self.dtiles.pop(id(t), None)
                    t.sem = None
            st.close()
            self._stack = prev

    def _wait(self, eng, tok):
        if tok[0] == "e":
            _, f, s = tok
            if f == eng and eng == "pe":
                return
            key = ("e", f)
            if self.waited[eng].get(key, 0) >= s:
                return
            self.waited[eng][key] = s
            self.q[eng].append(("w", self.sem[f], s))
        else:
            _, t, de = tok
            if not t.sem or de not in t.sem:
                return
            sem, cnt = t.sem[de]
            key = ("d", id(sem))
            if self.waited[eng].get(key, 0) >= cnt:
                return
            self.waited[eng][key] = cnt
            self.q[eng].append(("w", sem, cnt))

    def _deps(self, eng, reads, writes):
        for t in reads:
            for tok in t.w.values():
                self._wait(eng, tok)
            if t.psum:
                for tok in t.r.values():
                    if not (tok[0] == "e" and tok[1] == eng):
                        self._wait(eng, tok)
        for t in writes:
            for tok in t.w.values():
                self._wait(eng, tok)
            for tok in t.r.values():
                self._wait(eng, tok)

    @staticmethod
    def _key(tok):
        return (tok[0], tok[1]) if tok[0] == "e" else (tok[0], id(tok[1]), tok[2])

    def _commit(self, tok, reads, writes):
        k = self._key(tok)
        for t in reads:
            t.r[k] = tok
        for t in writes:
            t.w[k] = tok

    def op(self, eng, meth, reads=(), writes=(), **kw):
        self._deps(eng, reads, writes)
        self.seq[eng] += 1
        tok = ("e", eng, self.seq[eng])
        self.q[eng].append(("o", meth, kw))
        self._commit(tok, reads, writes)
        self.n += 1
        return tok

    def dma(self, eng, out_ap, in_ap, semt, reads=(), writes=(), meth="dma_start", fn=None, **kw):
        self._deps(eng, reads, writes)
        t = semt
        if t.sem is None:
            t.sem = {}
        if eng not in t.sem:
            fl = self.free_dsems.get(eng)
            if fl:
                t.sem[eng] = fl.pop()
            else:
                t.sem[eng] = [self.nc.alloc_semaphore(f"d_{eng}_{t.name}"), 0]
            self.dtiles[id(t)] = t
        t.sem[eng][1] += 16
        if fn is not None:
            self.q[eng].append(("f", fn, t.sem[eng][0]))
        elif meth == "dma_start":
            self.q[eng].append(("d", out_ap, in_ap, t.sem[eng][0], kw))
        else:
            self.q[eng].append(("m", meth, kw, t.sem[eng][0]))
        tok = ("d", t, eng)
        self._commit(tok, reads, writes)
        self.n += 1
        return tok

    def barrier(self):
        for f in self.ENGS:
            if f != "sp" and self.seq[f] > 0:
                self._wait("sp", ("e", f, self.seq[f]))
        for t in list(self.dtiles.values()):
            for de in list(t.sem.keys()):
                self._wait("sp", ("d", t, de))
        self.seq["sp"] += 1
        self.q["sp"].append(("o", "nop", {}))
        tok = ("e", "sp", self.seq["sp"])
        for e in self.ENGS:
            if e != "sp":
                self._wait(e, tok)

    def emit(self):
        nc = self.nc
        q = self.q
        sems = self.sem

        def run(e, name):
            for it in q[name]:
                if it[0] == "w":
                    e.wait_ge(it[1], it[2])
                elif it[0] == "o":
                    getattr(e, it[1])(**it[2]).then_inc(sems[name], 1)
                elif it[0] == "f":
                    it[1](e).then_inc(it[2], 16)
                elif it[0] == "m":
                    getattr(e, it[1])(**it[2]).then_inc(it[3], 16)
                else:
                    _, o, i, s, kw = it
                    e.dma_start(out=o, in_=i, **kw).then_inc(s, 16)

        with nc.Block() as block:
            @block.tensor
            def _(e):
                run(e, "pe")

            @block.scalar
            def _(e):
                run(e, "act")

            @block.vector
            def _(e):
                run(e, "dve")

            @block.gpsimd
            def _(e):
                run(e, "pool")

            @block.sync
            def _(e):
                run(e, "sp")


def mm(P, o, oap, l, lap, r, rap, start=True, stop=True):
    P.op("pe", "matmul", reads=[l, r], writes=[o], out=oap, lhsT=lap, rhs=rap, start=start, stop=stop)


INPUT_SPECS = {
    "xin": [NB, TT, D], "cT": [128, 8, 3], "mod_w": [2, D, 6 * D], "mod_b": [2, 128, 48],
    "norm_mix": [2, 128, 8], "norm_ffn": [2, 128, 8], "norm_final": [1, D], "ident": [128, 128],
    "rec_w_in": [D, 4352], "rec_w_out": [D, D],
    "rw_vec": [128, 4, 12], "rw_mu": [128, 14], "rw_w_up": [2, 64, 512], "rw_a_up": [64, 512], "rw_g_up": [128, 512],
    "blk64": [128, 128], "masks": [128, 4, 128], "scanmask": [128, W],
    "hg_lb": [2, 2, 128, 4], "hg_norm": [1, 128],
    "ffn_wg": [D, 2816], "ffn_wu": [D, 2816], "ffn_wd": [2816, D],
    "att_w_in": [D, 1536], "att_w_out": [D, D], "att_sink": [1, 16],
    "rope": [128, 2, TL], "perm": [128, 128],
    "moe_router": [D, 8], "moe_router_b": [1, 8],
    "moe_consts": [128, 512], "norm_ffn_row": [2, D],
    "moe_wg": [2048, 11264], "moe_wu": [2048, 11264], "moe_wd": [2048, 11264],
}


class Ctx:
    def __init__(self, P):
        self.__dict__["P"] = P
        self.__dict__["used_inputs"] = []

    def __getattr__(self, name):
        if name in INPUT_SPECS:
            t = self.P.dram(name, INPUT_SPECS[name], F32, kind="ExternalInput")
            self.__dict__[name] = t
            self.used_inputs.append(name)
            return t
        raise AttributeError(name)


def setup_globals(P, G):
    G.bank = [P.ps(f"bank{i}", [128, 512], F32) for i in range(8)]
    G.identf = P.sb("identf", [128, 128], F32)
    P.dma("sp", G.identf[:], G.ident[:], G.identf, reads=[G.ident], writes=[G.identf])
    G.identb = P.sb("identb", [128, 128], BF16)
    P.op("dve", "tensor_copy", out=G.identb[:], in_=G.identf[:], reads=[G.identf], writes=[G.identb])
    G.eps = P.sb("eps", [128, 1], F32)
    P.op("dve", "memset", ap=G.eps[:], constant=1e-6, writes=[G.eps])
    G.WTm = P.sb("WTm", [128, 32, 8], F32)
    G.LG = P.sb("LGp", [128, 32, 8], F32)
    G.modT = P.sb("modT", [128, 2, 48, 3], F32)
    G.GS = P.sb("GS", [128, 2, 2, 8, 3], F32)


def stage_adaln(P, G):
    bank = G.bank
    with P.scope():
        cT = P.sb("cTs", [128, 8, 3])
        P.dma("sp", cT[:], G.cT[:], cT, reads=[G.cT], writes=[cT])
        sc = P.sb("sc", [128, 8, 3])
        P.op("act", "activation", out=sc[:], in_=cT[:], func=AF.Silu, reads=[cT], writes=[sc])
        mb = P.sb("mb", [128, 2, 48])
        P.dma("sp", mb[:], G.mod_b[:].rearrange("l p j -> p l j"), mb, reads=[G.mod_b], writes=[mb])
        gm = P.sb("gm", [128, 2, 2, 8])
        P.dma("sp", gm[:, :, 0, :], G.norm_mix[:].rearrange("l p k -> p l k"), gm, reads=[G.norm_mix], writes=[gm])
        P.dma("sp", gm[:, :, 1, :], G.norm_ffn[:].rearrange("l p k -> p l k"), gm, reads=[G.norm_ffn], writes=[gm])
        wts = [P.sb(f"mw{i}", [128, 8, 768]) for i in range(2)]
        it = 0
        for l in range(2):
            for g in range(8):
                wt = wts[it % 2]
                for k in range(8):
                    P.dma("sp" if k % 2 == 0 else "pool", wt[:, k, :],
                          G.mod_w[l, k * 128:(k + 1) * 128, g * 768:(g + 1) * 768], wt,
                          reads=[G.mod_w], writes=[wt])
                for jj in range(6):
                    j = g * 6 + jj
                    ps = bank[j % 2]
                    for k in range(8):
                        mm(P, ps, ps[:, 0:3], wt, wt[:, k, jj * 128:(jj + 1) * 128], sc, sc[:, k, :],
                           start=(k == 0), stop=(k == 7))
                    P.op("act", "activation", out=G.modT[:, l, j, :], in_=ps[:, 0:3], func=AF.Identity,
                         bias=mb[:, l, j:j + 1], reads=[ps, mb], writes=[G.modT])
                it += 1
        for l in range(2):
            for kind in range(2):
                wh = 1 + 3 * kind
                P.op("dve", "tensor_scalar", out=G.GS[:, l, kind, :, :], in0=G.modT[:, l, wh * 8:(wh + 1) * 8, :],
                     scalar1=1.0, scalar2=None, op0=ALU.add, reads=[G.modT], writes=[G.GS])
                P.op("dve", "tensor_tensor", out=G.GS[:, l, kind, :, :], in0=G.GS[:, l, kind, :, :],
                     in1=gm[:, l, kind, :].unsqueeze(2).broadcast_to([128, 8, 3]), op=ALU.mult,
                     reads=[G.GS, gm], writes=[G.GS])
        tr = P.sb("mtr", [48, 128])
        for l in range(2):
            for j in range(3):
                ps = bank[2 + (l * 3 + j) % 2]
                P.op("pe", "transpose", out=ps[0:48, 0:128], in_=G.modT[:, l, :, j], identity=G.identf[:],
                     reads=[G.modT, G.identf], writes=[ps])
                P.op("dve", "tensor_copy", out=tr[:], in_=ps[0:48, 0:128], reads=[ps], writes=[tr])
                P.dma("sp", G.modD[l, j, :].rearrange("(c p) -> c p", p=128), tr[:], tr, reads=[tr], writes=[G.modD])


def stage_norm(P, G, src, T, l, kind, dst, ctx_len, dst32=None):
    bank = G.bank
    with P.scope():
        xt = [P.sb(f"xt{i}", [128, D]) for i in range(4)]
        sq = P.sb("sq", [128, D], BF16)
        ss = [P.sb(f"ss{i}", [128, 1]) for i in range(4)]
        rs = [P.sb(f"rs{i}", [128, 1]) for i in range(4)]
        xn = [P.sb(f"xn{i}", [128, D]) for i in range(4)]
        hT = [P.sb(f"hT{i}", [128, 8, 128], BF16) for i in range(4)]
        hT32 = [P.sb(f"hTf{i}", [128, 8, 128], F32) for i in range(4)] if dst32 is not None else None
        i = 0
        for b in range(NB):
            for tt in range(T // 128):
                j = 2 if tt * 128 < ctx_len else b
                s = i % 4
                P.dma("sp", xt[s][:], src[b, tt * 128:(tt + 1) * 128, :], xt[s], reads=[src], writes=[xt[s]])
                P.op("act", "activation", out=sq[:], in_=xt[s][:], func=AF.Square, accum_out=ss[s][:],
                     reads=[xt[s]], writes=[sq, ss[s]])
                P.op("act", "activation", out=rs[s][:], in_=ss[s][:], func=AF.Sqrt, scale=1.0 / D, bias=G.eps[:, 0:1],
                     reads=[ss[s], G.eps], writes=[rs[s]])
                P.op("dve", "reciprocal", out=rs[s][:], in_=rs[s][:], reads=[rs[s]], writes=[rs[s]])
                P.op("dve", "tensor_scalar", out=xn[s][:], in0=xt[s][:], scalar1=rs[s][:, 0:1], scalar2=None,
                     op0=ALU.mult, reads=[xt[s], rs[s]], writes=[xn[s]])
                for k in range(8):
                    ps = bank[(i % 4) * 2 + k // 4]
                    pv = ps[:, (k % 4) * 128:(k % 4 + 1) * 128]
                    P.op("pe", "transpose", out=pv, in_=xn[s][:, k * 128:(k + 1) * 128], identity=G.identf[:],
                         reads=[xn[s], G.identf], writes=[ps])
                    if dst is not None:
                        if dst32 is None and k % 4 >= 2:
                            P.op("dve", "tensor_scalar", out=hT[s][:, k, :], in0=pv,
                                 scalar1=G.GS[:, l, kind, k, j:j + 1], scalar2=G.modT[:, l, 3 * kind * 8 + k, j:j + 1],
                                 op0=ALU.mult, op1=ALU.add, reads=[ps, G.GS, G.modT], writes=[hT[s]])
                        else:
                            P.op("act", "activation", out=hT[s][:, k, :], in_=pv, func=AF.Identity,
                                 scale=G.GS[:, l, kind, k, j:j + 1], bias=G.modT[:, l, 3 * kind * 8 + k, j:j + 1],
                                 reads=[ps, G.GS, G.modT], writes=[hT[s]])
                    if dst32 is not None:
                        if dst is None and k % 2:
                            P.op("act", "activation", out=hT32[s][:, k, :], in_=pv, func=AF.Identity,
                                 scale=G.GS[:, l, kind, k, j:j + 1], bias=G.modT[:, l, 3 * kind * 8 + k, j:j + 1],
                                 reads=[ps, G.GS, G.modT], writes=[hT32[s]])
                        else:
                            P.op("dve", "tensor_scalar", out=hT32[s][:, k, :], in0=pv,
                                 scalar1=G.GS[:, l, kind, k, j:j + 1], scalar2=G.modT[:, l, 3 * kind * 8 + k, j:j + 1],
                                 op0=ALU.mult, op1=ALU.add, reads=[ps, G.GS, G.modT], writes=[hT32[s]])
                if dst is not None:
                    P.dma("pool", dst[b, :, :, tt * 128:(tt + 1) * 128], hT[s][:], hT[s], reads=[hT[s]], writes=[dst])
                if dst32 is not None:
                    P.dma("pool", dst32[b, :, :, tt * 128:(tt + 1) * 128], hT32[s][:], hT32[s],
                          reads=[hT32[s]], writes=[dst32])
                i += 1


TBLK = [(0, 256), (256, 768), (768, 1280), (1280, 1792), (1792, 2304)]


def load_w_chunk(P, wt, src, c0, ncols, eng="pool"):
    P.dma(eng, wt[:, :, 0:ncols], src[:, c0:c0 + ncols].rearrange("(k p) n -> p k n", p=128), wt,
          reads=[src], writes=[wt])


class WPrefetch:
    def __init__(self, P, src, seq, nbuf=3, ncols=128):
        self.P, self.src, self.seq, self.ncols = P, src, list(seq), ncols
        self.bufs = [P.sb(f"wpf{i}", [128, 8, ncols], BF16) for i in range(nbuf)]
        self.i = 0
        self._issue(0)

    def _issue(self, i):
        if i < len(self.seq):
            load_w_chunk(self.P, self.bufs[i % len(self.bufs)], self.src, self.seq[i] * self.ncols, self.ncols)

    def get(self, chunk):
        assert self.seq[self.i] == chunk, (self.seq[self.i], chunk)
        wt = self.bufs[self.i % len(self.bufs)]
        self._issue(self.i + 1)
        self.i += 1
        return wt


def inproj_fm(P, G, hT, wt, out, padded=True, banks=(0, 1, 2, 3), evac="act", func=None):
    for bi, (t0, t1) in enumerate(TBLK):
        ps = G.bank[banks[bi % len(banks)]]
        n = t1 - t0
        for k in range(8):
            mm(P, ps, ps[:, 0:n], wt, wt[:, k, 0:128], hT, hT[:, k, t0:t1], start=(k == 0), stop=(k == 7))
        c0 = pcol(t0) if padded else t0
        if evac == "act":
            P.op("act", "activation", out=out[:, c0:c0 + n], in_=ps[:, 0:n], func=(func or AF.Copy), reads=[ps], writes=[out])
        else:
            P.op("dve", "tensor_copy", out=out[:, c0:c0 + n], in_=ps[:, 0:n], reads=[ps], writes=[out])


def tshift(P, raw, tmp, out, muh, omm, vec):
    P.op("pool", "tensor_tensor", out=tmp[:, 1:W - 1], in0=raw[:, 0:W - 2], in1=raw[:, 2:W], op=ALU.add,
         reads=[raw], writes=[tmp])
    P.op("dve", "tensor_scalar", out=tmp[:, 1:W - 1], in0=tmp[:, 1:W - 1], scalar1=muh, scalar2=None, op0=ALU.mult,
         reads=[tmp, vec], writes=[tmp])
    P.op("dve", "scalar_tensor_tensor", out=out[:, 1:W - 1], in0=raw[:, 1:W - 1], scalar=omm, in1=tmp[:, 1:W - 1],
         op0=ALU.mult, op1=ALU.add, reads=[raw, tmp, vec], writes=[out])


def chunk_views(ap):
    return (ap[:, 1:1 + TC].rearrange("p (n t) -> p n t", t=128),
            ap[:, 3 + TC:3 + TT].rearrange("p (n t) -> p n t", t=128))


def stage_rwkv_feat(P, G, b):
    bank = G.bank
    with P.scope():
        hT = P.sb("hTres", [128, 8, TT], BF16)
        for k in range(8):
            P.dma("sp", hT[:, k, :], G.hT0[b, :, k, :], hT, reads=[G.hT0], writes=[hT])
        vec = P.sb("rwvec", [128, 4, 12])
        P.dma("sp", vec[:], G.rw_vec[:], vec, reads=[G.rw_vec], writes=[vec])
        mu = P.sb("rwmu", [128, 3, 14])
        P.dma("sp", mu[:, 0, :], G.rw_mu[:], mu, reads=[G.rw_mu], writes=[mu])
        P.op("dve", "tensor_scalar", out=mu[:, 1, :], in0=mu[:, 0, :], scalar1=0.5, scalar2=None, op0=ALU.mult,
             reads=[mu], writes=[mu])
        P.op("dve", "tensor_scalar", out=mu[:, 2, :], in0=mu[:, 0, :], scalar1=-1.0, scalar2=1.0, op0=ALU.mult,
             op1=ALU.add, reads=[mu], writes=[mu])
        P.op("dve", "tensor_scalar", out=vec[:, :, 9], in0=vec[:, :, 1], scalar1=-1.0, scalar2=1.0, op0=ALU.mult,
             op1=ALU.add, reads=[vec], writes=[vec])
        blk = P.sb("blk64", [128, 128])
        P.dma("sp", blk[:], G.blk64[:], blk, reads=[G.blk64], writes=[blk])
        smask = P.sb("smask", [128, W])
        P.dma("sp", smask[:], G.scanmask[:], smask, reads=[G.scanmask], writes=[smask])
        wup = P.sb("wup", [64, 2, 512], BF16)
        P.dma("pool", wup[:], G.rw_w_up[:].rearrange("d r c -> r d c"), wup, reads=[G.rw_w_up], writes=[wup])
        aup = P.sb("aup", [128, 512], BF16)
        P.dma("pool", aup[64:128, :], G.rw_a_up[:], aup, reads=[G.rw_a_up], writes=[aup])
        gup = P.sb("gup", [128, 512], BF16)
        P.dma("pool", gup[:], G.rw_g_up[:], gup, reads=[G.rw_g_up], writes=[gup])
        eps12 = P.sb("eps12", [128, 1])
        P.op("dve", "memset", ap=eps12[:], constant=1e-12, writes=[eps12])
        WP = WPrefetch(P, G.rec_w_in, [12, 13] + [x for c in range(4) for x in (c, 4 + c, 8 + c)])
        T = [P.sb(f"T{i}", [128, W]) for i in range(12)]
        for t in T:
            P.op("pool", "memset", ap=t[:], constant=0.0, writes=[t])
        twb = P.sb("twb", [64, W], BF16)
        adb = P.sb("adb", [128, W], BF16)
        sgb = P.sb("sgb", [128, W], BF16)
        vtok = P.sb("vtok", [128, 18, 128], BF16)
        bkt = P.sb("bkt", [128, 18, 2, 128], BF16)
        gl = P.sb("gl", [128, 18])
        wi = [0]

        def proj(chunk, out):
            wt = WP.get(chunk)
            inproj_fm(P, G, hT, wt, T[0])
            tshift(P, T[0], T[1], out, mu[:, 1, chunk:chunk + 1], mu[:, 2, chunk:chunk + 1], mu)

        proj(12, T[2])
        P.op("act", "activation", out=twb[:], in_=T[2][0:64, :], func=AF.Tanh, reads=[T[2]], writes=[twb])
        P.op("dve", "tensor_copy", out=adb[64:128, :], in_=T[2][64:128, :], reads=[T[2]], writes=[adb])
        proj(13, T[2])
        P.op("act", "activation", out=sgb[:], in_=T[2][:], func=AF.Sigmoid, reads=[T[2]], writes=[sgb])
        for c in range(4):
            cs = slice(c * 128, (c + 1) * 128)
            A, R, KR, KAP, T6, V = T[2], T[3], T[4], T[5], T[6], T[7]
            for bi, c0 in enumerate(range(0, W, 512)):
                n = min(512, W - c0)
                ps = bank[4 + bi % 2]
                mm(P, ps, ps[:, 0:n], gup, gup[:, cs], sgb, sgb[:, c0:c0 + n])
                P.op("act", "activation", out=T[1][:, c0:c0 + n], in_=ps[:, 0:n], func=AF.Copy, reads=[ps], writes=[T[1]])
                ps2 = bank[6 + bi % 2]
                mm(P, ps2, ps2[:, 0:n], aup, aup[64:128, cs], adb, adb[64:128, c0:c0 + n])
                P.op("act", "activation", out=A[:, c0:c0 + n], in_=ps2[:, 0:n], func=AF.Sigmoid, bias=vec[:, c, 3:4],
                     reads=[ps2, vec], writes=[A])
            P.dma("sp", G.gT[b, c, :, :], T[1][:], T[1], reads=[T[1]], writes=[G.gT])
            proj(c, R)
            proj(4 + c, KR)
            P.op("dve", "tensor_scalar", out=KAP[:], in0=KR[:], scalar1=vec[:, c, 0:1], scalar2=None, op0=ALU.mult,
                 reads=[KR, vec], writes=[KAP])
            P.op("pool", "tensor_tensor", out=T6[:], in0=KAP[:], in1=KAP[:], op=ALU.mult, reads=[KAP], writes=[T6])
            for bi, c0 in enumerate(range(0, W, 512)):
                n = min(512, W - c0)
                ps = bank[4 + bi % 2]
                mm(P, ps, ps[:, 0:n], blk, blk[:], T6, T6[:, c0:c0 + n])
                P.op("act", "activation", out=T[1][:, c0:c0 + n], in_=ps[:, 0:n], func=AF.Sqrt, reads=[ps], writes=[T[1]])
            P.op("dve", "tensor_scalar", out=T[1][:], in0=T[1][:], scalar1=eps12[:, 0:1], scalar2=None, op0=ALU.max,
                 reads=[T[1], eps12], writes=[T[1]])
            P.op("dve", "reciprocal", out=T[1][:], in_=T[1][:], reads=[T[1]], writes=[T[1]])
            P.op("dve", "tensor_tensor", out=KAP[:], in0=KAP[:], in1=T[1][:], op=ALU.mult, reads=[KAP, T[1]], writes=[KAP])
            P.op("dve", "tensor_scalar", out=T6[:], in0=A[:], scalar1=vec[:, c, 1:2], scalar2=vec[:, c, 9:10],
                 op0=ALU.mult, op1=ALU.add, reads=[A, vec], writes=[T6])
            KM = T6
            P.op("pool", "tensor_tensor", out=KM[:], in0=KM[:], in1=KR[:], op=ALU.mult, reads=[KM, KR], writes=[KM])
            BE = KR
            P.op("dve", "tensor_tensor", out=BE[:], in0=KAP[:], in1=A[:], op=ALU.mult, reads=[KAP, A], writes=[BE])
            P.op("dve", "scalar_tensor_tensor", out=T[1][:], in0=R[:], scalar=vec[:, c, 4:5], in1=KM[:],
                 op0=ALU.mult, op1=ALU.mult, reads=[R, KM, vec], writes=[T[1]])
            for bi, c0 in enumerate(range(0, W, 512)):
                n = min(512, W - c0)
                ps = bank[4 + bi % 2]
                mm(P, ps, ps[:, 0:n], blk, blk[:], T[1], T[1][:, c0:c0 + n])
                P.op("act", "activation", out=T[8][:, c0:c0 + n], in_=ps[:, 0:n], func=AF.Copy, reads=[ps], writes=[T[8]])
            P.dma("sp", G.bcT[b, c, :, :], T[8][:], T[8], reads=[T[8]], writes=[G.bcT])
            proj(8 + c, V)
            P.dma("sp", G.vT[b, c, :, :], V[:], V, reads=[V], writes=[G.vT])
            for n in range(18):
                ps = bank[4 + n % 4]
                c0 = pcol(n * 128)
                P.op("pe", "transpose", out=ps[:, 0:128], in_=V[:, c0:c0 + 128], identity=G.identf[:],
                     reads=[V, G.identf], writes=[ps])
                P.op("act" if n % 2 else "dve", "activation" if n % 2 else "tensor_copy", out=vtok[:, n, :],
                     in_=ps[:, 0:128], reads=[ps], writes=[vtok], **({"func": AF.Copy} if n % 2 else {}))
            P.dma("sp", G.Vt[b, :, :, cs].rearrange("n t c -> t n c"), vtok[:], vtok, reads=[vtok], writes=[G.Vt])
            for d in range(2):
                LW, CS, GE, GI, ENI = T[7], T[8], T[9], T[10], T[11]
                for bi, c0 in enumerate(range(0, W, 512)):
                    n = min(512, W - c0)
                    ps = bank[4 + bi % 2]
                    mm(P, ps, ps[:, 0:n], wup, wup[:, d, cs], twb, twb[:, c0:c0 + n])
                    P.op("act", "activation", out=LW[:, c0:c0 + n], in_=ps[:, 0:n], func=AF.Sigmoid,
                         bias=vec[:, c, 7 + d:8 + d], reads=[ps, vec], writes=[LW])
                P.op("dve", "tensor_tensor_scan", out=CS[:], data0=smask[:], data1=LW[:], initial=0.0,
                     op0=ALU.mult, op1=ALU.add, reads=[smask, LW], writes=[CS])
                for (cv, n0, nn) in ((chunk_views(CS[:])[0], 0, 2), (chunk_views(CS[:])[1], 2, 16)):
                    P.op("act", "activation", out=gl[:, n0:n0 + nn], in_=cv[:, :, 127], func=AF.Exp, scale=-C0,
                         reads=[CS], writes=[gl])
                P.dma("sp", G.GL[d][b, 2 * c:2 * c + 2, :, :].rearrange("h k n -> (h k) n"), gl[:], gl,
                      reads=[gl], writes=[G.GL[d]])
                if d == 0:
                    P.op("pool", "tensor_tensor", out=GE[:], in0=CS[:], in1=LW[:], op=ALU.subtract,
                         reads=[CS, LW], writes=[GE])
                    P.op("act", "activation", out=GI[:], in_=CS[:], func=AF.Exp, scale=-C0, reads=[CS], writes=[GI])
                    P.op("act", "activation", out=ENI[:], in_=CS[:], func=AF.Exp, scale=C0, reads=[CS], writes=[ENI])
                else:
                    for vi in range(2):
                        gev = chunk_views(GE[:])[vi]
                        csv = chunk_views(CS[:])[vi]
                        nn = 2 if vi == 0 else 16
                        P.op("dve", "tensor_tensor", out=gev, in0=csv[:, :, 127:128].broadcast_to([128, nn, 128]),
                             in1=csv, op=ALU.subtract, reads=[CS], writes=[GE])
                    P.op("pool", "tensor_tensor", out=GI[:], in0=GE[:], in1=LW[:], op=ALU.add,
                         reads=[GE, LW], writes=[GI])
                    P.op("act", "activation", out=ENI[:], in_=GI[:], func=AF.Exp, scale=C0, reads=[GI], writes=[ENI])
                    P.op("act", "activation", out=GI[:], in_=GI[:], func=AF.Exp, scale=-C0, reads=[GI], writes=[GI])
                P.op("act", "activation", out=GE[:], in_=GE[:], func=AF.Exp, scale=-C0, reads=[GE], writes=[GE])
                P.op("dve", "tensor_tensor", out=GI[:], in0=GI[:], in1=R[:], op=ALU.mult, reads=[GI, R], writes=[GI])
                P.op("pool", "tensor_tensor", out=GE[:], in0=GE[:], in1=KAP[:], op=ALU.mult, reads=[GE, KAP], writes=[GE])
                BH = CS
                P.op("dve", "tensor_tensor", out=BH[:], in0=ENI[:], in1=BE[:], op=ALU.mult, reads=[ENI, BE], writes=[BH])
                KH = ENI
                P.op("pool", "tensor_tensor", out=KH[:], in0=ENI[:], in1=KM[:], op=ALU.mult, reads=[ENI, KM], writes=[KH])
                hv = lambda t, j: t[b, 2 * c:2 * c + 2, :, j, :].rearrange("h k w -> (h k) w")
                P.dma("pool", hv(G.RS[d], 0), GE[:], GE, reads=[GE], writes=[G.RS[d]])
                P.dma("pool", hv(G.RS[d], 1), GI[:], GI, reads=[GI], writes=[G.RS[d]])
                P.dma("pool", hv(G.BK[d], 0), BH[:], BH, reads=[BH], writes=[G.BK[d]])
                P.dma("pool", hv(G.BK[d], 1), KH[:], KH, reads=[KH], writes=[G.BK[d]])
                for n in range(18):
                    c0 = pcol(n * 128)
                    for j, src in enumerate((BH, KH)):
                        ps = bank[(2 * n + j) % 4]
                        P.op("pe", "transpose", out=ps[:, 0:128], in_=src[:, c0:c0 + 128], identity=G.identf[:],
                             reads=[src, G.identf], writes=[ps])
                        if j == 0:
                            P.op("act", "activation", out=bkt[:, n, j, :], in_=ps[:, 0:128], func=AF.Copy,
                                 reads=[ps], writes=[bkt])
                        else:
                            P.op("dve", "tensor_copy", out=bkt[:, n, j, :], in_=ps[:, 0:128], reads=[ps], writes=[bkt])
                for j in range(2):
                    P.dma("sp", G.BKt[d][b, :, :, j, cs].rearrange("n t c -> t n c"), bkt[:, :, j, :], bkt,
                          reads=[bkt], writes=[G.BKt[d]])


def stage_rwkv_scan(P, G, bs=(0, 1), ds=(0, 1), nmax=18):
    bank = G.bank
    with P.scope():
        msk = P.sb("msk", [128, 4, 128])
        P.dma("sp", msk[:], G.masks[:], msk, reads=[G.masks], writes=[msk])
        rs = [P.sb(f"rs{i}", [64, 8, 2, 128], BF16) for i in range(2)]
        bk = [P.sb(f"bk{i}", [64, 8, 2, 128], BF16) for i in range(2)]
        bkt = [P.sb(f"bktl{i}", [128, 2, 512], BF16) for i in range(2)]
        vt = [P.sb(f"vt{i}", [128, 512], BF16) for i in range(2)]
        Ms = [P.sb(f"Ms{i}", [128, 4, 128], F32) for i in range(2)]
        MTs = [P.sb(f"MTs{i}", [128, 4, 128], F32) for i in range(2)]
        Nn = [[P.sb(f"N{i}_{j}", [128, 4, 128], F32) for j in range(2)] for i in range(2)]
        NT = [[P.sb(f"NT{i}_{j}", [128, 4, 128], F32) for j in range(2)] for i in range(2)]
        ABr = [[P.sb(f"ABr{p}_{i}", [128, 4, 128], BF16) for i in range(2)] for p in range(2)]
        GKs = [[P.sb(f"GKs{p}_{i}", [128, 4, 256], BF16) for i in range(2)] for p in range(2)]
        Pm = [[P.sb(f"Pm{p}_{i}", [128, 4, 128], F32) for i in range(2)] for p in range(2)]
        WT = P.sb("WT", [128, 512], F32)
        nZ = P.sb("nZ", [128, 512], BF16)
        ot = [P.sb(f"ot{i}", [128, 512]) for i in range(2)]
        Sf = P.sb("Sf", [64, 8, 64])
        Sb = P.sb("Sb", [64, 8, 64], BF16)
        Stmp = P.sb("Stmp", [64, 8, 64])
        GLt = P.sb("GLt", [64, 8, 18])
        b6, b7 = bank[6], bank[7]

        def emit_front(b, d, n, s):
            m2 = msk[:, 0:2, :] if d == 0 else msk[:, 2:4, :]
            mt = msk[:, 2, :] if d == 0 else msk[:, 0, :]
            c0 = pcol(n * 128)
            for j in range(2):
                P.dma("sp", rs[s][:, :, j, :], G.RS[d][b, :, :, j, c0:c0 + 128].rearrange("h k t -> k h t"),
                      rs[s], reads=[G.RS[d]], writes=[rs[s]])
                P.dma("sp", bk[s][:, :, j, :], G.BK[d][b, :, :, j, c0:c0 + 128].rearrange("h k t -> k h t"),
                      bk[s], reads=[G.BK[d]], writes=[bk[s]])
            P.dma("sp", bkt[s][:], G.BKt[d][b, n], bkt[s], reads=[G.BKt[d]], writes=[bkt[s]])
            P.dma("sp", vt[s][:], G.Vt[b, n], vt[s], reads=[G.Vt], writes=[vt[s]])
            R_, B_ = rs[s], bk[s]
            for hf in range(2):
                for i in range(4):
                    h = hf * 4 + i
                    pb = bank[hf * 3 + 0] if i < 2 else bank[hf * 3 + 1]
                    mm(P, pb, pb[:, (i % 2) * 256:(i % 2 + 1) * 256], B_, B_[:, h, 0, :], R_, R_[:, h, :, :])
                for i in range(4):
                    h = hf * 4 + i
                    pk = b6 if i < 2 else b7
                    mm(P, pk, pk[:, (i % 2) * 256:(i % 2 + 1) * 256], B_, B_[:, h, 1, :], R_, R_[:, h, :, :])
                pm = bank[hf * 3 + 2]
                for i in range(4):
                    h = hf * 4 + i
                    mm(P, pm, pm[:, i * 128:(i + 1) * 128], R_, R_[:, h, 0, :], B_, B_[:, h, 0, :])
                for half2 in range(2):
                    pb = bank[hf * 3 + half2]
                    pbv = pb[:, 0:512].rearrange("p (h j t) -> p h j t", h=2, j=2)
                    P.op("dve", "tensor_tensor", out=Ms[hf][:, 2 * half2:2 * half2 + 2, :], in0=pbv[:, :, 0, :],
                         in1=m2[:, 0, :].unsqueeze(1).broadcast_to([128, 2, 128]), op=ALU.mult,
                         reads=[pb, msk], writes=[Ms[hf]])
                    P.op("dve", "tensor_tensor", out=ABr[s][hf][:, 2 * half2:2 * half2 + 2, :], in0=pbv[:, :, 1, :],
                         in1=m2[:, 1, :].unsqueeze(1).broadcast_to([128, 2, 128]), op=ALU.mult,
                         reads=[pb, msk], writes=[ABr[s][hf]])
                    pk = bank[6 + half2]
                    P.op("dve", "tensor_tensor", out=GKs[s][hf][:, 2 * half2:2 * half2 + 2, :].rearrange("p h (j t) -> p h j t", j=2),
                         in0=pk[:, 0:512].rearrange("p (h j t) -> p h j t", h=2, j=2),
                         in1=m2.unsqueeze(1).broadcast_to([128, 2, 2, 128]), op=ALU.mult,
                         reads=[pk, msk], writes=[GKs[s][hf]])
                P.op("dve", "tensor_tensor", out=MTs[hf][:], in0=pm[:, 0:512].rearrange("p (h t) -> p h t", h=4),
                     in1=mt.unsqueeze(1).broadcast_to([128, 4, 128]), op=ALU.mult,
                     reads=[pm, msk], writes=[MTs[hf]])
                P.op("pool", "tensor_tensor", out=Pm[s][hf][:], in0=G.identf[:].unsqueeze(1).broadcast_to([128, 4, 128]),
                     in1=Ms[hf][:], op=ALU.subtract, reads=[Ms[hf], G.identf], writes=[Pm[s][hf]])
            for hf in range(2):
                pn, pt = bank[hf * 3 + 0], bank[hf * 3 + 1]
                for i in range(4):
                    mm(P, pn, pn[:, i * 128:(i + 1) * 128], MTs[hf], MTs[hf][:, i, :], Ms[hf], Ms[hf][:, i, :])
                for i in range(4):
                    mm(P, pt, pt[:, i * 128:(i + 1) * 128], Ms[hf], Ms[hf][:, i, :], MTs[hf], MTs[hf][:, i, :])
                P.op("act", "activation", out=Nn[hf][0][:], in_=pn[:, 0:512].rearrange("p (h t) -> p h t", h=4),
                     func=AF.Copy, reads=[pn], writes=[Nn[hf][0]])
                P.op("act", "activation", out=NT[hf][0][:], in_=pt[:, 0:512].rearrange("p (h t) -> p h t", h=4),
                     func=AF.Copy, reads=[pt], writes=[NT[hf][0]])

        def emit_level(s, lev):
            cur, nxt = lev % 2, 1 - lev % 2
            for hf in range(2):
                pp, pn, pt = bank[hf * 3 + 2], bank[hf * 3 + 0], bank[hf * 3 + 1]
                Pq = Pm[s][hf]
                for i in range(4):
                    mm(P, pp, pp[:, i * 128:(i + 1) * 128], NT[hf][cur], NT[hf][cur][:, i, :], Pq, Pq[:, i, :])
                if lev < 4:
                    for i in range(4):
                        mm(P, pn, pn[:, i * 128:(i + 1) * 128], NT[hf][cur], NT[hf][cur][:, i, :],
                           Nn[hf][cur], Nn[hf][cur][:, i, :])
                if lev < 5:
                    for i in range(4):
                        mm(P, pt, pt[:, i * 128:(i + 1) * 128], Nn[hf][cur], Nn[hf][cur][:, i, :],
                           NT[hf][cur], NT[hf][cur][:, i, :])
                P.op("dve", "tensor_tensor", out=Pq[:], in0=pp[:, 0:512].rearrange("p (h t) -> p h t", h=4),
                     in1=Pq[:], op=ALU.add, reads=[pp, Pq], writes=[Pq])
                if lev < 4:
                    P.op("act", "activation", out=Nn[hf][nxt][:], in_=pn[:, 0:512].rearrange("p (h t) -> p h t", h=4),
                         func=AF.Copy, reads=[pn], writes=[Nn[hf][nxt]])
                if lev < 5:
                    P.op("act", "activation", out=NT[hf][nxt][:], in_=pt[:, 0:512].rearrange("p (h t) -> p h t", h=4),
                         func=AF.Copy, reads=[pt], writes=[NT[hf][nxt]])

        def chain_steps(b, d, n, s):
            R_, BT_, V_ = rs[s], bkt[s], vt[s]

            def st_d():
                for h in range(8):
                    hf, i = h // 4, h % 4
                    hs = slice(h * 64, (h + 1) * 64)
                    mm(P, b6, b6[:, hs], R_, R_[:, h, 0, :], Sb, Sb[:, h, :], start=True, stop=False)
                    mm(P, b6, b6[:, hs], GKs[s][hf], GKs[s][hf][:, i, 0:128], V_, V_[:, hs], start=False, stop=True)
                P.op("act", "activation", out=WT[:], in_=b6[:, 0:512], func=AF.Copy, reads=[b6], writes=[WT])

            def st_e():
                for h in range(8):
                    hf, i = h // 4, h % 4
                    hs = slice(h * 64, (h + 1) * 64)
                    mm(P, b7, b7[:, hs], Pm[s][hf], Pm[s][hf][:, i, :], WT, WT[:, hs])
                P.op("act", "activation", out=nZ[:], in_=b7[:, 0:512], func=AF.Copy, scale=-1.0, reads=[b7], writes=[nZ])

            def st_f():
                for h in range(8):
                    hf, i = h // 4, h % 4
                    hs = slice(h * 64, (h + 1) * 64)
                    mm(P, b6, b6[:, hs], R_, R_[:, h, 1, :], Sb, Sb[:, h, :], start=True, stop=False)
                    mm(P, b6, b6[:, hs], GKs[s][hf], GKs[s][hf][:, i, 128:256], V_, V_[:, hs], start=False, stop=False)
                    mm(P, b6, b6[:, hs], ABr[s][hf], ABr[s][hf][:, i, :], nZ, nZ[:, hs], start=False, stop=True)
                o_ = ot[s]
                P.op("dve", "tensor_copy", out=o_[:], in_=b6[:, 0:512], reads=[b6], writes=[o_])
                P.dma("pool", G.oA[d][b, n], o_[:], o_, reads=[o_], writes=[G.oA[d]])

            def st_g():
                for h in range(8):
                    hs = slice(h * 64, (h + 1) * 64)
                    mm(P, b7, b7[0:64, hs], BT_, BT_[:, 1, hs], V_, V_[:, hs], start=True, stop=False)
                    mm(P, b7, b7[0:64, hs], BT_, BT_[:, 0, hs], nZ, nZ[:, hs], start=False, stop=True)
                P.op("dve", "tensor_tensor", out=Stmp[:], in0=b7[0:64, 0:512].rearrange("p (h v) -> p h v", h=8),
                     in1=Sf[:], op=ALU.add, reads=[b7, Sf], writes=[Stmp])
                P.op("dve", "tensor_tensor", out=Sf[:], in0=Stmp[:],
                     in1=GLt[:, :, n:n + 1].broadcast_to([64, 8, 64]), op=ALU.mult,
                     reads=[Stmp, GLt], writes=[Sf])
                P.op("act", "activation", out=Sb[:], in_=Sf[:], func=AF.Copy, reads=[Sf], writes=[Sb])

            return [st_d, st_e, st_f, st_g]

        it = 0
        for b in bs:
            for d in ds:
                P.op("dve", "memset", ap=Sf[:], constant=0.0, writes=[Sf])
                P.op("dve", "memset", ap=Sb[:], constant=0.0, writes=[Sb])
                P.dma("sp", GLt[:], G.GL[d][b].rearrange("h k n -> k h n"), GLt, reads=[G.GL[d]], writes=[GLt])
                order = list(range(18)) if d == 0 else [1, 0] + list(range(17, 1, -1))
                order = order[:nmax]
                pending = []
                for n in order + [None]:
                    if n is not None:
                        s = it % 2
                        it += 1
                        emit_front(b, d, n, s)
                    for lev in range(6):
                        if n is not None:
                            emit_level(s, lev)
                        if pending and lev >= 1:
                            pending.pop(0)()
                    while pending:
                        pending.pop(0)()
                    if n is not None:
                        pending = chain_steps(b, d, n, s)


def stage_rwkv_post(P, G):
    bank = G.bank
    with P.scope():
        vec = P.sb("rwvec2", [128, 4, 12])
        P.dma("sp", vec[:], G.rw_vec[:], vec, reads=[G.rw_vec], writes=[vec])
        gne = P.sb("gne", [128, 1])
        P.op("dve", "memset", ap=gne[:], constant=64e-5, writes=[gne])
        o0 = [P.sb(f"o0_{i}", [128, 512]) for i in range(3)]
        o1 = [P.sb(f"o1_{i}", [128, 512]) for i in range(3)]
        sq = P.sb("posq", [128, 512])
        st = [P.sb(f"pst{i}", [128, 4, 8]) for i in range(3)]
        on = [P.sb(f"on{i}", [128, 512]) for i in range(3)]
        ya = [P.sb(f"ya{i}", [128, 4, 128]) for i in range(3)]
        bc = [P.sb(f"bc{i}", [128, 4, 128]) for i in range(3)]
        vv = [P.sb(f"vv{i}", [128, 4, 128]) for i in range(3)]
        gg = [P.sb(f"gg{i}", [128, 4, 128]) for i in range(3)]
        yb = [P.sb(f"yab{i}", [128, 4, 128], BF16) for i in range(3)]
        it = 0
        for b in range(NB):
            for n in range(18):
                s = it % 3
                it += 1
                c0 = pcol(n * 128)
                P.dma("sp", o0[s][:], G.oA[0][b, n], o0[s], reads=[G.oA[0]], writes=[o0[s]])
                P.dma("sp", o1[s][:], G.oA[1][b, n], o1[s], reads=[G.oA[1]], writes=[o1[s]])
                P.dma("sp", bc[s][:], G.bcT[b, :, :, c0:c0 + 128].rearrange("c p t -> p c t"), bc[s], reads=[G.bcT], writes=[bc[s]])
                P.dma("sp", vv[s][:], G.vT[b, :, :, c0:c0 + 128].rearrange("c p t -> p c t"), vv[s], reads=[G.vT], writes=[vv[s]])
                P.dma("sp", gg[s][:], G.gT[b, :, :, c0:c0 + 128].rearrange("c p t -> p c t"), gg[s], reads=[G.gT], writes=[gg[s]])
                o_, S_ = o0[s], st[s]
                P.op("pool", "tensor_tensor", out=o_[:], in0=o_[:], in1=o1[s][:], op=ALU.add, reads=[o_, o1[s]], writes=[o_])
                ov = o_[:].rearrange("p (h v) -> p h v", h=8)
                P.op("dve", "tensor_reduce", out=S_[:, 0, :], in_=ov, axis=AX.X, op=ALU.add, reads=[o_], writes=[S_])
                P.op("pool", "tensor_tensor", out=sq[:], in0=o_[:], in1=o_[:], op=ALU.mult, reads=[o_], writes=[sq])
                P.op("dve", "tensor_reduce", out=S_[:, 1, :], in_=sq[:].rearrange("p (h v) -> p h v", h=8), axis=AX.X,
                     op=ALU.add, reads=[sq], writes=[S_])
                P.op("dve", "tensor_scalar", out=S_[:, 2, :], in0=S_[:, 0, :], scalar1=1.0 / 64, scalar2=None, op0=ALU.mult,
                     reads=[S_], writes=[S_])
                P.op("dve", "tensor_tensor", out=S_[:, 3, :], in0=S_[:, 2, :], in1=S_[:, 2, :], op=ALU.mult, reads=[S_], writes=[S_])
                P.op("dve", "scalar_tensor_tensor", out=S_[:, 3, :], in0=S_[:, 1, :], scalar=1.0 / 64, in1=S_[:, 3, :],
                     op0=ALU.mult, op1=ALU.subtract, reads=[S_], writes=[S_])
                P.op("act", "activation", out=S_[:, 3, :], in_=S_[:, 3, :], func=AF.Sqrt, bias=gne[:, 0:1], reads=[S_, gne], writes=[S_])
                P.op("dve", "reciprocal", out=S_[:, 3, :], in_=S_[:, 3, :], reads=[S_], writes=[S_])
                onv = on[s][:].rearrange("p (h v) -> p h v", h=8)
                P.op("dve", "tensor_tensor", out=onv, in0=ov, in1=S_[:, 2, :].unsqueeze(2).broadcast_to([128, 8, 64]),
                     op=ALU.subtract, reads=[o_, S_], writes=[on[s]])
                P.op("dve", "tensor_tensor", out=onv, in0=onv, in1=S_[:, 3, :].unsqueeze(2).broadcast_to([128, 8, 64]),
                     op=ALU.mult, reads=[on[s], S_], writes=[on[s]])
                for c in range(4):
                    ps = bank[it % 8]
                    P.op("pe", "transpose", out=ps[:, c * 128:(c + 1) * 128], in_=on[s][:, c * 128:(c + 1) * 128], identity=G.identf[:],
                         reads=[on[s], G.identf], writes=[ps])
                for c in range(4):
                    ps = bank[it % 8]
                    P.op("act", "activation", out=ya[s][:, c, :], in_=ps[:, c * 128:(c + 1) * 128], func=AF.Identity,
                         scale=vec[:, c, 5:6], bias=vec[:, c, 6:7], reads=[ps, vec], writes=[ya[s]])
                P.op("pool", "tensor_tensor", out=bc[s][:], in0=bc[s][:], in1=vv[s][:], op=ALU.mult, reads=[bc[s], vv[s]], writes=[bc[s]])
                P.op("dve", "tensor_tensor", out=ya[s][:], in0=ya[s][:], in1=bc[s][:], op=ALU.add, reads=[ya[s], bc[s]], writes=[ya[s]])
                P.op("dve", "tensor_tensor", out=yb[s][:], in0=ya[s][:], in1=gg[s][:], op=ALU.mult, reads=[ya[s], gg[s]], writes=[yb[s]])
                P.dma("pool", G.yT[b, 0:4, :, n * 128:(n + 1) * 128].rearrange("c p t -> p c t"), yb[s][:], yb[s],
                      reads=[yb[s]], writes=[G.yT])


def stage_hgrn_feat(P, G, b):
    bank = G.bank
    with P.scope():
        hT = P.sb("hTres", [128, 8, TT], BF16)
        for k in range(8):
            P.dma("sp", hT[:, k, :], G.hT0[b, :, k, :], hT, reads=[G.hT0], writes=[hT])
        lbr = P.sb("lbr", [128, 2, 2, 4])
        P.dma("sp", lbr[:], G.hg_lb[:].rearrange("d l p h -> p d l h"), lbr, reads=[G.hg_lb], writes=[lbr])
        lb = P.sb("lb", [128, 2, 2, 4])
        P.op("dve", "tensor_tensor", out=lb[:, :, 0, :], in0=lbr[:, :, 0, :], in1=lbr[:, :, 1, :], op=ALU.subtract,
             reads=[lbr], writes=[lb])
        P.op("act", "activation", out=lb[:, :, 0, :], in_=lb[:, :, 0, :], func=AF.Sigmoid, reads=[lb], writes=[lb])
        P.op("dve", "tensor_scalar", out=lb[:, :, 1, :], in0=lb[:, :, 0, :], scalar1=-1.0, scalar2=1.0, op0=ALU.mult,
             op1=ALU.add, reads=[lb], writes=[lb])
        sm64 = P.sb("sm64", [128, TT])
        P.op("pool", "memset", ap=sm64[:], constant=1.0, writes=[sm64])
        P.op("pool", "memset", ap=sm64[:].rearrange("p (n t) -> p n t", t=64)[:, :, 0:1], constant=0.0, writes=[sm64])
        WP = WPrefetch(P, G.rec_w_in, [x for h in range(4) for x in (14 + h, 22 + h, 26 + h)])
        wbig = P.sb("wbig", [128, 8, 512], BF16)
        T = [P.sb(f"H{i}", [128, TT]) for i in range(7)]
        hkt = P.sb("hkt", [128, 18, 128], BF16)
        glh = P.sb("glh", [128, 36])
        tok = [P.sb(f"tok{i}", [128, 512], BF16) for i in range(2)]
        wi = [0]

        def proj(chunk, out, func=None):
            wt = WP.get(chunk)
            inproj_fm(P, G, hT, wt, out, padded=False, func=func)

        for h in range(4):
            Q, F, LF, CS, GI, ENI, KF = T
            proj(14 + h, Q, AF.Silu)
            for d in range(2):
                proj(22 + 4 * d + h, F, AF.Sigmoid)
                P.op("dve", "tensor_scalar", out=F[:], in0=F[:], scalar1=lb[:, d, 1, h:h + 1], scalar2=lb[:, d, 0, h:h + 1],
                     op0=ALU.mult, op1=ALU.add, reads=[F, lb], writes=[F])
                P.op("act", "activation", out=LF[:], in_=F[:], func=AF.Ln, reads=[F], writes=[LF])
                P.op("pool", "tensor_scalar", out=KF[:], in0=F[:], scalar1=-1.0, scalar2=1.0, op0=ALU.mult, op1=ALU.add,
                     reads=[F], writes=[KF])
                P.op("dve", "tensor_tensor_scan", out=CS[:], data0=sm64[:], data1=LF[:], initial=0.0, op0=ALU.mult,
                     op1=ALU.add, reads=[sm64, LF], writes=[CS])
                csv = CS[:].rearrange("p (n t) -> p n t", t=64)
                P.op("act", "activation", out=glh[:], in_=csv[:, :, 63], func=AF.Exp, reads=[CS], writes=[glh])
                P.dma("sp", G.HGL[d][b, h], glh[:], glh, reads=[glh], writes=[G.HGL[d]])
                if d == 0:
                    gsrc = CS
                else:
                    P.op("dve", "tensor_tensor", out=GI[:].rearrange("p (n t) -> p n t", t=64),
                         in0=csv[:, :, 63:64].broadcast_to([128, 36, 64]), in1=csv, op=ALU.subtract, reads=[CS], writes=[GI])
                    P.op("pool", "tensor_tensor", out=GI[:], in0=GI[:], in1=LF[:], op=ALU.add, reads=[GI, LF], writes=[GI])
                    gsrc = GI
                P.op("act", "activation", out=ENI[:], in_=gsrc[:], func=AF.Exp, scale=-1.0, reads=[gsrc], writes=[ENI])
                P.op("act", "activation", out=GI[:], in_=gsrc[:], func=AF.Exp, reads=[gsrc], writes=[GI])
                P.op("dve", "tensor_tensor", out=GI[:], in0=GI[:], in1=Q[:], op=ALU.mult, reads=[GI, Q], writes=[GI])
                P.op("pool", "tensor_tensor", out=ENI[:], in0=ENI[:], in1=KF[:], op=ALU.mult, reads=[ENI, KF], writes=[ENI])
                P.dma("pool", G.HQK[d][b, h, :, 0, :], GI[:], GI, reads=[GI], writes=[G.HQK[d]])
                P.dma("pool", G.HQK[d][b, h, :, 1, :], ENI[:], ENI, reads=[ENI], writes=[G.HQK[d]])
                for n in range(18):
                    ps = bank[4 + n % 4]
                    P.op("pe", "transpose", out=ps[:, 0:128], in_=ENI[:, n * 128:(n + 1) * 128], identity=G.identf[:],
                         reads=[ENI, G.identf], writes=[ps])
                    if n % 2:
                        P.op("act", "activation", out=hkt[:, n, :], in_=ps[:, 0:128], func=AF.Copy, reads=[ps], writes=[hkt])
                    else:
                        P.op("dve", "tensor_copy", out=hkt[:, n, :], in_=ps[:, 0:128], reads=[ps], writes=[hkt])
                P.dma("sp", G.HKt[d][b, :, :, h * 128:(h + 1) * 128].rearrange("n t c -> t n c"), hkt[:], hkt,
                      reads=[hkt], writes=[G.HKt[d]])
        for which, c0, dst in ((0, 18 * 128, G.HVt), (1, 30 * 128, G.HGt)):
            load_w_chunk(P, wbig, G.rec_w_in, c0, 512)
            for tt in range(18):
                ps = bank[tt % 4]
                for k in range(8):
                    mm(P, ps, ps[:, 0:512], hT, hT[:, k, tt * 128:(tt + 1) * 128], wbig, wbig[:, k, :],
                       start=(k == 0), stop=(k == 7))
                tk = tok[tt % 2]
                P.op("act", "activation", out=tk[:], in_=ps[:, 0:512], func=(AF.Silu if which else AF.Copy),
                     reads=[ps], writes=[tk])
                P.dma("sp", dst[b, tt], tk[:], tk, reads=[tk], writes=[dst])


def stage_hgrn_scan(P, G, bs=(0, 1), ds=(0, 1)):
    bank = G.bank
    with P.scope():
        msk = P.sb("msk", [128, 4, 128])
        P.dma("sp", msk[:], G.masks[:], msk, reads=[G.masks], writes=[msk])
        qk = [[P.sb(f"qk{d}_{i}", [128, 4, 2, 128], BF16) for i in range(2)] for d in range(2)]
        kt = [[P.sb(f"hkt{d}_{i}", [128, 512], BF16) for i in range(2)] for d in range(2)]
        vt = [[P.sb(f"hvt{d}_{i}", [128, 512], BF16) for i in range(2)] for d in range(2)]
        att = [[P.sb(f"att{d}_{i}", [128, 4, 64], BF16) for i in range(2)] for d in range(2)]
        ot = [[P.sb(f"hot{d}_{i}", [128, 512]) for i in range(2)] for d in range(2)]
        Sf = [P.sb(f"hSf{d}", [128, 4, 128]) for d in range(2)]
        Sb = [P.sb(f"hSb{d}", [128, 4, 128], BF16) for d in range(2)]
        Stmp = [P.sb(f"hStmp{d}", [128, 4, 128]) for d in range(2)]
        GLt = [P.sb(f"hGLt{d}", [128, 4, 36]) for d in range(2)]
        it = 0
        for b in bs:
            for d in ds:
                P.op("dve", "memset", ap=Sf[d][:], constant=0.0, writes=[Sf[d]])
                P.op("dve", "memset", ap=Sb[d][:], constant=0.0, writes=[Sb[d]])
                P.dma("sp", GLt[d][:], G.HGL[d][b].rearrange("h k n -> k h n"), GLt[d], reads=[G.HGL[d]], writes=[GLt[d]])
            orders = {0: list(range(18)), 1: [1, 0] + list(range(17, 1, -1))}
            for step in range(18):
                s = it % 2
                it += 1
                for d in ds:
                    tt = orders[d][step]
                    for j in range(2):
                        P.dma("sp", qk[d][s][:, :, j, :], G.HQK[d][b, :, :, j, tt * 128:(tt + 1) * 128].rearrange("h k t -> k h t"),
                              qk[d][s], reads=[G.HQK[d]], writes=[qk[d][s]])
                    P.dma("sp", kt[d][s][:], G.HKt[d][b, tt], kt[d][s], reads=[G.HKt[d]], writes=[kt[d][s]])
                    P.dma("sp", vt[d][s][:], G.HVt[b, tt], vt[d][s], reads=[G.HVt], writes=[vt[d][s]])
                for ci in range(2):
                    for d in ds:
                        tt = orders[d][step]
                        mi = 1 if d == 0 else 3
                        half = ci if d == 0 else 1 - ci
                        QK, KT, VT = qk[d][s], kt[d][s], vt[d][s]
                        pO = bank[4 + d]
                        chunk = 2 * tt + half
                        lo = half * 64
                        pr = slice(lo, lo + 64)
                        pA = bank[2 * d + ci]
                        pS = bank[6 + d]
                        A_ = att[d][ci]
                        for h in range(4):
                            mm(P, pA, pA[pr, h * 64:(h + 1) * 64], QK, QK[:, h, 1, lo:lo + 64], QK, QK[:, h, 0, lo:lo + 64])
                        P.op("dve", "tensor_tensor", out=A_[pr, :, :], in0=pA[pr, 0:256].rearrange("p (h t) -> p h t", h=4),
                             in1=msk[pr, mi, lo:lo + 64].unsqueeze(1).broadcast_to([64, 4, 64]), op=ALU.mult,
                             reads=[pA, msk], writes=[A_])
                        for h in range(4):
                            hs = slice(h * 128, (h + 1) * 128)
                            mm(P, pO, pO[pr, hs], A_, A_[pr, h, :], VT, VT[pr, hs], start=True, stop=False)
                            mm(P, pO, pO[pr, hs], QK, QK[:, h, 0, lo:lo + 64], Sb[d], Sb[d][:, h, :], start=False, stop=True)
                        for h in range(4):
                            hs = slice(h * 128, (h + 1) * 128)
                            mm(P, pS, pS[:, hs], KT, KT[pr, hs], VT, VT[pr, hs])
                        P.op("dve", "tensor_tensor", out=Stmp[d][:], in0=pS[:, 0:512].rearrange("p (h v) -> p h v", h=4),
                             in1=Sf[d][:], op=ALU.add, reads=[pS, Sf[d]], writes=[Stmp[d]])
                        P.op("dve", "tensor_tensor", out=Sf[d][:], in0=Stmp[d][:],
                             in1=GLt[d][:, :, chunk:chunk + 1].broadcast_to([128, 4, 128]), op=ALU.mult,
                             reads=[Stmp[d], GLt[d]], writes=[Sf[d]])
                        P.op("act", "activation", out=Sb[d][:], in_=Sf[d][:], func=AF.Copy, reads=[Sf[d]], writes=[Sb[d]])
                for d in ds:
                    tt = orders[d][step]
                    o_ = ot[d][s]
                    pO = bank[4 + d]
                    P.op("act", "activation", out=o_[:], in_=pO[:, 0:512], func=AF.Copy, reads=[pO], writes=[o_])
                    P.dma("pool", G.oH[d][b, tt], o_[:], o_, reads=[o_], writes=[G.oH[d]])


def stage_hgrn_post(P, G):
    bank = G.bank
    with P.scope():
        gn = P.sb("gn", [128, 128])
        P.dma("sp", gn[:], G.hg_norm[0:1, :].partition_broadcast(128), gn, reads=[G.hg_norm], writes=[gn])
        o0 = [P.sb(f"ho0_{i}", [128, 512]) for i in range(3)]
        o1 = [P.sb(f"ho1_{i}", [128, 512]) for i in range(3)]
        gt = [P.sb(f"hgt{i}", [128, 512], BF16) for i in range(3)]
        sq = P.sb("hsq", [128, 512])
        st = [P.sb(f"hst{i}", [128, 4]) for i in range(3)]
        yb = [P.sb(f"hyb{i}", [128, 4, 128], BF16) for i in range(3)]
        it = 0
        for b in range(NB):
            for tt in range(18):
                s = it % 3
                it += 1
                P.dma("sp", o0[s][:], G.oH[0][b, tt], o0[s], reads=[G.oH[0]], writes=[o0[s]])
                P.dma("sp", o1[s][:], G.oH[1][b, tt], o1[s], reads=[G.oH[1]], writes=[o1[s]])
                P.dma("sp", gt[s][:], G.HGt[b, tt], gt[s], reads=[G.HGt], writes=[gt[s]])
                o_ = o0[s]
                P.op("pool", "tensor_tensor", out=o_[:], in0=o_[:], in1=o1[s][:], op=ALU.add, reads=[o_, o1[s]], writes=[o_])
                P.op("pool", "tensor_tensor", out=sq[:], in0=o_[:], in1=o_[:], op=ALU.mult, reads=[o_], writes=[sq])
                P.op("dve", "tensor_reduce", out=st[s][:], in_=sq[:].rearrange("p (h v) -> p h v", h=4), axis=AX.X, op=ALU.add,
                     reads=[sq], writes=[st[s]])
                P.op("act", "activation", out=st[s][:], in_=st[s][:], func=AF.Sqrt, scale=1.0 / 128, bias=G.eps[:, 0:1],
                     reads=[st[s], G.eps], writes=[st[s]])
                P.op("dve", "reciprocal", out=st[s][:], in_=st[s][:], reads=[st[s]], writes=[st[s]])
                ov = o_[:].rearrange("p (h v) -> p h v", h=4)
                P.op("dve", "tensor_tensor", out=ov, in0=ov, in1=st[s][:].unsqueeze(2).broadcast_to([128, 4, 128]), op=ALU.mult,
                     reads=[o_, st[s]], writes=[o_])
                P.op("dve", "tensor_tensor", out=ov, in0=ov, in1=gn[:].unsqueeze(1).broadcast_to([128, 4, 128]), op=ALU.mult,
                     reads=[o_, gn], writes=[o_])
                P.op("pool", "tensor_tensor", out=o_[:], in0=o_[:], in1=gt[s][:], op=ALU.mult, reads=[o_, gt[s]], writes=[o_])
                ps = bank[it % 8]
                for c in range(4):
                    P.op("pe", "transpose", out=ps[:, c * 128:(c + 1) * 128], in_=o_[:, c * 128:(c + 1) * 128], identity=G.identf[:],
                         reads=[o_, G.identf], writes=[ps])
                P.op("act", "activation", out=yb[s][:, 0:2, :], in_=ps[:, 0:256].rearrange("p (c t) -> p c t", c=2), func=AF.Copy,
                     reads=[ps], writes=[yb[s]])
                P.op("dve", "tensor_copy", out=yb[s][:, 2:4, :], in_=ps[:, 256:512].rearrange("p (c t) -> p c t", c=2),
                     reads=[ps], writes=[yb[s]])
                P.dma("pool", G.yT[b, 4:8, :, tt * 128:(tt + 1) * 128].rearrange("c p t -> p c t"), yb[s][:], yb[s],
                      reads=[yb[s]], writes=[G.yT])


def load_gates(P, G, l, which, name="gate"):
    gts = []
    for j in range(3):
        g = P.sb(f"{name}{j}", [128, D])
        P.dma("sp", g[:], G.modD[l, j:j + 1, which * D:(which + 1) * D].partition_broadcast(128), g,
              reads=[G.modD], writes=[g])
        gts.append(g)
    return gts


def stage_outproj(P, G, yT, wsrc, l, T, ctx_len, xin_ap, xout_ap, xin_t, xout_t):
    bank = G.bank
    with P.scope():
        gts = load_gates(P, G, l, 2)
        w = P.sb("wout", [128, 8, D], BF16)
        for k in range(8):
            P.dma("pool", w[:, k, :], wsrc[k * 128:(k + 1) * 128, :], w, reads=[wsrc], writes=[w])
        yt = [P.sb(f"yt{i}", [128, 8, 128], BF16) for i in range(3)]
        xt = [P.sb(f"xt{i}", [128, D]) for i in range(3)]
        rt = [P.sb(f"rt{i}", [128, D]) for i in range(3)]
        it = 0
        for b in range(NB):
            for tt in range(T // 128):
                s = it % 3
                it += 1
                j = 2 if tt * 128 < ctx_len else b
                P.dma("sp", yt[s][:], yT[b, :, :, tt * 128:(tt + 1) * 128].rearrange("c p t -> p c t"), yt[s],
                      reads=[yT], writes=[yt[s]])
                P.dma("sp", xt[s][:], xin_ap(b, tt), xt[s], reads=[xin_t], writes=[xt[s]])
                for half in range(2):
                    ps = bank[(it % 3) * 2 + half]
                    for c in range(8):
                        mm(P, ps, ps[:, 0:512], yt[s], yt[s][:, c, :], w, w[:, c, half * 512:(half + 1) * 512],
                           start=(c == 0), stop=(c == 7))
                    P.op("dve", "tensor_tensor", out=rt[s][:, half * 512:(half + 1) * 512], in0=ps[:, 0:512],
                         in1=gts[j][:, half * 512:(half + 1) * 512], op=ALU.mult, reads=[ps, gts[j]], writes=[rt[s]])
                P.op("dve", "tensor_tensor", out=rt[s][:], in0=rt[s][:], in1=xt[s][:], op=ALU.add,
                     reads=[rt[s], xt[s]], writes=[rt[s]])
                P.dma("pool", xout_ap(b, tt), rt[s][:], rt[s], reads=[rt[s]], writes=[xout_t])


def swiglu_pass(P, G, fT, blocks, wg_ap, wu_ap, wd_ap, nch, gts, ctx_len, xin_ap, xout_ap, xin_t, xout_t, wsrc_ts,
                tokw=None, tokw_col=None, tiles_per_b=None):
    bank = G.bank
    F_ = nch * 128
    with P.scope():
        wg = P.sb("wg", [128, 8, F_], BF16)
        wu = P.sb("wu", [128, 8, F_], BF16)
        wd = P.sb("wd", [128, nch, D], BF16)
        for k in range(8):
            P.dma("pool", wg[:, k, :], wg_ap[k * 128:(k + 1) * 128, :], wg, reads=wsrc_ts, writes=[wg])
            P.dma("pool", wu[:, k, :], wu_ap[k * 128:(k + 1) * 128, :], wu, reads=wsrc_ts, writes=[wu])
        for c in range(nch):
            P.dma("pool", wd[:, c, :], wd_ap[c * 128:(c + 1) * 128, :], wd, reads=wsrc_ts, writes=[wd])
        ft = [P.sb(f"ft{i}", [128, 8, 512], BF16) for i in range(2)]
        sg = [P.sb(f"sg{i}", [128, 512]) for i in range(2)]
        act = [P.sb(f"act{i}", [128, nch, 512], BF16) for i in range(2)]
        xt = [P.sb(f"fxt{i}", [128, D]) for i in range(2)]
        rt = [P.sb(f"frt{i}", [128, D]) for i in range(2)]
        bi = 0
        ti = 0
        for b in range(NB):
            for (t0, t1) in blocks:
                n = t1 - t0
                F = ft[bi % 2]
                A_ = act[bi % 2]
                bi += 1
                for k in range(8):
                    P.dma("sp", F[:, k, 0:n], fT[b, :, k, t0:t1], F, reads=[fT], writes=[F])
                for jc in range(nch):
                    pg, pu = bank[jc % 2], bank[2 + jc % 2]
                    cs = slice(jc * 128, (jc + 1) * 128)
                    for k in range(8):
                        mm(P, pg, pg[:, 0:n], wg, wg[:, k, cs], F, F[:, k, 0:n], start=(k == 0), stop=(k == 7))
                    for k in range(8):
                        mm(P, pu, pu[:, 0:n], wu, wu[:, k, cs], F, F[:, k, 0:n], start=(k == 0), stop=(k == 7))
                    S_ = sg[jc % 2]
                    P.op("act", "activation", out=S_[:, 0:n], in_=pg[:, 0:n], func=AF.Silu, reads=[pg], writes=[S_])
                    P.op("dve", "tensor_tensor", out=A_[:, jc, 0:n], in0=S_[:, 0:n], in1=pu[:, 0:n], op=ALU.mult,
                         reads=[S_, pu], writes=[A_])
                for ts in range(n // 128):
                    tt = t0 // 128 + ts
                    s = ti % 2
                    ti += 1
                    j = 2 if tt * 128 < ctx_len else b
                    P.dma("sp", xt[s][:], xin_ap(b, tt), xt[s], reads=[xin_t], writes=[xt[s]])
                    for half in range(2):
                        po = bank[4 + (ti % 2) * 2 + half]
                        for jc in range(nch):
                            mm(P, po, po[:, 0:512], A_, A_[:, jc, ts * 128:(ts + 1) * 128], wd,
                               wd[:, jc, half * 512:(half + 1) * 512], start=(jc == 0), stop=(jc == nch - 1))
                        hs = slice(half * 512, (half + 1) * 512)
                        if tokw is None:
                            P.op("dve", "tensor_tensor", out=rt[s][:, hs], in0=po[:, 0:512], in1=gts[j][:, hs], op=ALU.mult,
                                 reads=[po, gts[j]], writes=[rt[s]])
                        else:
                            tix = b * tiles_per_b + tt
                            P.op("dve", "scalar_tensor_tensor", out=rt[s][:, hs], in0=po[:, 0:512],
                                 scalar=tokw[:, tix, tokw_col:tokw_col + 1], in1=gts[j][:, hs], op0=ALU.mult, op1=ALU.mult,
                                 reads=[po, gts[j], tokw], writes=[rt[s]])
                    P.op("dve", "tensor_tensor", out=rt[s][:], in0=rt[s][:], in1=xt[s][:], op=ALU.add,
                         reads=[rt[s], xt[s]], writes=[rt[s]])
                    P.dma("pool", xout_ap(b, tt), rt[s][:], rt[s], reads=[rt[s]], writes=[xout_t])


def stage_ffn0(P, G):
    with P.scope():
        gts = load_gates(P, G, 0, 5)
        for hf in range(2):
            cs = slice(hf * 1408, (hf + 1) * 1408)
            xin_t = G.x1 if hf == 0 else G.x2
            swiglu_pass(P, G, G.fT0, TBLK, G.ffn_wg[:, cs], G.ffn_wu[:, cs], G.ffn_wd[cs, :], 11, gts, TC,
                        (lambda b, tt, X=xin_t: X[b, tt * 128:(tt + 1) * 128, :]),
                        (lambda b, tt: G.x2[b, tt * 128:(tt + 1) * 128, :]), xin_t, G.x2,
                        [G.ffn_wg, G.ffn_wu, G.ffn_wd])


LBLK = [(TC + i * 512, TC + (i + 1) * 512) for i in range(4)]


def stage_att_inproj(P, G, b):
    bank = G.bank
    with P.scope():
        hT = P.sb("hTres", [128, 8, TT], BF16)
        for k in range(8):
            P.dma("sp", hT[:, k, :], G.hT1[b, :, k, :], hT, reads=[G.hT1], writes=[hT])
        rope = P.sb("rope", [128, 2, TL])
        P.dma("sp", rope[:], G.rope[:], rope, reads=[G.rope], writes=[rope])
        perm = P.sb("perm", [128, 128], BF16)
        P.dma("pool", perm[:], G.perm[:], perm, reads=[G.perm], writes=[perm])
        WP = WPrefetch(P, G.att_w_in, list(range(10)))
        wv = P.sb("wv", [128, 8, 256], BF16)
        load_w_chunk(P, wv, G.att_w_in, 1280, 256)
        qraw = [P.sb(f"qraw{i}", [128, 512], BF16) for i in range(2)]
        t1 = [P.sb(f"t1_{i}", [128, 512]) for i in range(2)]
        t2 = [P.sb(f"t2_{i}", [128, 512]) for i in range(2)]
        qo = [P.sb(f"qo{i}", [128, 512], BF16) for i in range(2)]
        kc_ = [P.sb(f"kc{i}", [128, 256], BF16) for i in range(2)]
        vtk = [P.sb(f"vtk{i}", [128, 256], BF16) for i in range(2)]
        it = 0
        for ch in range(10):
            wt = WP.get(ch)
            if ch < 8:
                dst = lambda tl0, n, ch=ch: G.qT[b, 2 * ch:2 * ch + 2, :, tl0:tl0 + n].rearrange("h k t -> (h k) t")
                dt_ = G.qT
            else:
                kc = ch - 8
                dst = lambda tl0, n, kc=kc: G.kT[b, 2 * kc:2 * kc + 2, :, TC + tl0:TC + tl0 + n].rearrange("h k t -> (h k) t")
                dt_ = G.kT
                ps = bank[0]
                for k in range(8):
                    mm(P, ps, ps[:, 0:TC], wt, wt[:, k, :], hT, hT[:, k, 0:TC], start=(k == 0), stop=(k == 7))
                kk_ = kc_[kc % 2]
                P.op("act", "activation", out=kk_[:], in_=ps[:, 0:TC], func=AF.Copy, reads=[ps], writes=[kk_])
                P.dma("pool", G.kT[b, 2 * kc:2 * kc + 2, :, 0:TC].rearrange("h k t -> (h k) t"), kk_[:], kk_,
                      reads=[kk_], writes=[G.kT])
            for (t0, t1_) in LBLK:
                s = it % 2
                it += 1
                tl0 = t0 - TC
                p1, p2 = bank[(it % 2) * 2], bank[(it % 2) * 2 + 1]
                for k in range(8):
                    mm(P, p1, p1[:, 0:512], wt, wt[:, k, :], hT, hT[:, k, t0:t1_], start=(k == 0), stop=(k == 7))
                P.op("act", "activation", out=qraw[s][:], in_=p1[:, 0:512], func=AF.Copy, reads=[p1], writes=[qraw[s]])
                mm(P, p2, p2[:, 0:512], perm, perm[:], qraw[s], qraw[s][:])
                P.op("dve", "tensor_tensor", out=t1[s][:], in0=p1[:, 0:512], in1=rope[:, 0, tl0:tl0 + 512], op=ALU.mult,
                     reads=[p1, rope], writes=[t1[s]])
                P.op("dve", "tensor_tensor", out=t2[s][:], in0=p2[:, 0:512], in1=rope[:, 1, tl0:tl0 + 512], op=ALU.mult,
                     reads=[p2, rope], writes=[t2[s]])
                P.op("dve", "tensor_tensor", out=qo[s][:], in0=t1[s][:], in1=t2[s][:], op=ALU.add,
                     reads=[t1[s], t2[s]], writes=[qo[s]])
                P.dma("pool", dst(tl0, 512), qo[s][:], qo[s], reads=[qo[s]], writes=[dt_])
        for tt in range(18):
            ps = bank[4 + tt % 4]
            for k in range(8):
                mm(P, ps, ps[:, 0:256], hT, hT[:, k, tt * 128:(tt + 1) * 128], wv, wv[:, k, :], start=(k == 0), stop=(k == 7))
            v_ = vtk[tt % 2]
            P.op("act", "activation", out=v_[:], in_=ps[:, 0:256], func=AF.Copy, reads=[ps], writes=[v_])
            P.dma("sp", G.Vt1[b, tt], v_[:], v_, reads=[v_], writes=[G.Vt1])


def stage_attention(P, G):
    bank = G.bank
    with P.scope():
        msk = P.sb("mskb", [128, 4, 128], BF16)
        P.dma("pool", msk[:], G.masks[:], msk, reads=[G.masks], writes=[msk])
        es = P.sb("es", [64, 16])
        P.dma("sp", es[:], G.att_sink[0:1, :].partition_broadcast(64), es, reads=[G.att_sink], writes=[es])
        P.op("act", "activation", out=es[:], in_=es[:], func=AF.Exp, reads=[es], writes=[es])
        ones = P.sb("ones", [128, 64], BF16)
        P.op("dve", "memset", ap=ones[:], constant=1.0, writes=[ones])
        Kt = [P.sb(f"Kt{i}", [64, TT], BF16) for i in range(2)]
        Vv = [P.sb(f"Vv{i}", [128, 18, 64], BF16) for i in range(2)]
        Qt = [P.sb(f"Qt{i}", [64, 4, TL], BF16) for i in range(2)]
        E = [P.sb(f"E{i}", [128, 4, 128], BF16) for i in range(6)]
        den = [P.sb(f"den{i}", [64, 4, 128]) for i in range(2)]
        ob = [P.sb(f"ob{i}", [64, 4, 128], BF16) for i in range(2)]
        g = 0
        r = 0
        ii = 0
        for b in range(NB):
            for hk in range(4):
                K_, V_, Q_ = Kt[g % 2], Vv[g % 2], Qt[g % 2]
                g += 1
                P.dma("sp", K_[:], G.kT[b, hk], K_, reads=[G.kT], writes=[K_])
                P.dma("sp", V_[:], G.Vt1[b, :, :, hk * 64:(hk + 1) * 64].rearrange("n t c -> t n c"), V_,
                      reads=[G.Vt1], writes=[V_])
                P.dma("sp", Q_[:], G.qT[b, 4 * hk:4 * hk + 4].rearrange("g k t -> k g t"), Q_, reads=[G.qT], writes=[Q_])
                for i in range(16):
                    tiles = [(0, None), (1, None)]
                    if i > 0:
                        tiles.append((2 + i - 1, 3))
                    tiles.append((2 + i, None))
                    if i < 15:
                        tiles.append((2 + i + 1, 1))
                    pN, pD = bank[4 + ii % 2], bank[6 + ii % 2]
                    Es = []
                    for ti, (kt, mi) in enumerate(tiles):
                        pS = bank[r % 4]
                        E_ = E[r % 6]
                        r += 1
                        mm(P, pS, pS[:, 0:512], K_, K_[:, kt * 128:(kt + 1) * 128], Q_, Q_[:, :, i * 128:(i + 1) * 128])
                        P.op("act", "activation", out=E_[:], in_=pS[:, 0:512].rearrange("p (g t) -> p g t", g=4),
                             func=AF.Exp, scale=0.125, reads=[pS], writes=[E_])
                        if mi is not None:
                            P.op("dve", "tensor_tensor", out=E_[:], in0=E_[:],
                                 in1=msk[:, mi, :].unsqueeze(1).broadcast_to([128, 4, 128]), op=ALU.mult,
                                 reads=[E_, msk], writes=[E_])
                        Es.append(E_)
                    for ti, (kt, mi) in enumerate(tiles):
                        E_ = Es[ti]
                        first, last = ti == 0, ti == len(tiles) - 1
                        mm(P, pN, pN[0:64, 0:512], V_, V_[:, kt, :], E_, E_[:], start=first, stop=last)
                        mm(P, pD, pD[0:64, 0:512], ones, ones[:], E_, E_[:], start=first, stop=last)
                    s = ii % 2
                    ii += 1
                    P.op("dve", "tensor_tensor", out=den[s][:], in0=pD[0:64, 0:512].rearrange("p (g t) -> p g t", g=4),
                         in1=es[:, 4 * hk:4 * hk + 4].unsqueeze(2).broadcast_to([64, 4, 128]), op=ALU.add,
                         reads=[pD, es], writes=[den[s]])
                    P.op("dve", "reciprocal", out=den[s][:], in_=den[s][:], reads=[den[s]], writes=[den[s]])
                    P.op("dve", "tensor_tensor", out=ob[s][:], in0=pN[0:64, 0:512].rearrange("p (g t) -> p g t", g=4),
                         in1=den[s][:], op=ALU.mult, reads=[pN, den[s]], writes=[ob[s]])
                    P.dma("pool", G.oT1[b, 2 * hk:2 * hk + 2, :, i * 128:(i + 1) * 128].rearrange("c (hh k) t -> k (c hh) t", hh=2),
                          ob[s][:], ob[s], reads=[ob[s]], writes=[G.oT1])


def stage_router(P, G):
    bank = G.bank
    with P.scope():
        rw = P.sb("rw", [128, 8, 8])
        P.dma("sp", rw[:], G.moe_router[:].rearrange("(k p) e -> p k e", p=128), rw, reads=[G.moe_router], writes=[rw])
        rb = P.sb("rb", [128, 8])
        P.dma("sp", rb[:], G.moe_router_b[0:1, :].partition_broadcast(128), rb, reads=[G.moe_router_b], writes=[rb])
        ff = [P.sb(f"ff{i}", [128, 8, 128]) for i in range(2)]
        lg = [P.sb(f"lg{i}", [128, 8]) for i in range(2)]
        tm = [P.sb(f"tm{i}", [128, 4, 8]) for i in range(2)]
        sc = [P.sb(f"rsc{i}", [128, 4]) for i in range(2)]
        it = 0
        for b in range(NB):
            for tt in range(16):
                s = it % 2
                tix = b * 16 + tt
                it += 1
                P.dma("sp", ff[s][:], G.fT1f[b, :, :, tt * 128:(tt + 1) * 128], ff[s], reads=[G.fT1f], writes=[ff[s]])
                ps = bank[it % 2]
                for k in range(8):
                    mm(P, ps, ps[:, 0:8], ff[s], ff[s][:, k, :], rw, rw[:, k, :], start=(k == 0), stop=(k == 7))
                L_, T_, S_ = lg[s], tm[s], sc[s]
                P.op("dve", "tensor_tensor", out=L_[:], in0=ps[:, 0:8], in1=rb[:], op=ALU.add, reads=[ps, rb], writes=[L_])
                P.op("dve", "tensor_reduce", out=S_[:, 0:1], in_=L_[:], axis=AX.X, op=ALU.max, reads=[L_], writes=[S_])
                P.op("dve", "tensor_scalar", out=T_[:, 0, :], in0=L_[:], scalar1=S_[:, 0:1], scalar2=-1e30, op0=ALU.is_equal,
                     op1=ALU.mult, reads=[L_, S_], writes=[T_])
                P.op("dve", "tensor_tensor", out=T_[:, 0, :], in0=T_[:, 0, :], in1=L_[:], op=ALU.add, reads=[T_, L_], writes=[T_])
                P.op("dve", "tensor_reduce", out=S_[:, 1:2], in_=T_[:, 0, :], axis=AX.X, op=ALU.max, reads=[T_], writes=[S_])
                P.op("dve", "tensor_scalar", out=T_[:, 1, :], in0=L_[:], scalar1=S_[:, 1:2], scalar2=None, op0=ALU.is_ge,
                     reads=[L_, S_], writes=[T_])
                P.op("dve", "tensor_scalar", out=S_[:, 2:3], in0=S_[:, 0:1], scalar1=-1.0, scalar2=None, op0=ALU.mult,
                     reads=[S_], writes=[S_])
                P.op("act", "activation", out=T_[:, 2, :], in_=L_[:], func=AF.Exp, bias=S_[:, 2:3], reads=[L_, S_], writes=[T_])
                P.op("dve", "tensor_tensor", out=T_[:, 2, :], in0=T_[:, 2, :], in1=T_[:, 1, :], op=ALU.mult, reads=[T_], writes=[T_])
                P.op("dve", "tensor_reduce", out=S_[:, 3:4], in_=T_[:, 2, :], axis=AX.X, op=ALU.add, reads=[T_], writes=[S_])
                P.op("dve", "reciprocal", out=S_[:, 3:4], in_=S_[:, 3:4], reads=[S_], writes=[S_])
                P.op("dve", "tensor_scalar", out=G.WTm[:, tix, :], in0=T_[:, 2, :], scalar1=S_[:, 3:4], scalar2=None, op0=ALU.mult,
                     reads=[T_, S_], writes=[G.WTm])


def stage_moe(P, G, experts=range(8)):
    with P.scope():
        gts = load_gates(P, G, 1, 5)
        first = True
        blocks = [(i * 512, (i + 1) * 512) for i in range(4)]
        for e in experts:
            for hf in range(2):
                cs = slice(hf * 1408, (hf + 1) * 1408)
                xin_t = G.x3 if first else G.x4
                first = False
                swiglu_pass(P, G, G.fT1, blocks, G.moe_wg[e, :, cs], G.moe_wu[e, :, cs], G.moe_wd[e, cs, :], 11, gts, 0,
                            (lambda b, tt, X=xin_t: X[b, tt * 128:(tt + 1) * 128, :]),
                            (lambda b, tt: G.x4[b, tt * 128:(tt + 1) * 128, :]), xin_t, G.x4,
                            [G.moe_wg, G.moe_wu, G.moe_wd], tokw=G.WTm, tokw_col=e, tiles_per_b=16)


def stage_final(P, G, src):
    with P.scope():
        gf = P.sb("gfin", [128, D])
        P.dma("sp", gf[:], G.norm_final[0:1, :].partition_broadcast(128), gf, reads=[G.norm_final], writes=[gf])
        xt = [P.sb(f"zxt{i}", [128, D]) for i in range(2)]
        sq = P.sb("zsq", [128, D], BF16)
        ss = [P.sb(f"zss{i}", [128, 1]) for i in range(2)]
        yo = [P.sb(f"zyo{i}", [128, D]) for i in range(2)]
        it = 0
        for b in range(NB):
            for tt in range(16):
                s = it % 2
                it += 1
                P.dma("sp", xt[s][:], src[b, tt * 128:(tt + 1) * 128, :], xt[s], reads=[src], writes=[xt[s]])
                P.op("act", "activation", out=sq[:], in_=xt[s][:], func=AF.Square, accum_out=ss[s][:], reads=[xt[s]], writes=[sq, ss[s]])
                P.op("act", "activation", out=ss[s][:], in_=ss[s][:], func=AF.Sqrt, scale=1.0 / D, bias=G.eps[:, 0:1],
                     reads=[ss[s], G.eps], writes=[ss[s]])
                P.op("dve", "reciprocal", out=ss[s][:], in_=ss[s][:], reads=[ss[s]], writes=[ss[s]])
                P.op("dve", "scalar_tensor_tensor", out=yo[s][:], in0=xt[s][:], scalar=ss[s][:, 0:1], in1=gf[:], op0=ALU.mult,
                     op1=ALU.mult, reads=[xt[s], ss[s], gf], writes=[yo[s]])
                P.dma("pool", G.out[b, tt * 128:(tt + 1) * 128, :], yo[s][:], yo[s], reads=[yo[s]], writes=[G.out])


I32 = mybir.dt.int32
BS = 512
SUBS = BS // 512
NBLK = (NB * TL * 2) // BS + 8
NSLOT = NBLK * BS
DUMMY = NB * TL


def stage_moe_sparse(P, G, nblk=NBLK):
    bank = G.bank
    IOA = bass.IndirectOffsetOnAxis
    with P.scope():
        WAB = P.sb("WAB", [128, 32, 2])
        SAB = P.sb("SAB", [128, 2, 32], I32)
        IDXG = P.sb("IDXG", [128, NBLK, 2], I32)
        IDXD = P.sb("IDXD", [128, NBLK, 2], I32)
        _phaseA = P.scope()
        _phaseA.__enter__()
        LG = G.LG
        T3 = lambda nm: P.sb(nm, [128, 32, 8])
        T2 = lambda nm: P.sb(nm, [128, 32])
        bc3 = lambda t2: t2[:].unsqueeze(2).broadcast_to([128, 32, 8])
        M1, M2, NM1, DEN = T2("M1"), T2("M2"), T2("NM1"), T2("DEN")
        TMP, SEL, WT = T3("TMP"), T3("SEL"), T3("WT")
        P.op("dve", "tensor_reduce", out=M1[:], in_=LG[:], axis=AX.X, op=ALU.max, reads=[LG], writes=[M1])
        P.op("dve", "tensor_tensor", out=TMP[:], in0=LG[:], in1=bc3(M1), op=ALU.is_equal, reads=[LG, M1], writes=[TMP])
        P.op("dve", "scalar_tensor_tensor", out=TMP[:], in0=TMP[:], scalar=-1e30, in1=LG[:], op0=ALU.mult, op1=ALU.add,
             reads=[TMP, LG], writes=[TMP])
        P.op("dve", "tensor_reduce", out=M2[:], in_=TMP[:], axis=AX.X, op=ALU.max, reads=[TMP], writes=[M2])
        P.op("dve", "tensor_tensor", out=SEL[:], in0=LG[:], in1=bc3(M2), op=ALU.is_ge, reads=[LG, M2], writes=[SEL])
        P.op("dve", "tensor_tensor", out=TMP[:], in0=LG[:], in1=bc3(M1), op=ALU.subtract, reads=[LG, M1], writes=[TMP])
        P.op("act", "activation", out=TMP[:], in_=TMP[:], func=AF.Exp, reads=[TMP], writes=[TMP])
        P.op("dve", "tensor_tensor", out=TMP[:], in0=TMP[:], in1=SEL[:], op=ALU.mult, reads=[TMP, SEL], writes=[TMP])
        P.op("dve", "tensor_reduce", out=DEN[:], in_=TMP[:], axis=AX.X, op=ALU.add, reads=[TMP], writes=[DEN])
        P.op("dve", "reciprocal", out=DEN[:], in_=DEN[:], reads=[DEN], writes=[DEN])
        P.op("dve", "tensor_tensor", out=WT[:], in0=TMP[:], in1=bc3(DEN), op=ALU.mult, reads=[TMP, DEN], writes=[WT])
        cst = P.sb("mcst", [128, 512])
        P.dma("sp", cst[:], G.moe_consts[:], cst, reads=[G.moe_consts], writes=[cst])
        TH = cst[:, 0:64].rearrange("p (e m) -> p e m", m=8)
        JJ = cst[:, 64:64 + NBLK]
        CG = cst[:, 88:90]
        CD = cst[:, 104:106]
        TIDc = cst[:, 128:160]
        RM = cst[:, 256:512]
        selb = P.sb("selb", [128, 256], BF16)
        P.op("dve", "tensor_copy", out=selb[:], in_=SEL[:].rearrange("p i e -> p (i e)"), reads=[SEL], writes=[selb])
        mb_ = P.sb("mskb2", [128, 4, 128], BF16)
        P.dma("pool", mb_[:], G.masks[:], mb_, reads=[G.masks], writes=[mb_])
        onesb = P.sb("onesb", [128, 128], BF16)
        P.op("dve", "memset", ap=onesb[:], constant=1.0, writes=[onesb])
        mm(P, bank[2], bank[2][:, 0:256], mb_, mb_[:, 0, :], selb, selb[:])
        mm(P, bank[3], bank[3][:, 0:256], onesb, onesb[:], selb, selb[:])
        SLOT = T3("SLOT")
        P.op("dve", "tensor_copy", out=SLOT[:], in_=bank[2][:, 0:256].rearrange("p (i e) -> p i e", e=8), reads=[bank[2]], writes=[SLOT])
        TOTp = P.sb("TOTp", [128, 8, 32])
        P.op("dve", "tensor_copy", out=TOTp[:], in_=bank[3][:, 0:256].rearrange("p (i e) -> p e i", e=8), reads=[bank[3]], writes=[TOTp])
        INC = P.sb("INC", [128, 8, 32])
        P.op("dve", "tensor_tensor_scan", out=INC[:].rearrange("p e i -> p (e i)"), data0=RM, data1=TOTp[:].rearrange("p e i -> p (e i)"),
             initial=0.0, op0=ALU.mult, op1=ALU.add, reads=[cst, TOTp], writes=[INC])
        OFFp = P.sb("OFFp", [128, 8, 32])
        P.op("dve", "tensor_tensor", out=OFFp[:], in0=INC[:], in1=TOTp[:], op=ALU.subtract, reads=[INC, TOTp], writes=[OFFp])
        CMP = P.sb("CMP", [128, 8, 8])
        P.op("dve", "tensor_tensor", out=CMP[:], in0=INC[:, :, 31:32].broadcast_to([128, 8, 8]), in1=TH, op=ALU.is_gt,
             reads=[INC, cst], writes=[CMP])
        nbk = P.sb("nbk", [128, 8])
        P.op("dve", "tensor_reduce", out=nbk[:], in_=CMP[:], axis=AX.X, op=ALU.add, reads=[CMP], writes=[nbk])
        PEI = P.sb("PEI", [128, 8])
        P.op("dve", "tensor_tensor_scan", out=PEI[:], data0=onesb[:, 0:8], data1=nbk[:], initial=0.0, op0=ALU.mult, op1=ALU.add,
             reads=[onesb, nbk], writes=[PEI])
        PST = P.sb("PST", [128, 8])
        P.op("dve", "tensor_tensor", out=PST[:], in0=PEI[:], in1=nbk[:], op=ALU.subtract, reads=[PEI, nbk], writes=[PST])
        P.op("dve", "tensor_scalar", out=PST[:], in0=PST[:], scalar1=float(BS), scalar2=None, op0=ALU.mult, reads=[PST], writes=[PST])
        P.op("dve", "tensor_tensor", out=SLOT[:], in0=SLOT[:], in1=OFFp[:].rearrange("p e i -> p i e"), op=ALU.add,
             reads=[SLOT, OFFp], writes=[SLOT])
        P.op("dve", "tensor_tensor", out=SLOT[:], in0=SLOT[:], in1=PST[:].unsqueeze(1).broadcast_to([128, 32, 8]), op=ALU.add,
             reads=[SLOT, PST], writes=[SLOT])
        V = T3("V")
        P.op("dve", "scalar_tensor_tensor", out=V[:], in0=SLOT[:], scalar=1.0, in1=SEL[:], op0=ALU.add, op1=ALU.mult,
             reads=[SLOT, SEL], writes=[V])
        MA, MB = T2("MA"), T2("MB")
        P.op("dve", "tensor_reduce", out=MA[:], in_=V[:], axis=AX.X, op=ALU.max, reads=[V], writes=[MA])
        P.op("dve", "tensor_tensor", out=TMP[:], in0=V[:], in1=bc3(MA), op=ALU.is_equal, reads=[V, MA], writes=[TMP])
        IS2 = T3("IS2")
        P.op("dve", "tensor_tensor", out=IS2[:], in0=TMP[:], in1=WT[:], op=ALU.mult, reads=[TMP, WT], writes=[IS2])
        P.op("dve", "tensor_reduce", out=WAB[:, :, 0], in_=IS2[:], axis=AX.X, op=ALU.add, reads=[IS2], writes=[WAB])
        P.op("dve", "tensor_tensor", out=TMP[:], in0=TMP[:], in1=V[:], op=ALU.mult, reads=[TMP, V], writes=[TMP])
        P.op("dve", "tensor_tensor", out=V[:], in0=V[:], in1=TMP[:], op=ALU.subtract, reads=[V, TMP], writes=[V])
        P.op("dve", "tensor_reduce", out=MB[:], in_=V[:], axis=AX.X, op=ALU.max, reads=[V], writes=[MB])
        P.op("dve", "tensor_tensor", out=TMP[:], in0=V[:], in1=bc3(MB), op=ALU.is_equal, reads=[V, MB], writes=[TMP])
        P.op("dve", "tensor_tensor", out=IS2[:], in0=TMP[:], in1=WT[:], op=ALU.mult, reads=[TMP, WT], writes=[IS2])
        P.op("dve", "tensor_reduce", out=WAB[:, :, 1], in_=IS2[:], axis=AX.X, op=ALU.add, reads=[IS2], writes=[WAB])
        P.op("dve", "tensor_scalar", out=MA[:], in0=MA[:], scalar1=-1.0, scalar2=None, op0=ALU.add, reads=[MA], writes=[MA])
        P.op("dve", "tensor_scalar", out=MB[:], in0=MB[:], scalar1=-1.0, scalar2=None, op0=ALU.add, reads=[MB], writes=[MB])
        P.op("dve", "tensor_copy", out=SAB[:, 0, :], in_=MA[:], reads=[MA], writes=[SAB])
        P.op("dve", "tensor_copy", out=SAB[:, 1, :], in_=MB[:], reads=[MB], writes=[SAB])
        CJ = P.sb("CJ", [128, NBLK, 8])
        P.op("dve", "tensor_tensor", out=CJ[:], in0=PEI[:].unsqueeze(1).broadcast_to([128, NBLK, 8]),
             in1=JJ.unsqueeze(2).broadcast_to([128, NBLK, 8]), op=ALU.is_le, reads=[PEI, cst], writes=[CJ])
        EJ = P.sb("EJ", [128, NBLK])
        P.op("dve", "tensor_reduce", out=EJ[:], in_=CJ[:], axis=AX.X, op=ALU.add, reads=[CJ], writes=[EJ])
        P.op("dve", "tensor_scalar", out=EJ[:], in0=EJ[:], scalar1=7.0, scalar2=None, op0=ALU.min, reads=[EJ], writes=[EJ])
        IGf = P.sb("IGf", [128, NBLK, 2])
        P.op("dve", "scalar_tensor_tensor", out=IGf[:], in0=EJ[:].unsqueeze(2).broadcast_to([128, NBLK, 2]), scalar=256.0,
             in1=CG.unsqueeze(1).broadcast_to([128, NBLK, 2]), op0=ALU.mult, op1=ALU.add, reads=[EJ, cst], writes=[IGf])
        P.op("dve", "tensor_copy", out=IDXG[:], in_=IGf[:], reads=[IGf], writes=[IDXG])
        IDf = P.sb("IDf", [128, NBLK, 2])
        P.op("dve", "scalar_tensor_tensor", out=IDf[:], in0=EJ[:].unsqueeze(2).broadcast_to([128, NBLK, 2]), scalar=256.0,
             in1=CD.unsqueeze(1).broadcast_to([128, NBLK, 2]), op0=ALU.mult, op1=ALU.add, reads=[EJ, cst], writes=[IDf])
        P.op("dve", "tensor_copy", out=IDXD[:], in_=IDf[:], reads=[IDf], writes=[IDXD])
        ini = P.sb("ini", [128, NSLOT // 128], I32)
        P.op("dve", "memset", ap=ini[:], constant=DUMMY, writes=[ini])
        P.dma("sp", G.tokidx[:, 0].rearrange("(p c) -> p c", c=NSLOT // 128), ini[:], ini, reads=[ini], writes=[G.tokidx])
        tid = P.sb("tid", [128, 32], I32)
        P.op("dve", "tensor_copy", out=tid[:], in_=TIDc, reads=[cst], writes=[tid])
        for i in range(32):
            for ab in range(2):
                P.dma("pool", None, None, tid, reads=[tid, SAB], writes=[G.tokidx], meth="indirect_dma_start",
                      out=G.tokidx[:, :], out_offset=IOA(ap=SAB[:, ab, i:i + 1], axis=0), in_=tid[:, i:i + 1], in_offset=None)
        _phaseA.__exit__(None, None, None)
        WG2 = G.moe_wg[:, :]
        WU2 = G.moe_wu[:, :]
        WD2 = G.moe_wd[:, :]
        with P.scope():
            wg = [P.sb(f"swg{i}", [128, 8, 1408], BF16) for i in range(2)]
            wu = [P.sb(f"swu{i}", [128, 8, 1408], BF16) for i in range(2)]
            wd = [P.sb(f"swd{i}", [128, 11, D], BF16) for i in range(2)]
            idx = [P.sb(f"sidx{i}", [128, 1], I32) for i in range(8)]
            xg = [P.sb(f"sxg{i}", [128, D]) for i in range(4)]
            xTs = [P.sb(f"sxT{i}", [128, SUBS, 8, 512], BF16) for i in range(2)]
            sg = [P.sb(f"ssg{i}", [128, 512]) for i in range(2)]
            act = P.sb("sact", [128, 11, 512], BF16)
            yt = [P.sb(f"syt{i}", [128, D]) for i in range(3)]
            yi = 0
            gstate = {"gi": 0}

            def emit_gather(j):
                xT = xTs[j % 2]
                gi = gstate["gi"]
                for sub in range(SUBS):
                    for c in range(4):
                        ix = idx[gi % 8]
                        X_ = xg[gi % 4]
                        gi += 1
                        r0 = j * BS + sub * 512 + c * 128
                        P.dma("sp", ix[:], G.tokidx[r0:r0 + 128, :], ix, reads=[G.tokidx], writes=[ix])
                        P.dma("pool", None, None, X_, reads=[ix, G.f_tok], writes=[X_], meth="indirect_dma_start",
                              out=X_[:], out_offset=None, in_=G.f_tok[:, :], in_offset=IOA(ap=ix[:, 0:1], axis=0))
                        for k in range(8):
                            ps = bank[4 + (gi % 2) * 2 + k // 4]
                            P.op("pe", "transpose", out=ps[:, (k % 4) * 128:(k % 4 + 1) * 128], in_=X_[:, k * 128:(k + 1) * 128],
                                 identity=G.identf[:], reads=[X_, G.identf], writes=[ps])
                        for kh in range(2):
                            ps = bank[4 + (gi % 2) * 2 + kh]
                            P.op("act" if kh else "dve", "activation" if kh else "tensor_copy",
                                 out=xT[:, sub, kh * 4:(kh + 1) * 4, c * 128:(c + 1) * 128],
                                 in_=ps[:, 0:512].rearrange("p (k t) -> p k t", k=4), reads=[ps], writes=[xT],
                                 **({"func": AF.Copy} if kh else {}))
                gstate["gi"] = gi

            emit_gather(0)
            for j in range(nblk):
                xT = xTs[j % 2]
                for hf in range(2):
                    s = (2 * j + hf) % 2
                    if not (os.environ.get("NO_WGATHER") and j > 0):
                      P.dma("pool", None, None, wg[s], reads=[IDXG, G.moe_wg], writes=[wg[s]], meth="indirect_dma_start",
                          out=wg[s][:].rearrange("p k c -> p (k c)"), out_offset=None, in_=WG2, in_offset=IOA(ap=IDXG[:, j, hf:hf + 1], axis=0))
                    if not (os.environ.get("NO_WGATHER") and j > 0):
                      P.dma("pool", None, None, wu[s], reads=[IDXG, G.moe_wu], writes=[wu[s]], meth="indirect_dma_start",
                          out=wu[s][:].rearrange("p k c -> p (k c)"), out_offset=None, in_=WU2, in_offset=IOA(ap=IDXG[:, j, hf:hf + 1], axis=0))
                    if not (os.environ.get("NO_WGATHER") and j > 0):
                      P.dma("pool", None, None, wd[s], reads=[IDXD, G.moe_wd], writes=[wd[s]], meth="indirect_dma_start",
                          out=wd[s][:].rearrange("p c n -> p (c n)"), out_offset=None, in_=WD2, in_offset=IOA(ap=IDXD[:, j, hf:hf + 1], axis=0))
                    for sub in range(SUBS):
                        for jc in range(11):
                            pg, pu = bank[jc % 2], bank[2 + jc % 2]
                            cs = slice(jc * 128, (jc + 1) * 128)
                            for k in range(8):
                                mm(P, pg, pg[:, 0:512], wg[s], wg[s][:, k, cs], xT, xT[:, sub, k, :], start=(k == 0), stop=(k == 7))
                            for k in range(8):
                                mm(P, pu, pu[:, 0:512], wu[s], wu[s][:, k, cs], xT, xT[:, sub, k, :], start=(k == 0), stop=(k == 7))
                            S_ = sg[jc % 2]
                            P.op("act", "activation", out=S_[:], in_=pg[:, 0:512], func=AF.Silu, reads=[pg], writes=[S_])
                            P.op("dve", "tensor_tensor", out=act[:, jc, :], in0=S_[:], in1=pu[:, 0:512], op=ALU.mult,
                                 reads=[S_, pu], writes=[act])
                        if hf == 0 and sub == SUBS - 1 and j + 1 < nblk:
                            emit_gather(j + 1)
                        for tt in range(4):
                            Y_ = yt[yi % 3]
                            yi += 1
                            for half in range(2):
                                po = bank[4 + (tt * 2 + half) % 4]
                                for jc in range(11):
                                    mm(P, po, po[:, 0:512], act, act[:, jc, tt * 128:(tt + 1) * 128], wd[s],
                                       wd[s][:, jc, half * 512:(half + 1) * 512], start=(jc == 0), stop=(jc == 10))
                                hs = slice(half * 512, (half + 1) * 512)
                                if half == 0:
                                    P.op("act", "activation", out=Y_[:, hs], in_=po[:, 0:512], func=AF.Copy, reads=[po], writes=[Y_])
                                else:
                                    P.op("dve", "tensor_copy", out=Y_[:, hs], in_=po[:, 0:512], reads=[po], writes=[Y_])
                            r0 = j * BS + sub * 512 + tt * 128
                            P.dma("sp", G.Yb[r0:r0 + 128, hf, :], Y_[:], Y_, reads=[Y_], writes=[G.Yb])
        with P.scope():
            gts = load_gates(P, G, 1, 5)
            gf = P.sb("gfin", [128, D])
            P.dma("sp", gf[:], G.norm_final[0:1, :].partition_broadcast(128), gf, reads=[G.norm_final], writes=[gf])
            ya = [P.sb(f"cya{i}", [128, D]) for i in range(2)]
            yb = [P.sb(f"cyb{i}", [128, D]) for i in range(2)]
            yy = [P.sb(f"cyy{i}", [128, 2, D]) for i in range(2)]
            zz = [P.sb(f"czz{i}", [128, 2, D]) for i in range(2)]
            xt = [P.sb(f"cxt{i}", [128, D]) for i in range(2)]
            sq = P.sb("csq", [128, D], BF16)
            ss = [P.sb(f"css{i}", [128, 1]) for i in range(2)]
            for b in range(NB):
                for tt in range(16):
                    i = b * 16 + tt
                    s = i % 2
                    for (dst_, ab) in ((yy[s], 0), (zz[s], 1)):
                        P.dma("pool", None, None, dst_, reads=[SAB, G.Yb], writes=[dst_], meth="indirect_dma_start",
                              out=dst_[:].rearrange("p h d -> p (h d)"), out_offset=None, in_=G.Yb[:].rearrange("n h d -> n (h d)"),
                              in_offset=IOA(ap=SAB[:, ab, i:i + 1], axis=0))
                    P.op("dve", "tensor_tensor", out=ya[s][:], in0=yy[s][:, 0, :], in1=yy[s][:, 1, :], op=ALU.add, reads=[yy[s]], writes=[ya[s]])
                    P.op("dve", "tensor_tensor", out=yb[s][:], in0=zz[s][:, 0, :], in1=zz[s][:, 1, :], op=ALU.add, reads=[zz[s]], writes=[yb[s]])
                    P.dma("sp", xt[s][:], G.x3[b, tt * 128:(tt + 1) * 128, :], xt[s], reads=[G.x3], writes=[xt[s]])
                    P.op("dve", "tensor_scalar", out=ya[s][:], in0=ya[s][:], scalar1=WAB[:, i, 0:1], scalar2=None, op0=ALU.mult,
                         reads=[ya[s], WAB], writes=[ya[s]])
                    P.op("dve", "scalar_tensor_tensor", out=ya[s][:], in0=yb[s][:], scalar=WAB[:, i, 1:2], in1=ya[s][:],
                         op0=ALU.mult, op1=ALU.add, reads=[ya[s], yb[s], WAB], writes=[ya[s]])
                    P.op("dve", "tensor_tensor", out=ya[s][:], in0=ya[s][:], in1=gts[b][:], op=ALU.mult, reads=[ya[s], gts[b]], writes=[ya[s]])
                    P.op("dve", "tensor_tensor", out=xt[s][:], in0=xt[s][:], in1=ya[s][:], op=ALU.add, reads=[xt[s], ya[s]], writes=[xt[s]])
                    P.op("act", "activation", out=sq[:], in_=xt[s][:], func=AF.Square, accum_out=ss[s][:], reads=[xt[s]], writes=[sq, ss[s]])
                    P.op("act", "activation", out=ss[s][:], in_=ss[s][:], func=AF.Sqrt, scale=1.0 / D, bias=G.eps[:, 0:1],
                         reads=[ss[s], G.eps], writes=[ss[s]])
                    P.op("dve", "reciprocal", out=ss[s][:], in_=ss[s][:], reads=[ss[s]], writes=[ss[s]])
                    P.op("dve", "scalar_tensor_tensor", out=yb[s][:], in0=xt[s][:], scalar=ss[s][:, 0:1], in1=gf[:], op0=ALU.mult,
                         op1=ALU.mult, reads=[xt[s], ss[s], gf], writes=[yb[s]])
                    P.dma("act", G.out[b, tt * 128:(tt + 1) * 128, :], yb[s][:], yb[s], reads=[yb[s]], writes=[G.out])


def stage_norm_tok(P, G):
    with P.scope():
        grow = P.sb("grow", [128, D])
        P.dma("sp", grow[:], G.norm_ffn_row[1:2, :].partition_broadcast(128), grow, reads=[G.norm_ffn_row], writes=[grow])
        GSb, SHb = [], []
        for j in range(2):
            g_ = P.sb(f"GSb{j}", [128, D])
            P.dma("sp", g_[:], G.modD[1, j:j + 1, 4 * D:5 * D].partition_broadcast(128), g_, reads=[G.modD], writes=[g_])
            P.op("dve", "scalar_tensor_tensor", out=g_[:], in0=g_[:], scalar=1.0, in1=grow[:], op0=ALU.add, op1=ALU.mult,
                 reads=[g_, grow], writes=[g_])
            GSb.append(g_)
            h_ = P.sb(f"SHb{j}", [128, D])
            P.dma("sp", h_[:], G.modD[1, j:j + 1, 3 * D:4 * D].partition_broadcast(128), h_, reads=[G.modD], writes=[h_])
            SHb.append(h_)
        z = P.sb("zrow", [128, D])
        P.op("dve", "memset", ap=z[:], constant=0.0, writes=[z])
        P.dma("sp", G.f_tok[NB * TL:NB * TL + 128, :], z[:], z, reads=[z], writes=[G.f_tok])
        rw = P.sb("rw", [128, 8, 8])
        P.dma("sp", rw[:], G.moe_router[:].rearrange("(k p) e -> p k e", p=128), rw, reads=[G.moe_router], writes=[rw])
        rb = P.sb("rb", [128, 8])
        P.dma("sp", rb[:], G.moe_router_b[0:1, :].partition_broadcast(128), rb, reads=[G.moe_router_b], writes=[rb])
        fT = [P.sb(f"nfT{i}", [128, 8, 128]) for i in range(2)]
        xt = [P.sb(f"nxt{i}", [128, D]) for i in range(2)]
        sq = P.sb("nsq", [128, D], BF16)
        ss = [P.sb(f"nss{i}", [128, 1]) for i in range(2)]
        fo = [P.sb(f"nfo{i}", [128, D]) for i in range(2)]
        it = 0
        for b in range(NB):
            for tt in range(16):
                s = it % 2
                it += 1
                P.dma("sp", xt[s][:], G.x3[b, tt * 128:(tt + 1) * 128, :], xt[s], reads=[G.x3], writes=[xt[s]])
                P.op("act", "activation", out=sq[:], in_=xt[s][:], func=AF.Square, accum_out=ss[s][:], reads=[xt[s]], writes=[sq, ss[s]])
                P.op("act", "activation", out=ss[s][:], in_=ss[s][:], func=AF.Sqrt, scale=1.0 / D, bias=G.eps[:, 0:1],
                     reads=[ss[s], G.eps], writes=[ss[s]])
                P.op("dve", "reciprocal", out=ss[s][:], in_=ss[s][:], reads=[ss[s]], writes=[ss[s]])
                P.op("dve", "scalar_tensor_tensor", out=fo[s][:], in0=xt[s][:], scalar=ss[s][:, 0:1], in1=GSb[b][:], op0=ALU.mult,
                     op1=ALU.mult, reads=[xt[s], ss[s], GSb[b]], writes=[fo[s]])
                P.op("dve", "tensor_tensor", out=fo[s][:], in0=fo[s][:], in1=SHb[b][:], op=ALU.add, reads=[fo[s], SHb[b]], writes=[fo[s]])
                P.dma("pool", G.f_tok[b * TL + tt * 128:b * TL + (tt + 1) * 128, :], fo[s][:], fo[s], reads=[fo[s]], writes=[G.f_tok])
                tix = b * 16 + tt
                for k in range(8):
                    ps = G.bank[(it % 2) * 2 + k // 4]
                    P.op("pe", "transpose", out=ps[:, (k % 4) * 128:(k % 4 + 1) * 128], in_=fo[s][:, k * 128:(k + 1) * 128],
                         identity=G.identf[:], reads=[fo[s], G.identf], writes=[ps])
                for kh in range(2):
                    ps = G.bank[(it % 2) * 2 + kh]
                    if kh:
                        P.op("act", "activation", out=fT[s][:, 4:8, :], in_=ps[:, 0:512].rearrange("p (k t) -> p k t", k=4),
                             func=AF.Copy, reads=[ps], writes=[fT[s]])
                    else:
                        P.op("dve", "tensor_copy", out=fT[s][:, 0:4, :], in_=ps[:, 0:512].rearrange("p (k t) -> p k t", k=4),
                             reads=[ps], writes=[fT[s]])
                pl = G.bank[4 + it % 2]
                for k in range(8):
                    mm(P, pl, pl[:, 0:8], fT[s], fT[s][:, k, :], rw, rw[:, k, :], start=(k == 0), stop=(k == 7))
                P.op("dve", "tensor_tensor", out=G.LG[:, tix, :], in0=pl[:, 0:8], in1=rb[:], op=ALU.add, reads=[pl, rb], writes=[G.LG])


def build(upto="all", dbg=()):
    nc = bass.Bass("TRN2", target_bir_lowering=False)
    P = Prog(nc)
    G = Ctx(P)
    nc.G = G
    kinds = lambda n: "ExternalOutput" if n in dbg else "Internal"
    S = lambda n, s, dt=F32: P.dram(n, s, dt, kind=kinds(n))
    G.out = P.dram("out", [NB, TL, D], F32, kind="ExternalOutput")
    G.hT0 = S("hT0", [NB, 128, 8, TT], BF16)
    G.RS = [S(f"RS{d}", [NB, 8, 64, 2, W], BF16) for d in range(2)]
    G.BK = [S(f"BK{d}", [NB, 8, 64, 2, W], BF16) for d in range(2)]
    G.BKt = [S(f"BKt{d}", [NB, 18, 128, 2, 512], BF16) for d in range(2)]
    G.GL = [S(f"GL{d}", [NB, 8, 64, 18]) for d in range(2)]
    G.Vt = S("Vt", [NB, 18, 128, 512], BF16)
    G.gT = S("gT", [NB, 4, 128, W])
    G.vT = S("vT", [NB, 4, 128, W])
    G.bcT = S("bcT", [NB, 4, 128, W])
    G.oA = [S(f"oA{d}", [NB, 18, 128, 512]) for d in range(2)]
    G.yT = S("yT", [NB, 8, 128, TT], BF16)
    G.x1 = S("x1", [NB, TT, D])
    G.x2 = S("x2", [NB, TT, D])
    G.fT0 = S("fT0", [NB, 128, 8, TT], BF16)
    G.hT1 = S("hT1", [NB, 128, 8, TT], BF16)
    G.qT = S("qT", [NB, 16, 64, TL], BF16)
    G.kT = S("kT", [NB, 4, 64, TT], BF16)
    G.Vt1 = S("Vt1", [NB, 18, 128, 256], BF16)
    G.oT1 = S("oT1", [NB, 8, 128, TL], BF16)
    G.x3 = S("x3", [NB, TL, D])
    G.x4 = S("x4", [NB, TL, D])
    G.fT1 = S("fT1", [NB, 128, 8, TL], BF16)
    G.fT1f = S("fT1f", [NB, 128, 8, TL], F32)
    G.f_tok = S("f_tok", [NB * TL + 128, D])
    G.Yb = S("Yb", [NSLOT, 2, D])
    G.tokidx = P.dram("tokidx", [NSLOT, 1], I32, kind=kinds("tokidx"))
    G.HQK = [S(f"HQK{d}", [NB, 4, 128, 2, TT], BF16) for d in range(2)]
    G.HKt = [S(f"HKt{d}", [NB, 18, 128, 512], BF16) for d in range(2)]
    G.HGL = [S(f"HGL{d}", [NB, 4, 128, 36]) for d in range(2)]
    G.HVt = S("HVt", [NB, 18, 128, 512], BF16)
    G.HGt = S("HGt", [NB, 18, 128, 512], BF16)
    G.oH = [S(f"oH{d}", [NB, 18, 128, 512]) for d in range(2)]
    setup_globals(P, G)
    G.modD = S("modD", [2, 3, 6 * D])
    outs = [G.out]

    def done():
        pass
        with P.scope():
            pass
        P.emit()
        return nc

    stage_adaln(P, G)
    if upto == "adaln":
        return done()
    if upto.startswith("l1"):
        G.x2 = P.dram("x2in", [NB, TT, D], F32, kind="ExternalInput")
        G.used_inputs.append("x2in")
        stage_norm(P, G, G.x2, TT, 1, 0, G.hT1, TC)
        if upto == "l1n":
            return done()
        for b in range(NB):
            stage_att_inproj(P, G, b)
        if upto == "l1i":
            return done()
        stage_attention(P, G)
        if upto == "l1t":
            return done()
        stage_outproj(P, G, G.oT1, G.att_w_out, 1, TL, 0, lambda b, tt: G.x2[b, TC + tt * 128:TC + (tt + 1) * 128, :],
                      lambda b, tt: G.x3[b, tt * 128:(tt + 1) * 128, :], G.x2, G.x3)
        if upto == "l1a":
            return done()
        if upto == "l1d":
            stage_norm(P, G, G.x3, TL, 1, 1, G.fT1, 0, dst32=G.fT1f)
            stage_router(P, G)
            stage_moe(P, G)
            stage_final(P, G, G.x4)
            return done()
        stage_norm_tok(P, G)
        stage_moe_sparse(P, G, nblk=(2 if upto == "l1s2" else NBLK))
        return done()
    stage_norm(P, G, G.xin, TT, 0, 0, G.hT0, TC)
    if upto == "norm0":
        return done()
    for b in range(NB if upto != "rwf1" else 1):
        stage_rwkv_feat(P, G, b)
    if upto in ("rwf", "rwf1"):
        return done()
    if upto == "hg1":
        stage_hgrn_feat(P, G, 0)
        stage_hgrn_scan(P, G, bs=(0,))
        return done()
    if upto == "rws1":
        stage_rwkv_scan(P, G, bs=(0,), ds=(0, 1), nmax=18)
        return done()
    stage_rwkv_scan(P, G)
    if upto == "rws":
        return done()
    stage_rwkv_post(P, G)
    if upto == "rwp":
        return done()
    for b in range(NB):
        stage_hgrn_feat(P, G, b)
    stage_hgrn_scan(P, G)
    stage_hgrn_post(P, G)
    if upto == "hgp":
        return done()
    stage_outproj(P, G, G.yT, G.rec_w_out, 0, TT, TC, lambda b, tt: G.xin[b, tt * 128:(tt + 1) * 128, :],
                  lambda b, tt: G.x1[b, tt * 128:(tt + 1) * 128, :], G.xin, G.x1)
    if upto == "x1":
        return done()
    stage_norm(P, G, G.x1, TT, 0, 1, G.fT0, TC)
    stage_ffn0(P, G)
    if upto == "x2":
        return done()
    stage_norm(P, G, G.x2, TT, 1, 0, G.hT1, TC)
    for b in range(NB):
        stage_att_inproj(P, G, b)
    stage_attention(P, G)
    stage_outproj(P, G, G.oT1, G.att_w_out, 1, TL, 0, lambda b, tt: G.x2[b, TC + tt * 128:TC + (tt + 1) * 128, :],
                  lambda b, tt: G.x3[b, tt * 128:(tt + 1) * 128, :], G.x2, G.x3)
    if upto == "x3":
        return done()
    if upto == "dense":
        stage_norm(P, G, G.x3, TL, 1, 1, G.fT1, 0, dst32=G.fT1f)
        stage_router(P, G)
        stage_moe(P, G)
        stage_final(P, G, G.x4)
        return done()
    stage_norm_tok(P, G)
    stage_moe_sparse(P, G)
    return done()


def host_prep(inp, core):
    f = lambda a: np.ascontiguousarray(a, dtype=np.float32)
    b0 = core * NB
    m = {}
    m["xin"] = f(np.concatenate([inp["ctx"][b0:b0 + NB], inp["x"][b0:b0 + NB]], axis=1))
    cv = np.stack([inp["c"][b0], inp["c"][b0 + 1], inp["c_ctx"]], axis=-1)
    m["cT"] = f(cv.reshape(8, 128, 3).transpose(1, 0, 2))
    m["mod_w"] = f(inp["mod_w"])
    m["mod_b"] = f(inp["mod_b"].reshape(2, 48, 128).transpose(0, 2, 1))
    m["norm_mix"] = f(inp["norm_mix"].reshape(2, 8, 128).transpose(0, 2, 1))
    m["norm_ffn"] = f(inp["norm_ffn"].reshape(2, 8, 128).transpose(0, 2, 1))
    m["norm_final"] = f(inp["norm_final"].reshape(1, D))
    m["ident"] = np.eye(128, dtype=np.float32)
    m["rec_w_in"] = f(inp["rec_w_in"][0])
    m["rec_w_out"] = f(inp["rec_w_out"][0])
    fm = lambda v: np.asarray(v, np.float32).reshape(4, 128).T
    rv = np.zeros((128, 4, 12), np.float32)
    for i, v in enumerate([inp["rwkv_k_k"][0], inp["rwkv_k_a"][0], None, inp["rwkv_a0"][0],
                           inp["rwkv_r_k"][0].reshape(-1), inp["rwkv_ln_w"][0], inp["rwkv_ln_b"][0],
                           inp["rwkv_w0"][0, 0], inp["rwkv_w0"][0, 1]]):
        if v is not None:
            rv[:, :, i] = fm(v)
    m["rw_vec"] = rv
    m["rw_mu"] = f(inp["rwkv_mu"][0].reshape(14, 128).T)
    m["rw_w_up"] = f(inp["rwkv_w_up"][0])
    m["rw_a_up"] = f(inp["rwkv_a_up"][0])
    m["rw_g_up"] = f(inp["rwkv_g_up"][0])
    m["ffn_wg"] = f(inp["ffn_w_gate"][0])
    m["ffn_wu"] = f(inp["ffn_w_up"][0])
    m["ffn_wd"] = f(inp["ffn_w_down"][0])
    m["att_w_in"] = f(inp["att_w_in"][0])
    m["att_w_out"] = f(inp["att_w_out"][0])
    m["att_sink"] = f(inp["att_sink"][0].reshape(1, 16))
    m["moe_router"] = f(inp["moe_router"][0])
    m["moe_router_b"] = f(inp["moe_router_b"][0].reshape(1, 8))
    m["moe_wg"], m["moe_wu"], m["moe_wd"] = _moe_layout(inp)
    m["norm_ffn_row"] = f(inp["norm_ffn"])
    m["hg_lb"] = f(inp["hgrn_lb"].reshape(2, 2, 4, 128).transpose(0, 1, 3, 2))
    m["hg_norm"] = f(inp["hgrn_norm"][0].reshape(1, 128))
    m.update(CONSTS)
    return m


def _consts():
    c = {}
    blk = np.zeros((128, 128), np.float32)
    blk[:64, :64] = 1
    blk[64:, 64:] = 1
    c["blk64"] = blk
    s_ = np.arange(128)[:, None]
    t_ = np.arange(128)[None, :]
    c["masks"] = np.stack([s_ < t_, s_ <= t_, s_ > t_, s_ >= t_], 1).astype(np.float32)
    sm = np.ones((128, W), np.float32)
    for n in range(18):
        sm[:, pcol(n * 128)] = 0.0
    c["scanmask"] = sm
    t = np.arange(TL)
    row = (t // 64).astype(np.float32)
    col = (t % 64).astype(np.float32)
    inv = (10000.0 ** (-np.arange(0, 32, 2, dtype=np.float32) / 32)).astype(np.float32)
    rope = np.zeros((128, 2, TL), np.float32)
    perm = np.zeros((128, 128), np.float32)
    for p in range(128):
        dd = p % 64
        axis, half, jj = dd // 32, (dd % 32) // 16, dd % 16
        ang = ((row if axis == 0 else col) * inv[jj]).astype(np.float32)
        rope[p, 0] = np.cos(ang)
        rope[p, 1] = np.sin(ang) * (-1.0 if half == 0 else 1.0)
        sw = p + 16 if half == 0 else p - 16
        perm[sw, p] = 1.0
    c["rope"] = rope
    c["perm"] = perm
    mc = np.zeros((128, 512), np.float32)
    p_ = np.arange(128)[:, None]
    mc[:, 0:64] = np.tile(np.arange(8) * float(BS), 8)[None, :]
    mc[:, 64:88] = np.arange(24)[None, :] + 0.0
    mc[:, 88:90] = 2 * p_ + np.arange(2)[None, :]
    mc[:, 104:106] = np.arange(2)[None, :] * 128 + p_
    mc[:, 128:160] = np.arange(32)[None, :] * 128 + p_
    rm = np.ones((8, 32), np.float32)
    rm[:, 0] = 0.0
    mc[:, 256:512] = rm.reshape(1, 256)
    c["moe_consts"] = mc
    return c


CONSTS = _consts()


_MOE_CACHE = {}


def _moe_layout(inp):
    key = id(inp["moe_w_gate"])
    if key not in _MOE_CACHE:
        _MOE_CACHE.clear()
        def gu(w):
            w = np.asarray(w[0], np.float32).reshape(8, 8, 128, 2, 1408)
            return np.ascontiguousarray(w.transpose(0, 2, 3, 1, 4)).reshape(2048, 11264)
        wd = np.asarray(inp["moe_w_down"][0], np.float32).reshape(8, 2, 11, 128, 1024)
        wd = np.ascontiguousarray(wd.transpose(0, 1, 3, 2, 4)).reshape(2048, 11264)
        _MOE_CACHE[key] = (gu(inp["moe_w_gate"]), gu(inp["moe_w_up"]), wd)
    return _MOE_CACHE[key]


_NC_CACHE = {}


def kernel(**inp):
    inp = {k: np.asarray(v) for k, v in inp.items()}
    if "nc" not in _NC_CACHE:
        _NC_CACHE["nc"] = build()
    nc = _NC_CACHE["nc"]
    used = nc.G.used_inputs
    in_maps = []
    for c in range(8):
        m = host_prep(inp, c)
        in_maps.append({k: m[k] for k in used})
    res = run_bass_kernel_spmd(nc, in_maps, core_ids=list(range(8)))
    out = np.concatenate([r["out"] for r in res.results], axis=0)
    return out.astype(np.float32)
```

```python
import contextlib
import os
import numpy as np
import concourse.bass as bass
import concourse.mybir as mybir
from concourse.bass_utils import run_bass_kernel_spmd

F32 = mybir.dt.float32
BF16 = mybir.dt.bfloat16
AF = mybir.ActivationFunctionType
ALU = mybir.AluOpType
AX = mybir.AxisListType

NB = 2
TC = 256
TL = 2048
TT = TC + TL
D = 1024
W = TT + 4
C0 = float(np.exp(-0.5))


def pcol(t):
    return 1 + t if t < TC else 3 + t


class Tk:
    __slots__ = ("h", "name", "w", "r", "sem", "cnt", "psum")

    def __init__(self, h, name, psum=False):
        self.h = h
        self.name = name
        self.psum = psum
        self.w = {}
        self.r = {}
        self.sem = None
        self.cnt = 0

    def __getitem__(self, idx):
        return self.h[idx]


class Prog:
    ENGS = ("pe", "act", "dve", "pool", "sp")

    def __init__(self, nc):
        self.nc = nc
        self.q = {e: [] for e in self.ENGS}
        self.seq = {e: 0 for e in self.ENGS}
        self.sem = {e: nc.alloc_semaphore("s_" + e) for e in self.ENGS}
        self.waited = {e: {} for e in self.ENGS}
        self.n = 0
        self.uid = 0
        self._stack = None
        self._scope_tiles = []
        self.free_dsems = {}
        self.dtiles = {}

    def _nm(self, name):
        self.uid += 1
        return f"{name}_{self.uid}"

    def sb(self, name, shape, dt=F32):
        nm = self._nm(name)
        if self._stack is not None:
            h = self._stack.enter_context(self.nc.sbuf_tensor(nm, list(shape), dt))
        else:
            h = self.nc.alloc_sbuf_tensor(nm, list(shape), dt)
        t = Tk(h, nm)
        if self._scope_tiles:
            self._scope_tiles[-1].append(t)
        return t

    def ps(self, name, shape, dt=F32):
        nm = self._nm(name)
        return Tk(self.nc.alloc_psum_tensor(nm, list(shape), dt), nm, psum=True)

    def view(self, ap, name):
        return Tk(ap, self._nm(name))

    def dram(self, name, shape, dt=F32, kind="Internal"):
        return Tk(self.nc.dram_tensor(name, list(shape), dt, kind=kind), name)

    @contextlib.contextmanager
    def scope(self):
        st = contextlib.ExitStack()
        prev = self._stack
        self._stack = st
        self._scope_tiles.append([])
        try:
            yield
        finally:
            self.barrier()
            for t in self._scope_tiles.pop():
                if t.sem:
                    for e, sc in t.sem.items():
                        self.free_dsems.setdefault(e, []).append(sc)
                    self.dtiles.pop(id(t), None)
                    t.sem = None
            st.close()
            self._stack = prev

    def _wait(self, eng, tok):
        if tok[0] == "e":
            _, f, s = tok
            if f == eng and eng == "pe":
                return
            key = ("e", f)
            if self.waited[eng].get(key, 0) >= s:
                return
            self.waited[eng][key] = s
            self.q[eng].append(("w", self.sem[f], s))
        else:
            _, t, de = tok
            if not t.sem or de not in t.sem:
                return
            sem, cnt = t.sem[de]
            key = ("d", id(sem))
            if self.waited[eng].get(key, 0) >= cnt:
                return
            self.waited[eng][key] = cnt
            self.q[eng].append(("w", sem, cnt))

    def _deps(self, eng, reads, writes):
        for t in reads:
            for tok in t.w.values():
                self._wait(eng, tok)
            if t.psum:
                for tok in t.r.values():
                    if not (tok[0] == "e" and tok[1] == eng):
                        self._wait(eng, tok)
        for t in writes:
            for tok in t.w.values():
                self._wait(eng, tok)
            for tok in t.r.values():
                self._wait(eng, tok)

    @staticmethod
    def _key(tok):
        return (tok[0], tok[1]) if tok[0] == "e" else (tok[0], id(tok[1]), tok[2])

    def _commit(self, tok, reads, writes):
        k = self._key(tok)
        for t in reads:
            t.r[k] = tok
        for t in writes:
            t.w[k] = tok

    def op(self, eng, meth, reads=(), writes=(), **kw):
        self._deps(eng, reads, writes)
        self.seq[eng] += 1
        tok = ("e", eng, self.seq[eng])
        self.q[eng].append(("o", meth, kw))
        self._commit(tok, reads, writes)
        self.n += 1
        return tok

    def dma(self, eng, out_ap, in_ap, semt, reads=(), writes=(), meth="dma_start", fn=None, **kw):
        self._deps(eng, reads, writes)
        t = semt
        if t.sem is None:
            t.sem = {}
        if eng not in t.sem:
            fl = self.free_dsems.get(eng)
            if fl:
                t.sem[eng] = fl.pop()
            else:
                t.sem[eng] = [self.nc.alloc_semaphore(f"d_{eng}_{t.name}"), 0]
            self.dtiles[id(t)] = t
        t.sem[eng][1] += 16
        if fn is not None:
            self.q[eng].append(("f", fn, t.sem[eng][0]))
        elif meth == "dma_start":
            self.q[eng].append(("d", out_ap, in_ap, t.sem[eng][0], kw))
        else:
            self.q[eng].append(("m", meth, kw, t.sem[eng][0]))
        tok = ("d", t, eng)
        self._commit(tok, reads, writes)
        self.n += 1
        return tok

    def barrier(self):
        for f in self.ENGS:
            if f != "sp" and self.seq[f] > 0:
                self._wait("sp", ("e", f, self.seq[f]))
        for t in list(self.dtiles.values()):
            for de in list(t.sem.keys()):
                self._wait("sp", ("d", t, de))
        self.seq["sp"] += 1
        self.q["sp"].append(("o", "nop", {}))
        tok = ("e", "sp", self.seq["sp"])
        for e in self.ENGS:
            if e != "sp":
                self._wait(e, tok)

    def emit(self):
        nc = self.nc
        q = self.q
        sems = self.sem

        def run(e, name):
            for it in q[name]:
                if it[0] == "w":
                    e.wait_ge(it[1], it[2])
                elif it[0] == "o":
                    getattr(e, it[1])(**it[2]).then_inc(sems[name], 1)
                elif it[0] == "f":
                    it[1](e).then_inc(it[2], 16)
                elif it[0] == "m":
                    getattr(e, it[1])(**it[2]).then_inc(it[3], 16)
                else:
                    _, o, i, s, kw = it
                    e.dma_start(out=o, in_=i, **kw).then_inc(s, 16)

        with nc.Block() as block:
            @block.tensor
            def _(e):
                run(e, "pe")

            @block.scalar
            def _(e):
                run(e, "act")

            @block.vector
            def _(e):
                run(e, "dve")

            @block.gpsimd
            def _(e):
                run(e, "pool")

            @block.sync
            def _(e):
                run(e, "sp")


def mm(P, o, oap, l, lap, r, rap, start=True, stop=True):
    P.op("pe", "matmul", reads=[l, r], writes=[o], out=oap, lhsT=lap, rhs=rap, start=start, stop=stop)


INPUT_SPECS = {
    "xin": [NB, TT, D], "cT": [128, 8, 3], "mod_w": [2, D, 6 * D], "mod_b": [2, 128, 48],
    "norm_mix": [2, 128, 8], "norm_ffn": [2, 128, 8], "norm_final": [1, D], "ident": [128, 128],
    "rec_w_in": [D, 4352], "rec_w_out": [D, D],
    "rw_vec": [128, 4, 12], "rw_mu": [128, 14], "rw_w_up": [2, 64, 512], "rw_a_up": [64, 512], "rw_g_up": [128, 512],
    "blk64": [128, 128], "masks": [128, 4, 128], "scanmask": [128, W],
    "hg_lb": [2, 2, 128, 4], "hg_norm": [1, 128],
    "ffn_wg": [D, 2816], "ffn_wu": [D, 2816], "ffn_wd": [2816, D],
    "att_w_in": [D, 1536], "att_w_out": [D, D], "att_sink": [1, 16],
    "rope": [128, 2, TL], "perm": [128, 128],
    "moe_router": [D, 8], "moe_router_b": [1, 8],
    "moe_consts": [128, 512], "norm_ffn_row": [2, D],
    "moe_wg": [2048, 11264], "moe_wu": [2048, 11264], "moe_wd": [2048, 11264],
}


class Ctx:
    def __init__(self, P):
        self.__dict__["P"] = P
        self.__dict__["used_inputs"] = []

    def __getattr__(self, name):
        if name in INPUT_SPECS:
            t = self.P.dram(name, INPUT_SPECS[name], F32, kind="ExternalInput")
            self.__dict__[name] = t
            self.used_inputs.append(name)
            return t
        raise AttributeError(name)


def setup_globals(P, G):
    G.bank = [P.ps(f"bank{i}", [128, 512], F32) for i in range(8)]
    G.identf = P.sb("identf", [128, 128], F32)
    P.dma("sp", G.identf[:], G.ident[:], G.identf, reads=[G.ident], writes=[G.identf])
    G.identb = P.sb("identb", [128, 128], BF16)
    P.op("dve", "tensor_copy", out=G.identb[:], in_=G.identf[:], reads=[G.identf], writes=[G.identb])
    G.eps = P.sb("eps", [128, 1], F32)
    P.op("dve", "memset", ap=G.eps[:], constant=1e-6, writes=[G.eps])
    G.WTm = P.sb("WTm", [128, 32, 8], F32)
    G.LG = P.sb("LGp", [128, 32, 8], F32)
    G.modT = P.sb("modT", [128, 2, 48, 3], F32)
    G.GS = P.sb("GS", [128, 2, 2, 8, 3], F32)


def stage_adaln(P, G):
    bank = G.bank
    with P.scope():
        cT = P.sb("cTs", [128, 8, 3])
        P.dma("sp", cT[:], G.cT[:], cT, reads=[G.cT], writes=[cT])
        sc = P.sb("sc", [128, 8, 3])
        P.op("act", "activation", out=sc[:], in_=cT[:], func=AF.Silu, reads=[cT], writes=[sc])
        mb = P.sb("mb", [128, 2, 48])
        P.dma("sp", mb[:], G.mod_b[:].rearrange("l p j -> p l j"), mb, reads=[G.mod_b], writes=[mb])
        gm = P.sb("gm", [128, 2, 2, 8])
        P.dma("sp", gm[:, :, 0, :], G.norm_mix[:].rearrange("l p k -> p l k"), gm, reads=[G.norm_mix], writes=[gm])
        P.dma("sp", gm[:, :, 1, :], G.norm_ffn[:].rearrange("l p k -> p l k"), gm, reads=[G.norm_ffn], writes=[gm])
        wts = [P.sb(f"mw{i}", [128, 8, 768]) for i in range(2)]
        it = 0
        for l in range(2):
            for g in range(8):
                wt = wts[it % 2]
                for k in range(8):
                    P.dma("sp" if k % 2 == 0 else "pool", wt[:, k, :],
                          G.mod_w[l, k * 128:(k + 1) * 128, g * 768:(g + 1) * 768], wt,
                          reads=[G.mod_w], writes=[wt])
                for jj in range(6):
                    j = g * 6 + jj
                    ps = bank[j % 2]
                    for k in range(8):
                        mm(P, ps, ps[:, 0:3], wt, wt[:, k, jj * 128:(jj + 1) * 128], sc, sc[:, k, :],
                           start=(k == 0), stop=(k == 7))
                    P.op("act", "activation", out=G.modT[:, l, j, :], in_=ps[:, 0:3], func=AF.Identity,
                         bias=mb[:, l, j:j + 1], reads=[ps, mb], writes=[G.modT])
                it += 1
        for l in range(2):
            for kind in range(2):
                wh = 1 + 3 * kind
                P.op("dve", "tensor_scalar", out=G.GS[:, l, kind, :, :], in0=G.modT[:, l, wh * 8:(wh + 1) * 8, :],
                     scalar1=1.0, scalar2=None, op0=ALU.add, reads=[G.modT], writes=[G.GS])
                P.op("dve", "tensor_tensor", out=G.GS[:, l, kind, :, :], in0=G.GS[:, l, kind, :, :],
                     in1=gm[:, l, kind, :].unsqueeze(2).broadcast_to([128, 8, 3]), op=ALU.mult,
                     reads=[G.GS, gm], writes=[G.GS])
        tr = P.sb("mtr", [48, 128])
        for l in range(2):
            for j in range(3):
                ps = bank[2 + (l * 3 + j) % 2]
                P.op("pe", "transpose", out=ps[0:48, 0:128], in_=G.modT[:, l, :, j], identity=G.identf[:],
                     reads=[G.modT, G.identf], writes=[ps])
                P.op("dve", "tensor_copy", out=tr[:], in_=ps[0:48, 0:128], reads=[ps], writes=[tr])
                P.dma("sp", G.modD[l, j, :].rearrange("(c p) -> c p", p=128), tr[:], tr, reads=[tr], writes=[G.modD])


def stage_norm(P, G, src, T, l, kind, dst, ctx_len, dst32=None):
    bank = G.bank
    with P.scope():
        xt = [P.sb(f"xt{i}", [128, D]) for i in range(4)]
        sq = P.sb("sq", [128, D], BF16)
        ss = [P.sb(f"ss{i}", [128, 1]) for i in range(4)]
        rs = [P.sb(f"rs{i}", [128, 1]) for i in range(4)]
        xn = [P.sb(f"xn{i}", [128, D]) for i in range(4)]
        hT = [P.sb(f"hT{i}", [128, 8, 128], BF16) for i in range(4)]
        hT32 = [P.sb(f"hTf{i}", [128, 8, 128], F32) for i in range(4)] if dst32 is not None else None
        i = 0
        for b in range(NB):
            for tt in range(T // 128):
                j = 2 if tt * 128 < ctx_len else b
                s = i % 4
                P.dma("sp", xt[s][:], src[b, tt * 128:(tt + 1) * 128, :], xt[s], reads=[src], writes=[xt[s]])
                P.op("act", "activation", out=sq[:], in_=xt[s][:], func=AF.Square, accum_out=ss[s][:],
                     reads=[xt[s]], writes=[sq, ss[s]])
                P.op("act", "activation", out=rs[s][:], in_=ss[s][:], func=AF.Sqrt, scale=1.0 / D, bias=G.eps[:, 0:1],
                     reads=[ss[s], G.eps], writes=[rs[s]])
                P.op("dve", "reciprocal", out=rs[s][:], in_=rs[s][:], reads=[rs[s]], writes=[rs[s]])
                P.op("dve", "tensor_scalar", out=xn[s][:], in0=xt[s][:], scalar1=rs[s][:, 0:1], scalar2=None,
                     op0=ALU.mult, reads=[xt[s], rs[s]], writes=[xn[s]])
                for k in range(8):
                    ps = bank[(i % 4) * 2 + k // 4]
                    pv = ps[:, (k % 4) * 128:(k % 4 + 1) * 128]
                    P.op("pe", "transpose", out=pv, in_=xn[s][:, k * 128:(k + 1) * 128], identity=G.identf[:],
                         reads=[xn[s], G.identf], writes=[ps])
                    if dst is not None:
                        if dst32 is None and k % 4 >= 2:
                            P.op("dve", "tensor_scalar", out=hT[s][:, k, :], in0=pv,
                                 scalar1=G.GS[:, l, kind, k, j:j + 1], scalar2=G.modT[:, l, 3 * kind * 8 + k, j:j + 1],
                                 op0=ALU.mult, op1=ALU.add, reads=[ps, G.GS, G.modT], writes=[hT[s]])
                        else:
                            P.op("act", "activation", out=hT[s][:, k, :], in_=pv, func=AF.Identity,
                                 scale=G.GS[:, l, kind, k, j:j + 1], bias=G.modT[:, l, 3 * kind * 8 + k, j:j + 1],
                                 reads=[ps, G.GS, G.modT], writes=[hT[s]])
                    if dst32 is not None:
                        if dst is None and k % 2:
                            P.op("act", "activation", out=hT32[s][:, k, :], in_=pv, func=AF.Identity,
                                 scale=G.GS[:, l, kind, k, j:j + 1], bias=G.modT[:, l, 3 * kind * 8 + k, j:j + 1],
                                 reads=[ps, G.GS, G.modT], writes=[hT32[s]])
                        else:
                            P.op("dve", "tensor_scalar", out=hT32[s][:, k, :], in0=pv,
                                 scalar1=G.GS[:, l, kind, k, j:j + 1], scalar2=G.modT[:, l, 3 * kind * 8 + k, j:j + 1],
                                 op0=ALU.mult, op1=ALU.add, reads=[ps, G.GS, G.modT], writes=[hT32[s]])
                if dst is not None:
                    P.dma("pool", dst[b, :, :, tt * 128:(tt + 1) * 128], hT[s][:], hT[s], reads=[hT[s]], writes=[dst])
                if dst32 is not None:
                    P.dma("pool", dst32[b, :, :, tt * 128:(tt + 1) * 128], hT32[s][:], hT32[s],
                          reads=[hT32[s]], writes=[dst32])
                i += 1


TBLK = [(0, 256), (256, 768), (768, 1280), (1280, 1792), (1792, 2304)]


def load_w_chunk(P, wt, src, c0, ncols, eng="pool"):
    P.dma(eng, wt[:, :, 0:ncols], src[:, c0:c0 + ncols].rearrange("(k p) n -> p k n", p=128), wt,
          reads=[src], writes=[wt])


class WPrefetch:
    def __init__(self, P, src, seq, nbuf=3, ncols=128):
        self.P, self.src, self.seq, self.ncols = P, src, list(seq), ncols
        self.bufs = [P.sb(f"wpf{i}", [128, 8, ncols], BF16) for i in range(nbuf)]
        self.i = 0
        self._issue(0)

    def _issue(self, i):
        if i < len(self.seq):
            load_w_chunk(self.P, self.bufs[i % len(self.bufs)], self.src, self.seq[i] * self.ncols, self.ncols)

    def get(self, chunk):
        assert self.seq[self.i] == chunk, (self.seq[self.i], chunk)
        wt = self.bufs[self.i % len(self.bufs)]
        self._issue(self.i + 1)
        self.i += 1
        return wt


def inproj_fm(P, G, hT, wt, out, padded=True, banks=(0, 1, 2, 3), evac="act", func=None):
    for bi, (t0, t1) in enumerate(TBLK):
        ps = G.bank[banks[bi % len(banks)]]
        n = t1 - t0
        for k in range(8):
            mm(P, ps, ps[:, 0:n], wt, wt[:, k, 0:128], hT, hT[:, k, t0:t1], start=(k == 0), stop=(k == 7))
        c0 = pcol(t0) if padded else t0
        if evac == "act":
            P.op("act", "activation", out=out[:, c0:c0 + n], in_=ps[:, 0:n], func=(func or AF.Copy), reads=[ps], writes=[out])
        else:
            P.op("dve", "tensor_copy", out=out[:, c0:c0 + n], in_=ps[:, 0:n], reads=[ps], writes=[out])


def tshift(P, raw, tmp, out, muh, omm, vec):
    P.op("pool", "tensor_tensor", out=tmp[:, 1:W - 1], in0=raw[:, 0:W - 2], in1=raw[:, 2:W], op=ALU.add,
         reads=[raw], writes=[tmp])
    P.op("dve", "tensor_scalar", out=tmp[:, 1:W - 1], in0=tmp[:, 1:W - 1], scalar1=muh, scalar2=None, op0=ALU.mult,
         reads=[tmp, vec], writes=[tmp])
    P.op("dve", "scalar_tensor_tensor", out=out[:, 1:W - 1], in0=raw[:, 1:W - 1], scalar=omm, in1=tmp[:, 1:W - 1],
         op0=ALU.mult, op1=ALU.add, reads=[raw, tmp, vec], writes=[out])


def chunk_views(ap):
    return (ap[:, 1:1 + TC].rearrange("p (n t) -> p n t", t=128),
            ap[:, 3 + TC:3 + TT].rearrange("p (n t) -> p n t", t=128))


def stage_rwkv_feat(P, G, b):
    bank = G.bank
    with P.scope():
        hT = P.sb("hTres", [128, 8, TT], BF16)
        for k in range(8):
            P.dma("sp", hT[:, k, :], G.hT0[b, :, k, :], hT, reads=[G.hT0], writes=[hT])
        vec = P.sb("rwvec", [128, 4, 12])
        P.dma("sp", vec[:], G.rw_vec[:], vec, reads=[G.rw_vec], writes=[vec])
        mu = P.sb("rwmu", [128, 3, 14])
        P.dma("sp", mu[:, 0, :], G.rw_mu[:], mu, reads=[G.rw_mu], writes=[mu])
        P.op("dve", "tensor_scalar", out=mu[:, 1, :], in0=mu[:, 0, :], scalar1=0.5, scalar2=None, op0=ALU.mult,
             reads=[mu], writes=[mu])
        P.op("dve", "tensor_scalar", out=mu[:, 2, :], in0=mu[:, 0, :], scalar1=-1.0, scalar2=1.0, op0=ALU.mult,
             op1=ALU.add, reads=[mu], writes=[mu])
        P.op("dve", "tensor_scalar", out=vec[:, :, 9], in0=vec[:, :, 1], scalar1=-1.0, scalar2=1.0, op0=ALU.mult,
             op1=ALU.add, reads=[vec], writes=[vec])
        blk = P.sb("blk64", [128, 128])
        P.dma("sp", blk[:], G.blk64[:], blk, reads=[G.blk64], writes=[blk])
        smask = P.sb("smask", [128, W])
        P.dma("sp", smask[:], G.scanmask[:], smask, reads=[G.scanmask], writes=[smask])
        wup = P.sb("wup", [64, 2, 512], BF16)
        P.dma("pool", wup[:], G.rw_w_up[:].rearrange("d r c -> r d c"), wup, reads=[G.rw_w_up], writes=[wup])
        aup = P.sb("aup", [128, 512], BF16)
        P.dma("pool", aup[64:128, :], G.rw_a_up[:], aup, reads=[G.rw_a_up], writes=[aup])
        gup = P.sb("gup", [128, 512], BF16)
        P.dma("pool", gup[:], G.rw_g_up[:], gup, reads=[G.rw_g_up], writes=[gup])
        eps12 = P.sb("eps12", [128, 1])
        P.op("dve", "memset", ap=eps12[:], constant=1e-12, writes=[eps12])
        WP = WPrefetch(P, G.rec_w_in, [12, 13] + [x for c in range(4) for x in (c, 4 + c, 8 + c)])
        T = [P.sb(f"T{i}", [128, W]) for i in range(12)]
        for t in T:
            P.op("pool", "memset", ap=t[:], constant=0.0, writes=[t])
        twb = P.sb("twb", [64, W], BF16)
        adb = P.sb("adb", [128, W], BF16)
        sgb = P.sb("sgb", [128, W], BF16)
        vtok = P.sb("vtok", [128, 18, 128], BF16)
        bkt = P.sb("bkt", [128, 18, 2, 128], BF16)
        gl = P.sb("gl", [128, 18])
        wi = [0]

        def proj(chunk, out):
            wt = WP.get(chunk)
            inproj_fm(P, G, hT, wt, T[0])
            tshift(P, T[0], T[1], out, mu[:, 1, chunk:chunk + 1], mu[:, 2, chunk:chunk + 1], mu)

        proj(12, T[2])
        P.op("act", "activation", out=twb[:], in_=T[2][0:64, :], func=AF.Tanh, reads=[T[2]], writes=[twb])
        P.op("dve", "tensor_copy", out=adb[64:128, :], in_=T[2][64:128, :], reads=[T[2]], writes=[adb])
        proj(13, T[2])
        P.op("act", "activation", out=sgb[:], in_=T[2][:], func=AF.Sigmoid, reads=[T[2]], writes=[sgb])
        for c in range(4):
            cs = slice(c * 128, (c + 1) * 128)
            A, R, KR, KAP, T6, V = T[2], T[3], T[4], T[5], T[6], T[7]
            for bi, c0 in enumerate(range(0, W, 512)):
                n = min(512, W - c0)
                ps = bank[4 + bi % 2]
                mm(P, ps, ps[:, 0:n], gup, gup[:, cs], sgb, sgb[:, c0:c0 + n])
                P.op("act", "activation", out=T[1][:, c0:c0 + n], in_=ps[:, 0:n], func=AF.Copy, reads=[ps], writes=[T[1]])
                ps2 = bank[6 + bi % 2]
                mm(P, ps2, ps2[:, 0:n], aup, aup[64:128, cs], adb, adb[64:128, c0:c0 + n])
                P.op("act", "activation", out=A[:, c0:c0 + n], in_=ps2[:, 0:n], func=AF.Sigmoid, bias=vec[:, c, 3:4],
                     reads=[ps2, vec], writes=[A])
            P.dma("sp", G.gT[b, c, :, :], T[1][:], T[1], reads=[T[1]], writes=[G.gT])
            proj(c, R)
            proj(4 + c, KR)
            P.op("dve", "tensor_scalar", out=KAP[:], in0=KR[:], scalar1=vec[:, c, 0:1], scalar2=None, op0=ALU.mult,
                 reads=[KR, vec], writes=[KAP])
            P.op("pool", "tensor_tensor", out=T6[:], in0=KAP[:], in1=KAP[:], op=ALU.mult, reads=[KAP], writes=[T6])
            for bi, c0 in enumerate(range(0, W, 512)):
                n = min(512, W - c0)
                ps = bank[4 + bi % 2]
                mm(P, ps, ps[:, 0:n], blk, blk[:], T6, T6[:, c0:c0 + n])
                P.op("act", "activation", out=T[1][:, c0:c0 + n], in_=ps[:, 0:n], func=AF.Sqrt, reads=[ps], writes=[T[1]])
            P.op("dve", "tensor_scalar", out=T[1][:], in0=T[1][:], scalar1=eps12[:, 0:1], scalar2=None, op0=ALU.max,
                 reads=[T[1], eps12], writes=[T[1]])
            P.op("dve", "reciprocal", out=T[1][:], in_=T[1][:], reads=[T[1]], writes=[T[1]])
            P.op("dve", "tensor_tensor", out=KAP[:], in0=KAP[:], in1=T[1][:], op=ALU.mult, reads=[KAP, T[1]], writes=[KAP])
            P.op("dve", "tensor_scalar", out=T6[:], in0=A[:], scalar1=vec[:, c, 1:2], scalar2=vec[:, c, 9:10],
                 op0=ALU.mult, op1=ALU.add, reads=[A, vec], writes=[T6])
            KM = T6
            P.op("pool", "tensor_tensor", out=KM[:], in0=KM[:], in1=KR[:], op=ALU.mult, reads=[KM, KR], writes=[KM])
            BE = KR
            P.op("dve", "tensor_tensor", out=BE[:], in0=KAP[:], in1=A[:], op=ALU.mult, reads=[KAP, A], writes=[BE])
            P.op("dve", "scalar_tensor_tensor", out=T[1][:], in0=R[:], scalar=vec[:, c, 4:5], in1=KM[:],
                 op0=ALU.mult, op1=ALU.mult, reads=[R, KM, vec], writes=[T[1]])
            for bi, c0 in enumerate(range(0, W, 512)):
                n = min(512, W - c0)
                ps = bank[4 + bi % 2]
                mm(P, ps, ps[:, 0:n], blk, blk[:], T[1], T[1][:, c0:c0 + n])
                P.op("act", "activation", out=T[8][:, c0:c0 + n], in_=ps[:, 0:n], func=AF.Copy, reads=[ps], writes=[T[8]])
            P.dma("sp", G.bcT[b, c, :, :], T[8][:], T[8], reads=[T[8]], writes=[G.bcT])
            proj(8 + c, V)
            P.dma("sp", G.vT[b, c, :, :], V[:], V, reads=[V], writes=[G.vT])
            for n in range(18):
                ps = bank[4 + n % 4]
                c0 = pcol(n * 128)
                P.op("pe", "transpose", out=ps[:, 0:128], in_=V[:, c0:c0 + 128], identity=G.identf[:],
                     reads=[V, G.identf], writes=[ps])
                P.op("act" if n % 2 else "dve", "activation" if n % 2 else "tensor_copy", out=vtok[:, n, :],
                     in_=ps[:, 0:128], reads=[ps], writes=[vtok], **({"func": AF.Copy} if n % 2 else {}))
            P.dma("sp", G.Vt[b, :, :, cs].rearrange("n t c -> t n c"), vtok[:], vtok, reads=[vtok], writes=[G.Vt])
            for d in range(2):
                LW, CS, GE, GI, ENI = T[7], T[8], T[9], T[10], T[11]
                for bi, c0 in enumerate(range(0, W, 512)):
                    n = min(512, W - c0)
                    ps = bank[4 + bi % 2]
                    mm(P, ps, ps[:, 0:n], wup, wup[:, d, cs], twb, twb[:, c0:c0 + n])
                    P.op("act", "activation", out=LW[:, c0:c0 + n], in_=ps[:, 0:n], func=AF.Sigmoid,
                         bias=vec[:, c, 7 + d:8 + d], reads=[ps, vec], writes=[LW])
                P.op("dve", "tensor_tensor_scan", out=CS[:], data0=smask[:], data1=LW[:], initial=0.0,
                     op0=ALU.mult, op1=ALU.add, reads=[smask, LW], writes=[CS])
                for (cv, n0, nn) in ((chunk_views(CS[:])[0], 0, 2), (chunk_views(CS[:])[1], 2, 16)):
                    P.op("act", "activation", out=gl[:, n0:n0 + nn], in_=cv[:, :, 127], func=AF.Exp, scale=-C0,
                         reads=[CS], writes=[gl])
                P.dma("sp", G.GL[d][b, 2 * c:2 * c + 2, :, :].rearrange("h k n -> (h k) n"), gl[:], gl,
                      reads=[gl], writes=[G.GL[d]])
                if d == 0:
                    P.op("pool", "tensor_tensor", out=GE[:], in0=CS[:], in1=LW[:], op=ALU.subtract,
                         reads=[CS, LW], writes=[GE])
                    P.op("act", "activation", out=GI[:], in_=CS[:], func=AF.Exp, scale=-C0, reads=[CS], writes=[GI])
                    P.op("act", "activation", out=ENI[:], in_=CS[:], func=AF.Exp, scale=C0, reads=[CS], writes=[ENI])
                else:
                    for vi in range(2):
                        gev = chunk_views(GE[:])[vi]
                        csv = chunk_views(CS[:])[vi]
                        nn = 2 if vi == 0 else 16
                        P.op("dve", "tensor_tensor", out=gev, in0=csv[:, :, 127:128].broadcast_to([128, nn, 128]),
                             in1=csv, op=ALU.subtract, reads=[CS], writes=[GE])
                    P.op("pool", "tensor_tensor", out=GI[:], in0=GE[:], in1=LW[:], op=ALU.add,
                         reads=[GE, LW], writes=[GI])
                    P.op("act", "activation", out=ENI[:], in_=GI[:], func=AF.Exp, scale=C0, reads=[GI], writes=[ENI])
                    P.op("act", "activation", out=GI[:], in_=GI[:], func=AF.Exp, scale=-C0, reads=[GI], writes=[GI])
                P.op("act", "activation", out=GE[:], in_=GE[:], func=AF.Exp, scale=-C0, reads=[GE], writes=[GE])
                P.op("dve", "tensor_tensor", out=GI[:], in0=GI[:], in1=R[:], op=ALU.mult, reads=[GI, R], writes=[GI])
                P.op("pool", "tensor_tensor", out=GE[:], in0=GE[:], in1=KAP[:], op=ALU.mult, reads=[GE, KAP], writes=[GE])
                BH = CS
                P.op("dve", "tensor_tensor", out=BH[:], in0=ENI[:], in1=BE[:], op=ALU.mult, reads=[ENI, BE], writes=[BH])
                KH = ENI
                P.op("pool", "tensor_tensor", out=KH[:], in0=ENI[:], in1=KM[:], op=ALU.mult, reads=[ENI, KM], writes=[KH])
                hv = lambda t, j: t[b, 2 * c:2 * c + 2, :, j, :].rearrange("h k w -> (h k) w")
                P.dma("pool", hv(G.RS[d], 0), GE[:], GE, reads=[GE], writes=[G.RS[d]])
                P.dma("pool", hv(G.RS[d], 1), GI[:], GI, reads=[GI], writes=[G.RS[d]])
                P.dma("pool", hv(G.BK[d], 0), BH[:], BH, reads=[BH], writes=[G.BK[d]])
                P.dma("pool", hv(G.BK[d], 1), KH[:], KH, reads=[KH], writes=[G.BK[d]])
                for n in range(18):
                    c0 = pcol(n * 128)
                    for j, src in enumerate((BH, KH)):
                        ps = bank[(2 * n + j) % 4]
                        P.op("pe", "transpose", out=ps[:, 0:128], in_=src[:, c0:c0 + 128], identity=G.identf[:],
                             reads=[src, G.identf], writes=[ps])
                        if j == 0:
                            P.op("act", "activation", out=bkt[:, n, j, :], in_=ps[:, 0:128], func=AF.Copy,
                                 reads=[ps], writes=[bkt])
                        else:
                            P.op("dve", "tensor_copy", out=bkt[:, n, j, :], in_=ps[:, 0:128], reads=[ps], writes=[bkt])
                for j in range(2):
                    P.dma("sp", G.BKt[d][b, :, :, j, cs].rearrange("n t c -> t n c"), bkt[:, :, j, :], bkt,
                          reads=[bkt], writes=[G.BKt[d]])


def stage_rwkv_scan(P, G, bs=(0, 1), ds=(0, 1), nmax=18):
    bank = G.bank
    with P.scope():
        msk = P.sb("msk", [128, 4, 128])
        P.dma("sp", msk[:], G.masks[:], msk, reads=[G.masks], writes=[msk])
        rs = [P.sb(f"rs{i}", [64, 8, 2, 128], BF16) for i in range(2)]
        bk = [P.sb(f"bk{i}", [64, 8, 2, 128], BF16) for i in range(2)]
        bkt = [P.sb(f"bktl{i}", [128, 2, 512], BF16) for i in range(2)]
        vt = [P.sb(f"vt{i}", [128, 512], BF16) for i in range(2)]
        Ms = [P.sb(f"Ms{i}", [128, 4, 128], F32) for i in range(2)]
        MTs = [P.sb(f"MTs{i}", [128, 4, 128], F32) for i in range(2)]
        Nn = [[P.sb(f"N{i}_{j}", [128, 4, 128], F32) for j in range(2)] for i in range(2)]
        NT = [[P.sb(f"NT{i}_{j}", [128, 4, 128], F32) for j in range(2)] for i in range(2)]
        ABr = [[P.sb(f"ABr{p}_{i}", [128, 4, 128], BF16) for i in range(2)] for p in range(2)]
        GKs = [[P.sb(f"GKs{p}_{i}", [128, 4, 256], BF16) for i in range(2)] for p in range(2)]
        Pm = [[P.sb(f"Pm{p}_{i}", [128, 4, 128], F32) for i in range(2)] for p in range(2)]
        WT = P.sb("WT", [128, 512], F32)
        nZ = P.sb("nZ", [128, 512], BF16)
        ot = [P.sb(f"ot{i}", [128, 512]) for i in range(2)]
        Sf = P.sb("Sf", [64, 8, 64])
        Sb = P.sb("Sb", [64, 8, 64], BF16)
        Stmp = P.sb("Stmp", [64, 8, 64])
        GLt = P.sb("GLt", [64, 8, 18])
        b6, b7 = bank[6], bank[7]

        def emit_front(b, d, n, s):
            m2 = msk[:, 0:2, :] if d == 0 else msk[:, 2:4, :]
            mt = msk[:, 2, :] if d == 0 else msk[:, 0, :]
            c0 = pcol(n * 128)
            for j in range(2):
                P.dma("sp", rs[s][:, :, j, :], G.RS[d][b, :, :, j, c0:c0 + 128].rearrange("h k t -> k h t"),
                      rs[s], reads=[G.RS[d]], writes=[rs[s]])
                P.dma("sp", bk[s][:, :, j, :], G.BK[d][b, :, :, j, c0:c0 + 128].rearrange("h k t -> k h t"),
                      bk[s], reads=[G.BK[d]], writes=[bk[s]])
            P.dma("sp", bkt[s][:], G.BKt[d][b, n], bkt[s], reads=[G.BKt[d]], writes=[bkt[s]])
            P.dma("sp", vt[s][:], G.Vt[b, n], vt[s], reads=[G.Vt], writes=[vt[s]])
            R_, B_ = rs[s], bk[s]
            for hf in range(2):
                for i in range(4):
                    h = hf * 4 + i
                    pb = bank[hf * 3 + 0] if i < 2 else bank[hf * 3 + 1]
                    mm(P, pb, pb[:, (i % 2) * 256:(i % 2 + 1) * 256], B_, B_[:, h, 0, :], R_, R_[:, h, :, :])
                for i in range(4):
                    h = hf * 4 + i
                    pk = b6 if i < 2 else b7
                    mm(P, pk, pk[:, (i % 2) * 256:(i % 2 + 1) * 256], B_, B_[:, h, 1, :], R_, R_[:, h, :, :])
                pm = bank[hf * 3 + 2]
                for i in range(4):
                    h = hf * 4 + i
                    mm(P, pm, pm[:, i * 128:(i + 1) * 128], R_, R_[:, h, 0, :], B_, B_[:, h, 0, :])
                for half2 in range(2):
                    pb = bank[hf * 3 + half2]
                    pbv = pb[:, 0:512].rearrange("p (h j t) -> p h j t", h=2, j=2)
                    P.op("dve", "tensor_tensor", out=Ms[hf][:, 2 * half2:2 * half2 + 2, :], in0=pbv[:, :, 0, :],
                         in1=m2[:, 0, :].unsqueeze(1).broadcast_to([128, 2, 128]), op=ALU.mult,
                         reads=[pb, msk], writes=[Ms[hf]])
                    P.op("dve", "tensor_tensor", out=ABr[s][hf][:, 2 * half2:2 * half2 + 2, :], in0=pbv[:, :, 1, :],
                         in1=m2[:, 1, :].unsqueeze(1).broadcast_to([128, 2, 128]), op=ALU.mult,
                         reads=[pb, msk], writes=[ABr[s][hf]])
                    pk = bank[6 + half2]
                    P.op("dve", "tensor_tensor", out=GKs[s][hf][:, 2 * half2:2 * half2 + 2, :].rearrange("p h (j t) -> p h j t", j=2),
                         in0=pk[:, 0:512].rearrange("p (h j t) -> p h j t", h=2, j=2),
                         in1=m2.unsqueeze(1).broadcast_to([128, 2, 2, 128]), op=ALU.mult,
                         reads=[pk, msk], writes=[GKs[s][hf]])
                P.op("dve", "tensor_tensor", out=MTs[hf][:], in0=pm[:, 0:512].rearrange("p (h t) -> p h t", h=4),
                     in1=mt.unsqueeze(1).broadcast_to([128, 4, 128]), op=ALU.mult,
                     reads=[pm, msk], writes=[MTs[hf]])
                P.op("pool", "tensor_tensor", out=Pm[s][hf][:], in0=G.identf[:].unsqueeze(1).broadcast_to([128, 4, 128]),
                     in1=Ms[hf][:], op=ALU.subtract, reads=[Ms[hf], G.identf], writes=[Pm[s][hf]])
            for hf in range(2):
                pn, pt = bank[hf * 3 + 0], bank[hf * 3 + 1]
                for i in range(4):
                    mm(P, pn, pn[:, i * 128:(i + 1) * 128], MTs[hf], MTs[hf][:, i, :], Ms[hf], Ms[hf][:, i, :])
                for i in range(4):
                    mm(P, pt, pt[:, i * 128:(i + 1) * 128], Ms[hf], Ms[hf][:, i, :], MTs[hf], MTs[hf][:, i, :])
                P.op("act", "activation", out=Nn[hf][0][:], in_=pn[:, 0:512].rearrange("p (h t) -> p h t", h=4),
                     func=AF.Copy, reads=[pn], writes=[Nn[hf][0]])
                P.op("act", "activation", out=NT[hf][0][:], in_=pt[:, 0:512].rearrange("p (h t) -> p h t", h=4),
                     func=AF.Copy, reads=[pt], writes=[NT[hf][0]])

        def emit_level(s, lev):
            cur, nxt = lev % 2, 1 - lev % 2
            for hf in range(2):
                pp, pn, pt = bank[hf * 3 + 2], bank[hf * 3 + 0], bank[hf * 3 + 1]
                Pq = Pm[s][hf]
                for i in range(4):
                    mm(P, pp, pp[:, i * 128:(i + 1) * 128], NT[hf][cur], NT[hf][cur][:, i, :], Pq, Pq[:, i, :])
                if lev < 4:
                    for i in range(4):
                        mm(P, pn, pn[:, i * 128:(i + 1) * 128], NT[hf][cur], NT[hf][cur][:, i, :],
                           Nn[hf][cur], Nn[hf][cur][:, i, :])
                if lev < 5:
                    for i in range(4):
                        mm(P, pt, pt[:, i * 128:(i + 1) * 128], Nn[hf][cur], Nn[hf][cur][:, i, :],
                           NT[hf][cur], NT[hf][cur][:, i, :])
                P.op("dve", "tensor_tensor", out=Pq[:], in0=pp[:, 0:512].rearrange("p (h t) -> p h t", h=4),
                     in1=Pq[:], op=ALU.add, reads=[pp, Pq], writes=[Pq])
                if lev < 4:
                    P.op("act", "activation", out=Nn[hf][nxt][:], in_=pn[:, 0:512].rearrange("p (h t) -> p h t", h=4),
                         func=AF.Copy, reads=[pn], writes=[Nn[hf][nxt]])
                if lev < 5:
                    P.op("act", "activation", out=NT[hf][nxt][:], in_=pt[:, 0:512].rearrange("p (h t) -> p h t", h=4),
                         func=AF.Copy, reads=[pt], writes=[NT[hf][nxt]])

        def chain_steps(b, d, n, s):
            R_, BT_, V_ = rs[s], bkt[s], vt[s]

            def st_d():
                for h in range(8):
                    hf, i = h // 4, h % 4
                    hs = slice(h * 64, (h + 1) * 64)
                    mm(P, b6, b6[:, hs], R_, R_[:, h, 0, :], Sb, Sb[:, h, :], start=True, stop=False)
                    mm(P, b6, b6[:, hs], GKs[s][hf], GKs[s][hf][:, i, 0:128], V_, V_[:, hs], start=False, stop=True)
                P.op("act", "activation", out=WT[:], in_=b6[:, 0:512], func=AF.Copy, reads=[b6], writes=[WT])

            def st_e():
                for h in range(8):
                    hf, i = h // 4, h % 4
                    hs = slice(h * 64, (h + 1) * 64)
                    mm(P, b7, b7[:, hs], Pm[s][hf], Pm[s][hf][:, i, :], WT, WT[:, hs])
                P.op("act", "activation", out=nZ[:], in_=b7[:, 0:512], func=AF.Copy, scale=-1.0, reads=[b7], writes=[nZ])

            def st_f():
                for h in range(8):
                    hf, i = h // 4, h % 4
                    hs = slice(h * 64, (h + 1) * 64)
                    mm(P, b6, b6[:, hs], R_, R_[:, h, 1, :], Sb, Sb[:, h, :], start=True, stop=False)
                    mm(P, b6, b6[:, hs], GKs[s][hf], GKs[s][hf][:, i, 128:256], V_, V_[:, hs], start=False, stop=False)
                    mm(P, b6, b6[:, hs], ABr[s][hf], ABr[s][hf][:, i, :], nZ, nZ[:, hs], start=False, stop=True)
                o_ = ot[s]
                P.op("dve", "tensor_copy", out=o_[:], in_=b6[:, 0:512], reads=[b6], writes=[o_])
                P.dma("pool", G.oA[d][b, n], o_[:], o_, reads=[o_], writes=[G.oA[d]])

            def st_g():
                for h in range(8):
                    hs = slice(h * 64, (h + 1) * 64)
                    mm(P, b7, b7[0:64, hs], BT_, BT_[:, 1, hs], V_, V_[:, hs], start=True, stop=False)
                    mm(P, b7, b7[0:64, hs], BT_, BT_[:, 0, hs], nZ, nZ[:, hs], start=False, stop=True)
                P.op("dve", "tensor_tensor", out=Stmp[:], in0=b7[0:64, 0:512].rearrange("p (h v) -> p h v", h=8),
                     in1=Sf[:], op=ALU.add, reads=[b7, Sf], writes=[Stmp])
                P.op("dve", "tensor_tensor", out=Sf[:], in0=Stmp[:],
                     in1=GLt[:, :, n:n + 1].broadcast_to([64, 8, 64]), op=ALU.mult,
                     reads=[Stmp, GLt], writes=[Sf])
                P.op("act", "activation", out=Sb[:], in_=Sf[:], func=AF.Copy, reads=[Sf], writes=[Sb])

            return [st_d, st_e, st_f, st_g]

        it = 0
        for b in bs:
            for d in ds:
                P.op("dve", "memset", ap=Sf[:], constant=0.0, writes=[Sf])
                P.op("dve", "memset", ap=Sb[:], constant=0.0, writes=[Sb])
                P.dma("sp", GLt[:], G.GL[d][b].rearrange("h k n -> k h n"), GLt, reads=[G.GL[d]], writes=[GLt])
                order = list(range(18)) if d == 0 else [1, 0] + list(range(17, 1, -1))
                order = order[:nmax]
                pending = []
                for n in order + [None]:
                    if n is not None:
                        s = it % 2
                        it += 1
                        emit_front(b, d, n, s)
                    for lev in range(6):
                        if n is not None:
                            emit_level(s, lev)
                        if pending and lev >= 1:
                            pending.pop(0)()
                    while pending:
                        pending.pop(0)()
                    if n is not None:
                        pending = chain_steps(b, d, n, s)


def stage_rwkv_post(P, G):
    bank = G.bank
    with P.scope():
        vec = P.sb("rwvec2", [128, 4, 12])
        P.dma("sp", vec[:], G.rw_vec[:], vec, reads=[G.rw_vec], writes=[vec])
        gne = P.sb("gne", [128, 1])
        P.op("dve", "memset", ap=gne[:], constant=64e-5, writes=[gne])
        o0 = [P.sb(f"o0_{i}", [128, 512]) for i in range(3)]
        o1 = [P.sb(f"o1_{i}", [128, 512]) for i in range(3)]
        sq = P.sb("posq", [128, 512])
        st = [P.sb(f"pst{i}", [128, 4, 8]) for i in range(3)]
        on = [P.sb(f"on{i}", [128, 512]) for i in range(3)]
        ya = [P.sb(f"ya{i}", [128, 4, 128]) for i in range(3)]
        bc = [P.sb(f"bc{i}", [128, 4, 128]) for i in range(3)]
        vv = [P.sb(f"vv{i}", [128, 4, 128]) for i in range(3)]
        gg = [P.sb(f"gg{i}", [128, 4, 128]) for i in range(3)]
        yb = [P.sb(f"yab{i}", [128, 4, 128], BF16) for i in range(3)]
        it = 0
        for b in range(NB):
            for n in range(18):
                s = it % 3
                it += 1
                c0 = pcol(n * 128)
                P.dma("sp", o0[s][:], G.oA[0][b, n], o0[s], reads=[G.oA[0]], writes=[o0[s]])
                P.dma("sp", o1[s][:], G.oA[1][b, n], o1[s], reads=[G.oA[1]], writes=[o1[s]])
                P.dma("sp", bc[s][:], G.bcT[b, :, :, c0:c0 + 128].rearrange("c p t -> p c t"), bc[s], reads=[G.bcT], writes=[bc[s]])
                P.dma("sp", vv[s][:], G.vT[b, :, :, c0:c0 + 128].rearrange("c p t -> p c t"), vv[s], reads=[G.vT], writes=[vv[s]])
                P.dma("sp", gg[s][:], G.gT[b, :, :, c0:c0 + 128].rearrange("c p t -> p c t"), gg[s], reads=[G.gT], writes=[gg[s]])
                o_, S_ = o0[s], st[s]
                P.op("pool", "tensor_tensor", out=o_[:], in0=o_[:], in1=o1[s][:], op=ALU.add, reads=[o_, o1[s]], writes=[o_])
                ov = o_[:].rearrange("p (h v) -> p h v", h=8)
                P.op("dve", "tensor_reduce", out=S_[:, 0, :], in_=ov, axis=AX.X, op=ALU.add, reads=[o_], writes=[S_])
                P.op("pool", "tensor_tensor", out=sq[:], in0=o_[:], in1=o_[:], op=ALU.mult, reads=[o_], writes=[sq])
                P.op("dve", "tensor_reduce", out=S_[:, 1, :], in_=sq[:].rearrange("p (h v) -> p h v", h=8), axis=AX.X,
                     op=ALU.add, reads=[sq], writes=[S_])
                P.op("dve", "tensor_scalar", out=S_[:, 2, :], in0=S_[:, 0, :], scalar1=1.0 / 64, scalar2=None, op0=ALU.mult,
                     reads=[S_], writes=[S_])
                P.op("dve", "tensor_tensor", out=S_[:, 3, :], in0=S_[:, 2, :], in1=S_[:, 2, :], op=ALU.mult, reads=[S_], writes=[S_])
                P.op("dve", "scalar_tensor_tensor", out=S_[:, 3, :], in0=S_[:, 1, :], scalar=1.0 / 64, in1=S_[:, 3, :],
                     op0=ALU.mult, op1=ALU.subtract, reads=[S_], writes=[S_])
                P.op("act", "activation", out=S_[:, 3, :], in_=S_[:, 3, :], func=AF.Sqrt, bias=gne[:, 0:1], reads=[S_, gne], writes=[S_])
                P.op("dve", "reciprocal", out=S_[:, 3, :], in_=S_[:, 3, :], reads=[S_], writes=[S_])
                onv = on[s][:].rearrange("p (h v) -> p h v", h=8)
                P.op("dve", "tensor_tensor", out=onv, in0=ov, in1=S_[:, 2, :].unsqueeze(2).broadcast_to([128, 8, 64]),
                     op=ALU.subtract, reads=[o_, S_], writes=[on[s]])
                P.op("dve", "tensor_tensor", out=onv, in0=onv, in1=S_[:, 3, :].unsqueeze(2).broadcast_to([128, 8, 64]),
                     op=ALU.mult, reads=[on[s], S_], writes=[on[s]])
                for c in range(4):
                    ps = bank[it % 8]
                    P.op("pe", "transpose", out=ps[:, c * 128:(c + 1) * 128], in_=on[s][:, c * 128:(c + 1) * 128], identity=G.identf[:],
                         reads=[on[s], G.identf], writes=[ps])
                for c in range(4):
                    ps = bank[it % 8]
                    P.op("act", "activation", out=ya[s][:, c, :], in_=ps[:, c * 128:(c + 1) * 128], func=AF.Identity,
                         scale=vec[:, c, 5:6], bias=vec[:, c, 6:7], reads=[ps, vec], writes=[ya[s]])
                P.op("pool", "tensor_tensor", out=bc[s][:], in0=bc[s][:], in1=vv[s][:], op=ALU.mult, reads=[bc[s], vv[s]], writes=[bc[s]])
                P.op("dve", "tensor_tensor", out=ya[s][:], in0=ya[s][:], in1=bc[s][:], op=ALU.add, reads=[ya[s], bc[s]], writes=[ya[s]])
                P.op("dve", "tensor_tensor", out=yb[s][:], in0=ya[s][:], in1=gg[s][:], op=ALU.mult, reads=[ya[s], gg[s]], writes=[yb[s]])
                P.dma("pool", G.yT[b, 0:4, :, n * 128:(n + 1) * 128].rearrange("c p t -> p c t"), yb[s][:], yb[s],
                      reads=[yb[s]], writes=[G.yT])


def stage_hgrn_feat(P, G, b):
    bank = G.bank
    with P.scope():
        hT = P.sb("hTres", [128, 8, TT], BF16)
        for k in range(8):
            P.dma("sp", hT[:, k, :], G.hT0[b, :, k, :], hT, reads=[G.hT0], writes=[hT])
        lbr = P.sb("lbr", [128, 2, 2, 4])
        P.dma("sp", lbr[:], G.hg_lb[:].rearrange("d l p h -> p d l h"), lbr, reads=[G.hg_lb], writes=[lbr])
        lb = P.sb("lb", [128, 2, 2, 4])
        P.op("dve", "tensor_tensor", out=lb[:, :, 0, :], in0=lbr[:, :, 0, :], in1=lbr[:, :, 1, :], op=ALU.subtract,
             reads=[lbr], writes=[lb])
        P.op("act", "activation", out=lb[:, :, 0, :], in_=lb[:, :, 0, :], func=AF.Sigmoid, reads=[lb], writes=[lb])
        P.op("dve", "tensor_scalar", out=lb[:, :, 1, :], in0=lb[:, :, 0, :], scalar1=-1.0, scalar2=1.0, op0=ALU.mult,
             op1=ALU.add, reads=[lb], writes=[lb])
        sm64 = P.sb("sm64", [128, TT])
        P.op("pool", "memset", ap=sm64[:], constant=1.0, writes=[sm64])
        P.op("pool", "memset", ap=sm64[:].rearrange("p (n t) -> p n t", t=64)[:, :, 0:1], constant=0.0, writes=[sm64])
        WP = WPrefetch(P, G.rec_w_in, [x for h in range(4) for x in (14 + h, 22 + h, 26 + h)])
        wbig = P.sb("wbig", [128, 8, 512], BF16)
        T = [P.sb(f"H{i}", [128, TT]) for i in range(7)]
        hkt = P.sb("hkt", [128, 18, 128], BF16)
        glh = P.sb("glh", [128, 36])
        tok = [P.sb(f"tok{i}", [128, 512], BF16) for i in range(2)]
        wi = [0]

        def proj(chunk, out, func=None):
            wt = WP.get(chunk)
            inproj_fm(P, G, hT, wt, out, padded=False, func=func)

        for h in range(4):
            Q, F, LF, CS, GI, ENI, KF = T
            proj(14 + h, Q, AF.Silu)
            for d in range(2):
                proj(22 + 4 * d + h, F, AF.Sigmoid)
                P.op("dve", "tensor_scalar", out=F[:], in0=F[:], scalar1=lb[:, d, 1, h:h + 1], scalar2=lb[:, d, 0, h:h + 1],
                     op0=ALU.mult, op1=ALU.add, reads=[F, lb], writes=[F])
                P.op("act", "activation", out=LF[:], in_=F[:], func=AF.Ln, reads=[F], writes=[LF])
                P.op("pool", "tensor_scalar", out=KF[:], in0=F[:], scalar1=-1.0, scalar2=1.0, op0=ALU.mult, op1=ALU.add,
                     reads=[F], writes=[KF])
                P.op("dve", "tensor_tensor_scan", out=CS[:], data0=sm64[:], data1=LF[:], initial=0.0, op0=ALU.mult,
                     op1=ALU.add, reads=[sm64, LF], writes=[CS])
                csv = CS[:].rearrange("p (n t) -> p n t", t=64)
                P.op("act", "activation", out=glh[:], in_=csv[:, :, 63], func=AF.Exp, reads=[CS], writes=[glh])
                P.dma("sp", G.HGL[d][b, h], glh[:], glh, reads=[glh], writes=[G.HGL[d]])
                if d == 0:
                    gsrc = CS
                else:
                    P.op("dve", "tensor_tensor", out=GI[:].rearrange("p (n t) -> p n t", t=64),
                         in0=csv[:, :, 63:64].broadcast_to([128, 36, 64]), in1=csv, op=ALU.subtract, reads=[CS], writes=[GI])
                    P.op("pool", "tensor_tensor", out=GI[:], in0=GI[:], in1=LF[:], op=ALU.add, reads=[GI, LF], writes=[GI])
                    gsrc = GI
                P.op("act", "activation", out=ENI[:], in_=gsrc[:], func=AF.Exp, scale=-1.0, reads=[gsrc], writes=[ENI])
                P.op("act", "activation", out=GI[:], in_=gsrc[:], func=AF.Exp, reads=[gsrc], writes=[GI])
                P.op("dve", "tensor_tensor", out=GI[:], in0=GI[:], in1=Q[:], op=ALU.mult, reads=[GI, Q], writes=[GI])
                P.op("pool", "tensor_tensor", out=ENI[:], in0=ENI[:], in1=KF[:], op=ALU.mult, reads=[ENI, KF], writes=[ENI])
                P.dma("pool", G.HQK[d][b, h, :, 0, :], GI[:], GI, reads=[GI], writes=[G.HQK[d]])
                P.dma("pool", G.HQK[d][b, h, :, 1, :], ENI[:], ENI, reads=[ENI], writes=[G.HQK[d]])
                for n in range(18):
                    ps = bank[4 + n % 4]
                    P.op("pe", "transpose", out=ps[:, 0:128], in_=ENI[:, n * 128:(n + 1) * 128], identity=G.identf[:],
                         reads=[ENI, G.identf], writes=[ps])
                    if n % 2:
                        P.op("act", "activation", out=hkt[:, n, :], in_=ps[:, 0:128], func=AF.Copy, reads=[ps], writes=[hkt])
                    else:
                        P.op("dve", "tensor_copy", out=hkt[:, n, :], in_=ps[:, 0:128], reads=[ps], writes=[hkt])
                P.dma("sp", G.HKt[d][b, :, :, h * 128:(h + 1) * 128].rearrange("n t c -> t n c"), hkt[:], hkt,
                      reads=[hkt], writes=[G.HKt[d]])
        for which, c0, dst in ((0, 18 * 128, G.HVt), (1, 30 * 128, G.HGt)):
            load_w_chunk(P, wbig, G.rec_w_in, c0, 512)
            for tt in range(18):
                ps = bank[tt % 4]
                for k in range(8):
                    mm(P, ps, ps[:, 0:512], hT, hT[:, k, tt * 128:(tt + 1) * 128], wbig, wbig[:, k, :],
                       start=(k == 0), stop=(k == 7))
                tk = tok[tt % 2]
                P.op("act", "activation", out=tk[:], in_=ps[:, 0:512], func=(AF.Silu if which else AF.Copy),
                     reads=[ps], writes=[tk])
                P.dma("sp", dst[b, tt], tk[:], tk, reads=[tk], writes=[dst])


def stage_hgrn_scan(P, G, bs=(0, 1), ds=(0, 1)):
    bank = G.bank
    with P.scope():
        msk = P.sb("msk", [128, 4, 128])
        P.dma("sp", msk[:], G.masks[:], msk, reads=[G.masks], writes=[msk])
        qk = [[P.sb(f"qk{d}_{i}", [128, 4, 2, 128], BF16) for i in range(2)] for d in range(2)]
        kt = [[P.sb(f"hkt{d}_{i}", [128, 512], BF16) for i in range(2)] for d in range(2)]
        vt = [[P.sb(f"hvt{d}_{i}", [128, 512], BF16) for i in range(2)] for d in range(2)]
        att = [[P.sb(f"att{d}_{i}", [128, 4, 64], BF16) for i in range(2)] for d in range(2)]
        ot = [[P.sb(f"hot{d}_{i}", [128, 512]) for i in range(2)] for d in range(2)]
        Sf = [P.sb(f"hSf{d}", [128, 4, 128]) for d in range(2)]
        Sb = [P.sb(f"hSb{d}", [128, 4, 128], BF16) for d in range(2)]
        Stmp = [P.sb(f"hStmp{d}", [128, 4, 128]) for d in range(2)]
        GLt = [P.sb(f"hGLt{d}", [128, 4, 36]) for d in range(2)]
        it = 0
        for b in bs:
            for d in ds:
                P.op("dve", "memset", ap=Sf[d][:], constant=0.0, writes=[Sf[d]])
                P.op("dve", "memset", ap=Sb[d][:], constant=0.0, writes=[Sb[d]])
                P.dma("sp", GLt[d][:], G.HGL[d][b].rearrange("h k n -> k h n"), GLt[d], reads=[G.HGL[d]], writes=[GLt[d]])
            orders = {0: list(range(18)), 1: [1, 0] + list(range(17, 1, -1))}
            for step in range(18):
                s = it % 2
                it += 1
                for d in ds:
                    tt = orders[d][step]
                    for j in range(2):
                        P.dma("sp", qk[d][s][:, :, j, :], G.HQK[d][b, :, :, j, tt * 128:(tt + 1) * 128].rearrange("h k t -> k h t"),
                              qk[d][s], reads=[G.HQK[d]], writes=[qk[d][s]])
                    P.dma("sp", kt[d][s][:], G.HKt[d][b, tt], kt[d][s], reads=[G.HKt[d]], writes=[kt[d][s]])
                    P.dma("sp", vt[d][s][:], G.HVt[b, tt], vt[d][s], reads=[G.HVt], writes=[vt[d][s]])
                for ci in range(2):
                    for d in ds:
                        tt = orders[d][step]
                        mi = 1 if d == 0 else 3
                        half = ci if d == 0 else 1 - ci
                        QK, KT, VT = qk[d][s], kt[d][s], vt[d][s]
                        pO = bank[4 + d]
                        chunk = 2 * tt + half
                        lo = half * 64
                        pr = slice(lo, lo + 64)
                        pA = bank[2 * d + ci]
                        pS = bank[6 + d]
                        A_ = att[d][ci]
                        for h in range(4):
                            mm(P, pA, pA[pr, h * 64:(h + 1) * 64], QK, QK[:, h, 1, lo:lo + 64], QK, QK[:, h, 0, lo:lo + 64])
                        P.op("dve", "tensor_tensor", out=A_[pr, :, :], in0=pA[pr, 0:256].rearrange("p (h t) -> p h t", h=4),
                             in1=msk[pr, mi, lo:lo + 64].unsqueeze(1).broadcast_to([64, 4, 64]), op=ALU.mult,
                             reads=[pA, msk], writes=[A_])
                        for h in range(4):
                            hs = slice(h * 128, (h + 1) * 128)
                            mm(P, pO, pO[pr, hs], A_, A_[pr, h, :], VT, VT[pr, hs], start=True, stop=False)
                            mm(P, pO, pO[pr, hs], QK, QK[:, h, 0, lo:lo + 64], Sb[d], Sb[d][:, h, :], start=False, stop=True)
                        for h in range(4):
                            hs = slice(h * 128, (h + 1) * 128)
                            mm(P, pS, pS[:, hs], KT, KT[pr, hs], VT, VT[pr, hs])
                        P.op("dve", "tensor_tensor", out=Stmp[d][:], in0=pS[:, 0:512].rearrange("p (h v) -> p h v", h=4),
                             in1=Sf[d][:], op=ALU.add, reads=[pS, Sf[d]], writes=[Stmp[d]])
                        P.op("dve", "tensor_tensor", out=Sf[d][:], in0=Stmp[d][:],
                             in1=GLt[d][:, :, chunk:chunk + 1].broadcast_to([128, 4, 128]), op=ALU.mult,
                             reads=[Stmp[d], GLt[d]], writes=[Sf[d]])
                        P.op("act", "activation", out=Sb[d][:], in_=Sf[d][:], func=AF.Copy, reads=[Sf[d]], writes=[Sb[d]])
                for d in ds:
                    tt = orders[d][step]
                    o_ = ot[d][s]
                    pO = bank[4 + d]
                    P.op("act", "activation", out=o_[:], in_=pO[:, 0:512], func=AF.Copy, reads=[pO], writes=[o_])
                    P.dma("pool", G.oH[d][b, tt], o_[:], o_, reads=[o_], writes=[G.oH[d]])


def stage_hgrn_post(P, G):
    bank = G.bank
    with P.scope():
        gn = P.sb("gn", [128, 128])
        P.dma("sp", gn[:], G.hg_norm[0:1, :].partition_broadcast(128), gn, reads=[G.hg_norm], writes=[gn])
        o0 = [P.sb(f"ho0_{i}", [128, 512]) for i in range(3)]
        o1 = [P.sb(f"ho1_{i}", [128, 512]) for i in range(3)]
        gt = [P.sb(f"hgt{i}", [128, 512], BF16) for i in range(3)]
        sq = P.sb("hsq", [128, 512])
        st = [P.sb(f"hst{i}", [128, 4]) for i in range(3)]
        yb = [P.sb(f"hyb{i}", [128, 4, 128], BF16) for i in range(3)]
        it = 0
        for b in range(NB):
            for tt in range(18):
                s = it % 3
                it += 1
                P.dma("sp", o0[s][:], G.oH[0][b, tt], o0[s], reads=[G.oH[0]], writes=[o0[s]])
                P.dma("sp", o1[s][:], G.oH[1][b, tt], o1[s], reads=[G.oH[1]], writes=[o1[s]])
                P.dma("sp", gt[s][:], G.HGt[b, tt], gt[s], reads=[G.HGt], writes=[gt[s]])
                o_ = o0[s]
                P.op("pool", "tensor_tensor", out=o_[:], in0=o_[:], in1=o1[s][:], op=ALU.add, reads=[o_, o1[s]], writes=[o_])
                P.op("pool", "tensor_tensor", out=sq[:], in0=o_[:], in1=o_[:], op=ALU.mult, reads=[o_], writes=[sq])
                P.op("dve", "tensor_reduce", out=st[s][:], in_=sq[:].rearrange("p (h v) -> p h v", h=4), axis=AX.X, op=ALU.add,
                     reads=[sq], writes=[st[s]])
                P.op("act", "activation", out=st[s][:], in_=st[s][:], func=AF.Sqrt, scale=1.0 / 128, bias=G.eps[:, 0:1],
                     reads=[st[s], G.eps], writes=[st[s]])
                P.op("dve", "reciprocal", out=st[s][:], in_=st[s][:], reads=[st[s]], writes=[st[s]])
                ov = o_[:].rearrange("p (h v) -> p h v", h=4)
                P.op("dve", "tensor_tensor", out=ov, in0=ov, in1=st[s][:].unsqueeze(2).broadcast_to([128, 4, 128]), op=ALU.mult,
                     reads=[o_, st[s]], writes=[o_])
                P.op("dve", "tensor_tensor", out=ov, in0=ov, in1=gn[:].unsqueeze(1).broadcast_to([128, 4, 128]), op=ALU.mult,
                     reads=[o_, gn], writes=[o_])
                P.op("pool", "tensor_tensor", out=o_[:], in0=o_[:], in1=gt[s][:], op=ALU.mult, reads=[o_, gt[s]], writes=[o_])
                ps = bank[it % 8]
                for c in range(4):
                    P.op("pe", "transpose", out=ps[:, c * 128:(c + 1) * 128], in_=o_[:, c * 128:(c + 1) * 128], identity=G.identf[:],
                         reads=[o_, G.identf], writes=[ps])
                P.op("act", "activation", out=yb[s][:, 0:2, :], in_=ps[:, 0:256].rearrange("p (c t) -> p c t", c=2), func=AF.Copy,
                     reads=[ps], writes=[yb[s]])
                P.op("dve", "tensor_copy", out=yb[s][:, 2:4, :], in_=ps[:, 256:512].rearrange("p (c t) -> p c t", c=2),
                     reads=[ps], writes=[yb[s]])
                P.dma("pool", G.yT[b, 4:8, :, tt * 128:(tt + 1) * 128].rearrange("c p t -> p c t"), yb[s][:], yb[s],
                      reads=[yb[s]], writes=[G.yT])


def load_gates(P, G, l, which, name="gate"):
    gts = []
    for j in range(3):
        g = P.sb(f"{name}{j}", [128, D])
        P.dma("sp", g[:], G.modD[l, j:j + 1, which * D:(which + 1) * D].partition_broadcast(128), g,
              reads=[G.modD], writes=[g])
        gts.append(g)
    return gts


def stage_outproj(P, G, yT, wsrc, l, T, ctx_len, xin_ap, xout_ap, xin_t, xout_t):
    bank = G.bank
    with P.scope():
        gts = load_gates(P, G, l, 2)
        w = P.sb("wout", [128, 8, D], BF16)
        for k in range(8):
            P.dma("pool", w[:, k, :], wsrc[k * 128:(k + 1) * 128, :], w, reads=[wsrc], writes=[w])
        yt = [P.sb(f"yt{i}", [128, 8, 128], BF16) for i in range(3)]
        xt = [P.sb(f"xt{i}", [128, D]) for i in range(3)]
        rt = [P.sb(f"rt{i}", [128, D]) for i in range(3)]
        it = 0
        for b in range(NB):
            for tt in range(T // 128):
                s = it % 3
                it += 1
                j = 2 if tt * 128 < ctx_len else b
                P.dma("sp", yt[s][:], yT[b, :, :, tt * 128:(tt + 1) * 128].rearrange("c p t -> p c t"), yt[s],
                      reads=[yT], writes=[yt[s]])
                P.dma("sp", xt[s][:], xin_ap(b, tt), xt[s], reads=[xin_t], writes=[xt[s]])
                for half in range(2):
                    ps = bank[(it % 3) * 2 + half]
                    for c in range(8):
                        mm(P, ps, ps[:, 0:512], yt[s], yt[s][:, c, :], w, w[:, c, half * 512:(half + 1) * 512],
                           start=(c == 0), stop=(c == 7))
                    P.op("dve", "tensor_tensor", out=rt[s][:, half * 512:(half + 1) * 512], in0=ps[:, 0:512],
                         in1=gts[j][:, half * 512:(half + 1) * 512], op=ALU.mult, reads=[ps, gts[j]], writes=[rt[s]])
                P.op("dve", "tensor_tensor", out=rt[s][:], in0=rt[s][:], in1=xt[s][:], op=ALU.add,
                     reads=[rt[s], xt[s]], writes=[rt[s]])
                P.dma("pool", xout_ap(b, tt), rt[s][:], rt[s], reads=[rt[s]], writes=[xout_t])


def swiglu_pass(P, G, fT, blocks, wg_ap, wu_ap, wd_ap, nch, gts, ctx_len, xin_ap, xout_ap, xin_t, xout_t, wsrc_ts,
                tokw=None, tokw_col=None, tiles_per_b=None):
    bank = G.bank
    F_ = nch * 128
    with P.scope():
        wg = P.sb("wg", [128, 8, F_], BF16)
        wu = P.sb("wu", [128, 8, F_], BF16)
        wd = P.sb("wd", [128, nch, D], BF16)
        for k in range(8):
            P.dma("pool", wg[:, k, :], wg_ap[k * 128:(k + 1) * 128, :], wg, reads=wsrc_ts, writes=[wg])
            P.dma("pool", wu[:, k, :], wu_ap[k * 128:(k + 1) * 128, :], wu, reads=wsrc_ts, writes=[wu])
        for c in range(nch):
            P.dma("pool", wd[:, c, :], wd_ap[c * 128:(c + 1) * 128, :], wd, reads=wsrc_ts, writes=[wd])
        ft = [P.sb(f"ft{i}", [128, 8, 512], BF16) for i in range(2)]
        sg = [P.sb(f"sg{i}", [128, 512]) for i in range(2)]
        act = [P.sb(f"act{i}", [128, nch, 512], BF16) for i in range(2)]
        xt = [P.sb(f"fxt{i}", [128, D]) for i in range(2)]
        rt = [P.sb(f"frt{i}", [128, D]) for i in range(2)]
        bi = 0
        ti = 0
        blist = [(b, t0, t1) for b in range(NB) for (t0, t1) in blocks]

        def load_ft(i):
            if i < len(blist):
                b_, a0, a1 = blist[i]
                for k in range(8):
                    P.dma("sp", ft[i % 2][:, k, 0:a1 - a0], fT[b_, :, k, a0:a1], ft[i % 2], reads=[fT], writes=[ft[i % 2]])

        load_ft(0)
        for b in range(NB):
            for (t0, t1) in blocks:
                n = t1 - t0
                F = ft[bi % 2]
                A_ = act[bi % 2]
                bi += 1
                load_ft(bi)
                for jc in range(nch):
                    pg, pu = bank[jc % 2], bank[2 + jc % 2]
                    cs = slice(jc * 128, (jc + 1) * 128)
                    for k in range(8):
                        mm(P, pg, pg[:, 0:n], wg, wg[:, k, cs], F, F[:, k, 0:n], start=(k == 0), stop=(k == 7))
                    for k in range(8):
                        mm(P, pu, pu[:, 0:n], wu, wu[:, k, cs], F, F[:, k, 0:n], start=(k == 0), stop=(k == 7))
                    S_ = sg[jc % 2]
                    P.op("act", "activation", out=S_[:, 0:n], in_=pg[:, 0:n], func=AF.Silu, reads=[pg], writes=[S_])
                    P.op("dve", "tensor_tensor", out=A_[:, jc, 0:n], in0=S_[:, 0:n], in1=pu[:, 0:n], op=ALU.mult,
                         reads=[S_, pu], writes=[A_])
                for ts in range(n // 128):
                    tt = t0 // 128 + ts
                    s = ti % 2
                    ti += 1
                    j = 2 if tt * 128 < ctx_len else b
                    P.dma("sp", xt[s][:], xin_ap(b, tt), xt[s], reads=[xin_t], writes=[xt[s]])
                    for half in range(2):
                        po = bank[4 + (ti % 2) * 2 + half]
                        for jc in range(nch):
                            mm(P, po, po[:, 0:512], A_, A_[:, jc, ts * 128:(ts + 1) * 128], wd,
                               wd[:, jc, half * 512:(half + 1) * 512], start=(jc == 0), stop=(jc == nch - 1))
                        hs = slice(half * 512, (half + 1) * 512)
                        if tokw is None:
                            P.op("dve", "tensor_tensor", out=rt[s][:, hs], in0=po[:, 0:512], in1=gts[j][:, hs], op=ALU.mult,
                                 reads=[po, gts[j]], writes=[rt[s]])
                        else:
                            tix = b * tiles_per_b + tt
                            P.op("dve", "scalar_tensor_tensor", out=rt[s][:, hs], in0=po[:, 0:512],
                                 scalar=tokw[:, tix, tokw_col:tokw_col + 1], in1=gts[j][:, hs], op0=ALU.mult, op1=ALU.mult,
                                 reads=[po, gts[j], tokw], writes=[rt[s]])
                    P.op("dve", "tensor_tensor", out=rt[s][:], in0=rt[s][:], in1=xt[s][:], op=ALU.add,
                         reads=[rt[s], xt[s]], writes=[rt[s]])
                    P.dma("pool", xout_ap(b, tt), rt[s][:], rt[s], reads=[rt[s]], writes=[xout_t])


def stage_ffn0(P, G):
    with P.scope():
        gts = load_gates(P, G, 0, 5)
        for hf in range(2):
            cs = slice(hf * 1408, (hf + 1) * 1408)
            xin_t = G.x1 if hf == 0 else G.x2
            swiglu_pass(P, G, G.fT0, TBLK, G.ffn_wg[:, cs], G.ffn_wu[:, cs], G.ffn_wd[cs, :], 11, gts, TC,
                        (lambda b, tt, X=xin_t: X[b, tt * 128:(tt + 1) * 128, :]),
                        (lambda b, tt: G.x2[b, tt * 128:(tt + 1) * 128, :]), xin_t, G.x2,
                        [G.ffn_wg, G.ffn_wu, G.ffn_wd])


LBLK = [(TC + i * 512, TC + (i + 1) * 512) for i in range(4)]


def stage_att_inproj(P, G, b):
    bank = G.bank
    with P.scope():
        hT = P.sb("hTres", [128, 8, TT], BF16)
        for k in range(8):
            P.dma("sp", hT[:, k, :], G.hT1[b, :, k, :], hT, reads=[G.hT1], writes=[hT])
        rope = P.sb("rope", [128, 2, TL])
        P.dma("sp", rope[:], G.rope[:], rope, reads=[G.rope], writes=[rope])
        perm = P.sb("perm", [128, 128], BF16)
        P.dma("pool", perm[:], G.perm[:], perm, reads=[G.perm], writes=[perm])
        WP = WPrefetch(P, G.att_w_in, list(range(10)))
        wv = P.sb("wv", [128, 8, 256], BF16)
        load_w_chunk(P, wv, G.att_w_in, 1280, 256)
        qraw = [P.sb(f"qraw{i}", [128, 512], BF16) for i in range(2)]
        t1 = [P.sb(f"t1_{i}", [128, 512]) for i in range(2)]
        t2 = [P.sb(f"t2_{i}", [128, 512]) for i in range(2)]
        qo = [P.sb(f"qo{i}", [128, 512], BF16) for i in range(2)]
        kc_ = [P.sb(f"kc{i}", [128, 256], BF16) for i in range(2)]
        vtk = [P.sb(f"vtk{i}", [128, 256], BF16) for i in range(2)]
        it = 0
        for ch in range(10):
            wt = WP.get(ch)
            if ch < 8:
                dst = lambda tl0, n, ch=ch: G.qT[b, 2 * ch:2 * ch + 2, :, tl0:tl0 + n].rearrange("h k t -> (h k) t")
                dt_ = G.qT
            else:
                kc = ch - 8
                dst = lambda tl0, n, kc=kc: G.kT[b, 2 * kc:2 * kc + 2, :, TC + tl0:TC + tl0 + n].rearrange("h k t -> (h k) t")
                dt_ = G.kT
                ps = bank[0]
                for k in range(8):
                    mm(P, ps, ps[:, 0:TC], wt, wt[:, k, :], hT, hT[:, k, 0:TC], start=(k == 0), stop=(k == 7))
                kk_ = kc_[kc % 2]
                P.op("act", "activation", out=kk_[:], in_=ps[:, 0:TC], func=AF.Copy, reads=[ps], writes=[kk_])
                P.dma("pool", G.kT[b, 2 * kc:2 * kc + 2, :, 0:TC].rearrange("h k t -> (h k) t"), kk_[:], kk_,
                      reads=[kk_], writes=[G.kT])
            for (t0, t1_) in LBLK:
                s = it % 2
                it += 1
                tl0 = t0 - TC
                p1, p2 = bank[(it % 2) * 2], bank[(it % 2) * 2 + 1]
                for k in range(8):
                    mm(P, p1, p1[:, 0:512], wt, wt[:, k, :], hT, hT[:, k, t0:t1_], start=(k == 0), stop=(k == 7))
                P.op("act", "activation", out=qraw[s][:], in_=p1[:, 0:512], func=AF.Copy, reads=[p1], writes=[qraw[s]])
                mm(P, p2, p2[:, 0:512], perm, perm[:], qraw[s], qraw[s][:])
                P.op("dve", "tensor_tensor", out=t1[s][:], in0=p1[:, 0:512], in1=rope[:, 0, tl0:tl0 + 512], op=ALU.mult,
                     reads=[p1, rope], writes=[t1[s]])
                P.op("dve", "tensor_tensor", out=t2[s][:], in0=p2[:, 0:512], in1=rope[:, 1, tl0:tl0 + 512], op=ALU.mult,
                     reads=[p2, rope], writes=[t2[s]])
                P.op("dve", "tensor_tensor", out=qo[s][:], in0=t1[s][:], in1=t2[s][:], op=ALU.add,
                     reads=[t1[s], t2[s]], writes=[qo[s]])
                P.dma("pool", dst(tl0, 512), qo[s][:], qo[s], reads=[qo[s]], writes=[dt_])
        for tt in range(18):
            ps = bank[4 + tt % 4]
            for k in range(8):
                mm(P, ps, ps[:, 0:256], hT, hT[:, k, tt * 128:(tt + 1) * 128], wv, wv[:, k, :], start=(k == 0), stop=(k == 7))
            v_ = vtk[tt % 2]
            P.op("act", "activation", out=v_[:], in_=ps[:, 0:256], func=AF.Copy, reads=[ps], writes=[v_])
            P.dma("sp", G.Vt1[b, tt], v_[:], v_, reads=[v_], writes=[G.Vt1])


def stage_attention(P, G):
    bank = G.bank
    with P.scope():
        msk = P.sb("mskb", [128, 4, 128], BF16)
        P.dma("pool", msk[:], G.masks[:], msk, reads=[G.masks], writes=[msk])
        es = P.sb("es", [64, 16])
        P.dma("sp", es[:], G.att_sink[0:1, :].partition_broadcast(64), es, reads=[G.att_sink], writes=[es])
        P.op("act", "activation", out=es[:], in_=es[:], func=AF.Exp, reads=[es], writes=[es])
        ones = P.sb("ones", [128, 64], BF16)
        P.op("dve", "memset", ap=ones[:], constant=1.0, writes=[ones])
        Kt = [P.sb(f"Kt{i}", [64, TT], BF16) for i in range(2)]
        Vv = [P.sb(f"Vv{i}", [128, 18, 64], BF16) for i in range(2)]
        Qt = [P.sb(f"Qt{i}", [64, 4, TL], BF16) for i in range(2)]
        E = [P.sb(f"E{i}", [128, 4, 128], BF16) for i in range(6)]
        den = [P.sb(f"den{i}", [64, 4, 128]) for i in range(2)]
        ob = [P.sb(f"ob{i}", [64, 4, 128], BF16) for i in range(2)]
        g = 0
        r = 0
        ii = 0
        for b in range(NB):
            for hk in range(4):
                K_, V_, Q_ = Kt[g % 2], Vv[g % 2], Qt[g % 2]
                g += 1
                P.dma("sp", K_[:], G.kT[b, hk], K_, reads=[G.kT], writes=[K_])
                P.dma("sp", V_[:], G.Vt1[b, :, :, hk * 64:(hk + 1) * 64].rearrange("n t c -> t n c"), V_,
                      reads=[G.Vt1], writes=[V_])
                P.dma("sp", Q_[:], G.qT[b, 4 * hk:4 * hk + 4].rearrange("g k t -> k g t"), Q_, reads=[G.qT], writes=[Q_])
                for i in range(16):
                    tiles = [(0, None), (1, None)]
                    if i > 0:
                        tiles.append((2 + i - 1, 3))
                    tiles.append((2 + i, None))
                    if i < 15:
                        tiles.append((2 + i + 1, 1))
                    pN, pD = bank[4 + ii % 2], bank[6 + ii % 2]
                    Es = []
                    for ti, (kt, mi) in enumerate(tiles):
                        pS = bank[r % 4]
                        E_ = E[r % 6]
                        r += 1
                        mm(P, pS, pS[:, 0:512], K_, K_[:, kt * 128:(kt + 1) * 128], Q_, Q_[:, :, i * 128:(i + 1) * 128])
                        P.op("act", "activation", out=E_[:], in_=pS[:, 0:512].rearrange("p (g t) -> p g t", g=4),
                             func=AF.Exp, scale=0.125, reads=[pS], writes=[E_])
                        if mi is not None:
                            P.op("dve", "tensor_tensor", out=E_[:], in0=E_[:],
                                 in1=msk[:, mi, :].unsqueeze(1).broadcast_to([128, 4, 128]), op=ALU.mult,
                                 reads=[E_, msk], writes=[E_])
                        Es.append(E_)
                    for ti, (kt, mi) in enumerate(tiles):
                        E_ = Es[ti]
                        first, last = ti == 0, ti == len(tiles) - 1
                        mm(P, pN, pN[0:64, 0:512], V_, V_[:, kt, :], E_, E_[:], start=first, stop=last)
                        mm(P, pD, pD[0:64, 0:512], ones, ones[:], E_, E_[:], start=first, stop=last)
                    s = ii % 2
                    ii += 1
                    P.op("dve", "tensor_tensor", out=den[s][:], in0=pD[0:64, 0:512].rearrange("p (g t) -> p g t", g=4),
                         in1=es[:, 4 * hk:4 * hk + 4].unsqueeze(2).broadcast_to([64, 4, 128]), op=ALU.add,
                         reads=[pD, es], writes=[den[s]])
                    P.op("dve", "reciprocal", out=den[s][:], in_=den[s][:], reads=[den[s]], writes=[den[s]])
                    P.op("dve", "tensor_tensor", out=ob[s][:], in0=pN[0:64, 0:512].rearrange("p (g t) -> p g t", g=4),
                         in1=den[s][:], op=ALU.mult, reads=[pN, den[s]], writes=[ob[s]])
                    P.dma("pool", G.oT1[b, 2 * hk:2 * hk + 2, :, i * 128:(i + 1) * 128].rearrange("c (hh k) t -> k (c hh) t", hh=2),
                          ob[s][:], ob[s], reads=[ob[s]], writes=[G.oT1])


def stage_router(P, G):
    bank = G.bank
    with P.scope():
        rw = P.sb("rw", [128, 8, 8])
        P.dma("sp", rw[:], G.moe_router[:].rearrange("(k p) e -> p k e", p=128), rw, reads=[G.moe_router], writes=[rw])
        rb = P.sb("rb", [128, 8])
        P.dma("sp", rb[:], G.moe_router_b[0:1, :].partition_broadcast(128), rb, reads=[G.moe_router_b], writes=[rb])
        ff = [P.sb(f"ff{i}", [128, 8, 128]) for i in range(2)]
        lg = [P.sb(f"lg{i}", [128, 8]) for i in range(2)]
        tm = [P.sb(f"tm{i}", [128, 4, 8]) for i in range(2)]
        sc = [P.sb(f"rsc{i}", [128, 4]) for i in range(2)]
        it = 0
        for b in range(NB):
            for tt in range(16):
                s = it % 2
                tix = b * 16 + tt
                it += 1
                P.dma("sp", ff[s][:], G.fT1f[b, :, :, tt * 128:(tt + 1) * 128], ff[s], reads=[G.fT1f], writes=[ff[s]])
                ps = bank[it % 2]
                for k in range(8):
                    mm(P, ps, ps[:, 0:8], ff[s], ff[s][:, k, :], rw, rw[:, k, :], start=(k == 0), stop=(k == 7))
                L_, T_, S_ = lg[s], tm[s], sc[s]
                P.op("dve", "tensor_tensor", out=L_[:], in0=ps[:, 0:8], in1=rb[:], op=ALU.add, reads=[ps, rb], writes=[L_])
                P.op("dve", "tensor_reduce", out=S_[:, 0:1], in_=L_[:], axis=AX.X, op=ALU.max, reads=[L_], writes=[S_])
                P.op("dve", "tensor_scalar", out=T_[:, 0, :], in0=L_[:], scalar1=S_[:, 0:1], scalar2=-1e30, op0=ALU.is_equal,
                     op1=ALU.mult, reads=[L_, S_], writes=[T_])
                P.op("dve", "tensor_tensor", out=T_[:, 0, :], in0=T_[:, 0, :], in1=L_[:], op=ALU.add, reads=[T_, L_], writes=[T_])
                P.op("dve", "tensor_reduce", out=S_[:, 1:2], in_=T_[:, 0, :], axis=AX.X, op=ALU.max, reads=[T_], writes=[S_])
                P.op("dve", "tensor_scalar", out=T_[:, 1, :], in0=L_[:], scalar1=S_[:, 1:2], scalar2=None, op0=ALU.is_ge,
                     reads=[L_, S_], writes=[T_])
                P.op("dve", "tensor_scalar", out=S_[:, 2:3], in0=S_[:, 0:1], scalar1=-1.0, scalar2=None, op0=ALU.mult,
                     reads=[S_], writes=[S_])
                P.op("act", "activation", out=T_[:, 2, :], in_=L_[:], func=AF.Exp, bias=S_[:, 2:3], reads=[L_, S_], writes=[T_])
                P.op("dve", "tensor_tensor", out=T_[:, 2, :], in0=T_[:, 2, :], in1=T_[:, 1, :], op=ALU.mult, reads=[T_], writes=[T_])
                P.op("dve", "tensor_reduce", out=S_[:, 3:4], in_=T_[:, 2, :], axis=AX.X, op=ALU.add, reads=[T_], writes=[S_])
                P.op("dve", "reciprocal", out=S_[:, 3:4], in_=S_[:, 3:4], reads=[S_], writes=[S_])
                P.op("dve", "tensor_scalar", out=G.WTm[:, tix, :], in0=T_[:, 2, :], scalar1=S_[:, 3:4], scalar2=None, op0=ALU.mult,
                     reads=[T_, S_], writes=[G.WTm])


def stage_moe(P, G, experts=range(8)):
    with P.scope():
        gts = load_gates(P, G, 1, 5)
        first = True
        blocks = [(i * 512, (i + 1) * 512) for i in range(4)]
        for e in experts:
            for hf in range(2):
                cs = slice(hf * 1408, (hf + 1) * 1408)
                xin_t = G.x3 if first else G.x4
                first = False
                swiglu_pass(P, G, G.fT1, blocks, G.moe_wg[e, :, cs], G.moe_wu[e, :, cs], G.moe_wd[e, cs, :], 11, gts, 0,
                            (lambda b, tt, X=xin_t: X[b, tt * 128:(tt + 1) * 128, :]),
                            (lambda b, tt: G.x4[b, tt * 128:(tt + 1) * 128, :]), xin_t, G.x4,
                            [G.moe_wg, G.moe_wu, G.moe_wd], tokw=G.WTm, tokw_col=e, tiles_per_b=16)


def stage_final(P, G, src):
    with P.scope():
        gf = P.sb("gfin", [128, D])
        P.dma("sp", gf[:], G.norm_final[0:1, :].partition_broadcast(128), gf, reads=[G.norm_final], writes=[gf])
        xt = [P.sb(f"zxt{i}", [128, D]) for i in range(2)]
        sq = P.sb("zsq", [128, D], BF16)
        ss = [P.sb(f"zss{i}", [128, 1]) for i in range(2)]
        yo = [P.sb(f"zyo{i}", [128, D]) for i in range(2)]
        it = 0
        for b in range(NB):
            for tt in range(16):
                s = it % 2
                it += 1
                P.dma("sp", xt[s][:], src[b, tt * 128:(tt + 1) * 128, :], xt[s], reads=[src], writes=[xt[s]])
                P.op("act", "activation", out=sq[:], in_=xt[s][:], func=AF.Square, accum_out=ss[s][:], reads=[xt[s]], writes=[sq, ss[s]])
                P.op("act", "activation", out=ss[s][:], in_=ss[s][:], func=AF.Sqrt, scale=1.0 / D, bias=G.eps[:, 0:1],
                     reads=[ss[s], G.eps], writes=[ss[s]])
                P.op("dve", "reciprocal", out=ss[s][:], in_=ss[s][:], reads=[ss[s]], writes=[ss[s]])
                P.op("dve", "scalar_tensor_tensor", out=yo[s][:], in0=xt[s][:], scalar=ss[s][:, 0:1], in1=gf[:], op0=ALU.mult,
                     op1=ALU.mult, reads=[xt[s], ss[s], gf], writes=[yo[s]])
                P.dma("pool", G.out[b, tt * 128:(tt + 1) * 128, :], yo[s][:], yo[s], reads=[yo[s]], writes=[G.out])


I32 = mybir.dt.int32
BS = 512
SUBS = BS // 512
NBLK = (NB * TL * 2) // BS + 8
NSLOT = NBLK * BS
DUMMY = NB * TL


def stage_moe_sparse(P, G, nblk=NBLK):
    bank = G.bank
    IOA = bass.IndirectOffsetOnAxis
    with P.scope():
        WAB = P.sb("WAB", [128, 32, 2])
        SAB = P.sb("SAB", [128, 2, 32], I32)
        IDXG = P.sb("IDXG", [128, NBLK, 2], I32)
        IDXD = P.sb("IDXD", [128, NBLK, 2], I32)
        _phaseA = P.scope()
        _phaseA.__enter__()
        LG = G.LG
        T3 = lambda nm: P.sb(nm, [128, 32, 8])
        T2 = lambda nm: P.sb(nm, [128, 32])
        bc3 = lambda t2: t2[:].unsqueeze(2).broadcast_to([128, 32, 8])
        M1, M2, NM1, DEN = T2("M1"), T2("M2"), T2("NM1"), T2("DEN")
        TMP, SEL, WT = T3("TMP"), T3("SEL"), T3("WT")
        P.op("dve", "tensor_reduce", out=M1[:], in_=LG[:], axis=AX.X, op=ALU.max, reads=[LG], writes=[M1])
        P.op("dve", "tensor_tensor", out=TMP[:], in0=LG[:], in1=bc3(M1), op=ALU.is_equal, reads=[LG, M1], writes=[TMP])
        P.op("dve", "scalar_tensor_tensor", out=TMP[:], in0=TMP[:], scalar=-1e30, in1=LG[:], op0=ALU.mult, op1=ALU.add,
             reads=[TMP, LG], writes=[TMP])
        P.op("dve", "tensor_reduce", out=M2[:], in_=TMP[:], axis=AX.X, op=ALU.max, reads=[TMP], writes=[M2])
        P.op("dve", "tensor_tensor", out=SEL[:], in0=LG[:], in1=bc3(M2), op=ALU.is_ge, reads=[LG, M2], writes=[SEL])
        P.op("dve", "tensor_tensor", out=TMP[:], in0=LG[:], in1=bc3(M1), op=ALU.subtract, reads=[LG, M1], writes=[TMP])
        P.op("act", "activation", out=TMP[:], in_=TMP[:], func=AF.Exp, reads=[TMP], writes=[TMP])
        P.op("dve", "tensor_tensor", out=TMP[:], in0=TMP[:], in1=SEL[:], op=ALU.mult, reads=[TMP, SEL], writes=[TMP])
        P.op("dve", "tensor_reduce", out=DEN[:], in_=TMP[:], axis=AX.X, op=ALU.add, reads=[TMP], writes=[DEN])
        P.op("dve", "reciprocal", out=DEN[:], in_=DEN[:], reads=[DEN], writes=[DEN])
        P.op("dve", "tensor_tensor", out=WT[:], in0=TMP[:], in1=bc3(DEN), op=ALU.mult, reads=[TMP, DEN], writes=[WT])
        cst = P.sb("mcst", [128, 512])
        P.dma("sp", cst[:], G.moe_consts[:], cst, reads=[G.moe_consts], writes=[cst])
        TH = cst[:, 0:64].rearrange("p (e m) -> p e m", m=8)
        JJ = cst[:, 64:64 + NBLK]
        CG = cst[:, 88:90]
        CD = cst[:, 104:106]
        TIDc = cst[:, 128:160]
        RM = cst[:, 256:512]
        selb = P.sb("selb", [128, 256], BF16)
        P.op("dve", "tensor_copy", out=selb[:], in_=SEL[:].rearrange("p i e -> p (i e)"), reads=[SEL], writes=[selb])
        mb_ = P.sb("mskb2", [128, 4, 128], BF16)
        P.dma("pool", mb_[:], G.masks[:], mb_, reads=[G.masks], writes=[mb_])
        onesb = P.sb("onesb", [128, 128], BF16)
        P.op("dve", "memset", ap=onesb[:], constant=1.0, writes=[onesb])
        mm(P, bank[2], bank[2][:, 0:256], mb_, mb_[:, 0, :], selb, selb[:])
        mm(P, bank[3], bank[3][:, 0:256], onesb, onesb[:], selb, selb[:])
        SLOT = T3("SLOT")
        P.op("dve", "tensor_copy", out=SLOT[:], in_=bank[2][:, 0:256].rearrange("p (i e) -> p i e", e=8), reads=[bank[2]], writes=[SLOT])
        TOTp = P.sb("TOTp", [128, 8, 32])
        P.op("dve", "tensor_copy", out=TOTp[:], in_=bank[3][:, 0:256].rearrange("p (i e) -> p e i", e=8), reads=[bank[3]], writes=[TOTp])
        INC = P.sb("INC", [128, 8, 32])
        P.op("dve", "tensor_tensor_scan", out=INC[:].rearrange("p e i -> p (e i)"), data0=RM, data1=TOTp[:].rearrange("p e i -> p (e i)"),
             initial=0.0, op0=ALU.mult, op1=ALU.add, reads=[cst, TOTp], writes=[INC])
        OFFp = P.sb("OFFp", [128, 8, 32])
        P.op("dve", "tensor_tensor", out=OFFp[:], in0=INC[:], in1=TOTp[:], op=ALU.subtract, reads=[INC, TOTp], writes=[OFFp])
        CMP = P.sb("CMP", [128, 8, 8])
        P.op("dve", "tensor_tensor", out=CMP[:], in0=INC[:, :, 31:32].broadcast_to([128, 8, 8]), in1=TH, op=ALU.is_gt,
             reads=[INC, cst], writes=[CMP])
        nbk = P.sb("nbk", [128, 8])
        P.op("dve", "tensor_reduce", out=nbk[:], in_=CMP[:], axis=AX.X, op=ALU.add, reads=[CMP], writes=[nbk])
        PEI = P.sb("PEI", [128, 8])
        P.op("dve", "tensor_tensor_scan", out=PEI[:], data0=onesb[:, 0:8], data1=nbk[:], initial=0.0, op0=ALU.mult, op1=ALU.add,
             reads=[onesb, nbk], writes=[PEI])
        PST = P.sb("PST", [128, 8])
        P.op("dve", "tensor_tensor", out=PST[:], in0=PEI[:], in1=nbk[:], op=ALU.subtract, reads=[PEI, nbk], writes=[PST])
        P.op("dve", "tensor_scalar", out=PST[:], in0=PST[:], scalar1=float(BS), scalar2=None, op0=ALU.mult, reads=[PST], writes=[PST])
        P.op("dve", "tensor_tensor", out=SLOT[:], in0=SLOT[:], in1=OFFp[:].rearrange("p e i -> p i e"), op=ALU.add,
             reads=[SLOT, OFFp], writes=[SLOT])
        P.op("dve", "tensor_tensor", out=SLOT[:], in0=SLOT[:], in1=PST[:].unsqueeze(1).broadcast_to([128, 32, 8]), op=ALU.add,
             reads=[SLOT, PST], writes=[SLOT])
        V = T3("V")
        P.op("dve", "scalar_tensor_tensor", out=V[:], in0=SLOT[:], scalar=1.0, in1=SEL[:], op0=ALU.add, op1=ALU.mult,
             reads=[SLOT, SEL], writes=[V])
        MA, MB = T2("MA"), T2("MB")
        P.op("dve", "tensor_reduce", out=MA[:], in_=V[:], axis=AX.X, op=ALU.max, reads=[V], writes=[MA])
        P.op("dve", "tensor_tensor", out=TMP[:], in0=V[:], in1=bc3(MA), op=ALU.is_equal, reads=[V, MA], writes=[TMP])
        IS2 = T3("IS2")
        P.op("dve", "tensor_tensor", out=IS2[:], in0=TMP[:], in1=WT[:], op=ALU.mult, reads=[TMP, WT], writes=[IS2])
        P.op("dve", "tensor_reduce", out=WAB[:, :, 0], in_=IS2[:], axis=AX.X, op=ALU.add, reads=[IS2], writes=[WAB])
        P.op("dve", "tensor_tensor", out=TMP[:], in0=TMP[:], in1=V[:], op=ALU.mult, reads=[TMP, V], writes=[TMP])
        P.op("dve", "tensor_tensor", out=V[:], in0=V[:], in1=TMP[:], op=ALU.subtract, reads=[V, TMP], writes=[V])
        P.op("dve", "tensor_reduce", out=MB[:], in_=V[:], axis=AX.X, op=ALU.max, reads=[V], writes=[MB])
        P.op("dve", "tensor_tensor", out=TMP[:], in0=V[:], in1=bc3(MB), op=ALU.is_equal, reads=[V, MB], writes=[TMP])
        P.op("dve", "tensor_tensor", out=IS2[:], in0=TMP[:], in1=WT[:], op=ALU.mult, reads=[TMP, WT], writes=[IS2])
        P.op("dve", "tensor_reduce", out=WAB[:, :, 1], in_=IS2[:], axis=AX.X, op=ALU.add, reads=[IS2], writes=[WAB])
        P.op("dve", "tensor_scalar", out=MA[:], in0=MA[:], scalar1=-1.0, scalar2=None, op0=ALU.add, reads=[MA], writes=[MA])
        P.op("dve", "tensor_scalar", out=MB[:], in0=MB[:], scalar1=-1.0, scalar2=None, op0=ALU.add, reads=[MB], writes=[MB])
        P.op("dve", "tensor_copy", out=SAB[:, 0, :], in_=MA[:], reads=[MA], writes=[SAB])
        P.op("dve", "tensor_copy", out=SAB[:, 1, :], in_=MB[:], reads=[MB], writes=[SAB])
        CJ = P.sb("CJ", [128, NBLK, 8])
        P.op("dve", "tensor_tensor", out=CJ[:], in0=PEI[:].unsqueeze(1).broadcast_to([128, NBLK, 8]),
             in1=JJ.unsqueeze(2).broadcast_to([128, NBLK, 8]), op=ALU.is_le, reads=[PEI, cst], writes=[CJ])
        EJ = P.sb("EJ", [128, NBLK])
        P.op("dve", "tensor_reduce", out=EJ[:], in_=CJ[:], axis=AX.X, op=ALU.add, reads=[CJ], writes=[EJ])
        P.op("dve", "tensor_scalar", out=EJ[:], in0=EJ[:], scalar1=7.0, scalar2=None, op0=ALU.min, reads=[EJ], writes=[EJ])
        IGf = P.sb("IGf", [128, NBLK, 2])
        P.op("dve", "scalar_tensor_tensor", out=IGf[:], in0=EJ[:].unsqueeze(2).broadcast_to([128, NBLK, 2]), scalar=256.0,
             in1=CG.unsqueeze(1).broadcast_to([128, NBLK, 2]), op0=ALU.mult, op1=ALU.add, reads=[EJ, cst], writes=[IGf])
        P.op("dve", "tensor_copy", out=IDXG[:], in_=IGf[:], reads=[IGf], writes=[IDXG])
        IDf = P.sb("IDf", [128, NBLK, 2])
        P.op("dve", "scalar_tensor_tensor", out=IDf[:], in0=EJ[:].unsqueeze(2).broadcast_to([128, NBLK, 2]), scalar=256.0,
             in1=CD.unsqueeze(1).broadcast_to([128, NBLK, 2]), op0=ALU.mult, op1=ALU.add, reads=[EJ, cst], writes=[IDf])
        P.op("dve", "tensor_copy", out=IDXD[:], in_=IDf[:], reads=[IDf], writes=[IDXD])
        ini = P.sb("ini", [128, NSLOT // 128], I32)
        P.op("dve", "memset", ap=ini[:], constant=DUMMY, writes=[ini])
        P.dma("sp", G.tokidx[:, 0].rearrange("(p c) -> p c", c=NSLOT // 128), ini[:], ini, reads=[ini], writes=[G.tokidx])
        tid = P.sb("tid", [128, 32], I32)
        P.op("dve", "tensor_copy", out=tid[:], in_=TIDc, reads=[cst], writes=[tid])
        for i in range(32):
            for ab in range(2):
                P.dma("pool", None, None, tid, reads=[tid, SAB], writes=[G.tokidx], meth="indirect_dma_start",
                      out=G.tokidx[:, :], out_offset=IOA(ap=SAB[:, ab, i:i + 1], axis=0), in_=tid[:, i:i + 1], in_offset=None)
        _phaseA.__exit__(None, None, None)
        WG2 = G.moe_wg[:, :]
        WU2 = G.moe_wu[:, :]
        WD2 = G.moe_wd[:, :]
        with P.scope():
            wg = [P.sb(f"swg{i}", [128, 8, 1408], BF16) for i in range(2)]
            wu = [P.sb(f"swu{i}", [128, 8, 1408], BF16) for i in range(2)]
            wd = [P.sb(f"swd{i}", [128, 11, D], BF16) for i in range(2)]
            idx = [P.sb(f"sidx{i}", [128, 1], I32) for i in range(8)]
            xg = [P.sb(f"sxg{i}", [128, D]) for i in range(4)]
            xTs = [P.sb(f"sxT{i}", [128, SUBS, 8, 512], BF16) for i in range(2)]
            sg = [P.sb(f"ssg{i}", [128, 512]) for i in range(2)]
            act = P.sb("sact", [128, 11, 512], BF16)
            yt = [P.sb(f"syt{i}", [128, D]) for i in range(3)]
            yi = 0
            gstate = {"gi": 0}

            def emit_gather(j):
                xT = xTs[j % 2]
                gi = gstate["gi"]
                for sub in range(SUBS):
                    for c in range(4):
                        ix = idx[gi % 8]
                        X_ = xg[gi % 4]
                        gi += 1
                        r0 = j * BS + sub * 512 + c * 128
                        P.dma("sp", ix[:], G.tokidx[r0:r0 + 128, :], ix, reads=[G.tokidx], writes=[ix])
                        P.dma("pool", None, None, X_, reads=[ix, G.f_tok], writes=[X_], meth="indirect_dma_start",
                              out=X_[:], out_offset=None, in_=G.f_tok[:, :], in_offset=IOA(ap=ix[:, 0:1], axis=0))
                        for k in range(8):
                            ps = bank[4 + (gi % 2) * 2 + k // 4]
                            P.op("pe", "transpose", out=ps[:, (k % 4) * 128:(k % 4 + 1) * 128], in_=X_[:, k * 128:(k + 1) * 128],
                                 identity=G.identf[:], reads=[X_, G.identf], writes=[ps])
                        for kh in range(2):
                            ps = bank[4 + (gi % 2) * 2 + kh]
                            P.op("act" if kh else "dve", "activation" if kh else "tensor_copy",
                                 out=xT[:, sub, kh * 4:(kh + 1) * 4, c * 128:(c + 1) * 128],
                                 in_=ps[:, 0:512].rearrange("p (k t) -> p k t", k=4), reads=[ps], writes=[xT],
                                 **({"func": AF.Copy} if kh else {}))
                gstate["gi"] = gi

            emit_gather(0)
            for j in range(nblk):
                xT = xTs[j % 2]
                for hf in range(2):
                    s = (2 * j + hf) % 2
                    if not (os.environ.get("NO_WGATHER") and j > 0):
                      P.dma("pool", None, None, wg[s], reads=[IDXG, G.moe_wg], writes=[wg[s]], meth="indirect_dma_start",
                          out=wg[s][:].rearrange("p k c -> p (k c)"), out_offset=None, in_=WG2, in_offset=IOA(ap=IDXG[:, j, hf:hf + 1], axis=0))
                    if not (os.environ.get("NO_WGATHER") and j > 0):
                      P.dma("pool", None, None, wu[s], reads=[IDXG, G.moe_wu], writes=[wu[s]], meth="indirect_dma_start",
                          out=wu[s][:].rearrange("p k c -> p (k c)"), out_offset=None, in_=WU2, in_offset=IOA(ap=IDXG[:, j, hf:hf + 1], axis=0))
                    if not (os.environ.get("NO_WGATHER") and j > 0):
                      P.dma("pool", None, None, wd[s], reads=[IDXD, G.moe_wd], writes=[wd[s]], meth="indirect_dma_start",
                          out=wd[s][:].rearrange("p c n -> p (c n)"), out_offset=None, in_=WD2, in_offset=IOA(ap=IDXD[:, j, hf:hf + 1], axis=0))
                    for sub in range(SUBS):
                        for jc in range(11):
                            pg, pu = bank[jc % 2], bank[2 + jc % 2]
                            cs = slice(jc * 128, (jc + 1) * 128)
                            for k in range(8):
                                mm(P, pg, pg[:, 0:512], wg[s], wg[s][:, k, cs], xT, xT[:, sub, k, :], start=(k == 0), stop=(k == 7))
                            for k in range(8):
                                mm(P, pu, pu[:, 0:512], wu[s], wu[s][:, k, cs], xT, xT[:, sub, k, :], start=(k == 0), stop=(k == 7))
                            S_ = sg[jc % 2]
                            P.op("act", "activation", out=S_[:], in_=pg[:, 0:512], func=AF.Silu, reads=[pg], writes=[S_])
                            P.op("dve", "tensor_tensor", out=act[:, jc, :], in0=S_[:], in1=pu[:, 0:512], op=ALU.mult,
                                 reads=[S_, pu], writes=[act])
                        if hf == 0 and sub == SUBS - 1 and j + 1 < nblk:
                            emit_gather(j + 1)
                        for tt in range(4):
                            Y_ = yt[yi % 3]
                            yi += 1
                            for half in range(2):
                                po = bank[4 + (tt * 2 + half) % 4]
                                for jc in range(11):
                                    mm(P, po, po[:, 0:512], act, act[:, jc, tt * 128:(tt + 1) * 128], wd[s],
                                       wd[s][:, jc, half * 512:(half + 1) * 512], start=(jc == 0), stop=(jc == 10))
                                hs = slice(half * 512, (half + 1) * 512)
                                if half == 0:
                                    P.op("act", "activation", out=Y_[:, hs], in_=po[:, 0:512], func=AF.Copy, reads=[po], writes=[Y_])
                                else:
                                    P.op("dve", "tensor_copy", out=Y_[:, hs], in_=po[:, 0:512], reads=[po], writes=[Y_])
                            r0 = j * BS + sub * 512 + tt * 128
                            P.dma("sp", G.Yb[r0:r0 + 128, hf, :], Y_[:], Y_, reads=[Y_], writes=[G.Yb])
        with P.scope():
            gts = load_gates(P, G, 1, 5)
            gf = P.sb("gfin", [128, D])
            P.dma("sp", gf[:], G.norm_final[0:1, :].partition_broadcast(128), gf, reads=[G.norm_final], writes=[gf])
            ya = [P.sb(f"cya{i}", [128, D]) for i in range(2)]
            yb = [P.sb(f"cyb{i}", [128, D]) for i in range(2)]
            yy = [P.sb(f"cyy{i}", [128, 2, D]) for i in range(2)]
            zz = [P.sb(f"czz{i}", [128, 2, D]) for i in range(2)]
            xt = [P.sb(f"cxt{i}", [128, D]) for i in range(2)]
            sq = P.sb("csq", [128, D], BF16)
            ss = [P.sb(f"css{i}", [128, 1]) for i in range(2)]
            for b in range(NB):
                for tt in range(16):
                    i = b * 16 + tt
                    s = i % 2
                    for (dst_, ab) in ((yy[s], 0), (zz[s], 1)):
                        P.dma("pool", None, None, dst_, reads=[SAB, G.Yb], writes=[dst_], meth="indirect_dma_start",
                              out=dst_[:].rearrange("p h d -> p (h d)"), out_offset=None, in_=G.Yb[:].rearrange("n h d -> n (h d)"),
                              in_offset=IOA(ap=SAB[:, ab, i:i + 1], axis=0))
                    P.op("dve", "tensor_tensor", out=ya[s][:], in0=yy[s][:, 0, :], in1=yy[s][:, 1, :], op=ALU.add, reads=[yy[s]], writes=[ya[s]])
                    P.op("dve", "tensor_tensor", out=yb[s][:], in0=zz[s][:, 0, :], in1=zz[s][:, 1, :], op=ALU.add, reads=[zz[s]], writes=[yb[s]])
                    P.dma("sp", xt[s][:], G.x3[b, tt * 128:(tt + 1) * 128, :], xt[s], reads=[G.x3], writes=[xt[s]])
                    P.op("dve", "tensor_scalar", out=ya[s][:], in0=ya[s][:], scalar1=WAB[:, i, 0:1], scalar2=None, op0=ALU.mult,
                         reads=[ya[s], WAB], writes=[ya[s]])
                    P.op("dve", "scalar_tensor_tensor", out=ya[s][:], in0=yb[s][:], scalar=WAB[:, i, 1:2], in1=ya[s][:],
                         op0=ALU.mult, op1=ALU.add, reads=[ya[s], yb[s], WAB], writes=[ya[s]])
                    P.op("dve", "tensor_tensor", out=ya[s][:], in0=ya[s][:], in1=gts[b][:], op=ALU.mult, reads=[ya[s], gts[b]], writes=[ya[s]])
                    P.op("dve", "tensor_tensor", out=xt[s][:], in0=xt[s][:], in1=ya[s][:], op=ALU.add, reads=[xt[s], ya[s]], writes=[xt[s]])
                    P.op("act", "activation", out=sq[:], in_=xt[s][:], func=AF.Square, accum_out=ss[s][:], reads=[xt[s]], writes=[sq, ss[s]])
                    P.op("act", "activation", out=ss[s][:], in_=ss[s][:], func=AF.Sqrt, scale=1.0 / D, bias=G.eps[:, 0:1],
                         reads=[ss[s], G.eps], writes=[ss[s]])
                    P.op("dve", "reciprocal", out=ss[s][:], in_=ss[s][:], reads=[ss[s]], writes=[ss[s]])
                    P.op("dve", "scalar_tensor_tensor", out=yb[s][:], in0=xt[s][:], scalar=ss[s][:, 0:1], in1=gf[:], op0=ALU.mult,
                         op1=ALU.mult, reads=[xt[s], ss[s], gf], writes=[yb[s]])
                    P.dma("sp", G.out[b, tt * 128:(tt + 1) * 128, :], yb[s][:], yb[s], reads=[yb[s]], writes=[G.out])


def stage_norm_tok(P, G):
    with P.scope():
        grow = P.sb("grow", [128, D])
        P.dma("sp", grow[:], G.norm_ffn_row[1:2, :].partition_broadcast(128), grow, reads=[G.norm_ffn_row], writes=[grow])
        GSb, SHb = [], []
        for j in range(2):
            g_ = P.sb(f"GSb{j}", [128, D])
            P.dma("sp", g_[:], G.modD[1, j:j + 1, 4 * D:5 * D].partition_broadcast(128), g_, reads=[G.modD], writes=[g_])
            P.op("dve", "scalar_tensor_tensor", out=g_[:], in0=g_[:], scalar=1.0, in1=grow[:], op0=ALU.add, op1=ALU.mult,
                 reads=[g_, grow], writes=[g_])
            GSb.append(g_)
            h_ = P.sb(f"SHb{j}", [128, D])
            P.dma("sp", h_[:], G.modD[1, j:j + 1, 3 * D:4 * D].partition_broadcast(128), h_, reads=[G.modD], writes=[h_])
            SHb.append(h_)
        z = P.sb("zrow", [128, D])
        P.op("dve", "memset", ap=z[:], constant=0.0, writes=[z])
        P.dma("sp", G.f_tok[NB * TL:NB * TL + 128, :], z[:], z, reads=[z], writes=[G.f_tok])
        rw = P.sb("rw", [128, 8, 8])
        P.dma("sp", rw[:], G.moe_router[:].rearrange("(k p) e -> p k e", p=128), rw, reads=[G.moe_router], writes=[rw])
        rb = P.sb("rb", [128, 8])
        P.dma("sp", rb[:], G.moe_router_b[0:1, :].partition_broadcast(128), rb, reads=[G.moe_router_b], writes=[rb])
        fT = [P.sb(f"nfT{i}", [128, 8, 128]) for i in range(2)]
        xt = [P.sb(f"nxt{i}", [128, D]) for i in range(2)]
        sq = P.sb("nsq", [128, D], BF16)
        ss = [P.sb(f"nss{i}", [128, 1]) for i in range(2)]
        fo = [P.sb(f"nfo{i}", [128, D]) for i in range(2)]
        it = 0
        for b in range(NB):
            for tt in range(16):
                s = it % 2
                it += 1
                P.dma("sp", xt[s][:], G.x3[b, tt * 128:(tt + 1) * 128, :], xt[s], reads=[G.x3], writes=[xt[s]])
                P.op("act", "activation", out=sq[:], in_=xt[s][:], func=AF.Square, accum_out=ss[s][:], reads=[xt[s]], writes=[sq, ss[s]])
                P.op("act", "activation", out=ss[s][:], in_=ss[s][:], func=AF.Sqrt, scale=1.0 / D, bias=G.eps[:, 0:1],
                     reads=[ss[s], G.eps], writes=[ss[s]])
                P.op("dve", "reciprocal", out=ss[s][:], in_=ss[s][:], reads=[ss[s]], writes=[ss[s]])
                P.op("dve", "scalar_tensor_tensor", out=fo[s][:], in0=xt[s][:], scalar=ss[s][:, 0:1], in1=GSb[b][:], op0=ALU.mult,
                     op1=ALU.mult, reads=[xt[s], ss[s], GSb[b]], writes=[fo[s]])
                P.op("dve", "tensor_tensor", out=fo[s][:], in0=fo[s][:], in1=SHb[b][:], op=ALU.add, reads=[fo[s], SHb[b]], writes=[fo[s]])
                P.dma("pool", G.f_tok[b * TL + tt * 128:b * TL + (tt + 1) * 128, :], fo[s][:], fo[s], reads=[fo[s]], writes=[G.f_tok])
                tix = b * 16 + tt
                for k in range(8):
                    ps = G.bank[(it % 2) * 2 + k // 4]
                    P.op("pe", "transpose", out=ps[:, (k % 4) * 128:(k % 4 + 1) * 128], in_=fo[s][:, k * 128:(k + 1) * 128],
                         identity=G.identf[:], reads=[fo[s], G.identf], writes=[ps])
                for kh in range(2):
                    ps = G.bank[(it % 2) * 2 + kh]
                    if kh:
                        P.op("act", "activation", out=fT[s][:, 4:8, :], in_=ps[:, 0:512].rearrange("p (k t) -> p k t", k=4),
                             func=AF.Copy, reads=[ps], writes=[fT[s]])
                    else:
                        P.op("dve", "tensor_copy", out=fT[s][:, 0:4, :], in_=ps[:, 0:512].rearrange("p (k t) -> p k t", k=4),
                             reads=[ps], writes=[fT[s]])
                pl = G.bank[4 + it % 2]
                for k in range(8):
                    mm(P, pl, pl[:, 0:8], fT[s], fT[s][:, k, :], rw, rw[:, k, :], start=(k == 0), stop=(k == 7))
                P.op("dve", "tensor_tensor", out=G.LG[:, tix, :], in0=pl[:, 0:8], in1=rb[:], op=ALU.add, reads=[pl, rb], writes=[G.LG])


def build(upto="all", dbg=()):
    nc = bass.Bass("TRN2", target_bir_lowering=False)
    P = Prog(nc)
    G = Ctx(P)
    nc.G = G
    kinds = lambda n: "ExternalOutput" if n in dbg else "Internal"
    S = lambda n, s, dt=F32: P.dram(n, s, dt, kind=kinds(n))
    G.out = P.dram("out", [NB, TL, D], F32, kind="ExternalOutput")
    G.hT0 = S("hT0", [NB, 128, 8, TT], BF16)
    G.RS = [S(f"RS{d}", [NB, 8, 64, 2, W], BF16) for d in range(2)]
    G.BK = [S(f"BK{d}", [NB, 8, 64, 2, W], BF16) for d in range(2)]
    G.BKt = [S(f"BKt{d}", [NB, 18, 128, 2, 512], BF16) for d in range(2)]
    G.GL = [S(f"GL{d}", [NB, 8, 64, 18]) for d in range(2)]
    G.Vt = S("Vt", [NB, 18, 128, 512], BF16)
    G.gT = S("gT", [NB, 4, 128, W])
    G.vT = S("vT", [NB, 4, 128, W])
    G.bcT = S("bcT", [NB, 4, 128, W])
    G.oA = [S(f"oA{d}", [NB, 18, 128, 512]) for d in range(2)]
    G.yT = S("yT", [NB, 8, 128, TT], BF16)
    G.x1 = S("x1", [NB, TT, D])
    G.x2 = S("x2", [NB, TT, D])
    G.fT0 = S("fT0", [NB, 128, 8, TT], BF16)
    G.hT1 = S("hT1", [NB, 128, 8, TT], BF16)
    G.qT = S("qT", [NB, 16, 64, TL], BF16)
    G.kT = S("kT", [NB, 4, 64, TT], BF16)
    G.Vt1 = S("Vt1", [NB, 18, 128, 256], BF16)
    G.oT1 = S("oT1", [NB, 8, 128, TL], BF16)
    G.x3 = S("x3", [NB, TL, D])
    G.x4 = S("x4", [NB, TL, D])
    G.fT1 = S("fT1", [NB, 128, 8, TL], BF16)
    G.fT1f = S("fT1f", [NB, 128, 8, TL], F32)
    G.f_tok = S("f_tok", [NB * TL + 128, D])
    G.Yb = S("Yb", [NSLOT, 2, D])
    G.tokidx = P.dram("tokidx", [NSLOT, 1], I32, kind=kinds("tokidx"))
    G.HQK = [S(f"HQK{d}", [NB, 4, 128, 2, TT], BF16) for d in range(2)]
    G.HKt = [S(f"HKt{d}", [NB, 18, 128, 512], BF16) for d in range(2)]
    G.HGL = [S(f"HGL{d}", [NB, 4, 128, 36]) for d in range(2)]
    G.HVt = S("HVt", [NB, 18, 128, 512], BF16)
    G.HGt = S("HGt", [NB, 18, 128, 512], BF16)
    G.oH = [S(f"oH{d}", [NB, 18, 128, 512]) for d in range(2)]
    setup_globals(P, G)
    G.modD = S("modD", [2, 3, 6 * D])
    outs = [G.out]

    def done():
        pass
        with P.scope():
            pass
        P.emit()
        return nc

    stage_adaln(P, G)
    if upto == "adaln":
        return done()
    if upto.startswith("l1"):
        G.x2 = P.dram("x2in", [NB, TT, D], F32, kind="ExternalInput")
        G.used_inputs.append("x2in")
        stage_norm(P, G, G.x2, TT, 1, 0, G.hT1, TC)
        if upto == "l1n":
            return done()
        for b in range(NB):
            stage_att_inproj(P, G, b)
        if upto == "l1i":
            return done()
        stage_attention(P, G)
        if upto == "l1t":
            return done()
        stage_outproj(P, G, G.oT1, G.att_w_out, 1, TL, 0, lambda b, tt: G.x2[b, TC + tt * 128:TC + (tt + 1) * 128, :],
                      lambda b, tt: G.x3[b, tt * 128:(tt + 1) * 128, :], G.x2, G.x3)
        if upto == "l1a":
            return done()
        if upto == "l1d":
            stage_norm(P, G, G.x3, TL, 1, 1, G.fT1, 0, dst32=G.fT1f)
            stage_router(P, G)
            stage_moe(P, G)
            stage_final(P, G, G.x4)
            return done()
        stage_norm_tok(P, G)
        stage_moe_sparse(P, G, nblk=(2 if upto == "l1s2" else NBLK))
        return done()
    stage_norm(P, G, G.xin, TT, 0, 0, G.hT0, TC)
    if upto == "norm0":
        return done()
    for b in range(NB if upto != "rwf1" else 1):
        stage_rwkv_feat(P, G, b)
    if upto in ("rwf", "rwf1"):
        return done()
    if upto == "hg1":
        stage_hgrn_feat(P, G, 0)
        stage_hgrn_scan(P, G, bs=(0,))
        return done()
    if upto == "rws1":
        stage_rwkv_scan(P, G, bs=(0,), ds=(0, 1), nmax=18)
        return done()
    stage_rwkv_scan(P, G)
    if upto == "rws":
        return done()
    stage_rwkv_post(P, G)
    if upto == "rwp":
        return done()
    for b in range(NB):
        stage_hgrn_feat(P, G, b)
    stage_hgrn_scan(P, G)
    stage_hgrn_post(P, G)
    if upto == "hgp":
        return done()
    stage_outproj(P, G, G.yT, G.rec_w_out, 0, TT, TC, lambda b, tt: G.xin[b, tt * 128:(tt + 1) * 128, :],
                  lambda b, tt: G.x1[b, tt * 128:(tt + 1) * 128, :], G.xin, G.x1)
    if upto == "x1":
        return done()
    stage_norm(P, G, G.x1, TT, 0, 1, G.fT0, TC)
    stage_ffn0(P, G)
    if upto == "x2":
        return done()
    stage_norm(P, G, G.x2, TT, 1, 0, G.hT1, TC)
    for b in range(NB):
        stage_att_inproj(P, G, b)
    stage_attention(P, G)
    stage_outproj(P, G, G.oT1, G.att_w_out, 1, TL, 0, lambda b, tt: G.x2[b, TC + tt * 128:TC + (tt + 1) * 128, :],
                  lambda b, tt: G.x3[b, tt * 128:(tt + 1) * 128, :], G.x2, G.x3)
    if upto == "x3":
        return done()
    if upto == "dense":
        stage_norm(P, G, G.x3, TL, 1, 1, G.fT1, 0, dst32=G.fT1f)
        stage_router(P, G)
        stage_moe(P, G)
        stage_final(P, G, G.x4)
        return done()
    stage_norm_tok(P, G)
    stage_moe_sparse(P, G)
    return done()


def host_prep(inp, core):
    f = lambda a: np.ascontiguousarray(a, dtype=np.float32)
    b0 = core * NB
    m = {}
    m["xin"] = f(np.concatenate([inp["ctx"][b0:b0 + NB], inp["x"][b0:b0 + NB]], axis=1))
    cv = np.stack([inp["c"][b0], inp["c"][b0 + 1], inp["c_ctx"]], axis=-1)
    m["cT"] = f(cv.reshape(8, 128, 3).transpose(1, 0, 2))
    m["mod_w"] = f(inp["mod_w"])
    m["mod_b"] = f(inp["mod_b"].reshape(2, 48, 128).transpose(0, 2, 1))
    m["norm_mix"] = f(inp["norm_mix"].reshape(2, 8, 128).transpose(0, 2, 1))
    m["norm_ffn"] = f(inp["norm_ffn"].reshape(2, 8, 128).transpose(0, 2, 1))
    m["norm_final"] = f(inp["norm_final"].reshape(1, D))
    m["ident"] = np.eye(128, dtype=np.float32)
    m["rec_w_in"] = f(inp["rec_w_in"][0])
    m["rec_w_out"] = f(inp["rec_w_out"][0])
    fm = lambda v: np.asarray(v, np.float32).reshape(4, 128).T
    rv = np.zeros((128, 4, 12), np.float32)
    for i, v in enumerate([inp["rwkv_k_k"][0], inp["rwkv_k_a"][0], None, inp["rwkv_a0"][0],
                           inp["rwkv_r_k"][0].reshape(-1), inp["rwkv_ln_w"][0], inp["rwkv_ln_b"][0],
                           inp["rwkv_w0"][0, 0], inp["rwkv_w0"][0, 1]]):
        if v is not None:
            rv[:, :, i] = fm(v)
    m["rw_vec"] = rv
    m["rw_mu"] = f(inp["rwkv_mu"][0].reshape(14, 128).T)
    m["rw_w_up"] = f(inp["rwkv_w_up"][0])
    m["rw_a_up"] = f(inp["rwkv_a_up"][0])
    m["rw_g_up"] = f(inp["rwkv_g_up"][0])
    m["ffn_wg"] = f(inp["ffn_w_gate"][0])
    m["ffn_wu"] = f(inp["ffn_w_up"][0])
    m["ffn_wd"] = f(inp["ffn_w_down"][0])
    m["att_w_in"] = f(inp["att_w_in"][0])
    m["att_w_out"] = f(inp["att_w_out"][0])
    m["att_sink"] = f(inp["att_sink"][0].reshape(1, 16))
    m["moe_router"] = f(inp["moe_router"][0])
    m["moe_router_b"] = f(inp["moe_router_b"][0].reshape(1, 8))
    m["moe_wg"], m["moe_wu"], m["moe_wd"] = _moe_layout(inp)
    m["norm_ffn_row"] = f(inp["norm_ffn"])
    m["hg_lb"] = f(inp["hgrn_lb"].reshape(2, 2, 4, 128).transpose(0, 1, 3, 2))
    m["hg_norm"] = f(inp["hgrn_norm"][0].reshape(1, 128))
    m.update(CONSTS)
    return m


def _consts():
    c = {}
    blk = np.zeros((128, 128), np.float32)
    blk[:64, :64] = 1
    blk[64:, 64:] = 1
    c["blk64"] = blk
    s_ = np.arange(128)[:, None]
    t_ = np.arange(128)[None, :]
    c["masks"] = np.stack([s_ < t_, s_ <= t_, s_ > t_, s_ >= t_], 1).astype(np.float32)
    sm = np.ones((128, W), np.float32)
    for n in range(18):
        sm[:, pcol(n * 128)] = 0.0
    c["scanmask"] = sm
    t = np.arange(TL)
    row = (t // 64).astype(np.float32)
    col = (t % 64).astype(np.float32)
    inv = (10000.0 ** (-np.arange(0, 32, 2, dtype=np.float32) / 32)).astype(np.float32)
    rope = np.zeros((128, 2, TL), np.float32)
    perm = np.zeros((128, 128), np.float32)
    for p in range(128):
        dd = p % 64
        axis, half, jj = dd // 32, (dd % 32) // 16, dd % 16
        ang = ((row if axis == 0 else col) * inv[jj]).astype(np.float32)
        rope[p, 0] = np.cos(ang)
        rope[p, 1] = np.sin(ang) * (-1.0 if half == 0 else 1.0)
        sw = p + 16 if half == 0 else p - 16
        perm[sw, p] = 1.0
    c["rope"] = rope
    c["perm"] = perm
    mc = np.zeros((128, 512), np.float32)
    p_ = np.arange(128)[:, None]
    mc[:, 0:64] = np.tile(np.arange(8) * float(BS), 8)[None, :]
    mc[:, 64:88] = np.arange(24)[None, :] + 0.0
    mc[:, 88:90] = 2 * p_ + np.arange(2)[None, :]
    mc[:, 104:106] = np.arange(2)[None, :] * 128 + p_
    mc[:, 128:160] = np.arange(32)[None, :] * 128 + p_
    rm = np.ones((8, 32), np.float32)
    rm[:, 0] = 0.0
    mc[:, 256:512] = rm.reshape(1, 256)
    c["moe_consts"] = mc
    return c


CONSTS = _consts()


_MOE_CACHE = {}


def _moe_layout(inp):
    key = id(inp["moe_w_gate"])
    if key not in _MOE_CACHE:
        _MOE_CACHE.clear()
        def gu(w):
            w = np.asarray(w[0], np.float32).reshape(8, 8, 128, 2, 1408)
            return np.ascontiguousarray(w.transpose(0, 2, 3, 1, 4)).reshape(2048, 11264)
        wd = np.asarray(inp["moe_w_down"][0], np.float32).reshape(8, 2, 11, 128, 1024)
        wd = np.ascontiguousarray(wd.transpose(0, 1, 3, 2, 4)).reshape(2048, 11264)
        _MOE_CACHE[key] = (gu(inp["moe_w_gate"]), gu(inp["moe_w_up"]), wd)
    return _MOE_CACHE[key]


_NC_CACHE = {}


def kernel(**inp):
    inp = {k: np.asarray(v) for k, v in inp.items()}
    if "nc" not in _NC_CACHE:
        _NC_CACHE["nc"] = build()
    nc = _NC_CACHE["nc"]
    used = nc.G.used_inputs
    in_maps = []
    for c in range(8):
        m = host_prep(inp, c)
        in_maps.append({k: m[k] for k in used})
    res = run_bass_kernel_spmd(nc, in_maps, core_ids=list(range(8)))
    out = np.concatenate([r["out"] for r in res.results], axis=0)
    return out.astype(np.float32)
```

```python
import contextlib
import os
import numpy as np
import concourse.bass as bass
import concourse.mybir as mybir
from concourse.bass_utils import run_bass_kernel_spmd

F32 = mybir.dt.float32
BF16 = mybir.dt.bfloat16
AF = mybir.ActivationFunctionType
ALU = mybir.AluOpType
AX = mybir.AxisListType

NB = 2
TC = 256
TL = 2048
TT = TC + TL
D = 1024
W = TT + 4
C0 = float(np.exp(-0.5))


def pcol(t):
    return 1 + t if t < TC else 3 + t


class Tk:
    __slots__ = ("h", "name", "w", "r", "sem", "cnt", "psum")

    def __init__(self, h, name, psum=False):
        self.h = h
        self.name = name
        self.psum = psum
        self.w = {}
        self.r = {}
        self.sem = None
        self.cnt = 0

    def __getitem__(self, idx):
        return self.h[idx]


class Prog:
    ENGS = ("pe", "act", "dve", "pool", "sp")

    def __init__(self, nc):
        self.nc = nc
        self.q = {e: [] for e in self.ENGS}
        self.seq = {e: 0 for e in self.ENGS}
        self.sem = {e: nc.alloc_semaphore("s_" + e) for e in self.ENGS}
        self.waited = {e: {} for e in self.ENGS}
        self.n = 0
        self.uid = 0
        self._stack = None
        self._scope_tiles = []
        self.free_dsems = {}
        self.dtiles = {}

    def _nm(self, name):
        self.uid += 1
        return f"{name}_{self.uid}"

    def sb(self, name, shape, dt=F32):
        nm = self._nm(name)
        if self._stack is not None:
            h = self._stack.enter_context(self.nc.sbuf_tensor(nm, list(shape), dt))
        else:
            h = self.nc.alloc_sbuf_tensor(nm, list(shape), dt)
        t = Tk(h, nm)
        if self._scope_tiles:
            self._scope_tiles[-1].append(t)
        return t

    def ps(self, name, shape, dt=F32):
        nm = self._nm(name)
        return Tk(self.nc.alloc_psum_tensor(nm, list(shape), dt), nm, psum=True)

    def view(self, ap, name):
        return Tk(ap, self._nm(name))

    def dram(self, name, shape, dt=F32, kind="Internal"):
        return Tk(self.nc.dram_tensor(name, list(shape), dt, kind=kind), name)

    @contextlib.contextmanager
    def scope(self):
        st = contextlib.ExitStack()
        prev = self._stack
        self._stack = st
        self._scope_tiles.append([])
        try:
            yield
        finally:
            self.barrier()
            for t in self._scope_tiles.pop():
                if t.sem:
                    for e, sc in t.sem.items():
                        self.free_dsems.setdefault(e, []).append(sc)
                    self.dtiles.pop(id(t), None)
                    t.sem = None
            st.close()
            self._stack = prev

    def _wait(self, eng, tok):
        if tok[0] == "e":
            _, f, s = tok
            if f == eng and eng == "pe":
                return
            key = ("e", f)
            if self.waited[eng].get(key, 0) >= s:
                return
            self.waited[eng][key] = s
            self.q[eng].append(("w", self.sem[f], s))
        else:
            _, t, de = tok
            if not t.sem or de not in t.sem:
                return
            sem, cnt = t.sem[de]
            key = ("d", id(sem))
            if self.waited[eng].get(key, 0) >= cnt:
                return
            self.waited[eng][key] = cnt
            self.q[eng].append(("w", sem, cnt))

    def _deps(self, eng, reads, writes):
        for t in reads:
            for tok in t.w.values():
                self._wait(eng, tok)
            if t.psum:
                for tok in t.r.values():
                    if not (tok[0] == "e" and tok[1] == eng):
                        self._wait(eng, tok)
        for t in writes:
            for tok in t.w.values():
                self._wait(eng, tok)
            for tok in t.r.values():
                self._wait(eng, tok)

    @staticmethod
    def _key(tok):
        return (tok[0], tok[1]) if tok[0] == "e" else (tok[0], id(tok[1]), tok[2])

    def _commit(self, tok, reads, writes):
        k = self._key(tok)
        for t in reads:
            t.r[k] = tok
        for t in writes:
            t.w[k] = tok

    def op(self, eng, meth, reads=(), writes=(), **kw):
        self._deps(eng, reads, writes)
        self.seq[eng] += 1
        tok = ("e", eng, self.seq[eng])
        self.q[eng].append(("o", meth, kw))
        self._commit(tok, reads, writes)
        self.n += 1
        return tok

    def dma(self, eng, out_ap, in_ap, semt, reads=(), writes=(), meth="dma_start", fn=None, **kw):
        self._deps(eng, reads, writes)
        t = semt
        if t.sem is None:
            t.sem = {}
        if eng not in t.sem:
            fl = self.free_dsems.get(eng)
            if fl:
                t.sem[eng] = fl.pop()
            else:
                t.sem[eng] = [self.nc.alloc_semaphore(f"d_{eng}_{t.name}"), 0]
            self.dtiles[id(t)] = t
        t.sem[eng][1] += 16
        if fn is not None:
            self.q[eng].append(("f", fn, t.sem[eng][0]))
        elif meth == "dma_start":
            self.q[eng].append(("d", out_ap, in_ap, t.sem[eng][0], kw))
        else:
            self.q[eng].append(("m", meth, kw, t.sem[eng][0]))
        tok = ("d", t, eng)
        self._commit(tok, reads, writes)
        self.n += 1
        return tok

    def barrier(self):
        for f in self.ENGS:
            if f != "sp" and self.seq[f] > 0:
                self._wait("sp", ("e", f, self.seq[f]))
        for t in list(self.dtiles.values()):
            for de in list(t.sem.keys()):
                self._wait("sp", ("d", t, de))
        self.seq["sp"] += 1
        self.q["sp"].append(("o", "nop", {}))
        tok = ("e", "sp", self.seq["sp"])
        for e in self.ENGS:
            if e != "sp":
                self._wait(e, tok)

    def emit(self):
        nc = self.nc
        q = self.q
        sems = self.sem

        def run(e, name):
            for it in q[name]:
                if it[0] == "w":
                    e.wait_ge(it[1], it[2])
                elif it[0] == "o":
                    getattr(e, it[1])(**it[2]).then_inc(sems[name], 1)
                elif it[0] == "f":
                    it[1](e).then_inc(it[2], 16)
                elif it[0] == "m":
                    getattr(e, it[1])(**it[2]).then_inc(it[3], 16)
                else:
                    _, o, i, s, kw = it
                    e.dma_start(out=o, in_=i, **kw).then_inc(s, 16)

        with nc.Block() as block:
            @block.tensor
            def _(e):
                run(e, "pe")

            @block.scalar
            def _(e):
                run(e, "act")

            @block.vector
            def _(e):
                run(e, "dve")

            @block.gpsimd
            def _(e):
                run(e, "pool")

            @block.sync
            def _(e):
                run(e, "sp")


def mm(P, o, oap, l, lap, r, rap, start=True, stop=True):
    P.op("pe", "matmul", reads=[l, r], writes=[o], out=oap, lhsT=lap, rhs=rap, start=start, stop=stop)


INPUT_SPECS = {
    "xin": [NB, TT, D], "cT": [128, 8, 3], "mod_w": [2, D, 6 * D], "mod_b": [2, 128, 48],
    "norm_mix": [2, 128, 8], "norm_ffn": [2, 128, 8], "norm_final": [1, D], "ident": [128, 128],
    "rec_w_in": [D, 4352], "rec_w_out": [D, D],
    "rw_vec": [128, 4, 12], "rw_mu": [128, 14], "rw_w_up": [2, 64, 512], "rw_a_up": [64, 512], "rw_g_up": [128, 512],
    "blk64": [128, 128], "masks": [128, 4, 128], "scanmask": [128, W],
    "hg_lb": [2, 2, 128, 4], "hg_norm": [1, 128],
    "ffn_wg": [D, 2816], "ffn_wu": [D, 2816], "ffn_wd": [2816, D],
    "att_w_in": [D, 1536], "att_w_out": [D, D], "att_sink": [1, 16],
    "rope": [128, 2, TL], "perm": [128, 128],
    "moe_router": [D, 8], "moe_router_b": [1, 8],
    "moe_consts": [128, 512], "norm_ffn_row": [2, D],
    "moe_wg": [2048, 11264], "moe_wu": [2048, 11264], "moe_wd": [2048, 11264],
}


class Ctx:
    def __init__(self, P):
        self.__dict__["P"] = P
        self.__dict__["used_inputs"] = []

    def __getattr__(self, name):
        if name in INPUT_SPECS:
            t = self.P.dram(name, INPUT_SPECS[name], F32, kind="ExternalInput")
            self.__dict__[name] = t
            self.used_inputs.append(name)
            return t
        raise AttributeError(name)


def setup_globals(P, G):
    G.bank = [P.ps(f"bank{i}", [128, 512], F32) for i in range(8)]
    G.identf = P.sb("identf", [128, 128], F32)
    P.dma("sp", G.identf[:], G.ident[:], G.identf, reads=[G.ident], writes=[G.identf])
    G.identb = P.sb("identb", [128, 128], BF16)
    P.op("dve", "tensor_copy", out=G.identb[:], in_=G.identf[:], reads=[G.identf], writes=[G.identb])
    G.eps = P.sb("eps", [128, 1], F32)
    P.op("dve", "memset", ap=G.eps[:], constant=1e-6, writes=[G.eps])
    G.WTm = P.sb("WTm", [128, 32, 8], F32)
    G.LG = P.sb("LGp", [128, 32, 8], F32)
    G.modT = P.sb("modT", [128, 2, 48, 3], F32)
    G.GS = P.sb("GS", [128, 2, 2, 8, 3], F32)


def stage_adaln(P, G):
    bank = G.bank
    with P.scope():
        cT = P.sb("cTs", [128, 8, 3])
        P.dma("sp", cT[:], G.cT[:], cT, reads=[G.cT], writes=[cT])
        sc = P.sb("sc", [128, 8, 3])
        P.op("act", "activation", out=sc[:], in_=cT[:], func=AF.Silu, reads=[cT], writes=[sc])
        mb = P.sb("mb", [128, 2, 48])
        P.dma("sp", mb[:], G.mod_b[:].rearrange("l p j -> p l j"), mb, reads=[G.mod_b], writes=[mb])
        gm = P.sb("gm", [128, 2, 2, 8])
        P.dma("sp", gm[:, :, 0, :], G.norm_mix[:].rearrange("l p k -> p l k"), gm, reads=[G.norm_mix], writes=[gm])
        P.dma("sp", gm[:, :, 1, :], G.norm_ffn[:].rearrange("l p k -> p l k"), gm, reads=[G.norm_ffn], writes=[gm])
        wts = [P.sb(f"mw{i}", [128, 8, 768]) for i in range(2)]
        it = 0
        for l in range(2):
            for g in range(8):
                wt = wts[it % 2]
                for k in range(8):
                    P.dma("sp" if k % 2 == 0 else "pool", wt[:, k, :],
                          G.mod_w[l, k * 128:(k + 1) * 128, g * 768:(g + 1) * 768], wt,
                          reads=[G.mod_w], writes=[wt])
                for jj in range(6):
                    j = g * 6 + jj
                    ps = bank[j % 2]
                    for k in range(8):
                        mm(P, ps, ps[:, 0:3], wt, wt[:, k, jj * 128:(jj + 1) * 128], sc, sc[:, k, :],
                           start=(k == 0), stop=(k == 7))
                    P.op("act", "activation", out=G.modT[:, l, j, :], in_=ps[:, 0:3], func=AF.Identity,
                         bias=mb[:, l, j:j + 1], reads=[ps, mb], writes=[G.modT])
                it += 1
        for l in range(2):
            for kind in range(2):
                wh = 1 + 3 * kind
                P.op("dve", "tensor_scalar", out=G.GS[:, l, kind, :, :], in0=G.modT[:, l, wh * 8:(wh + 1) * 8, :],
                     scalar1=1.0, scalar2=None, op0=ALU.add, reads=[G.modT], writes=[G.GS])
                P.op("dve", "tensor_tensor", out=G.GS[:, l, kind, :, :], in0=G.GS[:, l, kind, :, :],
                     in1=gm[:, l, kind, :].unsqueeze(2).broadcast_to([128, 8, 3]), op=ALU.mult,
                     reads=[G.GS, gm], writes=[G.GS])
        tr = P.sb("mtr", [48, 128])
        for l in range(2):
            for j in range(3):
                ps = bank[2 + (l * 3 + j) % 2]
                P.op("pe", "transpose", out=ps[0:48, 0:128], in_=G.modT[:, l, :, j], identity=G.identf[:],
                     reads=[G.modT, G.identf], writes=[ps])
                P.op("dve", "tensor_copy", out=tr[:], in_=ps[0:48, 0:128], reads=[ps], writes=[tr])
                P.dma("sp", G.modD[l, j, :].rearrange("(c p) -> c p", p=128), tr[:], tr, reads=[tr], writes=[G.modD])


def stage_norm(P, G, src, T, l, kind, dst, ctx_len, dst32=None):
    bank = G.bank
    with P.scope():
        xt = [P.sb(f"xt{i}", [128, D]) for i in range(4)]
        sq = P.sb("sq", [128, D], BF16)
        ss = [P.sb(f"ss{i}", [128, 1]) for i in range(4)]
        rs = [P.sb(f"rs{i}", [128, 1]) for i in range(4)]
        xn = [P.sb(f"xn{i}", [128, D]) for i in range(4)]
        hT = [P.sb(f"hT{i}", [128, 8, 128], BF16) for i in range(4)]
        hT32 = [P.sb(f"hTf{i}", [128, 8, 128], F32) for i in range(4)] if dst32 is not None else None
        i = 0
        for b in range(NB):
            for tt in range(T // 128):
                j = 2 if tt * 128 < ctx_len else b
                s = i % 4
                P.dma("sp", xt[s][:], src[b, tt * 128:(tt + 1) * 128, :], xt[s], reads=[src], writes=[xt[s]])
                P.op("act", "activation", out=sq[:], in_=xt[s][:], func=AF.Square, accum_out=ss[s][:],
                     reads=[xt[s]], writes=[sq, ss[s]])
                P.op("act", "activation", out=rs[s][:], in_=ss[s][:], func=AF.Sqrt, scale=1.0 / D, bias=G.eps[:, 0:1],
                     reads=[ss[s], G.eps], writes=[rs[s]])
                P.op("dve", "reciprocal", out=rs[s][:], in_=rs[s][:], reads=[rs[s]], writes=[rs[s]])
                P.op("dve", "tensor_scalar", out=xn[s][:], in0=xt[s][:], scalar1=rs[s][:, 0:1], scalar2=None,
                     op0=ALU.mult, reads=[xt[s], rs[s]], writes=[xn[s]])
                for k in range(8):
                    ps = bank[(i % 4) * 2 + k // 4]
                    pv = ps[:, (k % 4) * 128:(k % 4 + 1) * 128]
                    P.op("pe", "transpose", out=pv, in_=xn[s][:, k * 128:(k + 1) * 128], identity=G.identf[:],
                         reads=[xn[s], G.identf], writes=[ps])
                    if dst is not None:
                        if dst32 is None and k % 4 >= 2:
                            P.op("dve", "tensor_scalar", out=hT[s][:, k, :], in0=pv,
                                 scalar1=G.GS[:, l, kind, k, j:j + 1], scalar2=G.modT[:, l, 3 * kind * 8 + k, j:j + 1],
                                 op0=ALU.mult, op1=ALU.add, reads=[ps, G.GS, G.modT], writes=[hT[s]])
                        else:
                            P.op("act", "activation", out=hT[s][:, k, :], in_=pv, func=AF.Identity,
                                 scale=G.GS[:, l, kind, k, j:j + 1], bias=G.modT[:, l, 3 * kind * 8 + k, j:j + 1],
                                 reads=[ps, G.GS, G.modT], writes=[hT[s]])
                    if dst32 is not None:
                        if dst is None and k % 2:
                            P.op("act", "activation", out=hT32[s][:, k, :], in_=pv, func=AF.Identity,
                                 scale=G.GS[:, l, kind, k, j:j + 1], bias=G.modT[:, l, 3 * kind * 8 + k, j:j + 1],
                                 reads=[ps, G.GS, G.modT], writes=[hT32[s]])
                        else:
                            P.op("dve", "tensor_scalar", out=hT32[s][:, k, :], in0=pv,
                                 scalar1=G.GS[:, l, kind, k, j:j + 1], scalar2=G.modT[:, l, 3 * kind * 8 + k, j:j + 1],
                                 op0=ALU.mult, op1=ALU.add, reads=[ps, G.GS, G.modT], writes=[hT32[s]])
                if dst is not None:
                    P.dma("pool", dst[b, :, :, tt * 128:(tt + 1) * 128], hT[s][:], hT[s], reads=[hT[s]], writes=[dst])
                if dst32 is not None:
                    P.dma("pool", dst32[b, :, :, tt * 128:(tt + 1) * 128], hT32[s][:], hT32[s],
                          reads=[hT32[s]], writes=[dst32])
                i += 1


TBLK = [(0, 256), (256, 768), (768, 1280), (1280, 1792), (1792, 2304)]


def load_w_chunk(P, wt, src, c0, ncols, eng="pool"):
    P.dma(eng, wt[:, :, 0:ncols], src[:, c0:c0 + ncols].rearrange("(k p) n -> p k n", p=128), wt,
          reads=[src], writes=[wt])


class WPrefetch:
    def __init__(self, P, src, seq, nbuf=3, ncols=128):
        self.P, self.src, self.seq, self.ncols = P, src, list(seq), ncols
        self.bufs = [P.sb(f"wpf{i}", [128, 8, ncols], BF16) for i in range(nbuf)]
        self.i = 0
        self._issue(0)

    def _issue(self, i):
        if i < len(self.seq):
            load_w_chunk(self.P, self.bufs[i % len(self.bufs)], self.src, self.seq[i] * self.ncols, self.ncols)

    def get(self, chunk):
        assert self.seq[self.i] == chunk, (self.seq[self.i], chunk)
        wt = self.bufs[self.i % len(self.bufs)]
        self._issue(self.i + 1)
        self.i += 1
        return wt


def inproj_fm(P, G, hT, wt, out, padded=True, banks=(0, 1, 2, 3), evac="act", func=None):
    for bi, (t0, t1) in enumerate(TBLK):
        ps = G.bank[banks[bi % len(banks)]]
        n = t1 - t0
        for k in range(8):
            mm(P, ps, ps[:, 0:n], wt, wt[:, k, 0:128], hT, hT[:, k, t0:t1], start=(k == 0), stop=(k == 7))
        c0 = pcol(t0) if padded else t0
        if evac == "act":
            P.op("act", "activation", out=out[:, c0:c0 + n], in_=ps[:, 0:n], func=(func or AF.Copy), reads=[ps], writes=[out])
        else:
            P.op("dve", "tensor_copy", out=out[:, c0:c0 + n], in_=ps[:, 0:n], reads=[ps], writes=[out])


def tshift(P, raw, tmp, out, muh, omm, vec):
    P.op("pool", "tensor_tensor", out=tmp[:, 1:W - 1], in0=raw[:, 0:W - 2], in1=raw[:, 2:W], op=ALU.add,
         reads=[raw], writes=[tmp])
    P.op("dve", "tensor_scalar", out=tmp[:, 1:W - 1], in0=tmp[:, 1:W - 1], scalar1=muh, scalar2=None, op0=ALU.mult,
         reads=[tmp, vec], writes=[tmp])
    P.op("dve", "scalar_tensor_tensor", out=out[:, 1:W - 1], in0=raw[:, 1:W - 1], scalar=omm, in1=tmp[:, 1:W - 1],
         op0=ALU.mult, op1=ALU.add, reads=[raw, tmp, vec], writes=[out])


def chunk_views(ap):
    return (ap[:, 1:1 + TC].rearrange("p (n t) -> p n t", t=128),
            ap[:, 3 + TC:3 + TT].rearrange("p (n t) -> p n t", t=128))


def stage_rwkv_feat(P, G, b):
    bank = G.bank
    with P.scope():
        hT = P.sb("hTres", [128, 8, TT], BF16)
        for k in range(8):
            P.dma("sp", hT[:, k, :], G.hT0[b, :, k, :], hT, reads=[G.hT0], writes=[hT])
        vec = P.sb("rwvec", [128, 4, 12])
        P.dma("sp", vec[:], G.rw_vec[:], vec, reads=[G.rw_vec], writes=[vec])
        mu = P.sb("rwmu", [128, 3, 14])
        P.dma("sp", mu[:, 0, :], G.rw_mu[:], mu, reads=[G.rw_mu], writes=[mu])
        P.op("dve", "tensor_scalar", out=mu[:, 1, :], in0=mu[:, 0, :], scalar1=0.5, scalar2=None, op0=ALU.mult,
             reads=[mu], writes=[mu])
        P.op("dve", "tensor_scalar", out=mu[:, 2, :], in0=mu[:, 0, :], scalar1=-1.0, scalar2=1.0, op0=ALU.mult,
             op1=ALU.add, reads=[mu], writes=[mu])
        P.op("dve", "tensor_scalar", out=vec[:, :, 9], in0=vec[:, :, 1], scalar1=-1.0, scalar2=1.0, op0=ALU.mult,
             op1=ALU.add, reads=[vec], writes=[vec])
        blk = P.sb("blk64", [128, 128])
        P.dma("sp", blk[:], G.blk64[:], blk, reads=[G.blk64], writes=[blk])
        smask = P.sb("smask", [128, W])
        P.dma("sp", smask[:], G.scanmask[:], smask, reads=[G.scanmask], writes=[smask])
        wup = P.sb("wup", [64, 2, 512], BF16)
        P.dma("pool", wup[:], G.rw_w_up[:].rearrange("d r c -> r d c"), wup, reads=[G.rw_w_up], writes=[wup])
        aup = P.sb("aup", [128, 512], BF16)
        P.dma("pool", aup[64:128, :], G.rw_a_up[:], aup, reads=[G.rw_a_up], writes=[aup])
        gup = P.sb("gup", [128, 512], BF16)
        P.dma("pool", gup[:], G.rw_g_up[:], gup, reads=[G.rw_g_up], writes=[gup])
        eps12 = P.sb("eps12", [128, 1])
        P.op("dve", "memset", ap=eps12[:], constant=1e-12, writes=[eps12])
        WP = WPrefetch(P, G.rec_w_in, [12, 13] + [x for c in range(4) for x in (c, 4 + c, 8 + c)])
        T = [P.sb(f"T{i}", [128, W]) for i in range(12)]
        for t in T:
            P.op("pool", "memset", ap=t[:], constant=0.0, writes=[t])
        twb = P.sb("twb", [64, W], BF16)
        adb = P.sb("adb", [128, W], BF16)
        sgb = P.sb("sgb", [128, W], BF16)
        vtok = P.sb("vtok", [128, 18, 128], BF16)
        bkt = P.sb("bkt", [128, 18, 2, 128], BF16)
        gl = P.sb("gl", [128, 18])
        wi = [0]

        def proj(chunk, out):
            wt = WP.get(chunk)
            inproj_fm(P, G, hT, wt, T[0])
            tshift(P, T[0], T[1], out, mu[:, 1, chunk:chunk + 1], mu[:, 2, chunk:chunk + 1], mu)

        proj(12, T[2])
        P.op("act", "activation", out=twb[:], in_=T[2][0:64, :], func=AF.Tanh, reads=[T[2]], writes=[twb])
        P.op("dve", "tensor_copy", out=adb[64:128, :], in_=T[2][64:128, :], reads=[T[2]], writes=[adb])
        proj(13, T[2])
        P.op("act", "activation", out=sgb[:], in_=T[2][:], func=AF.Sigmoid, reads=[T[2]], writes=[sgb])
        for c in range(4):
            cs = slice(c * 128, (c + 1) * 128)
            A, R, KR, KAP, T6, V = T[2], T[3], T[4], T[5], T[6], T[7]
            for bi, c0 in enumerate(range(0, W, 512)):
                n = min(512, W - c0)
                ps = bank[4 + bi % 2]
                mm(P, ps, ps[:, 0:n], gup, gup[:, cs], sgb, sgb[:, c0:c0 + n])
                P.op("act", "activation", out=T[1][:, c0:c0 + n], in_=ps[:, 0:n], func=AF.Copy, reads=[ps], writes=[T[1]])
                ps2 = bank[6 + bi % 2]
                mm(P, ps2, ps2[:, 0:n], aup, aup[64:128, cs], adb, adb[64:128, c0:c0 + n])
                P.op("act", "activation", out=A[:, c0:c0 + n], in_=ps2[:, 0:n], func=AF.Sigmoid, bias=vec[:, c, 3:4],
                     reads=[ps2, vec], writes=[A])
            P.dma("sp", G.gT[b, c, :, :], T[1][:], T[1], reads=[T[1]], writes=[G.gT])
            proj(c, R)
            proj(4 + c, KR)
            P.op("dve", "tensor_scalar", out=KAP[:], in0=KR[:], scalar1=vec[:, c, 0:1], scalar2=None, op0=ALU.mult,
                 reads=[KR, vec], writes=[KAP])
            P.op("pool", "tensor_tensor", out=T6[:], in0=KAP[:], in1=KAP[:], op=ALU.mult, reads=[KAP], writes=[T6])
            for bi, c0 in enumerate(range(0, W, 512)):
                n = min(512, W - c0)
                ps = bank[4 + bi % 2]
                mm(P, ps, ps[:, 0:n], blk, blk[:], T6, T6[:, c0:c0 + n])
                P.op("act", "activation", out=T[1][:, c0:c0 + n], in_=ps[:, 0:n], func=AF.Sqrt, reads=[ps], writes=[T[1]])
            P.op("dve", "tensor_scalar", out=T[1][:], in0=T[1][:], scalar1=eps12[:, 0:1], scalar2=None, op0=ALU.max,
                 reads=[T[1], eps12], writes=[T[1]])
            P.op("dve", "reciprocal", out=T[1][:], in_=T[1][:], reads=[T[1]], writes=[T[1]])
            P.op("dve", "tensor_tensor", out=KAP[:], in0=KAP[:], in1=T[1][:], op=ALU.mult, reads=[KAP, T[1]], writes=[KAP])
            P.op("dve", "tensor_scalar", out=T6[:], in0=A[:], scalar1=vec[:, c, 1:2], scalar2=vec[:, c, 9:10],
                 op0=ALU.mult, op1=ALU.add, reads=[A, vec], writes=[T6])
            KM = T6
            P.op("pool", "tensor_tensor", out=KM[:], in0=KM[:], in1=KR[:], op=ALU.mult, reads=[KM, KR], writes=[KM])
            BE = KR
            P.op("dve", "tensor_tensor", out=BE[:], in0=KAP[:], in1=A[:], op=ALU.mult, reads=[KAP, A], writes=[BE])
            P.op("dve", "scalar_tensor_tensor", out=T[1][:], in0=R[:], scalar=vec[:, c, 4:5], in1=KM[:],
                 op0=ALU.mult, op1=ALU.mult, reads=[R, KM, vec], writes=[T[1]])
            for bi, c0 in enumerate(range(0, W, 512)):
                n = min(512, W - c0)
                ps = bank[4 + bi % 2]
                mm(P, ps, ps[:, 0:n], blk, blk[:], T[1], T[1][:, c0:c0 + n])
                P.op("act", "activation", out=T[8][:, c0:c0 + n], in_=ps[:, 0:n], func=AF.Copy, reads=[ps], writes=[T[8]])
            P.dma("sp", G.bcT[b, c, :, :], T[8][:], T[8], reads=[T[8]], writes=[G.bcT])
            proj(8 + c, V)
            P.dma("sp", G.vT[b, c, :, :], V[:], V, reads=[V], writes=[G.vT])
            for n in range(18):
                ps = bank[4 + n % 4]
                c0 = pcol(n * 128)
                P.op("pe", "transpose", out=ps[:, 0:128], in_=V[:, c0:c0 + 128], identity=G.identf[:],
                     reads=[V, G.identf], writes=[ps])
                P.op("act" if n % 2 else "dve", "activation" if n % 2 else "tensor_copy", out=vtok[:, n, :],
                     in_=ps[:, 0:128], reads=[ps], writes=[vtok], **({"func": AF.Copy} if n % 2 else {}))
            P.dma("sp", G.Vt[b, :, :, cs].rearrange("n t c -> t n c"), vtok[:], vtok, reads=[vtok], writes=[G.Vt])
            for d in range(2):
                LW, CS, GE, GI, ENI = T[7], T[8], T[9], T[10], T[11]
                for bi, c0 in enumerate(range(0, W, 512)):
                    n = min(512, W - c0)
                    ps = bank[4 + bi % 2]
                    mm(P, ps, ps[:, 0:n], wup, wup[:, d, cs], twb, twb[:, c0:c0 + n])
                    P.op("act", "activation", out=LW[:, c0:c0 + n], in_=ps[:, 0:n], func=AF.Sigmoid,
                         bias=vec[:, c, 7 + d:8 + d], reads=[ps, vec], writes=[LW])
                P.op("dve", "tensor_tensor_scan", out=CS[:], data0=smask[:], data1=LW[:], initial=0.0,
                     op0=ALU.mult, op1=ALU.add, reads=[smask, LW], writes=[CS])
                for (cv, n0, nn) in ((chunk_views(CS[:])[0], 0, 2), (chunk_views(CS[:])[1], 2, 16)):
                    P.op("act", "activation", out=gl[:, n0:n0 + nn], in_=cv[:, :, 127], func=AF.Exp, scale=-C0,
                         reads=[CS], writes=[gl])
                P.dma("sp", G.GL[d][b, 2 * c:2 * c + 2, :, :].rearrange("h k n -> (h k) n"), gl[:], gl,
                      reads=[gl], writes=[G.GL[d]])
                if d == 0:
                    P.op("pool", "tensor_tensor", out=GE[:], in0=CS[:], in1=LW[:], op=ALU.subtract,
                         reads=[CS, LW], writes=[GE])
                    P.op("act", "activation", out=GI[:], in_=CS[:], func=AF.Exp, scale=-C0, reads=[CS], writes=[GI])
                    P.op("act", "activation", out=ENI[:], in_=CS[:], func=AF.Exp, scale=C0, reads=[CS], writes=[ENI])
                else:
                    for vi in range(2):
                        gev = chunk_views(GE[:])[vi]
                        csv = chunk_views(CS[:])[vi]
                        nn = 2 if vi == 0 else 16
                        P.op("dve", "tensor_tensor", out=gev, in0=csv[:, :, 127:128].broadcast_to([128, nn, 128]),
                             in1=csv, op=ALU.subtract, reads=[CS], writes=[GE])
                    P.op("pool", "tensor_tensor", out=GI[:], in0=GE[:], in1=LW[:], op=ALU.add,
                         reads=[GE, LW], writes=[GI])
                    P.op("act", "activation", out=ENI[:], in_=GI[:], func=AF.Exp, scale=C0, reads=[GI], writes=[ENI])
                    P.op("act", "activation", out=GI[:], in_=GI[:], func=AF.Exp, scale=-C0, reads=[GI], writes=[GI])
                P.op("act", "activation", out=GE[:], in_=GE[:], func=AF.Exp, scale=-C0, reads=[GE], writes=[GE])
                P.op("dve", "tensor_tensor", out=GI[:], in0=GI[:], in1=R[:], op=ALU.mult, reads=[GI, R], writes=[GI])
                P.op("pool", "tensor_tensor", out=GE[:], in0=GE[:], in1=KAP[:], op=ALU.mult, reads=[GE, KAP], writes=[GE])
                BH = CS
                P.op("dve", "tensor_tensor", out=BH[:], in0=ENI[:], in1=BE[:], op=ALU.mult, reads=[ENI, BE], writes=[BH])
                KH = ENI
                P.op("pool", "tensor_tensor", out=KH[:], in0=ENI[:], in1=KM[:], op=ALU.mult, reads=[ENI, KM], writes=[KH])
                hv = lambda t, j: t[b, 2 * c:2 * c + 2, :, j, :].rearrange("h k w -> (h k) w")
                P.dma("pool", hv(G.RS[d], 0), GE[:], GE, reads=[GE], writes=[G.RS[d]])
                P.dma("pool", hv(G.RS[d], 1), GI[:], GI, reads=[GI], writes=[G.RS[d]])
                P.dma("pool", hv(G.BK[d], 0), BH[:], BH, reads=[BH], writes=[G.BK[d]])
                P.dma("pool", hv(G.BK[d], 1), KH[:], KH, reads=[KH], writes=[G.BK[d]])
                for n in range(18):
                    c0 = pcol(n * 128)
                    for j, src in enumerate((BH, KH)):
                        ps = bank[(2 * n + j) % 4]
                        P.op("pe", "transpose", out=ps[:, 0:128], in_=src[:, c0:c0 + 128], identity=G.identf[:],
                             reads=[src, G.identf], writes=[ps])
                        if j == 0:
                            P.op("act", "activation", out=bkt[:, n, j, :], in_=ps[:, 0:128], func=AF.Copy,
                                 reads=[ps], writes=[bkt])
                        else:
                            P.op("dve", "tensor_copy", out=bkt[:, n, j, :], in_=ps[:, 0:128], reads=[ps], writes=[bkt])
                for j in range(2):
                    P.dma("sp", G.BKt[d][b, :, :, j, cs].rearrange("n t c -> t n c"), bkt[:, :, j, :], bkt,
                          reads=[bkt], writes=[G.BKt[d]])


def stage_rwkv_scan(P, G, bs=(0, 1), ds=(0, 1), nmax=18):
    bank = G.bank
    with P.scope():
        msk = P.sb("msk", [128, 4, 128])
        P.dma("sp", msk[:], G.masks[:], msk, reads=[G.masks], writes=[msk])
        rs = [P.sb(f"rs{i}", [64, 8, 2, 128], BF16) for i in range(3)]
        bk = [P.sb(f"bk{i}", [64, 8, 2, 128], BF16) for i in range(3)]
        bkt = [P.sb(f"bktl{i}", [128, 2, 512], BF16) for i in range(3)]
        vt = [P.sb(f"vt{i}", [128, 512], BF16) for i in range(3)]
        Ms = [P.sb(f"Ms{i}", [128, 4, 128], F32) for i in range(2)]
        MTs = [P.sb(f"MTs{i}", [128, 4, 128], F32) for i in range(2)]
        Nn = [[P.sb(f"N{i}_{j}", [128, 4, 128], F32) for j in range(2)] for i in range(2)]
        NT = [[P.sb(f"NT{i}_{j}", [128, 4, 128], F32) for j in range(2)] for i in range(2)]
        ABr = [[P.sb(f"ABr{p}_{i}", [128, 4, 128], BF16) for i in range(2)] for p in range(2)]
        GKs = [[P.sb(f"GKs{p}_{i}", [128, 4, 256], BF16) for i in range(2)] for p in range(2)]
        Pm = [[P.sb(f"Pm{p}_{i}", [128, 4, 128], F32) for i in range(2)] for p in range(2)]
        WT = P.sb("WT", [128, 512], F32)
        nZ = P.sb("nZ", [128, 512], BF16)
        ot = [P.sb(f"ot{i}", [128, 512]) for i in range(2)]
        Sf = P.sb("Sf", [64, 8, 64])
        Sb = P.sb("Sb", [64, 8, 64], BF16)
        Stmp = P.sb("Stmp", [64, 8, 64])
        GLt = P.sb("GLt", [64, 8, 18])
        b6, b7 = bank[6], bank[7]

        def emit_loads(b, d, n, q):
            c0 = pcol(n * 128)
            for j in range(2):
                P.dma("sp", rs[q][:, :, j, :], G.RS[d][b, :, :, j, c0:c0 + 128].rearrange("h k t -> k h t"),
                      rs[q], reads=[G.RS[d]], writes=[rs[q]])
                P.dma("sp", bk[q][:, :, j, :], G.BK[d][b, :, :, j, c0:c0 + 128].rearrange("h k t -> k h t"),
                      bk[q], reads=[G.BK[d]], writes=[bk[q]])
            P.dma("sp", bkt[q][:], G.BKt[d][b, n], bkt[q], reads=[G.BKt[d]], writes=[bkt[q]])
            P.dma("sp", vt[q][:], G.Vt[b, n], vt[q], reads=[G.Vt], writes=[vt[q]])

        def emit_front(b, d, n, s, q):
            m2 = msk[:, 0:2, :] if d == 0 else msk[:, 2:4, :]
            mt = msk[:, 2, :] if d == 0 else msk[:, 0, :]
            R_, B_ = rs[q], bk[q]
            for hf in range(2):
                for i in range(4):
                    h = hf * 4 + i
                    pb = bank[hf * 3 + 0] if i < 2 else bank[hf * 3 + 1]
                    mm(P, pb, pb[:, (i % 2) * 256:(i % 2 + 1) * 256], B_, B_[:, h, 0, :], R_, R_[:, h, :, :])
                for i in range(4):
                    h = hf * 4 + i
                    pk = b6 if i < 2 else b7
                    mm(P, pk, pk[:, (i % 2) * 256:(i % 2 + 1) * 256], B_, B_[:, h, 1, :], R_, R_[:, h, :, :])
                pm = bank[hf * 3 + 2]
                for i in range(4):
                    h = hf * 4 + i
                    mm(P, pm, pm[:, i * 128:(i + 1) * 128], R_, R_[:, h, 0, :], B_, B_[:, h, 0, :])
                for half2 in range(2):
                    pb = bank[hf * 3 + half2]
                    pbv = pb[:, 0:512].rearrange("p (h j t) -> p h j t", h=2, j=2)
                    P.op("dve", "tensor_tensor", out=Ms[hf][:, 2 * half2:2 * half2 + 2, :], in0=pbv[:, :, 0, :],
                         in1=m2[:, 0, :].unsqueeze(1).broadcast_to([128, 2, 128]), op=ALU.mult,
                         reads=[pb, msk], writes=[Ms[hf]])
                    P.op("dve", "tensor_tensor", out=ABr[s][hf][:, 2 * half2:2 * half2 + 2, :], in0=pbv[:, :, 1, :],
                         in1=m2[:, 1, :].unsqueeze(1).broadcast_to([128, 2, 128]), op=ALU.mult,
                         reads=[pb, msk], writes=[ABr[s][hf]])
                    pk = bank[6 + half2]
                    P.op("dve", "tensor_tensor", out=GKs[s][hf][:, 2 * half2:2 * half2 + 2, :].rearrange("p h (j t) -> p h j t", j=2),
                         in0=pk[:, 0:512].rearrange("p (h j t) -> p h j t", h=2, j=2),
                         in1=m2.unsqueeze(1).broadcast_to([128, 2, 2, 128]), op=ALU.mult,
                         reads=[pk, msk], writes=[GKs[s][hf]])
                P.op("dve", "tensor_tensor", out=MTs[hf][:], in0=pm[:, 0:512].rearrange("p (h t) -> p h t", h=4),
                     in1=mt.unsqueeze(1).broadcast_to([128, 4, 128]), op=ALU.mult,
                     reads=[pm, msk], writes=[MTs[hf]])
                P.op("pool", "tensor_tensor", out=Pm[s][hf][:], in0=G.identf[:].unsqueeze(1).broadcast_to([128, 4, 128]),
                     in1=Ms[hf][:], op=ALU.subtract, reads=[Ms[hf], G.identf], writes=[Pm[s][hf]])
            for hf in range(2):
                pn, pt = bank[hf * 3 + 0], bank[hf * 3 + 1]
                for i in range(4):
                    mm(P, pn, pn[:, i * 128:(i + 1) * 128], MTs[hf], MTs[hf][:, i, :], Ms[hf], Ms[hf][:, i, :])
                for i in range(4):
                    mm(P, pt, pt[:, i * 128:(i + 1) * 128], Ms[hf], Ms[hf][:, i, :], MTs[hf], MTs[hf][:, i, :])
                P.op("act", "activation", out=Nn[hf][0][:], in_=pn[:, 0:512].rearrange("p (h t) -> p h t", h=4),
                     func=AF.Copy, reads=[pn], writes=[Nn[hf][0]])
                P.op("act", "activation", out=NT[hf][0][:], in_=pt[:, 0:512].rearrange("p (h t) -> p h t", h=4),
                     func=AF.Copy, reads=[pt], writes=[NT[hf][0]])

        def emit_level(s, lev):
            cur, nxt = lev % 2, 1 - lev % 2
            for hf in range(2):
                pp, pn, pt = bank[hf * 3 + 2], bank[hf * 3 + 0], bank[hf * 3 + 1]
                Pq = Pm[s][hf]
                for i in range(4):
                    mm(P, pp, pp[:, i * 128:(i + 1) * 128], NT[hf][cur], NT[hf][cur][:, i, :], Pq, Pq[:, i, :])
                if lev < 4:
                    for i in range(4):
                        mm(P, pn, pn[:, i * 128:(i + 1) * 128], NT[hf][cur], NT[hf][cur][:, i, :],
                           Nn[hf][cur], Nn[hf][cur][:, i, :])
                if lev < 5:
                    for i in range(4):
                        mm(P, pt, pt[:, i * 128:(i + 1) * 128], Nn[hf][cur], Nn[hf][cur][:, i, :],
                           NT[hf][cur], NT[hf][cur][:, i, :])
                P.op("dve", "tensor_tensor", out=Pq[:], in0=pp[:, 0:512].rearrange("p (h t) -> p h t", h=4),
                     in1=Pq[:], op=ALU.add, reads=[pp, Pq], writes=[Pq])
                if lev < 4:
                    P.op("act", "activation", out=Nn[hf][nxt][:], in_=pn[:, 0:512].rearrange("p (h t) -> p h t", h=4),
                         func=AF.Copy, reads=[pn], writes=[Nn[hf][nxt]])
                if lev < 5:
                    P.op("act", "activation", out=NT[hf][nxt][:], in_=pt[:, 0:512].rearrange("p (h t) -> p h t", h=4),
                         func=AF.Copy, reads=[pt], writes=[NT[hf][nxt]])

        def chain_steps(b, d, n, s, q):
            R_, BT_, V_ = rs[q], bkt[q], vt[q]

            def st_d():
                for h in range(8):
                    hf, i = h // 4, h % 4
                    hs = slice(h * 64, (h + 1) * 64)
                    mm(P, b6, b6[:, hs], R_, R_[:, h, 0, :], Sb, Sb[:, h, :], start=True, stop=False)
                    mm(P, b6, b6[:, hs], GKs[s][hf], GKs[s][hf][:, i, 0:128], V_, V_[:, hs], start=False, stop=True)
                P.op("act", "activation", out=WT[:], in_=b6[:, 0:512], func=AF.Copy, reads=[b6], writes=[WT])

            def st_e():
                for h in range(8):
                    hf, i = h // 4, h % 4
                    hs = slice(h * 64, (h + 1) * 64)
                    mm(P, b7, b7[:, hs], Pm[s][hf], Pm[s][hf][:, i, :], WT, WT[:, hs])
                P.op("act", "activation", out=nZ[:], in_=b7[:, 0:512], func=AF.Copy, scale=-1.0, reads=[b7], writes=[nZ])

            def st_f():
                for h in range(8):
                    hf, i = h // 4, h % 4
                    hs = slice(h * 64, (h + 1) * 64)
                    mm(P, b6, b6[:, hs], R_, R_[:, h, 1, :], Sb, Sb[:, h, :], start=True, stop=False)
                    mm(P, b6, b6[:, hs], GKs[s][hf], GKs[s][hf][:, i, 128:256], V_, V_[:, hs], start=False, stop=False)
                    mm(P, b6, b6[:, hs], ABr[s][hf], ABr[s][hf][:, i, :], nZ, nZ[:, hs], start=False, stop=True)
                o_ = ot[s]
                P.op("dve", "tensor_copy", out=o_[:], in_=b6[:, 0:512], reads=[b6], writes=[o_])
                P.dma("pool", G.oA[d][b, n], o_[:], o_, reads=[o_], writes=[G.oA[d]])

            def st_g():
                for h in range(8):
                    hs = slice(h * 64, (h + 1) * 64)
                    mm(P, b7, b7[0:64, hs], BT_, BT_[:, 1, hs], V_, V_[:, hs], start=True, stop=False)
                    mm(P, b7, b7[0:64, hs], BT_, BT_[:, 0, hs], nZ, nZ[:, hs], start=False, stop=True)
                P.op("dve", "tensor_tensor", out=Stmp[:], in0=b7[0:64, 0:512].rearrange("p (h v) -> p h v", h=8),
                     in1=Sf[:], op=ALU.add, reads=[b7, Sf], writes=[Stmp])
                P.op("dve", "tensor_tensor", out=Sf[:], in0=Stmp[:],
                     in1=GLt[:, :, n:n + 1].broadcast_to([64, 8, 64]), op=ALU.mult,
                     reads=[Stmp, GLt], writes=[Sf])
                P.op("act", "activation", out=Sb[:], in_=Sf[:], func=AF.Copy, reads=[Sf], writes=[Sb])

            return [st_d, st_e, st_f, st_g]

        it = 0
        for b in bs:
            for d in ds:
                P.op("dve", "memset", ap=Sf[:], constant=0.0, writes=[Sf])
                P.op("dve", "memset", ap=Sb[:], constant=0.0, writes=[Sb])
                P.dma("sp", GLt[:], G.GL[d][b].rearrange("h k n -> k h n"), GLt, reads=[G.GL[d]], writes=[GLt])
                order = list(range(18)) if d == 0 else [1, 0] + list(range(17, 1, -1))
                order = order[:nmax]
                pending = []
                emit_loads(b, d, order[0], it % 3)
                for oi, n in enumerate(order + [None]):
                    if n is not None:
                        s = it % 2
                        q = it % 3
                        it += 1
                        if oi + 1 < len(order):
                            emit_loads(b, d, order[oi + 1], it % 3)
                        emit_front(b, d, n, s, q)
                    for lev in range(6):
                        if n is not None:
                            emit_level(s, lev)
                        if pending and lev >= 1:
                            pending.pop(0)()
                    while pending:
                        pending.pop(0)()
                    if n is not None:
                        pending = chain_steps(b, d, n, s, q)


def stage_rwkv_post(P, G):
    bank = G.bank
    with P.scope():
        vec = P.sb("rwvec2", [128, 4, 12])
        P.dma("sp", vec[:], G.rw_vec[:], vec, reads=[G.rw_vec], writes=[vec])
        gne = P.sb("gne", [128, 1])
        P.op("dve", "memset", ap=gne[:], constant=64e-5, writes=[gne])
        o0 = [P.sb(f"o0_{i}", [128, 512]) for i in range(3)]
        o1 = [P.sb(f"o1_{i}", [128, 512]) for i in range(3)]
        sq = P.sb("posq", [128, 512])
        st = [P.sb(f"pst{i}", [128, 4, 8]) for i in range(3)]
        on = [P.sb(f"on{i}", [128, 512]) for i in range(3)]
        ya = [P.sb(f"ya{i}", [128, 4, 128]) for i in range(3)]
        bc = [P.sb(f"bc{i}", [128, 4, 128]) for i in range(3)]
        vv = [P.sb(f"vv{i}", [128, 4, 128]) for i in range(3)]
        gg = [P.sb(f"gg{i}", [128, 4, 128]) for i in range(3)]
        yb = [P.sb(f"yab{i}", [128, 4, 128], BF16) for i in range(3)]
        it = 0
        for b in range(NB):
            for n in range(18):
                s = it % 3
                it += 1
                c0 = pcol(n * 128)
                P.dma("sp", o0[s][:], G.oA[0][b, n], o0[s], reads=[G.oA[0]], writes=[o0[s]])
                P.dma("sp", o1[s][:], G.oA[1][b, n], o1[s], reads=[G.oA[1]], writes=[o1[s]])
                P.dma("sp", bc[s][:], G.bcT[b, :, :, c0:c0 + 128].rearrange("c p t -> p c t"), bc[s], reads=[G.bcT], writes=[bc[s]])
                P.dma("sp", vv[s][:], G.vT[b, :, :, c0:c0 + 128].rearrange("c p t -> p c t"), vv[s], reads=[G.vT], writes=[vv[s]])
                P.dma("sp", gg[s][:], G.gT[b, :, :, c0:c0 + 128].rearrange("c p t -> p c t"), gg[s], reads=[G.gT], writes=[gg[s]])
                o_, S_ = o0[s], st[s]
                P.op("pool", "tensor_tensor", out=o_[:], in0=o_[:], in1=o1[s][:], op=ALU.add, reads=[o_, o1[s]], writes=[o_])
                ov = o_[:].rearrange("p (h v) -> p h v", h=8)
                P.op("dve", "tensor_reduce", out=S_[:, 0, :], in_=ov, axis=AX.X, op=ALU.add, reads=[o_], writes=[S_])
                P.op("pool", "tensor_tensor", out=sq[:], in0=o_[:], in1=o_[:], op=ALU.mult, reads=[o_], writes=[sq])
                P.op("dve", "tensor_reduce", out=S_[:, 1, :], in_=sq[:].rearrange("p (h v) -> p h v", h=8), axis=AX.X,
                     op=ALU.add, reads=[sq], writes=[S_])
                P.op("dve", "tensor_scalar", out=S_[:, 2, :], in0=S_[:, 0, :], scalar1=1.0 / 64, scalar2=None, op0=ALU.mult,
                     reads=[S_], writes=[S_])
                P.op("dve", "tensor_tensor", out=S_[:, 3, :], in0=S_[:, 2, :], in1=S_[:, 2, :], op=ALU.mult, reads=[S_], writes=[S_])
                P.op("dve", "scalar_tensor_tensor", out=S_[:, 3, :], in0=S_[:, 1, :], scalar=1.0 / 64, in1=S_[:, 3, :],
                     op0=ALU.mult, op1=ALU.subtract, reads=[S_], writes=[S_])
                P.op("act", "activation", out=S_[:, 3, :], in_=S_[:, 3, :], func=AF.Sqrt, bias=gne[:, 0:1], reads=[S_, gne], writes=[S_])
                P.op("dve", "reciprocal", out=S_[:, 3, :], in_=S_[:, 3, :], reads=[S_], writes=[S_])
                onv = on[s][:].rearrange("p (h v) -> p h v", h=8)
                P.op("dve", "tensor_tensor", out=onv, in0=ov, in1=S_[:, 2, :].unsqueeze(2).broadcast_to([128, 8, 64]),
                     op=ALU.subtract, reads=[o_, S_], writes=[on[s]])
                P.op("dve", "tensor_tensor", out=onv, in0=onv, in1=S_[:, 3, :].unsqueeze(2).broadcast_to([128, 8, 64]),
                     op=ALU.mult, reads=[on[s], S_], writes=[on[s]])
                for c in range(4):
                    ps = bank[it % 8]
                    P.op("pe", "transpose", out=ps[:, c * 128:(c + 1) * 128], in_=on[s][:, c * 128:(c + 1) * 128], identity=G.identf[:],
                         reads=[on[s], G.identf], writes=[ps])
                for c in range(4):
                    ps = bank[it % 8]
                    P.op("act", "activation", out=ya[s][:, c, :], in_=ps[:, c * 128:(c + 1) * 128], func=AF.Identity,
                         scale=vec[:, c, 5:6], bias=vec[:, c, 6:7], reads=[ps, vec], writes=[ya[s]])
                P.op("pool", "tensor_tensor", out=bc[s][:], in0=bc[s][:], in1=vv[s][:], op=ALU.mult, reads=[bc[s], vv[s]], writes=[bc[s]])
                P.op("dve", "tensor_tensor", out=ya[s][:], in0=ya[s][:], in1=bc[s][:], op=ALU.add, reads=[ya[s], bc[s]], writes=[ya[s]])
                P.op("dve", "tensor_tensor", out=yb[s][:], in0=ya[s][:], in1=gg[s][:], op=ALU.mult, reads=[ya[s], gg[s]], writes=[yb[s]])
                P.dma("pool", G.yT[b, 0:4, :, n * 128:(n + 1) * 128].rearrange("c p t -> p c t"), yb[s][:], yb[s],
                      reads=[yb[s]], writes=[G.yT])


def stage_hgrn_feat(P, G, b):
    bank = G.bank
    with P.scope():
        hT = P.sb("hTres", [128, 8, TT], BF16)
        for k in range(8):
            P.dma("sp", hT[:, k, :], G.hT0[b, :, k, :], hT, reads=[G.hT0], writes=[hT])
        lbr = P.sb("lbr", [128, 2, 2, 4])
        P.dma("sp", lbr[:], G.hg_lb[:].rearrange("d l p h -> p d l h"), lbr, reads=[G.hg_lb], writes=[lbr])
        lb = P.sb("lb", [128, 2, 2, 4])
        P.op("dve", "tensor_tensor", out=lb[:, :, 0, :], in0=lbr[:, :, 0, :], in1=lbr[:, :, 1, :], op=ALU.subtract,
             reads=[lbr], writes=[lb])
        P.op("act", "activation", out=lb[:, :, 0, :], in_=lb[:, :, 0, :], func=AF.Sigmoid, reads=[lb], writes=[lb])
        P.op("dve", "tensor_scalar", out=lb[:, :, 1, :], in0=lb[:, :, 0, :], scalar1=-1.0, scalar2=1.0, op0=ALU.mult,
             op1=ALU.add, reads=[lb], writes=[lb])
        sm64 = P.sb("sm64", [128, TT])
        P.op("pool", "memset", ap=sm64[:], constant=1.0, writes=[sm64])
        P.op("pool", "memset", ap=sm64[:].rearrange("p (n t) -> p n t", t=64)[:, :, 0:1], constant=0.0, writes=[sm64])
        WP = WPrefetch(P, G.rec_w_in, [x for h in range(4) for x in (14 + h, 22 + h, 26 + h)])
        wbig = P.sb("wbig", [128, 8, 512], BF16)
        T = [P.sb(f"H{i}", [128, TT]) for i in range(7)]
        hkt = P.sb("hkt", [128, 18, 128], BF16)
        glh = P.sb("glh", [128, 36])
        tok = [P.sb(f"tok{i}", [128, 512], BF16) for i in range(2)]
        wi = [0]

        def proj(chunk, out, func=None):
            wt = WP.get(chunk)
            inproj_fm(P, G, hT, wt, out, padded=False, func=func)

        for h in range(4):
            Q, F, LF, CS, GI, ENI, KF = T
            proj(14 + h, Q, AF.Silu)
            for d in range(2):
                proj(22 + 4 * d + h, F, AF.Sigmoid)
                P.op("dve", "tensor_scalar", out=F[:], in0=F[:], scalar1=lb[:, d, 1, h:h + 1], scalar2=lb[:, d, 0, h:h + 1],
                     op0=ALU.mult, op1=ALU.add, reads=[F, lb], writes=[F])
                P.op("act", "activation", out=LF[:], in_=F[:], func=AF.Ln, reads=[F], writes=[LF])
                P.op("pool", "tensor_scalar", out=KF[:], in0=F[:], scalar1=-1.0, scalar2=1.0, op0=ALU.mult, op1=ALU.add,
                     reads=[F], writes=[KF])
                P.op("dve", "tensor_tensor_scan", out=CS[:], data0=sm64[:], data1=LF[:], initial=0.0, op0=ALU.mult,
                     op1=ALU.add, reads=[sm64, LF], writes=[CS])
                csv = CS[:].rearrange("p (n t) -> p n t", t=64)
                P.op("act", "activation", out=glh[:], in_=csv[:, :, 63], func=AF.Exp, reads=[CS], writes=[glh])
                P.dma("sp", G.HGL[d][b, h], glh[:], glh, reads=[glh], writes=[G.HGL[d]])
                if d == 0:
                    gsrc = CS
                else:
                    P.op("dve", "tensor_tensor", out=GI[:].rearrange("p (n t) -> p n t", t=64),
                         in0=csv[:, :, 63:64].broadcast_to([128, 36, 64]), in1=csv, op=ALU.subtract, reads=[CS], writes=[GI])
                    P.op("pool", "tensor_tensor", out=GI[:], in0=GI[:], in1=LF[:], op=ALU.add, reads=[GI, LF], writes=[GI])
                    gsrc = GI
                P.op("act", "activation", out=ENI[:], in_=gsrc[:], func=AF.Exp, scale=-1.0, reads=[gsrc], writes=[ENI])
                P.op("act", "activation", out=GI[:], in_=gsrc[:], func=AF.Exp, reads=[gsrc], writes=[GI])
                P.op("dve", "tensor_tensor", out=GI[:], in0=GI[:], in1=Q[:], op=ALU.mult, reads=[GI, Q], writes=[GI])
                P.op("pool", "tensor_tensor", out=ENI[:], in0=ENI[:], in1=KF[:], op=ALU.mult, reads=[ENI, KF], writes=[ENI])
                P.dma("pool", G.HQK[d][b, h, :, 0, :], GI[:], GI, reads=[GI], writes=[G.HQK[d]])
                P.dma("pool", G.HQK[d][b, h, :, 1, :], ENI[:], ENI, reads=[ENI], writes=[G.HQK[d]])
                for n in range(18):
                    ps = bank[4 + n % 4]
                    P.op("pe", "transpose", out=ps[:, 0:128], in_=ENI[:, n * 128:(n + 1) * 128], identity=G.identf[:],
                         reads=[ENI, G.identf], writes=[ps])
                    if n % 2:
                        P.op("act", "activation", out=hkt[:, n, :], in_=ps[:, 0:128], func=AF.Copy, reads=[ps], writes=[hkt])
                    else:
                        P.op("dve", "tensor_copy", out=hkt[:, n, :], in_=ps[:, 0:128], reads=[ps], writes=[hkt])
                P.dma("sp", G.HKt[d][b, :, :, h * 128:(h + 1) * 128].rearrange("n t c -> t n c"), hkt[:], hkt,
                      reads=[hkt], writes=[G.HKt[d]])
        for which, c0, dst in ((0, 18 * 128, G.HVt), (1, 30 * 128, G.HGt)):
            load_w_chunk(P, wbig, G.rec_w_in, c0, 512)
            for tt in range(18):
                ps = bank[tt % 4]
                for k in range(8):
                    mm(P, ps, ps[:, 0:512], hT, hT[:, k, tt * 128:(tt + 1) * 128], wbig, wbig[:, k, :],
                       start=(k == 0), stop=(k == 7))
                tk = tok[tt % 2]
                P.op("act", "activation", out=tk[:], in_=ps[:, 0:512], func=(AF.Silu if which else AF.Copy),
                     reads=[ps], writes=[tk])
                P.dma("sp", dst[b, tt], tk[:], tk, reads=[tk], writes=[dst])


def stage_hgrn_scan(P, G, bs=(0, 1), ds=(0, 1)):
    bank = G.bank
    with P.scope():
        msk = P.sb("msk", [128, 4, 128])
        P.dma("sp", msk[:], G.masks[:], msk, reads=[G.masks], writes=[msk])
        qk = [[P.sb(f"qk{d}_{i}", [128, 4, 2, 128], BF16) for i in range(2)] for d in range(2)]
        kt = [[P.sb(f"hkt{d}_{i}", [128, 512], BF16) for i in range(2)] for d in range(2)]
        vt = [[P.sb(f"hvt{d}_{i}", [128, 512], BF16) for i in range(2)] for d in range(2)]
        att = [[P.sb(f"att{d}_{i}", [128, 4, 64], BF16) for i in range(2)] for d in range(2)]
        ot = [[P.sb(f"hot{d}_{i}", [128, 512]) for i in range(2)] for d in range(2)]
        Sf = [P.sb(f"hSf{d}", [128, 4, 128]) for d in range(2)]
        Sb = [P.sb(f"hSb{d}", [128, 4, 128], BF16) for d in range(2)]
        Stmp = [P.sb(f"hStmp{d}", [128, 4, 128]) for d in range(2)]
        GLt = [P.sb(f"hGLt{d}", [128, 4, 36]) for d in range(2)]
        it = 0
        for b in bs:
            for d in ds:
                P.op("dve", "memset", ap=Sf[d][:], constant=0.0, writes=[Sf[d]])
                P.op("dve", "memset", ap=Sb[d][:], constant=0.0, writes=[Sb[d]])
                P.dma("sp", GLt[d][:], G.HGL[d][b].rearrange("h k n -> k h n"), GLt[d], reads=[G.HGL[d]], writes=[GLt[d]])
            orders = {0: list(range(18)), 1: [1, 0] + list(range(17, 1, -1))}
            for step in range(18):
                s = it % 2
                it += 1
                for d in ds:
                    tt = orders[d][step]
                    for j in range(2):
                        P.dma("sp", qk[d][s][:, :, j, :], G.HQK[d][b, :, :, j, tt * 128:(tt + 1) * 128].rearrange("h k t -> k h t"),
                              qk[d][s], reads=[G.HQK[d]], writes=[qk[d][s]])
                    P.dma("sp", kt[d][s][:], G.HKt[d][b, tt], kt[d][s], reads=[G.HKt[d]], writes=[kt[d][s]])
                    P.dma("sp", vt[d][s][:], G.HVt[b, tt], vt[d][s], reads=[G.HVt], writes=[vt[d][s]])
                for ci in range(2):
                    for d in ds:
                        tt = orders[d][step]
                        mi = 1 if d == 0 else 3
                        half = ci if d == 0 else 1 - ci
                        QK, KT, VT = qk[d][s], kt[d][s], vt[d][s]
                        pO = bank[4 + d]
                        chunk = 2 * tt + half
                        lo = half * 64
                        pr = slice(lo, lo + 64)
                        pA = bank[2 * d + ci]
                        pS = bank[6 + d]
                        A_ = att[d][ci]
                        for h in range(4):
                            mm(P, pA, pA[pr, h * 64:(h + 1) * 64], QK, QK[:, h, 1, lo:lo + 64], QK, QK[:, h, 0, lo:lo + 64])
                        P.op("dve", "tensor_tensor", out=A_[pr, :, :], in0=pA[pr, 0:256].rearrange("p (h t) -> p h t", h=4),
                             in1=msk[pr, mi, lo:lo + 64].unsqueeze(1).broadcast_to([64, 4, 64]), op=ALU.mult,
                             reads=[pA, msk], writes=[A_])
                        for h in range(4):
                            hs = slice(h * 128, (h + 1) * 128)
                            mm(P, pO, pO[pr, hs], A_, A_[pr, h, :], VT, VT[pr, hs], start=True, stop=False)
                            mm(P, pO, pO[pr, hs], QK, QK[:, h, 0, lo:lo + 64], Sb[d], Sb[d][:, h, :], start=False, stop=True)
                        for h in range(4):
                            hs = slice(h * 128, (h + 1) * 128)
                            mm(P, pS, pS[:, hs], KT, KT[pr, hs], VT, VT[pr, hs])
                        P.op("dve", "tensor_tensor", out=Stmp[d][:], in0=pS[:, 0:512].rearrange("p (h v) -> p h v", h=4),
                             in1=Sf[d][:], op=ALU.add, reads=[pS, Sf[d]], writes=[Stmp[d]])
                        P.op("dve", "tensor_tensor", out=Sf[d][:], in0=Stmp[d][:],
                             in1=GLt[d][:, :, chunk:chunk + 1].broadcast_to([128, 4, 128]), op=ALU.mult,
                             reads=[Stmp[d], GLt[d]], writes=[Sf[d]])
                        P.op("act", "activation", out=Sb[d][:], in_=Sf[d][:], func=AF.Copy, reads=[Sf[d]], writes=[Sb[d]])
                for d in ds:
                    tt = orders[d][step]
                    o_ = ot[d][s]
                    pO = bank[4 + d]
                    P.op("act", "activation", out=o_[:], in_=pO[:, 0:512], func=AF.Copy, reads=[pO], writes=[o_])
                    P.dma("pool", G.oH[d][b, tt], o_[:], o_, reads=[o_], writes=[G.oH[d]])


def stage_hgrn_post(P, G):
    bank = G.bank
    with P.scope():
        gn = P.sb("gn", [128, 128])
        P.dma("sp", gn[:], G.hg_norm[0:1, :].partition_broadcast(128), gn, reads=[G.hg_norm], writes=[gn])
        o0 = [P.sb(f"ho0_{i}", [128, 512]) for i in range(3)]
        o1 = [P.sb(f"ho1_{i}", [128, 512]) for i in range(3)]
        gt = [P.sb(f"hgt{i}", [128, 512], BF16) for i in range(3)]
        sq = P.sb("hsq", [128, 512])
        st = [P.sb(f"hst{i}", [128, 4]) for i in range(3)]
        yb = [P.sb(f"hyb{i}", [128, 4, 128], BF16) for i in range(3)]
        it = 0
        for b in range(NB):
            for tt in range(18):
                s = it % 3
                it += 1
                P.dma("sp", o0[s][:], G.oH[0][b, tt], o0[s], reads=[G.oH[0]], writes=[o0[s]])
                P.dma("sp", o1[s][:], G.oH[1][b, tt], o1[s], reads=[G.oH[1]], writes=[o1[s]])
                P.dma("sp", gt[s][:], G.HGt[b, tt], gt[s], reads=[G.HGt], writes=[gt[s]])
                o_ = o0[s]
                P.op("pool", "tensor_tensor", out=o_[:], in0=o_[:], in1=o1[s][:], op=ALU.add, reads=[o_, o1[s]], writes=[o_])
                P.op("pool", "tensor_tensor", out=sq[:], in0=o_[:], in1=o_[:], op=ALU.mult, reads=[o_], writes=[sq])
                P.op("dve", "tensor_reduce", out=st[s][:], in_=sq[:].rearrange("p (h v) -> p h v", h=4), axis=AX.X, op=ALU.add,
                     reads=[sq], writes=[st[s]])
                P.op("act", "activation", out=st[s][:], in_=st[s][:], func=AF.Sqrt, scale=1.0 / 128, bias=G.eps[:, 0:1],
                     reads=[st[s], G.eps], writes=[st[s]])
                P.op("dve", "reciprocal", out=st[s][:], in_=st[s][:], reads=[st[s]], writes=[st[s]])
                ov = o_[:].rearrange("p (h v) -> p h v", h=4)
                P.op("dve", "tensor_tensor", out=ov, in0=ov, in1=st[s][:].unsqueeze(2).broadcast_to([128, 4, 128]), op=ALU.mult,
                     reads=[o_, st[s]], writes=[o_])
                P.op("dve", "tensor_tensor", out=ov, in0=ov, in1=gn[:].unsqueeze(1).broadcast_to([128, 4, 128]), op=ALU.mult,
                     reads=[o_, gn], writes=[o_])
                P.op("pool", "tensor_tensor", out=o_[:], in0=o_[:], in1=gt[s][:], op=ALU.mult, reads=[o_, gt[s]], writes=[o_])
                ps = bank[it % 8]
                for c in range(4):
                    P.op("pe", "transpose", out=ps[:, c * 128:(c + 1) * 128], in_=o_[:, c * 128:(c + 1) * 128], identity=G.identf[:],
                         reads=[o_, G.identf], writes=[ps])
                P.op("act", "activation", out=yb[s][:, 0:2, :], in_=ps[:, 0:256].rearrange("p (c t) -> p c t", c=2), func=AF.Copy,
                     reads=[ps], writes=[yb[s]])
                P.op("dve", "tensor_copy", out=yb[s][:, 2:4, :], in_=ps[:, 256:512].rearrange("p (c t) -> p c t", c=2),
                     reads=[ps], writes=[yb[s]])
                P.dma("pool", G.yT[b, 4:8, :, tt * 128:(tt + 1) * 128].rearrange("c p t -> p c t"), yb[s][:], yb[s],
                      reads=[yb[s]], writes=[G.yT])


def load_gates(P, G, l, which, name="gate"):
    gts = []
    for j in range(3):
        g = P.sb(f"{name}{j}", [128, D])
        P.dma("sp", g[:], G.modD[l, j:j + 1, which * D:(which + 1) * D].partition_broadcast(128), g,
              reads=[G.modD], writes=[g])
        gts.append(g)
    return gts


def stage_outproj(P, G, yT, wsrc, l, T, ctx_len, xin_ap, xout_ap, xin_t, xout_t):
    bank = G.bank
    with P.scope():
        gts = load_gates(P, G, l, 2)
        w = P.sb("wout", [128, 8, D], BF16)
        for k in range(8):
            P.dma("pool", w[:, k, :], wsrc[k * 128:(k + 1) * 128, :], w, reads=[wsrc], writes=[w])
        yt = [P.sb(f"yt{i}", [128, 8, 128], BF16) for i in range(3)]
        xt = [P.sb(f"xt{i}", [128, D]) for i in range(3)]
        rt = [P.sb(f"rt{i}", [128, D]) for i in range(3)]
        it = 0
        for b in range(NB):
            for tt in range(T // 128):
                s = it % 3
                it += 1
                j = 2 if tt * 128 < ctx_len else b
                P.dma("sp", yt[s][:], yT[b, :, :, tt * 128:(tt + 1) * 128].rearrange("c p t -> p c t"), yt[s],
                      reads=[yT], writes=[yt[s]])
                P.dma("sp", xt[s][:], xin_ap(b, tt), xt[s], reads=[xin_t], writes=[xt[s]])
                for half in range(2):
                    ps = bank[(it % 3) * 2 + half]
                    for c in range(8):
                        mm(P, ps, ps[:, 0:512], yt[s], yt[s][:, c, :], w, w[:, c, half * 512:(half + 1) * 512],
                           start=(c == 0), stop=(c == 7))
                    P.op("dve", "tensor_tensor", out=rt[s][:, half * 512:(half + 1) * 512], in0=ps[:, 0:512],
                         in1=gts[j][:, half * 512:(half + 1) * 512], op=ALU.mult, reads=[ps, gts[j]], writes=[rt[s]])
                P.op("dve", "tensor_tensor", out=rt[s][:], in0=rt[s][:], in1=xt[s][:], op=ALU.add,
                     reads=[rt[s], xt[s]], writes=[rt[s]])
                P.dma("pool", xout_ap(b, tt), rt[s][:], rt[s], reads=[rt[s]], writes=[xout_t])


def swiglu_pass(P, G, fT, blocks, wg_ap, wu_ap, wd_ap, nch, gts, ctx_len, xin_ap, xout_ap, xin_t, xout_t, wsrc_ts,
                tokw=None, tokw_col=None, tiles_per_b=None):
    bank = G.bank
    F_ = nch * 128
    with P.scope():
        wg = P.sb("wg", [128, 8, F_], BF16)
        wu = P.sb("wu", [128, 8, F_], BF16)
        wd = P.sb("wd", [128, nch, D], BF16)
        for k in range(8):
            P.dma("pool", wg[:, k, :], wg_ap[k * 128:(k + 1) * 128, :], wg, reads=wsrc_ts, writes=[wg])
            P.dma("pool", wu[:, k, :], wu_ap[k * 128:(k + 1) * 128, :], wu, reads=wsrc_ts, writes=[wu])
        for c in range(nch):
            P.dma("pool", wd[:, c, :], wd_ap[c * 128:(c + 1) * 128, :], wd, reads=wsrc_ts, writes=[wd])
        ft = [P.sb(f"ft{i}", [128, 8, 512], BF16) for i in range(2)]
        sg = [P.sb(f"sg{i}", [128, 512]) for i in range(2)]
        act = [P.sb(f"act{i}", [128, nch, 512], BF16) for i in range(2)]
        xt = [P.sb(f"fxt{i}", [128, D]) for i in range(2)]
        rt = [P.sb(f"frt{i}", [128, D]) for i in range(2)]
        bi = 0
        ti = 0
        blist = [(b, t0, t1) for b in range(NB) for (t0, t1) in blocks]

        def load_ft(i):
            if i < len(blist):
                b_, a0, a1 = blist[i]
                for k in range(8):
                    P.dma("sp", ft[i % 2][:, k, 0:a1 - a0], fT[b_, :, k, a0:a1], ft[i % 2], reads=[fT], writes=[ft[i % 2]])

        load_ft(0)
        for b in range(NB):
            for (t0, t1) in blocks:
                n = t1 - t0
                F = ft[bi % 2]
                A_ = act[bi % 2]
                bi += 1
                load_ft(bi)
                for jc in range(nch):
                    pg, pu = bank[jc % 2], bank[2 + jc % 2]
                    cs = slice(jc * 128, (jc + 1) * 128)
                    for k in range(8):
                        mm(P, pg, pg[:, 0:n], wg, wg[:, k, cs], F, F[:, k, 0:n], start=(k == 0), stop=(k == 7))
                    for k in range(8):
                        mm(P, pu, pu[:, 0:n], wu, wu[:, k, cs], F, F[:, k, 0:n], start=(k == 0), stop=(k == 7))
                    S_ = sg[jc % 2]
                    P.op("act", "activation", out=S_[:, 0:n], in_=pg[:, 0:n], func=AF.Silu, reads=[pg], writes=[S_])
                    P.op("dve", "tensor_tensor", out=A_[:, jc, 0:n], in0=S_[:, 0:n], in1=pu[:, 0:n], op=ALU.mult,
                         reads=[S_, pu], writes=[A_])
                for ts in range(n // 128):
                    tt = t0 // 128 + ts
                    s = ti % 2
                    ti += 1
                    j = 2 if tt * 128 < ctx_len else b
                    P.dma("sp", xt[s][:], xin_ap(b, tt), xt[s], reads=[xin_t], writes=[xt[s]])
                    for half in range(2):
                        po = bank[4 + (ti % 2) * 2 + half]
                        for jc in range(nch):
                            mm(P, po, po[:, 0:512], A_, A_[:, jc, ts * 128:(ts + 1) * 128], wd,
                               wd[:, jc, half * 512:(half + 1) * 512], start=(jc == 0), stop=(jc == nch - 1))
                        hs = slice(half * 512, (half + 1) * 512)
                        if tokw is None:
                            P.op("dve", "tensor_tensor", out=rt[s][:, hs], in0=po[:, 0:512], in1=gts[j][:, hs], op=ALU.mult,
                                 reads=[po, gts[j]], writes=[rt[s]])
                        else:
                            tix = b * tiles_per_b + tt
                            P.op("dve", "scalar_tensor_tensor", out=rt[s][:, hs], in0=po[:, 0:512],
                                 scalar=tokw[:, tix, tokw_col:tokw_col + 1], in1=gts[j][:, hs], op0=ALU.mult, op1=ALU.mult,
                                 reads=[po, gts[j], tokw], writes=[rt[s]])
                    P.op("dve", "tensor_tensor", out=rt[s][:], in0=rt[s][:], in1=xt[s][:], op=ALU.add,
                         reads=[rt[s], xt[s]], writes=[rt[s]])
                    P.dma("pool", xout_ap(b, tt), rt[s][:], rt[s], reads=[rt[s]], writes=[xout_t])


def stage_ffn0(P, G):
    with P.scope():
        gts = load_gates(P, G, 0, 5)
        for hf in range(2):
            cs = slice(hf * 1408, (hf + 1) * 1408)
            xin_t = G.x1 if hf == 0 else G.x2
            swiglu_pass(P, G, G.fT0, TBLK, G.ffn_wg[:, cs], G.ffn_wu[:, cs], G.ffn_wd[cs, :], 11, gts, TC,
                        (lambda b, tt, X=xin_t: X[b, tt * 128:(tt + 1) * 128, :]),
                        (lambda b, tt: G.x2[b, tt * 128:(tt + 1) * 128, :]), xin_t, G.x2,
                        [G.ffn_wg, G.ffn_wu, G.ffn_wd])


LBLK = [(TC + i * 512, TC + (i + 1) * 512) for i in range(4)]


def stage_att_inproj(P, G, b):
    bank = G.bank
    with P.scope():
        hT = P.sb("hTres", [128, 8, TT], BF16)
        for k in range(8):
            P.dma("sp", hT[:, k, :], G.hT1[b, :, k, :], hT, reads=[G.hT1], writes=[hT])
        rope = P.sb("rope", [128, 2, TL])
        P.dma("sp", rope[:], G.rope[:], rope, reads=[G.rope], writes=[rope])
        perm = P.sb("perm", [128, 128], BF16)
        P.dma("pool", perm[:], G.perm[:], perm, reads=[G.perm], writes=[perm])
        WP = WPrefetch(P, G.att_w_in, list(range(10)))
        wv = P.sb("wv", [128, 8, 256], BF16)
        load_w_chunk(P, wv, G.att_w_in, 1280, 256)
        qraw = [P.sb(f"qraw{i}", [128, 512], BF16) for i in range(2)]
        t1 = [P.sb(f"t1_{i}", [128, 512]) for i in range(2)]
        t2 = [P.sb(f"t2_{i}", [128, 512]) for i in range(2)]
        qo = [P.sb(f"qo{i}", [128, 512], BF16) for i in range(2)]
        kc_ = [P.sb(f"kc{i}", [128, 256], BF16) for i in range(2)]
        vtk = [P.sb(f"vtk{i}", [128, 256], BF16) for i in range(2)]
        it = 0
        for ch in range(10):
            wt = WP.get(ch)
            if ch < 8:
                dst = lambda tl0, n, ch=ch: G.qT[b, 2 * ch:2 * ch + 2, :, tl0:tl0 + n].rearrange("h k t -> (h k) t")
                dt_ = G.qT
            else:
                kc = ch - 8
                dst = lambda tl0, n, kc=kc: G.kT[b, 2 * kc:2 * kc + 2, :, TC + tl0:TC + tl0 + n].rearrange("h k t -> (h k) t")
                dt_ = G.kT
                ps = bank[0]
                for k in range(8):
                    mm(P, ps, ps[:, 0:TC], wt, wt[:, k, :], hT, hT[:, k, 0:TC], start=(k == 0), stop=(k == 7))
                kk_ = kc_[kc % 2]
                P.op("act", "activation", out=kk_[:], in_=ps[:, 0:TC], func=AF.Copy, reads=[ps], writes=[kk_])
                P.dma("pool", G.kT[b, 2 * kc:2 * kc + 2, :, 0:TC].rearrange("h k t -> (h k) t"), kk_[:], kk_,
                      reads=[kk_], writes=[G.kT])
            for (t0, t1_) in LBLK:
                s = it % 2
                it += 1
                tl0 = t0 - TC
                p1, p2 = bank[(it % 2) * 2], bank[(it % 2) * 2 + 1]
                for k in range(8):
                    mm(P, p1, p1[:, 0:512], wt, wt[:, k, :], hT, hT[:, k, t0:t1_], start=(k == 0), stop=(k == 7))
                P.op("act", "activation", out=qraw[s][:], in_=p1[:, 0:512], func=AF.Copy, reads=[p1], writes=[qraw[s]])
                mm(P, p2, p2[:, 0:512], perm, perm[:], qraw[s], qraw[s][:])
                P.op("dve", "tensor_tensor", out=t1[s][:], in0=p1[:, 0:512], in1=rope[:, 0, tl0:tl0 + 512], op=ALU.mult,
                     reads=[p1, rope], writes=[t1[s]])
                P.op("dve", "tensor_tensor", out=t2[s][:], in0=p2[:, 0:512], in1=rope[:, 1, tl0:tl0 + 512], op=ALU.mult,
                     reads=[p2, rope], writes=[t2[s]])
                P.op("dve", "tensor_tensor", out=qo[s][:], in0=t1[s][:], in1=t2[s][:], op=ALU.add,
                     reads=[t1[s], t2[s]], writes=[qo[s]])
                P.dma("pool", dst(tl0, 512), qo[s][:], qo[s], reads=[qo[s]], writes=[dt_])
        for tt in range(18):
            ps = bank[4 + tt % 4]
            for k in range(8):
                mm(P, ps, ps[:, 0:256], hT, hT[:, k, tt * 128:(tt + 1) * 128], wv, wv[:, k, :], start=(k == 0), stop=(k == 7))
            v_ = vtk[tt % 2]
            P.op("act", "activation", out=v_[:], in_=ps[:, 0:256], func=AF.Copy, reads=[ps], writes=[v_])
            P.dma("sp", G.Vt1[b, tt], v_[:], v_, reads=[v_], writes=[G.Vt1])


def stage_attention(P, G):
    bank = G.bank
    with P.scope():
        msk = P.sb("mskb", [128, 4, 128], BF16)
        P.dma("pool", msk[:], G.masks[:], msk, reads=[G.masks], writes=[msk])
        es = P.sb("es", [64, 16])
        P.dma("sp", es[:], G.att_sink[0:1, :].partition_broadcast(64), es, reads=[G.att_sink], writes=[es])
        P.op("act", "activation", out=es[:], in_=es[:], func=AF.Exp, reads=[es], writes=[es])
        ones = P.sb("ones", [128, 64], BF16)
        P.op("dve", "memset", ap=ones[:], constant=1.0, writes=[ones])
        Kt = [P.sb(f"Kt{i}", [64, TT], BF16) for i in range(2)]
        Vv = [P.sb(f"Vv{i}", [128, 18, 64], BF16) for i in range(2)]
        Qt = [P.sb(f"Qt{i}", [64, 4, TL], BF16) for i in range(2)]
        E = [P.sb(f"E{i}", [128, 4, 128], BF16) for i in range(6)]
        den = [P.sb(f"den{i}", [64, 4, 128]) for i in range(2)]
        ob = [P.sb(f"ob{i}", [64, 4, 128], BF16) for i in range(2)]
        g = 0
        r = 0
        ii = 0
        for b in range(NB):
            for hk in range(4):
                K_, V_, Q_ = Kt[g % 2], Vv[g % 2], Qt[g % 2]
                g += 1
                P.dma("sp", K_[:], G.kT[b, hk], K_, reads=[G.kT], writes=[K_])
                P.dma("sp", V_[:], G.Vt1[b, :, :, hk * 64:(hk + 1) * 64].rearrange("n t c -> t n c"), V_,
                      reads=[G.Vt1], writes=[V_])
                P.dma("sp", Q_[:], G.qT[b, 4 * hk:4 * hk + 4].rearrange("g k t -> k g t"), Q_, reads=[G.qT], writes=[Q_])
                for i in range(16):
                    tiles = [(0, None), (1, None)]
                    if i > 0:
                        tiles.append((2 + i - 1, 3))
                    tiles.append((2 + i, None))
                    if i < 15:
                        tiles.append((2 + i + 1, 1))
                    pN, pD = bank[4 + ii % 2], bank[6 + ii % 2]
                    Es = []
                    for ti, (kt, mi) in enumerate(tiles):
                        pS = bank[r % 4]
                        E_ = E[r % 6]
                        r += 1
                        mm(P, pS, pS[:, 0:512], K_, K_[:, kt * 128:(kt + 1) * 128], Q_, Q_[:, :, i * 128:(i + 1) * 128])
                        P.op("act", "activation", out=E_[:], in_=pS[:, 0:512].rearrange("p (g t) -> p g t", g=4),
                             func=AF.Exp, scale=0.125, reads=[pS], writes=[E_])
                        if mi is not None:
                            P.op("dve", "tensor_tensor", out=E_[:], in0=E_[:],
                                 in1=msk[:, mi, :].unsqueeze(1).broadcast_to([128, 4, 128]), op=ALU.mult,
                                 reads=[E_, msk], writes=[E_])
                        Es.append(E_)
                    for ti, (kt, mi) in enumerate(tiles):
                        E_ = Es[ti]
                        first, last = ti == 0, ti == len(tiles) - 1
                        mm(P, pN, pN[0:64, 0:512], V_, V_[:, kt, :], E_, E_[:], start=first, stop=last)
                        mm(P, pD, pD[0:64, 0:512], ones, ones[:], E_, E_[:], start=first, stop=last)
                    s = ii % 2
                    ii += 1
                    P.op("dve", "tensor_tensor", out=den[s][:], in0=pD[0:64, 0:512].rearrange("p (g t) -> p g t", g=4),
                         in1=es[:, 4 * hk:4 * hk + 4].unsqueeze(2).broadcast_to([64, 4, 128]), op=ALU.add,
                         reads=[pD, es], writes=[den[s]])
                    P.op("dve", "reciprocal", out=den[s][:], in_=den[s][:], reads=[den[s]], writes=[den[s]])
                    P.op("dve", "tensor_tensor", out=ob[s][:], in0=pN[0:64, 0:512].rearrange("p (g t) -> p g t", g=4),
                         in1=den[s][:], op=ALU.mult, reads=[pN, den[s]], writes=[ob[s]])
                    P.dma("pool", G.oT1[b, 2 * hk:2 * hk + 2, :, i * 128:(i + 1) * 128].rearrange("c (hh k) t -> k (c hh) t", hh=2),
                          ob[s][:], ob[s], reads=[ob[s]], writes=[G.oT1])


def stage_router(P, G):
    bank = G.bank
    with P.scope():
        rw = P.sb("rw", [128, 8, 8])
        P.dma("sp", rw[:], G.moe_router[:].rearrange("(k p) e -> p k e", p=128), rw, reads=[G.moe_router], writes=[rw])
        rb = P.sb("rb", [128, 8])
        P.dma("sp", rb[:], G.moe_router_b[0:1, :].partition_broadcast(128), rb, reads=[G.moe_router_b], writes=[rb])
        ff = [P.sb(f"ff{i}", [128, 8, 128]) for i in range(2)]
        lg = [P.sb(f"lg{i}", [128, 8]) for i in range(2)]
        tm = [P.sb(f"tm{i}", [128, 4, 8]) for i in range(2)]
        sc = [P.sb(f"rsc{i}", [128, 4]) for i in range(2)]
        it = 0
        for b in range(NB):
            for tt in range(16):
                s = it % 2
                tix = b * 16 + tt
                it += 1
                P.dma("sp", ff[s][:], G.fT1f[b, :, :, tt * 128:(tt + 1) * 128], ff[s], reads=[G.fT1f], writes=[ff[s]])
                ps = bank[it % 2]
                for k in range(8):
                    mm(P, ps, ps[:, 0:8], ff[s], ff[s][:, k, :], rw, rw[:, k, :], start=(k == 0), stop=(k == 7))
                L_, T_, S_ = lg[s], tm[s], sc[s]
                P.op("dve", "tensor_tensor", out=L_[:], in0=ps[:, 0:8], in1=rb[:], op=ALU.add, reads=[ps, rb], writes=[L_])
                P.op("dve", "tensor_reduce", out=S_[:, 0:1], in_=L_[:], axis=AX.X, op=ALU.max, reads=[L_], writes=[S_])
                P.op("dve", "tensor_scalar", out=T_[:, 0, :], in0=L_[:], scalar1=S_[:, 0:1], scalar2=-1e30, op0=ALU.is_equal,
                     op1=ALU.mult, reads=[L_, S_], writes=[T_])
                P.op("dve", "tensor_tensor", out=T_[:, 0, :], in0=T_[:, 0, :], in1=L_[:], op=ALU.add, reads=[T_, L_], writes=[T_])
                P.op("dve", "tensor_reduce", out=S_[:, 1:2], in_=T_[:, 0, :], axis=AX.X, op=ALU.max, reads=[T_], writes=[S_])
                P.op("dve", "tensor_scalar", out=T_[:, 1, :], in0=L_[:], scalar1=S_[:, 1:2], scalar2=None, op0=ALU.is_ge,
                     reads=[L_, S_], writes=[T_])
                P.op("dve", "tensor_scalar", out=S_[:, 2:3], in0=S_[:, 0:1], scalar1=-1.0, scalar2=None, op0=ALU.mult,
                     reads=[S_], writes=[S_])
                P.op("act", "activation", out=T_[:, 2, :], in_=L_[:], func=AF.Exp, bias=S_[:, 2:3], reads=[L_, S_], writes=[T_])
                P.op("dve", "tensor_tensor", out=T_[:, 2, :], in0=T_[:, 2, :], in1=T_[:, 1, :], op=ALU.mult, reads=[T_], writes=[T_])
                P.op("dve", "tensor_reduce", out=S_[:, 3:4], in_=T_[:, 2, :], axis=AX.X, op=ALU.add, reads=[T_], writes=[S_])
                P.op("dve", "reciprocal", out=S_[:, 3:4], in_=S_[:, 3:4], reads=[S_], writes=[S_])
                P.op("dve", "tensor_scalar", out=G.WTm[:, tix, :], in0=T_[:, 2, :], scalar1=S_[:, 3:4], scalar2=None, op0=ALU.mult,
                     reads=[T_, S_], writes=[G.WTm])


def stage_moe(P, G, experts=range(8)):
    with P.scope():
        gts = load_gates(P, G, 1, 5)
        first = True
        blocks = [(i * 512, (i + 1) * 512) for i in range(4)]
        for e in experts:
            for hf in range(2):
                cs = slice(hf * 1408, (hf + 1) * 1408)
                xin_t = G.x3 if first else G.x4
                first = False
                swiglu_pass(P, G, G.fT1, blocks, G.moe_wg[e, :, cs], G.moe_wu[e, :, cs], G.moe_wd[e, cs, :], 11, gts, 0,
                            (lambda b, tt, X=xin_t: X[b, tt * 128:(tt + 1) * 128, :]),
                            (lambda b, tt: G.x4[b, tt * 128:(tt + 1) * 128, :]), xin_t, G.x4,
                            [G.moe_wg, G.moe_wu, G.moe_wd], tokw=G.WTm, tokw_col=e, tiles_per_b=16)


def stage_final(P, G, src):
    with P.scope():
        gf = P.sb("gfin", [128, D])
        P.dma("sp", gf[:], G.norm_final[0:1, :].partition_broadcast(128), gf, reads=[G.norm_final], writes=[gf])
        xt = [P.sb(f"zxt{i}", [128, D]) for i in range(2)]
        sq = P.sb("zsq", [128, D], BF16)
        ss = [P.sb(f"zss{i}", [128, 1]) for i in range(2)]
        yo = [P.sb(f"zyo{i}", [128, D]) for i in range(2)]
        it = 0
        for b in range(NB):
            for tt in range(16):
                s = it % 2
                it += 1
                P.dma("sp", xt[s][:], src[b, tt * 128:(tt + 1) * 128, :], xt[s], reads=[src], writes=[xt[s]])
                P.op("act", "activation", out=sq[:], in_=xt[s][:], func=AF.Square, accum_out=ss[s][:], reads=[xt[s]], writes=[sq, ss[s]])
                P.op("act", "activation", out=ss[s][:], in_=ss[s][:], func=AF.Sqrt, scale=1.0 / D, bias=G.eps[:, 0:1],
                     reads=[ss[s], G.eps], writes=[ss[s]])
                P.op("dve", "reciprocal", out=ss[s][:], in_=ss[s][:], reads=[ss[s]], writes=[ss[s]])
                P.op("dve", "scalar_tensor_tensor", out=yo[s][:], in0=xt[s][:], scalar=ss[s][:, 0:1], in1=gf[:], op0=ALU.mult,
                     op1=ALU.mult, reads=[xt[s], ss[s], gf], writes=[yo[s]])
                P.dma("pool", G.out[b, tt * 128:(tt + 1) * 128, :], yo[s][:], yo[s], reads=[yo[s]], writes=[G.out])


I32 = mybir.dt.int32
BS = 512
SUBS = BS // 512
NBLK = (NB * TL * 2) // BS + 8
NSLOT = NBLK * BS
DUMMY = NB * TL


def stage_moe_sparse(P, G, nblk=NBLK):
    bank = G.bank
    IOA = bass.IndirectOffsetOnAxis
    with P.scope():
        WAB = P.sb("WAB", [128, 32, 2])
        SAB = P.sb("SAB", [128, 2, 32], I32)
        IDXG = P.sb("IDXG", [128, NBLK, 2], I32)
        IDXD = P.sb("IDXD", [128, NBLK, 2], I32)
        _phaseA = P.scope()
        _phaseA.__enter__()
        LG = G.LG
        T3 = lambda nm: P.sb(nm, [128, 32, 8])
        T2 = lambda nm: P.sb(nm, [128, 32])
        bc3 = lambda t2: t2[:].unsqueeze(2).broadcast_to([128, 32, 8])
        M1, M2, NM1, DEN = T2("M1"), T2("M2"), T2("NM1"), T2("DEN")
        TMP, SEL, WT = T3("TMP"), T3("SEL"), T3("WT")
        P.op("dve", "tensor_reduce", out=M1[:], in_=LG[:], axis=AX.X, op=ALU.max, reads=[LG], writes=[M1])
        P.op("dve", "tensor_tensor", out=TMP[:], in0=LG[:], in1=bc3(M1), op=ALU.is_equal, reads=[LG, M1], writes=[TMP])
        P.op("dve", "scalar_tensor_tensor", out=TMP[:], in0=TMP[:], scalar=-1e30, in1=LG[:], op0=ALU.mult, op1=ALU.add,
             reads=[TMP, LG], writes=[TMP])
        P.op("dve", "tensor_reduce", out=M2[:], in_=TMP[:], axis=AX.X, op=ALU.max, reads=[TMP], writes=[M2])
        P.op("dve", "tensor_tensor", out=SEL[:], in0=LG[:], in1=bc3(M2), op=ALU.is_ge, reads=[LG, M2], writes=[SEL])
        P.op("dve", "tensor_tensor", out=TMP[:], in0=LG[:], in1=bc3(M1), op=ALU.subtract, reads=[LG, M1], writes=[TMP])
        P.op("act", "activation", out=TMP[:], in_=TMP[:], func=AF.Exp, reads=[TMP], writes=[TMP])
        P.op("dve", "tensor_tensor", out=TMP[:], in0=TMP[:], in1=SEL[:], op=ALU.mult, reads=[TMP, SEL], writes=[TMP])
        P.op("dve", "tensor_reduce", out=DEN[:], in_=TMP[:], axis=AX.X, op=ALU.add, reads=[TMP], writes=[DEN])
        P.op("dve", "reciprocal", out=DEN[:], in_=DEN[:], reads=[DEN], writes=[DEN])
        P.op("dve", "tensor_tensor", out=WT[:], in0=TMP[:], in1=bc3(DEN), op=ALU.mult, reads=[TMP, DEN], writes=[WT])
        cst = P.sb("mcst", [128, 512])
        P.dma("sp", cst[:], G.moe_consts[:], cst, reads=[G.moe_consts], writes=[cst])
        TH = cst[:, 0:64].rearrange("p (e m) -> p e m", m=8)
        JJ = cst[:, 64:64 + NBLK]
        CG = cst[:, 88:90]
        CD = cst[:, 104:106]
        TIDc = cst[:, 128:160]
        RM = cst[:, 256:512]
        selb = P.sb("selb", [128, 256], BF16)
        P.op("dve", "tensor_copy", out=selb[:], in_=SEL[:].rearrange("p i e -> p (i e)"), reads=[SEL], writes=[selb])
        mb_ = P.sb("mskb2", [128, 4, 128], BF16)
        P.dma("pool", mb_[:], G.masks[:], mb_, reads=[G.masks], writes=[mb_])
        onesb = P.sb("onesb", [128, 128], BF16)
        P.op("dve", "memset", ap=onesb[:], constant=1.0, writes=[onesb])
        mm(P, bank[2], bank[2][:, 0:256], mb_, mb_[:, 0, :], selb, selb[:])
        mm(P, bank[3], bank[3][:, 0:256], onesb, onesb[:], selb, selb[:])
        SLOT = T3("SLOT")
        P.op("dve", "tensor_copy", out=SLOT[:], in_=bank[2][:, 0:256].rearrange("p (i e) -> p i e", e=8), reads=[bank[2]], writes=[SLOT])
        TOTp = P.sb("TOTp", [128, 8, 32])
        P.op("dve", "tensor_copy", out=TOTp[:], in_=bank[3][:, 0:256].rearrange("p (i e) -> p e i", e=8), reads=[bank[3]], writes=[TOTp])
        INC = P.sb("INC", [128, 8, 32])
        P.op("dve", "tensor_tensor_scan", out=INC[:].rearrange("p e i -> p (e i)"), data0=RM, data1=TOTp[:].rearrange("p e i -> p (e i)"),
             initial=0.0, op0=ALU.mult, op1=ALU.add, reads=[cst, TOTp], writes=[INC])
        OFFp = P.sb("OFFp", [128, 8, 32])
        P.op("dve", "tensor_tensor", out=OFFp[:], in0=INC[:], in1=TOTp[:], op=ALU.subtract, reads=[INC, TOTp], writes=[OFFp])
        CMP = P.sb("CMP", [128, 8, 8])
        P.op("dve", "tensor_tensor", out=CMP[:], in0=INC[:, :, 31:32].broadcast_to([128, 8, 8]), in1=TH, op=ALU.is_gt,
             reads=[INC, cst], writes=[CMP])
        nbk = P.sb("nbk", [128, 8])
        P.op("dve", "tensor_reduce", out=nbk[:], in_=CMP[:], axis=AX.X, op=ALU.add, reads=[CMP], writes=[nbk])
        PEI = P.sb("PEI", [128, 8])
        P.op("dve", "tensor_tensor_scan", out=PEI[:], data0=onesb[:, 0:8], data1=nbk[:], initial=0.0, op0=ALU.mult, op1=ALU.add,
             reads=[onesb, nbk], writes=[PEI])
        PST = P.sb("PST", [128, 8])
        P.op("dve", "tensor_tensor", out=PST[:], in0=PEI[:], in1=nbk[:], op=ALU.subtract, reads=[PEI, nbk], writes=[PST])
        P.op("dve", "tensor_scalar", out=PST[:], in0=PST[:], scalar1=float(BS), scalar2=None, op0=ALU.mult, reads=[PST], writes=[PST])
        P.op("dve", "tensor_tensor", out=SLOT[:], in0=SLOT[:], in1=OFFp[:].rearrange("p e i -> p i e"), op=ALU.add,
             reads=[SLOT, OFFp], writes=[SLOT])
        P.op("dve", "tensor_tensor", out=SLOT[:], in0=SLOT[:], in1=PST[:].unsqueeze(1).broadcast_to([128, 32, 8]), op=ALU.add,
             reads=[SLOT, PST], writes=[SLOT])
        V = T3("V")
        P.op("dve", "scalar_tensor_tensor", out=V[:], in0=SLOT[:], scalar=1.0, in1=SEL[:], op0=ALU.add, op1=ALU.mult,
             reads=[SLOT, SEL], writes=[V])
        MA, MB = T2("MA"), T2("MB")
        P.op("dve", "tensor_reduce", out=MA[:], in_=V[:], axis=AX.X, op=ALU.max, reads=[V], writes=[MA])
        P.op("dve", "tensor_tensor", out=TMP[:], in0=V[:], in1=bc3(MA), op=ALU.is_equal, reads=[V, MA], writes=[TMP])
        IS2 = T3("IS2")
        P.op("dve", "tensor_tensor", out=IS2[:], in0=TMP[:], in1=WT[:], op=ALU.mult, reads=[TMP, WT], writes=[IS2])
        P.op("dve", "tensor_reduce", out=WAB[:, :, 0], in_=IS2[:], axis=AX.X, op=ALU.add, reads=[IS2], writes=[WAB])
        P.op("dve", "tensor_tensor", out=TMP[:], in0=TMP[:], in1=V[:], op=ALU.mult, reads=[TMP, V], writes=[TMP])
        P.op("dve", "tensor_tensor", out=V[:], in0=V[:], in1=TMP[:], op=ALU.subtract, reads=[V, TMP], writes=[V])
        P.op("dve", "tensor_reduce", out=MB[:], in_=V[:], axis=AX.X, op=ALU.max, reads=[V], writes=[MB])
        P.op("dve", "tensor_tensor", out=TMP[:], in0=V[:], in1=bc3(MB), op=ALU.is_equal, reads=[V, MB], writes=[TMP])
        P.op("dve", "tensor_tensor", out=IS2[:], in0=TMP[:], in1=WT[:], op=ALU.mult, reads=[TMP, WT], writes=[IS2])
        P.op("dve", "tensor_reduce", out=WAB[:, :, 1], in_=IS2[:], axis=AX.X, op=ALU.add, reads=[IS2], writes=[WAB])
        P.op("dve", "tensor_scalar", out=MA[:], in0=MA[:], scalar1=-1.0, scalar2=None, op0=ALU.add, reads=[MA], writes=[MA])
        P.op("dve", "tensor_scalar", out=MB[:], in0=MB[:], scalar1=-1.0, scalar2=None, op0=ALU.add, reads=[MB], writes=[MB])
        P.op("dve", "tensor_copy", out=SAB[:, 0, :], in_=MA[:], reads=[MA], writes=[SAB])
        P.op("dve", "tensor_copy", out=SAB[:, 1, :], in_=MB[:], reads=[MB], writes=[SAB])
        CJ = P.sb("CJ", [128, NBLK, 8])
        P.op("dve", "tensor_tensor", out=CJ[:], in0=PEI[:].unsqueeze(1).broadcast_to([128, NBLK, 8]),
             in1=JJ.unsqueeze(2).broadcast_to([128, NBLK, 8]), op=ALU.is_le, reads=[PEI, cst], writes=[CJ])
        EJ = P.sb("EJ", [128, NBLK])
        P.op("dve", "tensor_reduce", out=EJ[:], in_=CJ[:], axis=AX.X, op=ALU.add, reads=[CJ], writes=[EJ])
        P.op("dve", "tensor_scalar", out=EJ[:], in0=EJ[:], scalar1=7.0, scalar2=None, op0=ALU.min, reads=[EJ], writes=[EJ])
        IGf = P.sb("IGf", [128, NBLK, 2])
        P.op("dve", "scalar_tensor_tensor", out=IGf[:], in0=EJ[:].unsqueeze(2).broadcast_to([128, NBLK, 2]), scalar=256.0,
             in1=CG.unsqueeze(1).broadcast_to([128, NBLK, 2]), op0=ALU.mult, op1=ALU.add, reads=[EJ, cst], writes=[IGf])
        P.op("dve", "tensor_copy", out=IDXG[:], in_=IGf[:], reads=[IGf], writes=[IDXG])
        IDf = P.sb("IDf", [128, NBLK, 2])
        P.op("dve", "scalar_tensor_tensor", out=IDf[:], in0=EJ[:].unsqueeze(2).broadcast_to([128, NBLK, 2]), scalar=256.0,
             in1=CD.unsqueeze(1).broadcast_to([128, NBLK, 2]), op0=ALU.mult, op1=ALU.add, reads=[EJ, cst], writes=[IDf])
        P.op("dve", "tensor_copy", out=IDXD[:], in_=IDf[:], reads=[IDf], writes=[IDXD])
        ini = P.sb("ini", [128, NSLOT // 128], I32)
        P.op("dve", "memset", ap=ini[:], constant=DUMMY, writes=[ini])
        P.dma("sp", G.tokidx[:, 0].rearrange("(p c) -> p c", c=NSLOT // 128), ini[:], ini, reads=[ini], writes=[G.tokidx])
        tid = P.sb("tid", [128, 32], I32)
        P.op("dve", "tensor_copy", out=tid[:], in_=TIDc, reads=[cst], writes=[tid])
        for i in range(32):
            for ab in range(2):
                P.dma("pool", None, None, tid, reads=[tid, SAB], writes=[G.tokidx], meth="indirect_dma_start",
                      out=G.tokidx[:, :], out_offset=IOA(ap=SAB[:, ab, i:i + 1], axis=0), in_=tid[:, i:i + 1], in_offset=None)
        _phaseA.__exit__(None, None, None)
        WG2 = G.moe_wg[:, :]
        WU2 = G.moe_wu[:, :]
        WD2 = G.moe_wd[:, :]
        with P.scope():
            wg = [P.sb(f"swg{i}", [128, 8, 1408], BF16) for i in range(2)]
            wu = [P.sb(f"swu{i}", [128, 8, 1408], BF16) for i in range(2)]
            wd = [P.sb(f"swd{i}", [128, 11, D], BF16) for i in range(2)]
            idx = [P.sb(f"sidx{i}", [128, 1], I32) for i in range(8)]
            xg = [P.sb(f"sxg{i}", [128, D]) for i in range(4)]
            xTs = [P.sb(f"sxT{i}", [128, SUBS, 8, 512], BF16) for i in range(2)]
            sg = [P.sb(f"ssg{i}", [128, 512]) for i in range(2)]
            act = P.sb("sact", [128, 11, 512], BF16)
            yt = [P.sb(f"syt{i}", [128, D]) for i in range(3)]
            yi = 0
            gstate = {"gi": 0}

            def emit_gather(j):
                xT = xTs[j % 2]
                gi = gstate["gi"]
                for sub in range(SUBS):
                    for c in range(4):
                        ix = idx[gi % 8]
                        X_ = xg[gi % 4]
                        gi += 1
                        r0 = j * BS + sub * 512 + c * 128
                        P.dma("sp", ix[:], G.tokidx[r0:r0 + 128, :], ix, reads=[G.tokidx], writes=[ix])
                        P.dma("pool", None, None, X_, reads=[ix, G.f_tok], writes=[X_], meth="indirect_dma_start",
                              out=X_[:], out_offset=None, in_=G.f_tok[:, :], in_offset=IOA(ap=ix[:, 0:1], axis=0))
                        for k in range(8):
                            ps = bank[4 + (gi % 2) * 2 + k // 4]
                            P.op("pe", "transpose", out=ps[:, (k % 4) * 128:(k % 4 + 1) * 128], in_=X_[:, k * 128:(k + 1) * 128],
                                 identity=G.identf[:], reads=[X_, G.identf], writes=[ps])
                        for kh in range(2):
                            ps = bank[4 + (gi % 2) * 2 + kh]
                            P.op("act" if kh else "dve", "activation" if kh else "tensor_copy",
                                 out=xT[:, sub, kh * 4:(kh + 1) * 4, c * 128:(c + 1) * 128],
                                 in_=ps[:, 0:512].rearrange("p (k t) -> p k t", k=4), reads=[ps], writes=[xT],
                                 **({"func": AF.Copy} if kh else {}))
                gstate["gi"] = gi

            emit_gather(0)
            for j in range(nblk):
                xT = xTs[j % 2]
                for hf in range(2):
                    s = (2 * j + hf) % 2
                    if not (os.environ.get("NO_WGATHER") and j > 0):
                      P.dma("pool", None, None, wg[s], reads=[IDXG, G.moe_wg], writes=[wg[s]], meth="indirect_dma_start",
                          out=wg[s][:].rearrange("p k c -> p (k c)"), out_offset=None, in_=WG2, in_offset=IOA(ap=IDXG[:, j, hf:hf + 1], axis=0))
                    if not (os.environ.get("NO_WGATHER") and j > 0):
                      P.dma("pool", None, None, wu[s], reads=[IDXG, G.moe_wu], writes=[wu[s]], meth="indirect_dma_start",
                          out=wu[s][:].rearrange("p k c -> p (k c)"), out_offset=None, in_=WU2, in_offset=IOA(ap=IDXG[:, j, hf:hf + 1], axis=0))
                    if not (os.environ.get("NO_WGATHER") and j > 0):
                      P.dma("pool", None, None, wd[s], reads=[IDXD, G.moe_wd], writes=[wd[s]], meth="indirect_dma_start",
                          out=wd[s][:].rearrange("p c n -> p (c n)"), out_offset=None, in_=WD2, in_offset=IOA(ap=IDXD[:, j, hf:hf + 1], axis=0))
                    for sub in range(SUBS):
                        for jc in range(11):
                            pg, pu = bank[jc % 2], bank[2 + jc % 2]
                            cs = slice(jc * 128, (jc + 1) * 128)
                            for k in range(8):
                                mm(P, pg, pg[:, 0:512], wg[s], wg[s][:, k, cs], xT, xT[:, sub, k, :], start=(k == 0), stop=(k == 7))
                            for k in range(8):
                                mm(P, pu, pu[:, 0:512], wu[s], wu[s][:, k, cs], xT, xT[:, sub, k, :], start=(k == 0), stop=(k == 7))
                            S_ = sg[jc % 2]
                            P.op("act", "activation", out=S_[:], in_=pg[:, 0:512], func=AF.Silu, reads=[pg], writes=[S_])
                            P.op("dve", "tensor_tensor", out=act[:, jc, :], in0=S_[:], in1=pu[:, 0:512], op=ALU.mult,
                                 reads=[S_, pu], writes=[act])
                        if hf == 0 and sub == SUBS - 1 and j + 1 < nblk:
                            emit_gather(j + 1)
                        for tt in range(4):
                            Y_ = yt[yi % 3]
                            yi += 1
                            for half in range(2):
                                po = bank[4 + (tt * 2 + half) % 4]
                                for jc in range(11):
                                    mm(P, po, po[:, 0:512], act, act[:, jc, tt * 128:(tt + 1) * 128], wd[s],
                                       wd[s][:, jc, half * 512:(half + 1) * 512], start=(jc == 0), stop=(jc == 10))
                                hs = slice(half * 512, (half + 1) * 512)
                                if half == 0:
                                    P.op("act", "activation", out=Y_[:, hs], in_=po[:, 0:512], func=AF.Copy, reads=[po], writes=[Y_])
                                else:
                                    P.op("dve", "tensor_copy", out=Y_[:, hs], in_=po[:, 0:512], reads=[po], writes=[Y_])
                            r0 = j * BS + sub * 512 + tt * 128
                            P.dma("sp", G.Yb[r0:r0 + 128, hf, :], Y_[:], Y_, reads=[Y_], writes=[G.Yb])
        with P.scope():
            gts = load_gates(P, G, 1, 5)
            gf = P.sb("gfin", [128, D])
            P.dma("sp", gf[:], G.norm_final[0:1, :].partition_broadcast(128), gf, reads=[G.norm_final], writes=[gf])
            ya = [P.sb(f"cya{i}", [128, D]) for i in range(2)]
            yb = [P.sb(f"cyb{i}", [128, D]) for i in range(2)]
            yy = [P.sb(f"cyy{i}", [128, 2, D]) for i in range(2)]
            zz = [P.sb(f"czz{i}", [128, 2, D]) for i in range(2)]
            xt = [P.sb(f"cxt{i}", [128, D]) for i in range(2)]
            sq = P.sb("csq", [128, D], BF16)
            ss = [P.sb(f"css{i}", [128, 1]) for i in range(2)]
            for b in range(NB):
                for tt in range(16):
                    i = b * 16 + tt
                    s = i % 2
                    for (dst_, ab) in ((yy[s], 0), (zz[s], 1)):
                        P.dma("pool", None, None, dst_, reads=[SAB, G.Yb], writes=[dst_], meth="indirect_dma_start",
                              out=dst_[:].rearrange("p h d -> p (h d)"), out_offset=None, in_=G.Yb[:].rearrange("n h d -> n (h d)"),
                              in_offset=IOA(ap=SAB[:, ab, i:i + 1], axis=0))
                    P.op("dve", "tensor_tensor", out=ya[s][:], in0=yy[s][:, 0, :], in1=yy[s][:, 1, :], op=ALU.add, reads=[yy[s]], writes=[ya[s]])
                    P.op("dve", "tensor_tensor", out=yb[s][:], in0=zz[s][:, 0, :], in1=zz[s][:, 1, :], op=ALU.add, reads=[zz[s]], writes=[yb[s]])
                    P.dma("sp", xt[s][:], G.x3[b, tt * 128:(tt + 1) * 128, :], xt[s], reads=[G.x3], writes=[xt[s]])
                    P.op("dve", "tensor_scalar", out=ya[s][:], in0=ya[s][:], scalar1=WAB[:, i, 0:1], scalar2=None, op0=ALU.mult,
                         reads=[ya[s], WAB], writes=[ya[s]])
                    P.op("dve", "scalar_tensor_tensor", out=ya[s][:], in0=yb[s][:], scalar=WAB[:, i, 1:2], in1=ya[s][:],
                         op0=ALU.mult, op1=ALU.add, reads=[ya[s], yb[s], WAB], writes=[ya[s]])
                    P.op("dve", "tensor_tensor", out=ya[s][:], in0=ya[s][:], in1=gts[b][:], op=ALU.mult, reads=[ya[s], gts[b]], writes=[ya[s]])
                    P.op("dve", "tensor_tensor", out=xt[s][:], in0=xt[s][:], in1=ya[s][:], op=ALU.add, reads=[xt[s], ya[s]], writes=[xt[s]])
                    P.op("act", "activation", out=sq[:], in_=xt[s][:], func=AF.Square, accum_out=ss[s][:], reads=[xt[s]], writes=[sq, ss[s]])
                    P.op("act", "activation", out=ss[s][:], in_=ss[s][:], func=AF.Sqrt, scale=1.0 / D, bias=G.eps[:, 0:1],
                         reads=[ss[s], G.eps], writes=[ss[s]])
                    P.op("dve", "reciprocal", out=ss[s][:], in_=ss[s][:], reads=[ss[s]], writes=[ss[s]])
                    P.op("dve", "scalar_tensor_tensor", out=yb[s][:], in0=xt[s][:], scalar=ss[s][:, 0:1], in1=gf[:], op0=ALU.mult,
                         op1=ALU.mult, reads=[xt[s], ss[s], gf], writes=[yb[s]])
                    P.dma("sp", G.out[b, tt * 128:(tt + 1) * 128, :], yb[s][:], yb[s], reads=[yb[s]], writes=[G.out])


def stage_norm_tok(P, G):
    with P.scope():
        grow = P.sb("grow", [128, D])
        P.dma("sp", grow[:], G.norm_ffn_row[1:2, :].partition_broadcast(128), grow, reads=[G.norm_ffn_row], writes=[grow])
        GSb, SHb = [], []
        for j in range(2):
            g_ = P.sb(f"GSb{j}", [128, D])
            P.dma("sp", g_[:], G.modD[1, j:j + 1, 4 * D:5 * D].partition_broadcast(128), g_, reads=[G.modD], writes=[g_])
            P.op("dve", "scalar_tensor_tensor", out=g_[:], in0=g_[:], scalar=1.0, in1=grow[:], op0=ALU.add, op1=ALU.mult,
                 reads=[g_, grow], writes=[g_])
            GSb.append(g_)
            h_ = P.sb(f"SHb{j}", [128, D])
            P.dma("sp", h_[:], G.modD[1, j:j + 1, 3 * D:4 * D].partition_broadcast(128), h_, reads=[G.modD], writes=[h_])
            SHb.append(h_)
        z = P.sb("zrow", [128, D])
        P.op("dve", "memset", ap=z[:], constant=0.0, writes=[z])
        P.dma("sp", G.f_tok[NB * TL:NB * TL + 128, :], z[:], z, reads=[z], writes=[G.f_tok])
        rw = P.sb("rw", [128, 8, 8])
        P.dma("sp", rw[:], G.moe_router[:].rearrange("(k p) e -> p k e", p=128), rw, reads=[G.moe_router], writes=[rw])
        rb = P.sb("rb", [128, 8])
        P.dma("sp", rb[:], G.moe_router_b[0:1, :].partition_broadcast(128), rb, reads=[G.moe_router_b], writes=[rb])
        fT = [P.sb(f"nfT{i}", [128, 8, 128]) for i in range(2)]
        xt = [P.sb(f"nxt{i}", [128, D]) for i in range(2)]
        sq = P.sb("nsq", [128, D], BF16)
        ss = [P.sb(f"nss{i}", [128, 1]) for i in range(2)]
        fo = [P.sb(f"nfo{i}", [128, D]) for i in range(2)]
        it = 0
        for b in range(NB):
            for tt in range(16):
                s = it % 2
                it += 1
                P.dma("sp", xt[s][:], G.x3[b, tt * 128:(tt + 1) * 128, :], xt[s], reads=[G.x3], writes=[xt[s]])
                P.op("act", "activation", out=sq[:], in_=xt[s][:], func=AF.Square, accum_out=ss[s][:], reads=[xt[s]], writes=[sq, ss[s]])
                P.op("act", "activation", out=ss[s][:], in_=ss[s][:], func=AF.Sqrt, scale=1.0 / D, bias=G.eps[:, 0:1],
                     reads=[ss[s], G.eps], writes=[ss[s]])
                P.op("dve", "reciprocal", out=ss[s][:], in_=ss[s][:], reads=[ss[s]], writes=[ss[s]])
                P.op("dve", "scalar_tensor_tensor", out=fo[s][:], in0=xt[s][:], scalar=ss[s][:, 0:1], in1=GSb[b][:], op0=ALU.mult,
                     op1=ALU.mult, reads=[xt[s], ss[s], GSb[b]], writes=[fo[s]])
                P.op("dve", "tensor_tensor", out=fo[s][:], in0=fo[s][:], in1=SHb[b][:], op=ALU.add, reads=[fo[s], SHb[b]], writes=[fo[s]])
                P.dma("pool", G.f_tok[b * TL + tt * 128:b * TL + (tt + 1) * 128, :], fo[s][:], fo[s], reads=[fo[s]], writes=[G.f_tok])
                tix = b * 16 + tt
                for k in range(8):
                    ps = G.bank[(it % 2) * 2 + k // 4]
                    P.op("pe", "transpose", out=ps[:, (k % 4) * 128:(k % 4 + 1) * 128], in_=fo[s][:, k * 128:(k + 1) * 128],
                         identity=G.identf[:], reads=[fo[s], G.identf], writes=[ps])
                for kh in range(2):
                    ps = G.bank[(it % 2) * 2 + kh]
                    if kh:
                        P.op("act", "activation", out=fT[s][:, 4:8, :], in_=ps[:, 0:512].rearrange("p (k t) -> p k t", k=4),
                             func=AF.Copy, reads=[ps], writes=[fT[s]])
                    else:
                        P.op("dve", "tensor_copy", out=fT[s][:, 0:4, :], in_=ps[:, 0:512].rearrange("p (k t) -> p k t", k=4),
                             reads=[ps], writes=[fT[s]])
                pl = G.bank[4 + it % 2]
                for k in range(8):
                    mm(P, pl, pl[:, 0:8], fT[s], fT[s][:, k, :], rw, rw[:, k, :], start=(k == 0), stop=(k == 7))
                P.op("dve", "tensor_tensor", out=G.LG[:, tix, :], in0=pl[:, 0:8], in1=rb[:], op=ALU.add, reads=[pl, rb], writes=[G.LG])


def build(upto="all", dbg=()):
    nc = bass.Bass("TRN2", target_bir_lowering=False)
    P = Prog(nc)
    G = Ctx(P)
    nc.G = G
    kinds = lambda n: "ExternalOutput" if n in dbg else "Internal"
    S = lambda n, s, dt=F32: P.dram(n, s, dt, kind=kinds(n))
    G.out = P.dram("out", [NB, TL, D], F32, kind="ExternalOutput")
    G.hT0 = S("hT0", [NB, 128, 8, TT], BF16)
    G.RS = [S(f"RS{d}", [NB, 8, 64, 2, W], BF16) for d in range(2)]
    G.BK = [S(f"BK{d}", [NB, 8, 64, 2, W], BF16) for d in range(2)]
    G.BKt = [S(f"BKt{d}", [NB, 18, 128, 2, 512], BF16) for d in range(2)]
    G.GL = [S(f"GL{d}", [NB, 8, 64, 18]) for d in range(2)]
    G.Vt = S("Vt", [NB, 18, 128, 512], BF16)
    G.gT = S("gT", [NB, 4, 128, W])
    G.vT = S("vT", [NB, 4, 128, W])
    G.bcT = S("bcT", [NB, 4, 128, W])
    G.oA = [S(f"oA{d}", [NB, 18, 128, 512]) for d in range(2)]
    G.yT = S("yT", [NB, 8, 128, TT], BF16)
    G.x1 = S("x1", [NB, TT, D])
    G.x2 = S("x2", [NB, TT, D])
    G.fT0 = S("fT0", [NB, 128, 8, TT], BF16)
    G.hT1 = S("hT1", [NB, 128, 8, TT], BF16)
    G.qT = S("qT", [NB, 16, 64, TL], BF16)
    G.kT = S("kT", [NB, 4, 64, TT], BF16)
    G.Vt1 = S("Vt1", [NB, 18, 128, 256], BF16)
    G.oT1 = S("oT1", [NB, 8, 128, TL], BF16)
    G.x3 = S("x3", [NB, TL, D])
    G.x4 = S("x4", [NB, TL, D])
    G.fT1 = S("fT1", [NB, 128, 8, TL], BF16)
    G.fT1f = S("fT1f", [NB, 128, 8, TL], F32)
    G.f_tok = S("f_tok", [NB * TL + 128, D])
    G.Yb = S("Yb", [NSLOT, 2, D])
    G.tokidx = P.dram("tokidx", [NSLOT, 1], I32, kind=kinds("tokidx"))
    G.HQK = [S(f"HQK{d}", [NB, 4, 128, 2, TT], BF16) for d in range(2)]
    G.HKt = [S(f"HKt{d}", [NB, 18, 128, 512], BF16) for d in range(2)]
    G.HGL = [S(f"HGL{d}", [NB, 4, 128, 36]) for d in range(2)]
    G.HVt = S("HVt", [NB, 18, 128, 512], BF16)
    G.HGt = S("HGt", [NB, 18, 128, 512], BF16)
    G.oH = [S(f"oH{d}", [NB, 18, 128, 512]) for d in range(2)]
    setup_globals(P, G)
    G.modD = S("modD", [2, 3, 6 * D])
    outs = [G.out]

    def done():
        pass
        with P.scope():
            pass
        P.emit()
        return nc

    stage_adaln(P, G)
    if upto == "adaln":
        return done()
    if upto.startswith("l1"):
        G.x2 = P.dram("x2in", [NB, TT, D], F32, kind="ExternalInput")
        G.used_inputs.append("x2in")
        stage_norm(P, G, G.x2, TT, 1, 0, G.hT1, TC)
        if upto == "l1n":
            return done()
        for b in range(NB):
            stage_att_inproj(P, G, b)
        if upto == "l1i":
            return done()
        stage_attention(P, G)
        if upto == "l1t":
            return done()
        stage_outproj(P, G, G.oT1, G.att_w_out, 1, TL, 0, lambda b, tt: G.x2[b, TC + tt * 128:TC + (tt + 1) * 128, :],
                      lambda b, tt: G.x3[b, tt * 128:(tt + 1) * 128, :], G.x2, G.x3)
        if upto == "l1a":
            return done()
        if upto == "l1d":
            stage_norm(P, G, G.x3, TL, 1, 1, G.fT1, 0, dst32=G.fT1f)
            stage_router(P, G)
            stage_moe(P, G)
            stage_final(P, G, G.x4)
            return done()
        stage_norm_tok(P, G)
        stage_moe_sparse(P, G, nblk=(2 if upto == "l1s2" else NBLK))
        return done()
    stage_norm(P, G, G.xin, TT, 0, 0, G.hT0, TC)
    if upto == "norm0":
        return done()
    for b in range(NB if upto != "rwf1" else 1):
        stage_rwkv_feat(P, G, b)
    if upto in ("rwf", "rwf1"):
        return done()
    if upto == "hg1":
        stage_hgrn_feat(P, G, 0)
        stage_hgrn_scan(P, G, bs=(0,))
        return done()
    if upto == "rws1":
        stage_rwkv_scan(P, G, bs=(0,), ds=(0, 1), nmax=18)
        return done()
    stage_rwkv_scan(P, G)
    if upto == "rws":
        return done()
    stage_rwkv_post(P, G)
    if upto == "rwp":
        return done()
    for b in range(NB):
        stage_hgrn_feat(P, G, b)
    stage_hgrn_scan(P, G)
    stage_hgrn_post(P, G)
    if upto == "hgp":
        return done()
    stage_outproj(P, G, G.yT, G.rec_w_out, 0, TT, TC, lambda b, tt: G.xin[b, tt * 128:(tt + 1) * 128, :],
                  lambda b, tt: G.x1[b, tt * 128:(tt + 1) * 128, :], G.xin, G.x1)
    if upto == "x1":
        return done()
    stage_norm(P, G, G.x1, TT, 0, 1, G.fT0, TC)
    stage_ffn0(P, G)
    if upto == "x2":
        return done()
    stage_norm(P, G, G.x2, TT, 1, 0, G.hT1, TC)
    for b in range(NB):
        stage_att_inproj(P, G, b)
    stage_attention(P, G)
    stage_outproj(P, G, G.oT1, G.att_w_out, 1, TL, 0, lambda b, tt: G.x2[b, TC + tt * 128:TC + (tt + 1) * 128, :],
                  lambda b, tt: G.x3[b, tt * 128:(tt + 1) * 128, :], G.x2, G.x3)
    if upto == "x3":
        return done()
    if upto == "dense":
        stage_norm(P, G, G.x3, TL, 1, 1, G.fT1, 0, dst32=G.fT1f)
        stage_router(P, G)
        stage_moe(P, G)
        stage_final(P, G, G.x4)
        return done()
    stage_norm_tok(P, G)
    stage_moe_sparse(P, G)
    return done()


def host_prep(inp, core):
    f = lambda a: np.ascontiguousarray(a, dtype=np.float32)
    b0 = core * NB
    m = {}
    m["xin"] = f(np.concatenate([inp["ctx"][b0:b0 + NB], inp["x"][b0:b0 + NB]], axis=1))
    cv = np.stack([inp["c"][b0], inp["c"][b0 + 1], inp["c_ctx"]], axis=-1)
    m["cT"] = f(cv.reshape(8, 128, 3).transpose(1, 0, 2))
    m["mod_w"] = f(inp["mod_w"])
    m["mod_b"] = f(inp["mod_b"].reshape(2, 48, 128).transpose(0, 2, 1))
    m["norm_mix"] = f(inp["norm_mix"].reshape(2, 8, 128).transpose(0, 2, 1))
    m["norm_ffn"] = f(inp["norm_ffn"].reshape(2, 8, 128).transpose(0, 2, 1))
    m["norm_final"] = f(inp["norm_final"].reshape(1, D))
    m["ident"] = np.eye(128, dtype=np.float32)
    m["rec_w_in"] = f(inp["rec_w_in"][0])
    m["rec_w_out"] = f(inp["rec_w_out"][0])
    fm = lambda v: np.asarray(v, np.float32).reshape(4, 128).T
    rv = np.zeros((128, 4, 12), np.float32)
    for i, v in enumerate([inp["rwkv_k_k"][0], inp["rwkv_k_a"][0], None, inp["rwkv_a0"][0],
                           inp["rwkv_r_k"][0].reshape(-1), inp["rwkv_ln_w"][0], inp["rwkv_ln_b"][0],
                           inp["rwkv_w0"][0, 0], inp["rwkv_w0"][0, 1]]):
        if v is not None:
            rv[:, :, i] = fm(v)
    m["rw_vec"] = rv
    m["rw_mu"] = f(inp["rwkv_mu"][0].reshape(14, 128).T)
    m["rw_w_up"] = f(inp["rwkv_w_up"][0])
    m["rw_a_up"] = f(inp["rwkv_a_up"][0])
    m["rw_g_up"] = f(inp["rwkv_g_up"][0])
    m["ffn_wg"] = f(inp["ffn_w_gate"][0])
    m["ffn_wu"] = f(inp["ffn_w_up"][0])
    m["ffn_wd"] = f(inp["ffn_w_down"][0])
    m["att_w_in"] = f(inp["att_w_in"][0])
    m["att_w_out"] = f(inp["att_w_out"][0])
    m["att_sink"] = f(inp["att_sink"][0].reshape(1, 16))
    m["moe_router"] = f(inp["moe_router"][0])
    m["moe_router_b"] = f(inp["moe_router_b"][0].reshape(1, 8))
    m["moe_wg"], m["moe_wu"], m["moe_wd"] = _moe_layout(inp)
    m["norm_ffn_row"] = f(inp["norm_ffn"])
    m["hg_lb"] = f(inp["hgrn_lb"].reshape(2, 2, 4, 128).transpose(0, 1, 3, 2))
    m["hg_norm"] = f(inp["hgrn_norm"][0].reshape(1, 128))
    m.update(CONSTS)
    return m


def _consts():
    c = {}
    blk = np.zeros((128, 128), np.float32)
    blk[:64, :64] = 1
    blk[64:, 64:] = 1
    c["blk64"] = blk
    s_ = np.arange(128)[:, None]
    t_ = np.arange(128)[None, :]
    c["masks"] = np.stack([s_ < t_, s_ <= t_, s_ > t_, s_ >= t_], 1).astype(np.float32)
    sm = np.ones((128, W), np.float32)
    for n in range(18):
        sm[:, pcol(n * 128)] = 0.0
    c["scanmask"] = sm
    t = np.arange(TL)
    row = (t // 64).astype(np.float32)
    col = (t % 64).astype(np.float32)
    inv = (10000.0 ** (-np.arange(0, 32, 2, dtype=np.float32) / 32)).astype(np.float32)
    rope = np.zeros((128, 2, TL), np.float32)
    perm = np.zeros((128, 128), np.float32)
    for p in range(128):
        dd = p % 64
        axis, half, jj = dd // 32, (dd % 32) // 16, dd % 16
        ang = ((row if axis == 0 else col) * inv[jj]).astype(np.float32)
        rope[p, 0] = np.cos(ang)
        rope[p, 1] = np.sin(ang) * (-1.0 if half == 0 else 1.0)
        sw = p + 16 if half == 0 else p - 16
        perm[sw, p] = 1.0
    c["rope"] = rope
    c["perm"] = perm
    mc = np.zeros((128, 512), np.float32)
    p_ = np.arange(128)[:, None]
    mc[:, 0:64] = np.tile(np.arange(8) * float(BS), 8)[None, :]
    mc[:, 64:88] = np.arange(24)[None, :] + 0.0
    mc[:, 88:90] = 2 * p_ + np.arange(2)[None, :]
    mc[:, 104:106] = np.arange(2)[None, :] * 128 + p_
    mc[:, 128:160] = np.arange(32)[None, :] * 128 + p_
    rm = np.ones((8, 32), np.float32)
    rm[:, 0] = 0.0
    mc[:, 256:512] = rm.reshape(1, 256)
    c["moe_consts"] = mc
    return c


CONSTS = _consts()


_MOE_CACHE = {}


def _moe_layout(inp):
    key = id(inp["moe_w_gate"])
    if key not in _MOE_CACHE:
        _MOE_CACHE.clear()
        def gu(w):
            w = np.asarray(w[0], np.float32).reshape(8, 8, 128, 2, 1408)
            return np.ascontiguousarray(w.transpose(0, 2, 3, 1, 4)).reshape(2048, 11264)
        wd = np.asarray(inp["moe_w_down"][0], np.float32).reshape(8, 2, 11, 128, 1024)
        wd = np.ascontiguousarray(wd.transpose(0, 1, 3, 2, 4)).reshape(2048, 11264)
        _MOE_CACHE[key] = (gu(inp["moe_w_gate"]), gu(inp["moe_w_up"]), wd)
    return _MOE_CACHE[key]


_NC_CACHE = {}


def kernel(**inp):
    inp = {k: np.asarray(v) for k, v in inp.items()}
    if "nc" not in _NC_CACHE:
        _NC_CACHE["nc"] = build()
    nc = _NC_CACHE["nc"]
    used = nc.G.used_inputs
    in_maps = []
    for c in range(8):
        m = host_prep(inp, c)
        in_maps.append({k: m[k] for k in used})
    res = run_bass_kernel_spmd(nc, in_maps, core_ids=list(range(8)))
    out = np.concatenate([r["out"] for r in res.results], axis=0)
    return out.astype(np.float32)
```

```python
import contextlib
import os
import numpy as np
import concourse.bass as bass
import concourse.mybir as mybir
from concourse.bass_utils import run_bass_kernel_spmd

F32 = mybir.dt.float32
BF16 = mybir.dt.bfloat16
AF = mybir.ActivationFunctionType
ALU = mybir.AluOpType
AX = mybir.AxisListType

NB = 2
TC = 256
TL = 2048
TT = TC + TL
D = 1024
W = TT + 4
C0 = float(np.exp(-0.5))


def pcol(t):
    return 1 + t if t < TC else 3 + t


class Tk:
    __slots__ = ("h", "name", "w", "r", "sem", "cnt", "psum")

    def __init__(self, h, name, psum=False):
        self.h = h
        self.name = name
        self.psum = psum
        self.w = {}
        self.r = {}
        self.sem = None
        self.cnt = 0

    def __getitem__(self, idx):
        return self.h[idx]


class Prog:
    ENGS = ("pe", "act", "dve", "pool", "sp")

    def __init__(self, nc):
        self.nc = nc
        self.q = {e: [] for e in self.ENGS}
        self.seq = {e: 0 for e in self.ENGS}
        self.sem = {e: nc.alloc_semaphore("s_" + e) for e in self.ENGS}
        self.waited = {e: {} for e in self.ENGS}
        self.n = 0
        self.uid = 0
        self._stack = None
        self._scope_tiles = []
        self.free_dsems = {}
        self.dtiles = {}

    def _nm(self, name):
        self.uid += 1
        return f"{name}_{self.uid}"

    def sb(self, name, shape, dt=F32):
        nm = self._nm(name)
        if self._stack is not None:
            h = self._stack.enter_context(self.nc.sbuf_tensor(nm, list(shape), dt))
        else:
            h = self.nc.alloc_sbuf_tensor(nm, list(shape), dt)
        t = Tk(h, nm)
        if self._scope_tiles:
            self._scope_tiles[-1].append(t)
        return t

    def ps(self, name, shape, dt=F32):
        nm = self._nm(name)
        return Tk(self.nc.alloc_psum_tensor(nm, list(shape), dt), nm, psum=True)

    def view(self, ap, name):
        return Tk(ap, self._nm(name))

    def dram(self, name, shape, dt=F32, kind="Internal"):
        return Tk(self.nc.dram_tensor(name, list(shape), dt, kind=kind), name)

    @contextlib.contextmanager
    def scope(self):
        st = contextlib.ExitStack()
        prev = self._stack
        self._stack = st
        self._scope_tiles.append([])
        try:
            yield
        finally:
            self.barrier()
            for t in self._scope_tiles.pop():
                if t.sem:
                    for e, sc in t.sem.items():
                        self.free_dsems.setdefault(e, []).append(sc)
                    self.dtiles.pop(id(t), None)
                    t.sem = None
            st.close()
            self._stack = prev

    def _wait(self, eng, tok):
        if tok[0] == "e":
            _, f, s = tok
            if f == eng and eng == "pe":
                return
            key = ("e", f)
            if self.waited[eng].get(key, 0) >= s:
                return
            self.waited[eng][key] = s
            self.q[eng].append(("w", self.sem[f], s))
        else:
            _, t, de = tok
            if not t.sem or de not in t.sem:
                return
            sem, cnt = t.sem[de]
            key = ("d", id(sem))
            if self.waited[eng].get(key, 0) >= cnt:
                return
            self.waited[eng][key] = cnt
            self.q[eng].append(("w", sem, cnt))

    def _deps(self, eng, reads, writes):
        for t in reads:
            for tok in t.w.values():
                self._wait(eng, tok)
            if t.psum:
                for tok in t.r.values():
                    if not (tok[0] == "e" and tok[1] == eng):
                        self._wait(eng, tok)
        for t in writes:
            for tok in t.w.values():
                self._wait(eng, tok)
            for tok in t.r.values():
                self._wait(eng, tok)

    @staticmethod
    def _key(tok):
        return (tok[0], tok[1]) if tok[0] == "e" else (tok[0], id(tok[1]), tok[2])

    def _commit(self, tok, reads, writes):
        k = self._key(tok)
        for t in reads:
            t.r[k] = tok
        for t in writes:
            t.w[k] = tok

    def op(self, eng, meth, reads=(), writes=(), **kw):
        self._deps(eng, reads, writes)
        self.seq[eng] += 1
        tok = ("e", eng, self.seq[eng])
        self.q[eng].append(("o", meth, kw))
        self._commit(tok, reads, writes)
        self.n += 1
        return tok

    def dma(self, eng, out_ap, in_ap, semt, reads=(), writes=(), meth="dma_start", fn=None, **kw):
        self._deps(eng, reads, writes)
        t = semt
        if t.sem is None:
            t.sem = {}
        if eng not in t.sem:
            fl = self.free_dsems.get(eng)
            if fl:
                t.sem[eng] = fl.pop()
            else:
                t.sem[eng] = [self.nc.alloc_semaphore(f"d_{eng}_{t.name}"), 0]
            self.dtiles[id(t)] = t
        t.sem[eng][1] += 16
        if fn is not None:
            self.q[eng].append(("f", fn, t.sem[eng][0]))
        elif meth == "dma_start":
            self.q[eng].append(("d", out_ap, in_ap, t.sem[eng][0], kw))
        else:
            self.q[eng].append(("m", meth, kw, t.sem[eng][0]))
        tok = ("d", t, eng)
        self._commit(tok, reads, writes)
        self.n += 1
        return tok

    def barrier(self):
        for f in self.ENGS:
            if f != "sp" and self.seq[f] > 0:
                self._wait("sp", ("e", f, self.seq[f]))
        for t in list(self.dtiles.values()):
            for de in list(t.sem.keys()):
                self._wait("sp", ("d", t, de))
        self.seq["sp"] += 1
        self.q["sp"].append(("o", "nop", {}))
        tok = ("e", "sp", self.seq["sp"])
        for e in self.ENGS:
            if e != "sp":
                self._wait(e, tok)

    def emit(self):
        nc = self.nc
        q = self.q
        sems = self.sem

        def run(e, name):
            for it in q[name]:
                if it[0] == "w":
                    e.wait_ge(it[1], it[2])
                elif it[0] == "o":
                    getattr(e, it[1])(**it[2]).then_inc(sems[name], 1)
                elif it[0] == "f":
                    it[1](e).then_inc(it[2], 16)
                elif it[0] == "m":
                    getattr(e, it[1])(**it[2]).then_inc(it[3], 16)
                else:
                    _, o, i, s, kw = it
                    e.dma_start(out=o, in_=i, **kw).then_inc(s, 16)

        with nc.Block() as block:
            @block.tensor
            def _(e):
                run(e, "pe")

            @block.scalar
            def _(e):
                run(e, "act")

            @block.vector
            def _(e):
                run(e, "dve")

            @block.gpsimd
            def _(e):
                run(e, "pool")

            @block.sync
            def _(e):
                run(e, "sp")


def mm(P, o, oap, l, lap, r, rap, start=True, stop=True):
    P.op("pe", "matmul", reads=[l, r], writes=[o], out=oap, lhsT=lap, rhs=rap, start=start, stop=stop)


INPUT_SPECS = {
    "xin": [NB, TT, D], "cT": [128, 8, 3], "mod_w": [2, D, 6 * D], "mod_b": [2, 128, 48],
    "norm_mix": [2, 128, 8], "norm_ffn": [2, 128, 8], "norm_final": [1, D], "ident": [128, 128],
    "rec_w_in": [D, 4352], "rec_w_out": [D, D],
    "rw_vec": [128, 4, 12], "rw_mu": [128, 14], "rw_w_up": [2, 64, 512], "rw_a_up": [64, 512], "rw_g_up": [128, 512],
    "blk64": [128, 128], "masks": [128, 4, 128], "scanmask": [128, W],
    "hg_lb": [2, 2, 128, 4], "hg_norm": [1, 128],
    "ffn_wg": [D, 2816], "ffn_wu": [D, 2816], "ffn_wd": [2816, D],
    "att_w_in": [D, 1536], "att_w_out": [D, D], "att_sink": [1, 16],
    "rope": [128, 2, TL], "perm": [128, 128],
    "moe_router": [D, 8], "moe_router_b": [1, 8],
    "moe_consts": [128, 512], "norm_ffn_row": [2, D],
    "moe_wg": [2048, 11264], "moe_wu": [2048, 11264], "moe_wd": [2048, 11264],
}


class Ctx:
    def __init__(self, P):
        self.__dict__["P"] = P
        self.__dict__["used_inputs"] = []

    def __getattr__(self, name):
        if name in INPUT_SPECS:
            t = self.P.dram(name, INPUT_SPECS[name], F32, kind="ExternalInput")
            self.__dict__[name] = t
            self.used_inputs.append(name)
            return t
        raise AttributeError(name)


def setup_globals(P, G):
    G.bank = [P.ps(f"bank{i}", [128, 512], F32) for i in range(8)]
    G.identf = P.sb("identf", [128, 128], F32)
    P.dma("sp", G.identf[:], G.ident[:], G.identf, reads=[G.ident], writes=[G.identf])
    G.identb = P.sb("identb", [128, 128], BF16)
    P.op("dve", "tensor_copy", out=G.identb[:], in_=G.identf[:], reads=[G.identf], writes=[G.identb])
    G.eps = P.sb("eps", [128, 1], F32)
    P.op("dve", "memset", ap=G.eps[:], constant=1e-6, writes=[G.eps])
    G.WTm = P.sb("WTm", [128, 32, 8], F32)
    G.LG = P.sb("LGp", [128, 32, 8], F32)
    G.modT = P.sb("modT", [128, 2, 48, 3], F32)
    G.GS = P.sb("GS", [128, 2, 2, 8, 3], F32)


def stage_adaln(P, G):
    bank = G.bank
    with P.scope():
        cT = P.sb("cTs", [128, 8, 3])
        P.dma("sp", cT[:], G.cT[:], cT, reads=[G.cT], writes=[cT])
        sc = P.sb("sc", [128, 8, 3])
        P.op("act", "activation", out=sc[:], in_=cT[:], func=AF.Silu, reads=[cT], writes=[sc])
        mb = P.sb("mb", [128, 2, 48])
        P.dma("sp", mb[:], G.mod_b[:].rearrange("l p j -> p l j"), mb, reads=[G.mod_b], writes=[mb])
        gm = P.sb("gm", [128, 2, 2, 8])
        P.dma("sp", gm[:, :, 0, :], G.norm_mix[:].rearrange("l p k -> p l k"), gm, reads=[G.norm_mix], writes=[gm])
        P.dma("sp", gm[:, :, 1, :], G.norm_ffn[:].rearrange("l p k -> p l k"), gm, reads=[G.norm_ffn], writes=[gm])
        wts = [P.sb(f"mw{i}", [128, 8, 768]) for i in range(2)]
        it = 0
        for l in range(2):
            for g in range(8):
                wt = wts[it % 2]
                for k in range(8):
                    P.dma("sp" if k % 2 == 0 else "pool", wt[:, k, :],
                          G.mod_w[l, k * 128:(k + 1) * 128, g * 768:(g + 1) * 768], wt,
                          reads=[G.mod_w], writes=[wt])
                for jj in range(6):
                    j = g * 6 + jj
                    ps = bank[j % 2]
                    for k in range(8):
                        mm(P, ps, ps[:, 0:3], wt, wt[:, k, jj * 128:(jj + 1) * 128], sc, sc[:, k, :],
                           start=(k == 0), stop=(k == 7))
                    P.op("act", "activation", out=G.modT[:, l, j, :], in_=ps[:, 0:3], func=AF.Identity,
                         bias=mb[:, l, j:j + 1], reads=[ps, mb], writes=[G.modT])
                it += 1
        for l in range(2):
            for kind in range(2):
                wh = 1 + 3 * kind
                P.op("dve", "tensor_scalar", out=G.GS[:, l, kind, :, :], in0=G.modT[:, l, wh * 8:(wh + 1) * 8, :],
                     scalar1=1.0, scalar2=None, op0=ALU.add, reads=[G.modT], writes=[G.GS])
                P.op("dve", "tensor_tensor", out=G.GS[:, l, kind, :, :], in0=G.GS[:, l, kind, :, :],
                     in1=gm[:, l, kind, :].unsqueeze(2).broadcast_to([128, 8, 3]), op=ALU.mult,
                     reads=[G.GS, gm], writes=[G.GS])
        tr = P.sb("mtr", [48, 128])
        for l in range(2):
            for j in range(3):
                ps = bank[2 + (l * 3 + j) % 2]
                P.op("pe", "transpose", out=ps[0:48, 0:128], in_=G.modT[:, l, :, j], identity=G.identf[:],
                     reads=[G.modT, G.identf], writes=[ps])
                P.op("dve", "tensor_copy", out=tr[:], in_=ps[0:48, 0:128], reads=[ps], writes=[tr])
                P.dma("sp", G.modD[l, j, :].rearrange("(c p) -> c p", p=128), tr[:], tr, reads=[tr], writes=[G.modD])


def stage_norm(P, G, src, T, l, kind, dst, ctx_len, dst32=None):
    bank = G.bank
    with P.scope():
        xt = [P.sb(f"xt{i}", [128, D]) for i in range(4)]
        sq = P.sb("sq", [128, D], BF16)
        ss = [P.sb(f"ss{i}", [128, 1]) for i in range(4)]
        rs = [P.sb(f"rs{i}", [128, 1]) for i in range(4)]
        xn = [P.sb(f"xn{i}", [128, D]) for i in range(4)]
        hT = [P.sb(f"hT{i}", [128, 8, 128], BF16) for i in range(4)]
        hT32 = [P.sb(f"hTf{i}", [128, 8, 128], F32) for i in range(4)] if dst32 is not None else None
        i = 0
        for b in range(NB):
            for tt in range(T // 128):
                j = 2 if tt * 128 < ctx_len else b
                s = i % 4
                P.dma("sp", xt[s][:], src[b, tt * 128:(tt + 1) * 128, :], xt[s], reads=[src], writes=[xt[s]])
                P.op("act", "activation", out=sq[:], in_=xt[s][:], func=AF.Square, accum_out=ss[s][:],
                     reads=[xt[s]], writes=[sq, ss[s]])
                P.op("act", "activation", out=rs[s][:], in_=ss[s][:], func=AF.Sqrt, scale=1.0 / D, bias=G.eps[:, 0:1],
                     reads=[ss[s], G.eps], writes=[rs[s]])
                P.op("dve", "reciprocal", out=rs[s][:], in_=rs[s][:], reads=[rs[s]], writes=[rs[s]])
                P.op("dve", "tensor_scalar", out=xn[s][:], in0=xt[s][:], scalar1=rs[s][:, 0:1], scalar2=None,
                     op0=ALU.mult, reads=[xt[s], rs[s]], writes=[xn[s]])
                for k in range(8):
                    ps = bank[(i % 4) * 2 + k // 4]
                    pv = ps[:, (k % 4) * 128:(k % 4 + 1) * 128]
                    P.op("pe", "transpose", out=pv, in_=xn[s][:, k * 128:(k + 1) * 128], identity=G.identf[:],
                         reads=[xn[s], G.identf], writes=[ps])
                    if dst is not None:
                        if dst32 is None and k % 4 >= 2:
                            P.op("dve", "tensor_scalar", out=hT[s][:, k, :], in0=pv,
                                 scalar1=G.GS[:, l, kind, k, j:j + 1], scalar2=G.modT[:, l, 3 * kind * 8 + k, j:j + 1],
                                 op0=ALU.mult, op1=ALU.add, reads=[ps, G.GS, G.modT], writes=[hT[s]])
                        else:
                            P.op("act", "activation", out=hT[s][:, k, :], in_=pv, func=AF.Identity,
                                 scale=G.GS[:, l, kind, k, j:j + 1], bias=G.modT[:, l, 3 * kind * 8 + k, j:j + 1],
                                 reads=[ps, G.GS, G.modT], writes=[hT[s]])
                    if dst32 is not None:
                        if dst is None and k % 2:
                            P.op("act", "activation", out=hT32[s][:, k, :], in_=pv, func=AF.Identity,
                                 scale=G.GS[:, l, kind, k, j:j + 1], bias=G.modT[:, l, 3 * kind * 8 + k, j:j + 1],
                                 reads=[ps, G.GS, G.modT], writes=[hT32[s]])
                        else:
                            P.op("dve", "tensor_scalar", out=hT32[s][:, k, :], in0=pv,
                                 scalar1=G.GS[:, l, kind, k, j:j + 1], scalar2=G.modT[:, l, 3 * kind * 8 + k, j:j + 1],
                                 op0=ALU.mult, op1=ALU.add, reads=[ps, G.GS, G.modT], writes=[hT32[s]])
                if dst is not None:
                    P.dma("pool", dst[b, :, :, tt * 128:(tt + 1) * 128], hT[s][:], hT[s], reads=[hT[s]], writes=[dst])
                if dst32 is not None:
                    P.dma("pool", dst32[b, :, :, tt * 128:(tt + 1) * 128], hT32[s][:], hT32[s],
                          reads=[hT32[s]], writes=[dst32])
                i += 1


TBLK = [(0, 256), (256, 768), (768, 1280), (1280, 1792), (1792, 2304)]


def load_w_chunk(P, wt, src, c0, ncols, eng="pool"):
    P.dma(eng, wt[:, :, 0:ncols], src[:, c0:c0 + ncols].rearrange("(k p) n -> p k n", p=128), wt,
          reads=[src], writes=[wt])


class WPrefetch:
    def __init__(self, P, src, seq, nbuf=3, ncols=128):
        self.P, self.src, self.seq, self.ncols = P, src, list(seq), ncols
        self.bufs = [P.sb(f"wpf{i}", [128, 8, ncols], BF16) for i in range(nbuf)]
        self.i = 0
        self._issue(0)

    def _issue(self, i):
        if i < len(self.seq):
            load_w_chunk(self.P, self.bufs[i % len(self.bufs)], self.src, self.seq[i] * self.ncols, self.ncols)

    def get(self, chunk):
        assert self.seq[self.i] == chunk, (self.seq[self.i], chunk)
        wt = self.bufs[self.i % len(self.bufs)]
        self._issue(self.i + 1)
        self.i += 1
        return wt


def inproj_fm(P, G, hT, wt, out, padded=True, banks=(0, 1, 2, 3), evac="act", func=None):
    for bi, (t0, t1) in enumerate(TBLK):
        ps = G.bank[banks[bi % len(banks)]]
        n = t1 - t0
        for k in range(8):
            mm(P, ps, ps[:, 0:n], wt, wt[:, k, 0:128], hT, hT[:, k, t0:t1], start=(k == 0), stop=(k == 7))
        c0 = pcol(t0) if padded else t0
        if evac == "act":
            P.op("act", "activation", out=out[:, c0:c0 + n], in_=ps[:, 0:n], func=(func or AF.Copy), reads=[ps], writes=[out])
        else:
            P.op("dve", "tensor_copy", out=out[:, c0:c0 + n], in_=ps[:, 0:n], reads=[ps], writes=[out])


def tshift(P, raw, tmp, out, muh, omm, vec):
    P.op("pool", "tensor_tensor", out=tmp[:, 1:W - 1], in0=raw[:, 0:W - 2], in1=raw[:, 2:W], op=ALU.add,
         reads=[raw], writes=[tmp])
    P.op("dve", "tensor_scalar", out=tmp[:, 1:W - 1], in0=tmp[:, 1:W - 1], scalar1=muh, scalar2=None, op0=ALU.mult,
         reads=[tmp, vec], writes=[tmp])
    P.op("dve", "scalar_tensor_tensor", out=out[:, 1:W - 1], in0=raw[:, 1:W - 1], scalar=omm, in1=tmp[:, 1:W - 1],
         op0=ALU.mult, op1=ALU.add, reads=[raw, tmp, vec], writes=[out])


def chunk_views(ap):
    return (ap[:, 1:1 + TC].rearrange("p (n t) -> p n t", t=128),
            ap[:, 3 + TC:3 + TT].rearrange("p (n t) -> p n t", t=128))


def stage_rwkv_feat(P, G, b):
    bank = G.bank
    with P.scope():
        hT = P.sb("hTres", [128, 8, TT], BF16)
        for k in range(8):
            P.dma("sp", hT[:, k, :], G.hT0[b, :, k, :], hT, reads=[G.hT0], writes=[hT])
        vec = P.sb("rwvec", [128, 4, 12])
        P.dma("sp", vec[:], G.rw_vec[:], vec, reads=[G.rw_vec], writes=[vec])
        mu = P.sb("rwmu", [128, 3, 14])
        P.dma("sp", mu[:, 0, :], G.rw_mu[:], mu, reads=[G.rw_mu], writes=[mu])
        P.op("dve", "tensor_scalar", out=mu[:, 1, :], in0=mu[:, 0, :], scalar1=0.5, scalar2=None, op0=ALU.mult,
             reads=[mu], writes=[mu])
        P.op("dve", "tensor_scalar", out=mu[:, 2, :], in0=mu[:, 0, :], scalar1=-1.0, scalar2=1.0, op0=ALU.mult,
             op1=ALU.add, reads=[mu], writes=[mu])
        P.op("dve", "tensor_scalar", out=vec[:, :, 9], in0=vec[:, :, 1], scalar1=-1.0, scalar2=1.0, op0=ALU.mult,
             op1=ALU.add, reads=[vec], writes=[vec])
        blk = P.sb("blk64", [128, 128])
        P.dma("sp", blk[:], G.blk64[:], blk, reads=[G.blk64], writes=[blk])
        smask = P.sb("smask", [128, W])
        P.dma("sp", smask[:], G.scanmask[:], smask, reads=[G.scanmask], writes=[smask])
        wup = P.sb("wup", [64, 2, 512], BF16)
        P.dma("pool", wup[:], G.rw_w_up[:].rearrange("d r c -> r d c"), wup, reads=[G.rw_w_up], writes=[wup])
        aup = P.sb("aup", [128, 512], BF16)
        P.dma("pool", aup[64:128, :], G.rw_a_up[:], aup, reads=[G.rw_a_up], writes=[aup])
        gup = P.sb("gup", [128, 512], BF16)
        P.dma("pool", gup[:], G.rw_g_up[:], gup, reads=[G.rw_g_up], writes=[gup])
        eps12 = P.sb("eps12", [128, 1])
        P.op("dve", "memset", ap=eps12[:], constant=1e-12, writes=[eps12])
        WP = WPrefetch(P, G.rec_w_in, [12, 13] + [x for c in range(4) for x in (c, 4 + c, 8 + c)])
        T = [P.sb(f"T{i}", [128, W]) for i in range(12)]
        for t in T:
            P.op("pool", "memset", ap=t[:], constant=0.0, writes=[t])
        twb = P.sb("twb", [64, W], BF16)
        adb = P.sb("adb", [128, W], BF16)
        sgb = P.sb("sgb", [128, W], BF16)
        vtok = P.sb("vtok", [128, 18, 128], BF16)
        bkt = P.sb("bkt", [128, 18, 2, 128], BF16)
        gl = P.sb("gl", [128, 18])
        wi = [0]

        def proj(chunk, out):
            wt = WP.get(chunk)
            inproj_fm(P, G, hT, wt, T[0])
            tshift(P, T[0], T[1], out, mu[:, 1, chunk:chunk + 1], mu[:, 2, chunk:chunk + 1], mu)

        proj(12, T[2])
        P.op("act", "activation", out=twb[:], in_=T[2][0:64, :], func=AF.Tanh, reads=[T[2]], writes=[twb])
        P.op("dve", "tensor_copy", out=adb[64:128, :], in_=T[2][64:128, :], reads=[T[2]], writes=[adb])
        proj(13, T[2])
        P.op("act", "activation", out=sgb[:], in_=T[2][:], func=AF.Sigmoid, reads=[T[2]], writes=[sgb])
        for c in range(4):
            cs = slice(c * 128, (c + 1) * 128)
            A, R, KR, KAP, T6, V = T[2], T[3], T[4], T[5], T[6], T[7]
            for bi, c0 in enumerate(range(0, W, 512)):
                n = min(512, W - c0)
                ps = bank[4 + bi % 2]
                mm(P, ps, ps[:, 0:n], gup, gup[:, cs], sgb, sgb[:, c0:c0 + n])
                P.op("act", "activation", out=T[1][:, c0:c0 + n], in_=ps[:, 0:n], func=AF.Copy, reads=[ps], writes=[T[1]])
                ps2 = bank[6 + bi % 2]
                mm(P, ps2, ps2[:, 0:n], aup, aup[64:128, cs], adb, adb[64:128, c0:c0 + n])
                P.op("act", "activation", out=A[:, c0:c0 + n], in_=ps2[:, 0:n], func=AF.Sigmoid, bias=vec[:, c, 3:4],
                     reads=[ps2, vec], writes=[A])
            P.dma("sp", G.gT[b, c, :, :], T[1][:], T[1], reads=[T[1]], writes=[G.gT])
            proj(c, R)
            proj(4 + c, KR)
            P.op("dve", "tensor_scalar", out=KAP[:], in0=KR[:], scalar1=vec[:, c, 0:1], scalar2=None, op0=ALU.mult,
                 reads=[KR, vec], writes=[KAP])
            P.op("pool", "tensor_tensor", out=T6[:], in0=KAP[:], in1=KAP[:], op=ALU.mult, reads=[KAP], writes=[T6])
            for bi, c0 in enumerate(range(0, W, 512)):
                n = min(512, W - c0)
                ps = bank[4 + bi % 2]
                mm(P, ps, ps[:, 0:n], blk, blk[:], T6, T6[:, c0:c0 + n])
                P.op("act", "activation", out=T[1][:, c0:c0 + n], in_=ps[:, 0:n], func=AF.Sqrt, reads=[ps], writes=[T[1]])
            P.op("dve", "tensor_scalar", out=T[1][:], in0=T[1][:], scalar1=eps12[:, 0:1], scalar2=None, op0=ALU.max,
                 reads=[T[1], eps12], writes=[T[1]])
            P.op("dve", "reciprocal", out=T[1][:], in_=T[1][:], reads=[T[1]], writes=[T[1]])
            P.op("dve", "tensor_tensor", out=KAP[:], in0=KAP[:], in1=T[1][:], op=ALU.mult, reads=[KAP, T[1]], writes=[KAP])
            P.op("dve", "tensor_scalar", out=T6[:], in0=A[:], scalar1=vec[:, c, 1:2], scalar2=vec[:, c, 9:10],
                 op0=ALU.mult, op1=ALU.add, reads=[A, vec], writes=[T6])
            KM = T6
            P.op("pool", "tensor_tensor", out=KM[:], in0=KM[:], in1=KR[:], op=ALU.mult, reads=[KM, KR], writes=[KM])
            BE = KR
            P.op("dve", "tensor_tensor", out=BE[:], in0=KAP[:], in1=A[:], op=ALU.mult, reads=[KAP, A], writes=[BE])
            P.op("dve", "scalar_tensor_tensor", out=T[1][:], in0=R[:], scalar=vec[:, c, 4:5], in1=KM[:],
                 op0=ALU.mult, op1=ALU.mult, reads=[R, KM, vec], writes=[T[1]])
            for bi, c0 in enumerate(range(0, W, 512)):
                n = min(512, W - c0)
                ps = bank[4 + bi % 2]
                mm(P, ps, ps[:, 0:n], blk, blk[:], T[1], T[1][:, c0:c0 + n])
                P.op("act", "activation", out=T[8][:, c0:c0 + n], in_=ps[:, 0:n], func=AF.Copy, reads=[ps], writes=[T[8]])
            P.dma("sp", G.bcT[b, c, :, :], T[8][:], T[8], reads=[T[8]], writes=[G.bcT])
            proj(8 + c, V)
            P.dma("sp", G.vT[b, c, :, :], V[:], V, reads=[V], writes=[G.vT])
            for n in range(18):
                ps = bank[4 + n % 4]
                c0 = pcol(n * 128)
                P.op("pe", "transpose", out=ps[:, 0:128], in_=V[:, c0:c0 + 128], identity=G.identf[:],
                     reads=[V, G.identf], writes=[ps])
                P.op("act" if n % 2 else "dve", "activation" if n % 2 else "tensor_copy", out=vtok[:, n, :],
                     in_=ps[:, 0:128], reads=[ps], writes=[vtok], **({"func": AF.Copy} if n % 2 else {}))
            P.dma("sp", G.Vt[b, :, :, cs].rearrange("n t c -> t n c"), vtok[:], vtok, reads=[vtok], writes=[G.Vt])
            for d in range(2):
                LW, CS, GE, GI, ENI = T[7], T[8], T[9], T[10], T[11]
                for bi, c0 in enumerate(range(0, W, 512)):
                    n = min(512, W - c0)
                    ps = bank[4 + bi % 2]
                    mm(P, ps, ps[:, 0:n], wup, wup[:, d, cs], twb, twb[:, c0:c0 + n])
                    P.op("act", "activation", out=LW[:, c0:c0 + n], in_=ps[:, 0:n], func=AF.Sigmoid,
                         bias=vec[:, c, 7 + d:8 + d], reads=[ps, vec], writes=[LW])
                P.op("dve", "tensor_tensor_scan", out=CS[:], data0=smask[:], data1=LW[:], initial=0.0,
                     op0=ALU.mult, op1=ALU.add, reads=[smask, LW], writes=[CS])
                for (cv, n0, nn) in ((chunk_views(CS[:])[0], 0, 2), (chunk_views(CS[:])[1], 2, 16)):
                    P.op("act", "activation", out=gl[:, n0:n0 + nn], in_=cv[:, :, 127], func=AF.Exp, scale=-C0,
                         reads=[CS], writes=[gl])
                P.dma("sp", G.GL[d][b, 2 * c:2 * c + 2, :, :].rearrange("h k n -> (h k) n"), gl[:], gl,
                      reads=[gl], writes=[G.GL[d]])
                if d == 0:
                    P.op("pool", "tensor_tensor", out=GE[:], in0=CS[:], in1=LW[:], op=ALU.subtract,
                         reads=[CS, LW], writes=[GE])
                    P.op("act", "activation", out=GI[:], in_=CS[:], func=AF.Exp, scale=-C0, reads=[CS], writes=[GI])
                    P.op("act", "activation", out=ENI[:], in_=CS[:], func=AF.Exp, scale=C0, reads=[CS], writes=[ENI])
                else:
                    for vi in range(2):
                        gev = chunk_views(GE[:])[vi]
                        csv = chunk_views(CS[:])[vi]
                        nn = 2 if vi == 0 else 16
                        P.op("dve", "tensor_tensor", out=gev, in0=csv[:, :, 127:128].broadcast_to([128, nn, 128]),
                             in1=csv, op=ALU.subtract, reads=[CS], writes=[GE])
                    P.op("pool", "tensor_tensor", out=GI[:], in0=GE[:], in1=LW[:], op=ALU.add,
                         reads=[GE, LW], writes=[GI])
                    P.op("act", "activation", out=ENI[:], in_=GI[:], func=AF.Exp, scale=C0, reads=[GI], writes=[ENI])
                    P.op("act", "activation", out=GI[:], in_=GI[:], func=AF.Exp, scale=-C0, reads=[GI], writes=[GI])
                P.op("act", "activation", out=GE[:], in_=GE[:], func=AF.Exp, scale=-C0, reads=[GE], writes=[GE])
                P.op("dve", "tensor_tensor", out=GI[:], in0=GI[:], in1=R[:], op=ALU.mult, reads=[GI, R], writes=[GI])
                P.op("pool", "tensor_tensor", out=GE[:], in0=GE[:], in1=KAP[:], op=ALU.mult, reads=[GE, KAP], writes=[GE])
                BH = CS
                P.op("dve", "tensor_tensor", out=BH[:], in0=ENI[:], in1=BE[:], op=ALU.mult, reads=[ENI, BE], writes=[BH])
                KH = ENI
                P.op("pool", "tensor_tensor", out=KH[:], in0=ENI[:], in1=KM[:], op=ALU.mult, reads=[ENI, KM], writes=[KH])
                hv = lambda t, j: t[b, 2 * c:2 * c + 2, :, j, :].rearrange("h k w -> (h k) w")
                P.dma("pool", hv(G.RS[d], 0), GE[:], GE, reads=[GE], writes=[G.RS[d]])
                P.dma("pool", hv(G.RS[d], 1), GI[:], GI, reads=[GI], writes=[G.RS[d]])
                P.dma("pool", hv(G.BK[d], 0), BH[:], BH, reads=[BH], writes=[G.BK[d]])
                P.dma("pool", hv(G.BK[d], 1), KH[:], KH, reads=[KH], writes=[G.BK[d]])
                for n in range(18):
                    c0 = pcol(n * 128)
                    for j, src in enumerate((BH, KH)):
                        ps = bank[(2 * n + j) % 4]
                        P.op("pe", "transpose", out=ps[:, 0:128], in_=src[:, c0:c0 + 128], identity=G.identf[:],
                             reads=[src, G.identf], writes=[ps])
                        if j == 0:
                            P.op("act", "activation", out=bkt[:, n, j, :], in_=ps[:, 0:128], func=AF.Copy,
                                 reads=[ps], writes=[bkt])
                        else:
                            P.op("dve", "tensor_copy", out=bkt[:, n, j, :], in_=ps[:, 0:128], reads=[ps], writes=[bkt])
                for j in range(2):
                    P.dma("sp", G.BKt[d][b, :, :, j, cs].rearrange("n t c -> t n c"), bkt[:, :, j, :], bkt,
                          reads=[bkt], writes=[G.BKt[d]])


def stage_rwkv_scan(P, G, bs=(0, 1), ds=(0, 1), nmax=18):
    bank = G.bank
    with P.scope():
        msk = P.sb("msk", [128, 4, 128])
        P.dma("sp", msk[:], G.masks[:], msk, reads=[G.masks], writes=[msk])
        rs = [P.sb(f"rs{i}", [64, 8, 2, 128], BF16) for i in range(3)]
        bk = [P.sb(f"bk{i}", [64, 8, 2, 128], BF16) for i in range(3)]
        bkt = [P.sb(f"bktl{i}", [128, 2, 512], BF16) for i in range(3)]
        vt = [P.sb(f"vt{i}", [128, 512], BF16) for i in range(3)]
        Ms = [P.sb(f"Ms{i}", [128, 4, 128], F32) for i in range(2)]
        MTs = [P.sb(f"MTs{i}", [128, 4, 128], F32) for i in range(2)]
        Nn = [[P.sb(f"N{i}_{j}", [128, 4, 128], F32) for j in range(2)] for i in range(2)]
        NT = [[P.sb(f"NT{i}_{j}", [128, 4, 128], F32) for j in range(2)] for i in range(2)]
        ABr = [[P.sb(f"ABr{p}_{i}", [128, 4, 128], BF16) for i in range(2)] for p in range(2)]
        GKs = [[P.sb(f"GKs{p}_{i}", [128, 4, 256], BF16) for i in range(2)] for p in range(2)]
        Pm = [[P.sb(f"Pm{p}_{i}", [128, 4, 128], F32) for i in range(2)] for p in range(2)]
        WT = P.sb("WT", [128, 512], F32)
        nZ = P.sb("nZ", [128, 512], BF16)
        ot = [P.sb(f"ot{i}", [128, 512]) for i in range(2)]
        Sf = P.sb("Sf", [64, 8, 64])
        Sb = P.sb("Sb", [64, 8, 64], BF16)
        Stmp = P.sb("Stmp", [64, 8, 64])
        GLt = P.sb("GLt", [64, 8, 18])
        b6, b7 = bank[6], bank[7]

        def emit_loads(b, d, n, q):
            c0 = pcol(n * 128)
            for j in range(2):
                P.dma("sp", rs[q][:, :, j, :], G.RS[d][b, :, :, j, c0:c0 + 128].rearrange("h k t -> k h t"),
                      rs[q], reads=[G.RS[d]], writes=[rs[q]])
                P.dma("sp", bk[q][:, :, j, :], G.BK[d][b, :, :, j, c0:c0 + 128].rearrange("h k t -> k h t"),
                      bk[q], reads=[G.BK[d]], writes=[bk[q]])
            P.dma("sp", bkt[q][:], G.BKt[d][b, n], bkt[q], reads=[G.BKt[d]], writes=[bkt[q]])
            P.dma("sp", vt[q][:], G.Vt[b, n], vt[q], reads=[G.Vt], writes=[vt[q]])

        def emit_front(b, d, n, s, q):
            m2 = msk[:, 0:2, :] if d == 0 else msk[:, 2:4, :]
            mt = msk[:, 2, :] if d == 0 else msk[:, 0, :]
            R_, B_ = rs[q], bk[q]
            for hf in range(2):
                for i in range(4):
                    h = hf * 4 + i
                    pb = bank[hf * 3 + 0] if i < 2 else bank[hf * 3 + 1]
                    mm(P, pb, pb[:, (i % 2) * 256:(i % 2 + 1) * 256], B_, B_[:, h, 0, :], R_, R_[:, h, :, :])
                for i in range(4):
                    h = hf * 4 + i
                    pk = b6 if i < 2 else b7
                    mm(P, pk, pk[:, (i % 2) * 256:(i % 2 + 1) * 256], B_, B_[:, h, 1, :], R_, R_[:, h, :, :])
                pm = bank[hf * 3 + 2]
                for i in range(4):
                    h = hf * 4 + i
                    mm(P, pm, pm[:, i * 128:(i + 1) * 128], R_, R_[:, h, 0, :], B_, B_[:, h, 0, :])
                for half2 in range(2):
                    pb = bank[hf * 3 + half2]
                    pbv = pb[:, 0:512].rearrange("p (h j t) -> p h j t", h=2, j=2)
                    P.op("dve", "tensor_tensor", out=Ms[hf][:, 2 * half2:2 * half2 + 2, :], in0=pbv[:, :, 0, :],
                         in1=m2[:, 0, :].unsqueeze(1).broadcast_to([128, 2, 128]), op=ALU.mult,
                         reads=[pb, msk], writes=[Ms[hf]])
                    P.op("dve", "tensor_tensor", out=ABr[s][hf][:, 2 * half2:2 * half2 + 2, :], in0=pbv[:, :, 1, :],
                         in1=m2[:, 1, :].unsqueeze(1).broadcast_to([128, 2, 128]), op=ALU.mult,
                         reads=[pb, msk], writes=[ABr[s][hf]])
                    pk = bank[6 + half2]
                    P.op("dve", "tensor_tensor", out=GKs[s][hf][:, 2 * half2:2 * half2 + 2, :].rearrange("p h (j t) -> p h j t", j=2),
                         in0=pk[:, 0:512].rearrange("p (h j t) -> p h j t", h=2, j=2),
                         in1=m2.unsqueeze(1).broadcast_to([128, 2, 2, 128]), op=ALU.mult,
                         reads=[pk, msk], writes=[GKs[s][hf]])
                P.op("dve", "tensor_tensor", out=MTs[hf][:], in0=pm[:, 0:512].rearrange("p (h t) -> p h t", h=4),
                     in1=mt.unsqueeze(1).broadcast_to([128, 4, 128]), op=ALU.mult,
                     reads=[pm, msk], writes=[MTs[hf]])
                P.op("pool", "tensor_tensor", out=Pm[s][hf][:], in0=G.identf[:].unsqueeze(1).broadcast_to([128, 4, 128]),
                     in1=Ms[hf][:], op=ALU.subtract, reads=[Ms[hf], G.identf], writes=[Pm[s][hf]])
            for hf in range(2):
                pn, pt = bank[hf * 3 + 0], bank[hf * 3 + 1]
                for i in range(4):
                    mm(P, pn, pn[:, i * 128:(i + 1) * 128], MTs[hf], MTs[hf][:, i, :], Ms[hf], Ms[hf][:, i, :])
                for i in range(4):
                    mm(P, pt, pt[:, i * 128:(i + 1) * 128], Ms[hf], Ms[hf][:, i, :], MTs[hf], MTs[hf][:, i, :])
                P.op("act", "activation", out=Nn[hf][0][:], in_=pn[:, 0:512].rearrange("p (h t) -> p h t", h=4),
                     func=AF.Copy, reads=[pn], writes=[Nn[hf][0]])
                P.op("act", "activation", out=NT[hf][0][:], in_=pt[:, 0:512].rearrange("p (h t) -> p h t", h=4),
                     func=AF.Copy, reads=[pt], writes=[NT[hf][0]])

        def emit_level(s, lev):
            cur, nxt = lev % 2, 1 - lev % 2
            for hf in range(2):
                pp, pn, pt = bank[hf * 3 + 2], bank[hf * 3 + 0], bank[hf * 3 + 1]
                Pq = Pm[s][hf]
                for i in range(4):
                    mm(P, pp, pp[:, i * 128:(i + 1) * 128], NT[hf][cur], NT[hf][cur][:, i, :], Pq, Pq[:, i, :])
                if lev < 4:
                    for i in range(4):
                        mm(P, pn, pn[:, i * 128:(i + 1) * 128], NT[hf][cur], NT[hf][cur][:, i, :],
                           Nn[hf][cur], Nn[hf][cur][:, i, :])
                if lev < 5:
                    for i in range(4):
                        mm(P, pt, pt[:, i * 128:(i + 1) * 128], Nn[hf][cur], Nn[hf][cur][:, i, :],
                           NT[hf][cur], NT[hf][cur][:, i, :])
                P.op("dve", "tensor_tensor", out=Pq[:], in0=pp[:, 0:512].rearrange("p (h t) -> p h t", h=4),
                     in1=Pq[:], op=ALU.add, reads=[pp, Pq], writes=[Pq])
                if lev < 4:
                    P.op("act", "activation", out=Nn[hf][nxt][:], in_=pn[:, 0:512].rearrange("p (h t) -> p h t", h=4),
                         func=AF.Copy, reads=[pn], writes=[Nn[hf][nxt]])
                if lev < 5:
                    P.op("act", "activation", out=NT[hf][nxt][:], in_=pt[:, 0:512].rearrange("p (h t) -> p h t", h=4),
                         func=AF.Copy, reads=[pt], writes=[NT[hf][nxt]])

        def chain_steps(b, d, n, s, q):
            R_, BT_, V_ = rs[q], bkt[q], vt[q]

            def st_d():
                for h in range(8):
                    hf, i = h // 4, h % 4
                    hs = slice(h * 64, (h + 1) * 64)
                    mm(P, b6, b6[:, hs], R_, R_[:, h, 0, :], Sb, Sb[:, h, :], start=True, stop=False)
                    mm(P, b6, b6[:, hs], GKs[s][hf], GKs[s][hf][:, i, 0:128], V_, V_[:, hs], start=False, stop=True)
                P.op("act", "activation", out=WT[:], in_=b6[:, 0:512], func=AF.Copy, reads=[b6], writes=[WT])

            def st_e():
                for h in range(8):
                    hf, i = h // 4, h % 4
                    hs = slice(h * 64, (h + 1) * 64)
                    mm(P, b7, b7[:, hs], Pm[s][hf], Pm[s][hf][:, i, :], WT, WT[:, hs])
                P.op("act", "activation", out=nZ[:], in_=b7[:, 0:512], func=AF.Copy, scale=-1.0, reads=[b7], writes=[nZ])

            def st_f():
                for h in range(8):
                    hf, i = h // 4, h % 4
                    hs = slice(h * 64, (h + 1) * 64)
                    mm(P, b6, b6[:, hs], R_, R_[:, h, 1, :], Sb, Sb[:, h, :], start=True, stop=False)
                    mm(P, b6, b6[:, hs], GKs[s][hf], GKs[s][hf][:, i, 128:256], V_, V_[:, hs], start=False, stop=False)
                    mm(P, b6, b6[:, hs], ABr[s][hf], ABr[s][hf][:, i, :], nZ, nZ[:, hs], start=False, stop=True)
                o_ = ot[s]
                P.op("dve", "tensor_copy", out=o_[:], in_=b6[:, 0:512], reads=[b6], writes=[o_])
                P.dma("pool", G.oA[d][b, n], o_[:], o_, reads=[o_], writes=[G.oA[d]])

            def st_g():
                for h in range(8):
                    hs = slice(h * 64, (h + 1) * 64)
                    mm(P, b7, b7[0:64, hs], BT_, BT_[:, 1, hs], V_, V_[:, hs], start=True, stop=False)
                    mm(P, b7, b7[0:64, hs], BT_, BT_[:, 0, hs], nZ, nZ[:, hs], start=False, stop=True)
                P.op("dve", "tensor_tensor", out=Stmp[:], in0=b7[0:64, 0:512].rearrange("p (h v) -> p h v", h=8),
                     in1=Sf[:], op=ALU.add, reads=[b7, Sf], writes=[Stmp])
                P.op("dve", "tensor_tensor", out=Sf[:], in0=Stmp[:],
                     in1=GLt[:, :, n:n + 1].broadcast_to([64, 8, 64]), op=ALU.mult,
                     reads=[Stmp, GLt], writes=[Sf])
                P.op("act", "activation", out=Sb[:], in_=Sf[:], func=AF.Copy, reads=[Sf], writes=[Sb])

            return [st_d, st_e, st_f, st_g]

        it = 0
        for b in bs:
            for d in ds:
                P.op("dve", "memset", ap=Sf[:], constant=0.0, writes=[Sf])
                P.op("dve", "memset", ap=Sb[:], constant=0.0, writes=[Sb])
                P.dma("sp", GLt[:], G.GL[d][b].rearrange("h k n -> k h n"), GLt, reads=[G.GL[d]], writes=[GLt])
                order = list(range(18)) if d == 0 else [1, 0] + list(range(17, 1, -1))
                order = order[:nmax]
                pending = []
                emit_loads(b, d, order[0], it % 3)
                for oi, n in enumerate(order + [None]):
                    if n is not None:
                        s = it % 2
                        q = it % 3
                        it += 1
                        if oi + 1 < len(order):
                            emit_loads(b, d, order[oi + 1], it % 3)
                        emit_front(b, d, n, s, q)
                    for lev in range(6):
                        if n is not None:
                            emit_level(s, lev)
                        if pending and lev >= 1:
                            pending.pop(0)()
                    while pending:
                        pending.pop(0)()
                    if n is not None:
                        pending = chain_steps(b, d, n, s, q)


def stage_rwkv_post(P, G):
    bank = G.bank
    with P.scope():
        vec = P.sb("rwvec2", [128, 4, 12])
        P.dma("sp", vec[:], G.rw_vec[:], vec, reads=[G.rw_vec], writes=[vec])
        gne = P.sb("gne", [128, 1])
        P.op("dve", "memset", ap=gne[:], constant=64e-5, writes=[gne])
        o0 = [P.sb(f"o0_{i}", [128, 512]) for i in range(3)]
        o1 = [P.sb(f"o1_{i}", [128, 512]) for i in range(3)]
        sq = P.sb("posq", [128, 512])
        st = [P.sb(f"pst{i}", [128, 4, 8]) for i in range(3)]
        on = [P.sb(f"on{i}", [128, 512]) for i in range(3)]
        ya = [P.sb(f"ya{i}", [128, 4, 128]) for i in range(3)]
        bc = [P.sb(f"bc{i}", [128, 4, 128]) for i in range(3)]
        vv = [P.sb(f"vv{i}", [128, 4, 128]) for i in range(3)]
        gg = [P.sb(f"gg{i}", [128, 4, 128]) for i in range(3)]
        yb = [P.sb(f"yab{i}", [128, 4, 128], BF16) for i in range(3)]
        it = 0
        for b in range(NB):
            for n in range(18):
                s = it % 3
                it += 1
                c0 = pcol(n * 128)
                P.dma("sp", o0[s][:], G.oA[0][b, n], o0[s], reads=[G.oA[0]], writes=[o0[s]])
                P.dma("sp", o1[s][:], G.oA[1][b, n], o1[s], reads=[G.oA[1]], writes=[o1[s]])
                P.dma("sp", bc[s][:], G.bcT[b, :, :, c0:c0 + 128].rearrange("c p t -> p c t"), bc[s], reads=[G.bcT], writes=[bc[s]])
                P.dma("sp", vv[s][:], G.vT[b, :, :, c0:c0 + 128].rearrange("c p t -> p c t"), vv[s], reads=[G.vT], writes=[vv[s]])
                P.dma("sp", gg[s][:], G.gT[b, :, :, c0:c0 + 128].rearrange("c p t -> p c t"), gg[s], reads=[G.gT], writes=[gg[s]])
                o_, S_ = o0[s], st[s]
                P.op("pool", "tensor_tensor", out=o_[:], in0=o_[:], in1=o1[s][:], op=ALU.add, reads=[o_, o1[s]], writes=[o_])
                ov = o_[:].rearrange("p (h v) -> p h v", h=8)
                P.op("dve", "tensor_reduce", out=S_[:, 0, :], in_=ov, axis=AX.X, op=ALU.add, reads=[o_], writes=[S_])
                P.op("pool", "tensor_tensor", out=sq[:], in0=o_[:], in1=o_[:], op=ALU.mult, reads=[o_], writes=[sq])
                P.op("dve", "tensor_reduce", out=S_[:, 1, :], in_=sq[:].rearrange("p (h v) -> p h v", h=8), axis=AX.X,
                     op=ALU.add, reads=[sq], writes=[S_])
                P.op("dve", "tensor_scalar", out=S_[:, 2, :], in0=S_[:, 0, :], scalar1=1.0 / 64, scalar2=None, op0=ALU.mult,
                     reads=[S_], writes=[S_])
                P.op("dve", "tensor_tensor", out=S_[:, 3, :], in0=S_[:, 2, :], in1=S_[:, 2, :], op=ALU.mult, reads=[S_], writes=[S_])
                P.op("dve", "scalar_tensor_tensor", out=S_[:, 3, :], in0=S_[:, 1, :], scalar=1.0 / 64, in1=S_[:, 3, :],
                     op0=ALU.mult, op1=ALU.subtract, reads=[S_], writes=[S_])
                P.op("act", "activation", out=S_[:, 3, :], in_=S_[:, 3, :], func=AF.Sqrt, bias=gne[:, 0:1], reads=[S_, gne], writes=[S_])
                P.op("dve", "reciprocal", out=S_[:, 3, :], in_=S_[:, 3, :], reads=[S_], writes=[S_])
                onv = on[s][:].rearrange("p (h v) -> p h v", h=8)
                P.op("dve", "tensor_tensor", out=onv, in0=ov, in1=S_[:, 2, :].unsqueeze(2).broadcast_to([128, 8, 64]),
                     op=ALU.subtract, reads=[o_, S_], writes=[on[s]])
                P.op("dve", "tensor_tensor", out=onv, in0=onv, in1=S_[:, 3, :].unsqueeze(2).broadcast_to([128, 8, 64]),
                     op=ALU.mult, reads=[on[s], S_], writes=[on[s]])
                for c in range(4):
                    ps = bank[it % 8]
                    P.op("pe", "transpose", out=ps[:, c * 128:(c + 1) * 128], in_=on[s][:, c * 128:(c + 1) * 128], identity=G.identf[:],
                         reads=[on[s], G.identf], writes=[ps])
                for c in range(4):
                    ps = bank[it % 8]
                    P.op("act", "activation", out=ya[s][:, c, :], in_=ps[:, c * 128:(c + 1) * 128], func=AF.Identity,
                         scale=vec[:, c, 5:6], bias=vec[:, c, 6:7], reads=[ps, vec], writes=[ya[s]])
                P.op("pool", "tensor_tensor", out=bc[s][:], in0=bc[s][:], in1=vv[s][:], op=ALU.mult, reads=[bc[s], vv[s]], writes=[bc[s]])
                P.op("dve", "tensor_tensor", out=ya[s][:], in0=ya[s][:], in1=bc[s][:], op=ALU.add, reads=[ya[s], bc[s]], writes=[ya[s]])
                P.op("dve", "tensor_tensor", out=yb[s][:], in0=ya[s][:], in1=gg[s][:], op=ALU.mult, reads=[ya[s], gg[s]], writes=[yb[s]])
                P.dma("pool", G.yT[b, 0:4, :, n * 128:(n + 1) * 128].rearrange("c p t -> p c t"), yb[s][:], yb[s],
                      reads=[yb[s]], writes=[G.yT])


def stage_hgrn_feat(P, G, b):
    bank = G.bank
    with P.scope():
        hT = P.sb("hTres", [128, 8, TT], BF16)
        for k in range(8):
            P.dma("sp", hT[:, k, :], G.hT0[b, :, k, :], hT, reads=[G.hT0], writes=[hT])
        lbr = P.sb("lbr", [128, 2, 2, 4])
        P.dma("sp", lbr[:], G.hg_lb[:].rearrange("d l p h -> p d l h"), lbr, reads=[G.hg_lb], writes=[lbr])
        lb = P.sb("lb", [128, 2, 2, 4])
        P.op("dve", "tensor_tensor", out=lb[:, :, 0, :], in0=lbr[:, :, 0, :], in1=lbr[:, :, 1, :], op=ALU.subtract,
             reads=[lbr], writes=[lb])
        P.op("act", "activation", out=lb[:, :, 0, :], in_=lb[:, :, 0, :], func=AF.Sigmoid, reads=[lb], writes=[lb])
        P.op("dve", "tensor_scalar", out=lb[:, :, 1, :], in0=lb[:, :, 0, :], scalar1=-1.0, scalar2=1.0, op0=ALU.mult,
             op1=ALU.add, reads=[lb], writes=[lb])
        sm64 = P.sb("sm64", [128, TT])
        P.op("pool", "memset", ap=sm64[:], constant=1.0, writes=[sm64])
        P.op("pool", "memset", ap=sm64[:].rearrange("p (n t) -> p n t", t=64)[:, :, 0:1], constant=0.0, writes=[sm64])
        WP = WPrefetch(P, G.rec_w_in, [x for h in range(4) for x in (14 + h, 22 + h, 26 + h)])
        wbig = P.sb("wbig", [128, 8, 512], BF16)
        T = [P.sb(f"H{i}", [128, TT]) for i in range(7)]
        hkt = P.sb("hkt", [128, 18, 128], BF16)
        glh = P.sb("glh", [128, 36])
        tok = [P.sb(f"tok{i}", [128, 512], BF16) for i in range(2)]
        wi = [0]

        def proj(chunk, out, func=None):
            wt = WP.get(chunk)
            inproj_fm(P, G, hT, wt, out, padded=False, func=func)

        for h in range(4):
            Q, F, LF, CS, GI, ENI, KF = T
            proj(14 + h, Q, AF.Silu)
            for d in range(2):
                proj(22 + 4 * d + h, F, AF.Sigmoid)
                P.op("dve", "tensor_scalar", out=F[:], in0=F[:], scalar1=lb[:, d, 1, h:h + 1], scalar2=lb[:, d, 0, h:h + 1],
                     op0=ALU.mult, op1=ALU.add, reads=[F, lb], writes=[F])
                P.op("act", "activation", out=LF[:], in_=F[:], func=AF.Ln, reads=[F], writes=[LF])
                P.op("pool", "tensor_scalar", out=KF[:], in0=F[:], scalar1=-1.0, scalar2=1.0, op0=ALU.mult, op1=ALU.add,
                     reads=[F], writes=[KF])
                P.op("dve", "tensor_tensor_scan", out=CS[:], data0=sm64[:], data1=LF[:], initial=0.0, op0=ALU.mult,
                     op1=ALU.add, reads=[sm64, LF], writes=[CS])
                csv = CS[:].rearrange("p (n t) -> p n t", t=64)
                P.op("act", "activation", out=glh[:], in_=csv[:, :, 63], func=AF.Exp, reads=[CS], writes=[glh])
                P.dma("sp", G.HGL[d][b, h], glh[:], glh, reads=[glh], writes=[G.HGL[d]])
                if d == 0:
                    gsrc = CS
                else:
                    P.op("dve", "tensor_tensor", out=GI[:].rearrange("p (n t) -> p n t", t=64),
                         in0=csv[:, :, 63:64].broadcast_to([128, 36, 64]), in1=csv, op=ALU.subtract, reads=[CS], writes=[GI])
                    P.op("pool", "tensor_tensor", out=GI[:], in0=GI[:], in1=LF[:], op=ALU.add, reads=[GI, LF], writes=[GI])
                    gsrc = GI
                P.op("act", "activation", out=ENI[:], in_=gsrc[:], func=AF.Exp, scale=-1.0, reads=[gsrc], writes=[ENI])
                P.op("act", "activation", out=GI[:], in_=gsrc[:], func=AF.Exp, reads=[gsrc], writes=[GI])
                P.op("dve", "tensor_tensor", out=GI[:], in0=GI[:], in1=Q[:], op=ALU.mult, reads=[GI, Q], writes=[GI])
                P.op("pool", "tensor_tensor", out=ENI[:], in0=ENI[:], in1=KF[:], op=ALU.mult, reads=[ENI, KF], writes=[ENI])
                P.dma("pool", G.HQK[d][b, h, :, 0, :], GI[:], GI, reads=[GI], writes=[G.HQK[d]])
                P.dma("pool", G.HQK[d][b, h, :, 1, :], ENI[:], ENI, reads=[ENI], writes=[G.HQK[d]])
                for n in range(18):
                    ps = bank[4 + n % 4]
                    P.op("pe", "transpose", out=ps[:, 0:128], in_=ENI[:, n * 128:(n + 1) * 128], identity=G.identf[:],
                         reads=[ENI, G.identf], writes=[ps])
                    if n % 2:
                        P.op("act", "activation", out=hkt[:, n, :], in_=ps[:, 0:128], func=AF.Copy, reads=[ps], writes=[hkt])
                    else:
                        P.op("dve", "tensor_copy", out=hkt[:, n, :], in_=ps[:, 0:128], reads=[ps], writes=[hkt])
                P.dma("sp", G.HKt[d][b, :, :, h * 128:(h + 1) * 128].rearrange("n t c -> t n c"), hkt[:], hkt,
                      reads=[hkt], writes=[G.HKt[d]])
        for which, c0, dst in ((0, 18 * 128, G.HVt), (1, 30 * 128, G.HGt)):
            load_w_chunk(P, wbig, G.rec_w_in, c0, 512)
            for tt in range(18):
                ps = bank[tt % 4]
                for k in range(8):
                    mm(P, ps, ps[:, 0:512], hT, hT[:, k, tt * 128:(tt + 1) * 128], wbig, wbig[:, k, :],
                       start=(k == 0), stop=(k == 7))
                tk = tok[tt % 2]
                P.op("act", "activation", out=tk[:], in_=ps[:, 0:512], func=(AF.Silu if which else AF.Copy),
                     reads=[ps], writes=[tk])
                P.dma("sp", dst[b, tt], tk[:], tk, reads=[tk], writes=[dst])


def stage_hgrn_scan(P, G, bs=(0, 1), ds=(0, 1)):
    bank = G.bank
    with P.scope():
        msk = P.sb("msk", [128, 4, 128])
        P.dma("sp", msk[:], G.masks[:], msk, reads=[G.masks], writes=[msk])
        qk = [[P.sb(f"qk{d}_{i}", [128, 4, 2, 128], BF16) for i in range(3)] for d in range(2)]
        kt = [[P.sb(f"hkt{d}_{i}", [128, 512], BF16) for i in range(3)] for d in range(2)]
        vt = [[P.sb(f"hvt{d}_{i}", [128, 512], BF16) for i in range(3)] for d in range(2)]
        att = [[P.sb(f"att{d}_{i}", [128, 4, 64], BF16) for i in range(2)] for d in range(2)]
        ot = [[P.sb(f"hot{d}_{i}", [128, 512]) for i in range(2)] for d in range(2)]
        Sf = [P.sb(f"hSf{d}", [128, 4, 128]) for d in range(2)]
        Sb = [P.sb(f"hSb{d}", [128, 4, 128], BF16) for d in range(2)]
        Stmp = [P.sb(f"hStmp{d}", [128, 4, 128]) for d in range(2)]
        GLt = [P.sb(f"hGLt{d}", [128, 4, 36]) for d in range(2)]
        it = 0
        for b in bs:
            for d in ds:
                P.op("dve", "memset", ap=Sf[d][:], constant=0.0, writes=[Sf[d]])
                P.op("dve", "memset", ap=Sb[d][:], constant=0.0, writes=[Sb[d]])
                P.dma("sp", GLt[d][:], G.HGL[d][b].rearrange("h k n -> k h n"), GLt[d], reads=[G.HGL[d]], writes=[GLt[d]])
            orders = {0: list(range(18)), 1: [1, 0] + list(range(17, 1, -1))}
            def hloads(step, q):
                for d in ds:
                    tt = orders[d][step]
                    for j in range(2):
                        P.dma("sp", qk[d][q][:, :, j, :], G.HQK[d][b, :, :, j, tt * 128:(tt + 1) * 128].rearrange("h k t -> k h t"),
                              qk[d][q], reads=[G.HQK[d]], writes=[qk[d][q]])
                    P.dma("sp", kt[d][q][:], G.HKt[d][b, tt], kt[d][q], reads=[G.HKt[d]], writes=[kt[d][q]])
                    P.dma("sp", vt[d][q][:], G.HVt[b, tt], vt[d][q], reads=[G.HVt], writes=[vt[d][q]])

            hloads(0, it % 3)
            for step in range(18):
                s = it % 2
                q = it % 3
                it += 1
                if step + 1 < 18:
                    hloads(step + 1, it % 3)
                for ci in range(2):
                    for d in ds:
                        tt = orders[d][step]
                        mi = 1 if d == 0 else 3
                        half = ci if d == 0 else 1 - ci
                        QK, KT, VT = qk[d][q], kt[d][q], vt[d][q]
                        pO = bank[4 + d]
                        chunk = 2 * tt + half
                        lo = half * 64
                        pr = slice(lo, lo + 64)
                        pA = bank[2 * d + ci]
                        pS = bank[6 + d]
                        A_ = att[d][ci]
                        for h in range(4):
                            mm(P, pA, pA[pr, h * 64:(h + 1) * 64], QK, QK[:, h, 1, lo:lo + 64], QK, QK[:, h, 0, lo:lo + 64])
                        P.op("dve", "tensor_tensor", out=A_[pr, :, :], in0=pA[pr, 0:256].rearrange("p (h t) -> p h t", h=4),
                             in1=msk[pr, mi, lo:lo + 64].unsqueeze(1).broadcast_to([64, 4, 64]), op=ALU.mult,
                             reads=[pA, msk], writes=[A_])
                        for h in range(4):
                            hs = slice(h * 128, (h + 1) * 128)
                            mm(P, pO, pO[pr, hs], A_, A_[pr, h, :], VT, VT[pr, hs], start=True, stop=False)
                            mm(P, pO, pO[pr, hs], QK, QK[:, h, 0, lo:lo + 64], Sb[d], Sb[d][:, h, :], start=False, stop=True)
                        for h in range(4):
                            hs = slice(h * 128, (h + 1) * 128)
                            mm(P, pS, pS[:, hs], KT, KT[pr, hs], VT, VT[pr, hs])
                        P.op("dve", "tensor_tensor", out=Stmp[d][:], in0=pS[:, 0:512].rearrange("p (h v) -> p h v", h=4),
                             in1=Sf[d][:], op=ALU.add, reads=[pS, Sf[d]], writes=[Stmp[d]])
                        P.op("dve", "tensor_tensor", out=Sf[d][:], in0=Stmp[d][:],
                             in1=GLt[d][:, :, chunk:chunk + 1].broadcast_to([128, 4, 128]), op=ALU.mult,
                             reads=[Stmp[d], GLt[d]], writes=[Sf[d]])
                        P.op("act", "activation", out=Sb[d][:], in_=Sf[d][:], func=AF.Copy, reads=[Sf[d]], writes=[Sb[d]])
                for d in ds:
                    tt = orders[d][step]
                    o_ = ot[d][s]
                    pO = bank[4 + d]
                    P.op("act", "activation", out=o_[:], in_=pO[:, 0:512], func=AF.Copy, reads=[pO], writes=[o_])
                    P.dma("pool", G.oH[d][b, tt], o_[:], o_, reads=[o_], writes=[G.oH[d]])


def stage_hgrn_post(P, G):
    bank = G.bank
    with P.scope():
        gn = P.sb("gn", [128, 128])
        P.dma("sp", gn[:], G.hg_norm[0:1, :].partition_broadcast(128), gn, reads=[G.hg_norm], writes=[gn])
        o0 = [P.sb(f"ho0_{i}", [128, 512]) for i in range(3)]
        o1 = [P.sb(f"ho1_{i}", [128, 512]) for i in range(3)]
        gt = [P.sb(f"hgt{i}", [128, 512], BF16) for i in range(3)]
        sq = P.sb("hsq", [128, 512])
        st = [P.sb(f"hst{i}", [128, 4]) for i in range(3)]
        yb = [P.sb(f"hyb{i}", [128, 4, 128], BF16) for i in range(3)]
        it = 0
        for b in range(NB):
            for tt in range(18):
                s = it % 3
                it += 1
                P.dma("sp", o0[s][:], G.oH[0][b, tt], o0[s], reads=[G.oH[0]], writes=[o0[s]])
                P.dma("sp", o1[s][:], G.oH[1][b, tt], o1[s], reads=[G.oH[1]], writes=[o1[s]])
                P.dma("sp", gt[s][:], G.HGt[b, tt], gt[s], reads=[G.HGt], writes=[gt[s]])
                o_ = o0[s]
                P.op("pool", "tensor_tensor", out=o_[:], in0=o_[:], in1=o1[s][:], op=ALU.add, reads=[o_, o1[s]], writes=[o_])
                P.op("pool", "tensor_tensor", out=sq[:], in0=o_[:], in1=o_[:], op=ALU.mult, reads=[o_], writes=[sq])
                P.op("dve", "tensor_reduce", out=st[s][:], in_=sq[:].rearrange("p (h v) -> p h v", h=4), axis=AX.X, op=ALU.add,
                     reads=[sq], writes=[st[s]])
                P.op("act", "activation", out=st[s][:], in_=st[s][:], func=AF.Sqrt, scale=1.0 / 128, bias=G.eps[:, 0:1],
                     reads=[st[s], G.eps], writes=[st[s]])
                P.op("dve", "reciprocal", out=st[s][:], in_=st[s][:], reads=[st[s]], writes=[st[s]])
                ov = o_[:].rearrange("p (h v) -> p h v", h=4)
                P.op("dve", "tensor_tensor", out=ov, in0=ov, in1=st[s][:].unsqueeze(2).broadcast_to([128, 4, 128]), op=ALU.mult,
                     reads=[o_, st[s]], writes=[o_])
                P.op("dve", "tensor_tensor", out=ov, in0=ov, in1=gn[:].unsqueeze(1).broadcast_to([128, 4, 128]), op=ALU.mult,
                     reads=[o_, gn], writes=[o_])
                P.op("pool", "tensor_tensor", out=o_[:], in0=o_[:], in1=gt[s][:], op=ALU.mult, reads=[o_, gt[s]], writes=[o_])
                ps = bank[it % 8]
                for c in range(4):
                    P.op("pe", "transpose", out=ps[:, c * 128:(c + 1) * 128], in_=o_[:, c * 128:(c + 1) * 128], identity=G.identf[:],
                         reads=[o_, G.identf], writes=[ps])
                P.op("act", "activation", out=yb[s][:, 0:2, :], in_=ps[:, 0:256].rearrange("p (c t) -> p c t", c=2), func=AF.Copy,
                     reads=[ps], writes=[yb[s]])
                P.op("dve", "tensor_copy", out=yb[s][:, 2:4, :], in_=ps[:, 256:512].rearrange("p (c t) -> p c t", c=2),
                     reads=[ps], writes=[yb[s]])
                P.dma("pool", G.yT[b, 4:8, :, tt * 128:(tt + 1) * 128].rearrange("c p t -> p c t"), yb[s][:], yb[s],
                      reads=[yb[s]], writes=[G.yT])


def load_gates(P, G, l, which, name="gate"):
    gts = []
    for j in range(3):
        g = P.sb(f"{name}{j}", [128, D])
        P.dma("sp", g[:], G.modD[l, j:j + 1, which * D:(which + 1) * D].partition_broadcast(128), g,
              reads=[G.modD], writes=[g])
        gts.append(g)
    return gts


def stage_outproj(P, G, yT, wsrc, l, T, ctx_len, xin_ap, xout_ap, xin_t, xout_t):
    bank = G.bank
    with P.scope():
        gts = load_gates(P, G, l, 2)
        w = P.sb("wout", [128, 8, D], BF16)
        for k in range(8):
            P.dma("pool", w[:, k, :], wsrc[k * 128:(k + 1) * 128, :], w, reads=[wsrc], writes=[w])
        yt = [P.sb(f"yt{i}", [128, 8, 128], BF16) for i in range(3)]
        xt = [P.sb(f"xt{i}", [128, D]) for i in range(3)]
        rt = [P.sb(f"rt{i}", [128, D]) for i in range(3)]
        it = 0
        for b in range(NB):
            for tt in range(T // 128):
                s = it % 3
                it += 1
                j = 2 if tt * 128 < ctx_len else b
                P.dma("sp", yt[s][:], yT[b, :, :, tt * 128:(tt + 1) * 128].rearrange("c p t -> p c t"), yt[s],
                      reads=[yT], writes=[yt[s]])
                P.dma("sp", xt[s][:], xin_ap(b, tt), xt[s], reads=[xin_t], writes=[xt[s]])
                for half in range(2):
                    ps = bank[(it % 3) * 2 + half]
                    for c in range(8):
                        mm(P, ps, ps[:, 0:512], yt[s], yt[s][:, c, :], w, w[:, c, half * 512:(half + 1) * 512],
                           start=(c == 0), stop=(c == 7))
                    P.op("dve", "tensor_tensor", out=rt[s][:, half * 512:(half + 1) * 512], in0=ps[:, 0:512],
                         in1=gts[j][:, half * 512:(half + 1) * 512], op=ALU.mult, reads=[ps, gts[j]], writes=[rt[s]])
                P.op("dve", "tensor_tensor", out=rt[s][:], in0=rt[s][:], in1=xt[s][:], op=ALU.add,
                     reads=[rt[s], xt[s]], writes=[rt[s]])
                P.dma("pool", xout_ap(b, tt), rt[s][:], rt[s], reads=[rt[s]], writes=[xout_t])


def swiglu_pass(P, G, fT, blocks, wg_ap, wu_ap, wd_ap, nch, gts, ctx_len, xin_ap, xout_ap, xin_t, xout_t, wsrc_ts,
                tokw=None, tokw_col=None, tiles_per_b=None):
    bank = G.bank
    F_ = nch * 128
    with P.scope():
        wg = P.sb("wg", [128, 8, F_], BF16)
        wu = P.sb("wu", [128, 8, F_], BF16)
        wd = P.sb("wd", [128, nch, D], BF16)
        for k in range(8):
            P.dma("pool", wg[:, k, :], wg_ap[k * 128:(k + 1) * 128, :], wg, reads=wsrc_ts, writes=[wg])
            P.dma("pool", wu[:, k, :], wu_ap[k * 128:(k + 1) * 128, :], wu, reads=wsrc_ts, writes=[wu])
        for c in range(nch):
            P.dma("pool", wd[:, c, :], wd_ap[c * 128:(c + 1) * 128, :], wd, reads=wsrc_ts, writes=[wd])
        ft = [P.sb(f"ft{i}", [128, 8, 512], BF16) for i in range(2)]
        sg = [P.sb(f"sg{i}", [128, 512]) for i in range(2)]
        act = [P.sb(f"act{i}", [128, nch, 512], BF16) for i in range(2)]
        xt = [P.sb(f"fxt{i}", [128, D]) for i in range(2)]
        rt = [P.sb(f"frt{i}", [128, D]) for i in range(2)]
        bi = 0
        ti = 0
        blist = [(b, t0, t1) for b in range(NB) for (t0, t1) in blocks]

        def load_ft(i):
            if i < len(blist):
                b_, a0, a1 = blist[i]
                for k in range(8):
                    P.dma("sp", ft[i % 2][:, k, 0:a1 - a0], fT[b_, :, k, a0:a1], ft[i % 2], reads=[fT], writes=[ft[i % 2]])

        load_ft(0)
        for b in range(NB):
            for (t0, t1) in blocks:
                n = t1 - t0
                F = ft[bi % 2]
                A_ = act[bi % 2]
                bi += 1
                load_ft(bi)
                for jc in range(nch):
                    pg, pu = bank[jc % 2], bank[2 + jc % 2]
                    cs = slice(jc * 128, (jc + 1) * 128)
                    for k in range(8):
                        mm(P, pg, pg[:, 0:n], wg, wg[:, k, cs], F, F[:, k, 0:n], start=(k == 0), stop=(k == 7))
                    for k in range(8):
                        mm(P, pu, pu[:, 0:n], wu, wu[:, k, cs], F, F[:, k, 0:n], start=(k == 0), stop=(k == 7))
                    S_ = sg[jc % 2]
                    P.op("act", "activation", out=S_[:, 0:n], in_=pg[:, 0:n], func=AF.Silu, reads=[pg], writes=[S_])
                    P.op("dve", "tensor_tensor", out=A_[:, jc, 0:n], in0=S_[:, 0:n], in1=pu[:, 0:n], op=ALU.mult,
                         reads=[S_, pu], writes=[A_])
                for ts in range(n // 128):
                    tt = t0 // 128 + ts
                    s = ti % 2
                    ti += 1
                    j = 2 if tt * 128 < ctx_len else b
                    P.dma("sp", xt[s][:], xin_ap(b, tt), xt[s], reads=[xin_t], writes=[xt[s]])
                    for half in range(2):
                        po = bank[4 + (ti % 2) * 2 + half]
                        for jc in range(nch):
                            mm(P, po, po[:, 0:512], A_, A_[:, jc, ts * 128:(ts + 1) * 128], wd,
                               wd[:, jc, half * 512:(half + 1) * 512], start=(jc == 0), stop=(jc == nch - 1))
                        hs = slice(half * 512, (half + 1) * 512)
                        if tokw is None:
                            P.op("dve", "tensor_tensor", out=rt[s][:, hs], in0=po[:, 0:512], in1=gts[j][:, hs], op=ALU.mult,
                                 reads=[po, gts[j]], writes=[rt[s]])
                        else:
                            tix = b * tiles_per_b + tt
                            P.op("dve", "scalar_tensor_tensor", out=rt[s][:, hs], in0=po[:, 0:512],
                                 scalar=tokw[:, tix, tokw_col:tokw_col + 1], in1=gts[j][:, hs], op0=ALU.mult, op1=ALU.mult,
                                 reads=[po, gts[j], tokw], writes=[rt[s]])
                    P.op("dve", "tensor_tensor", out=rt[s][:], in0=rt[s][:], in1=xt[s][:], op=ALU.add,
                         reads=[rt[s], xt[s]], writes=[rt[s]])
                    P.dma("pool", xout_ap(b, tt), rt[s][:], rt[s], reads=[rt[s]], writes=[xout_t])


def stage_ffn0(P, G):
    with P.scope():
        gts = load_gates(P, G, 0, 5)
        for hf in range(2):
            cs = slice(hf * 1408, (hf + 1) * 1408)
            xin_t = G.x1 if hf == 0 else G.x2
            swiglu_pass(P, G, G.fT0, TBLK, G.ffn_wg[:, cs], G.ffn_wu[:, cs], G.ffn_wd[cs, :], 11, gts, TC,
                        (lambda b, tt, X=xin_t: X[b, tt * 128:(tt + 1) * 128, :]),
                        (lambda b, tt: G.x2[b, tt * 128:(tt + 1) * 128, :]), xin_t, G.x2,
                        [G.ffn_wg, G.ffn_wu, G.ffn_wd])


LBLK = [(TC + i * 512, TC + (i + 1) * 512) for i in range(4)]


def stage_att_inproj(P, G, b):
    bank = G.bank
    with P.scope():
        hT = P.sb("hTres", [128, 8, TT], BF16)
        for k in range(8):
            P.dma("sp", hT[:, k, :], G.hT1[b, :, k, :], hT, reads=[G.hT1], writes=[hT])
        rope = P.sb("rope", [128, 2, TL])
        P.dma("sp", rope[:], G.rope[:], rope, reads=[G.rope], writes=[rope])
        perm = P.sb("perm", [128, 128], BF16)
        P.dma("pool", perm[:], G.perm[:], perm, reads=[G.perm], writes=[perm])
        WP = WPrefetch(P, G.att_w_in, list(range(10)))
        wv = P.sb("wv", [128, 8, 256], BF16)
        load_w_chunk(P, wv, G.att_w_in, 1280, 256)
        qraw = [P.sb(f"qraw{i}", [128, 512], BF16) for i in range(2)]
        t1 = [P.sb(f"t1_{i}", [128, 512]) for i in range(2)]
        t2 = [P.sb(f"t2_{i}", [128, 512]) for i in range(2)]
        qo = [P.sb(f"qo{i}", [128, 512], BF16) for i in range(2)]
        kc_ = [P.sb(f"kc{i}", [128, 256], BF16) for i in range(2)]
        vtk = [P.sb(f"vtk{i}", [128, 256], BF16) for i in range(2)]
        it = 0
        for ch in range(10):
            wt = WP.get(ch)
            if ch < 8:
                dst = lambda tl0, n, ch=ch: G.qT[b, 2 * ch:2 * ch + 2, :, tl0:tl0 + n].rearrange("h k t -> (h k) t")
                dt_ = G.qT
            else:
                kc = ch - 8
                dst = lambda tl0, n, kc=kc: G.kT[b, 2 * kc:2 * kc + 2, :, TC + tl0:TC + tl0 + n].rearrange("h k t -> (h k) t")
                dt_ = G.kT
                ps = bank[0]
                for k in range(8):
                    mm(P, ps, ps[:, 0:TC], wt, wt[:, k, :], hT, hT[:, k, 0:TC], start=(k == 0), stop=(k == 7))
                kk_ = kc_[kc % 2]
                P.op("act", "activation", out=kk_[:], in_=ps[:, 0:TC], func=AF.Copy, reads=[ps], writes=[kk_])
                P.dma("pool", G.kT[b, 2 * kc:2 * kc + 2, :, 0:TC].rearrange("h k t -> (h k) t"), kk_[:], kk_,
                      reads=[kk_], writes=[G.kT])
            for (t0, t1_) in LBLK:
                s = it % 2
                it += 1
                tl0 = t0 - TC
                p1, p2 = bank[(it % 2) * 2], bank[(it % 2) * 2 + 1]
                for k in range(8):
                    mm(P, p1, p1[:, 0:512], wt, wt[:, k, :], hT, hT[:, k, t0:t1_], start=(k == 0), stop=(k == 7))
                P.op("act", "activation", out=qraw[s][:], in_=p1[:, 0:512], func=AF.Copy, reads=[p1], writes=[qraw[s]])
                mm(P, p2, p2[:, 0:512], perm, perm[:], qraw[s], qraw[s][:])
                P.op("dve", "tensor_tensor", out=t1[s][:], in0=p1[:, 0:512], in1=rope[:, 0, tl0:tl0 + 512], op=ALU.mult,
                     reads=[p1, rope], writes=[t1[s]])
                P.op("dve", "tensor_tensor", out=t2[s][:], in0=p2[:, 0:512], in1=rope[:, 1, tl0:tl0 + 512], op=ALU.mult,
                     reads=[p2, rope], writes=[t2[s]])
                P.op("dve", "tensor_tensor", out=qo[s][:], in0=t1[s][:], in1=t2[s][:], op=ALU.add,
                     reads=[t1[s], t2[s]], writes=[qo[s]])
                P.dma("pool", dst(tl0, 512), qo[s][:], qo[s], reads=[qo[s]], writes=[dt_])
        for tt in range(18):
            ps = bank[4 + tt % 4]
            for k in range(8):
                mm(P, ps, ps[:, 0:256], hT, hT[:, k, tt * 128:(tt + 1) * 128], wv, wv[:, k, :], start=(k == 0), stop=(k == 7))
            v_ = vtk[tt % 2]
            P.op("act", "activation", out=v_[:], in_=ps[:, 0:256], func=AF.Copy, reads=[ps], writes=[v_])
            P.dma("sp", G.Vt1[b, tt], v_[:], v_, reads=[v_], writes=[G.Vt1])


def stage_attention(P, G):
    bank = G.bank
    with P.scope():
        msk = P.sb("mskb", [128, 4, 128], BF16)
        P.dma("pool", msk[:], G.masks[:], msk, reads=[G.masks], writes=[msk])
        es = P.sb("es", [64, 16])
        P.dma("sp", es[:], G.att_sink[0:1, :].partition_broadcast(64), es, reads=[G.att_sink], writes=[es])
        P.op("act", "activation", out=es[:], in_=es[:], func=AF.Exp, reads=[es], writes=[es])
        ones = P.sb("ones", [128, 64], BF16)
        P.op("dve", "memset", ap=ones[:], constant=1.0, writes=[ones])
        Kt = [P.sb(f"Kt{i}", [64, TT], BF16) for i in range(2)]
        Vv = [P.sb(f"Vv{i}", [128, 18, 64], BF16) for i in range(2)]
        Qt = [P.sb(f"Qt{i}", [64, 4, TL], BF16) for i in range(2)]
        E = [P.sb(f"E{i}", [128, 4, 128], BF16) for i in range(6)]
        den = [P.sb(f"den{i}", [64, 4, 128]) for i in range(2)]
        ob = [P.sb(f"ob{i}", [64, 4, 128], BF16) for i in range(2)]
        g = 0
        r = 0
        ii = 0
        for b in range(NB):
            for hk in range(4):
                K_, V_, Q_ = Kt[g % 2], Vv[g % 2], Qt[g % 2]
                g += 1
                P.dma("sp", K_[:], G.kT[b, hk], K_, reads=[G.kT], writes=[K_])
                P.dma("sp", V_[:], G.Vt1[b, :, :, hk * 64:(hk + 1) * 64].rearrange("n t c -> t n c"), V_,
                      reads=[G.Vt1], writes=[V_])
                P.dma("sp", Q_[:], G.qT[b, 4 * hk:4 * hk + 4].rearrange("g k t -> k g t"), Q_, reads=[G.qT], writes=[Q_])
                for i in range(16):
                    tiles = [(0, None), (1, None)]
                    if i > 0:
                        tiles.append((2 + i - 1, 3))
                    tiles.append((2 + i, None))
                    if i < 15:
                        tiles.append((2 + i + 1, 1))
                    pN, pD = bank[4 + ii % 2], bank[6 + ii % 2]
                    Es = []
                    for ti, (kt, mi) in enumerate(tiles):
                        pS = bank[r % 4]
                        E_ = E[r % 6]
                        r += 1
                        mm(P, pS, pS[:, 0:512], K_, K_[:, kt * 128:(kt + 1) * 128], Q_, Q_[:, :, i * 128:(i + 1) * 128])
                        P.op("act", "activation", out=E_[:], in_=pS[:, 0:512].rearrange("p (g t) -> p g t", g=4),
                             func=AF.Exp, scale=0.125, reads=[pS], writes=[E_])
                        if mi is not None:
                            P.op("dve", "tensor_tensor", out=E_[:], in0=E_[:],
                                 in1=msk[:, mi, :].unsqueeze(1).broadcast_to([128, 4, 128]), op=ALU.mult,
                                 reads=[E_, msk], writes=[E_])
                        Es.append(E_)
                    for ti, (kt, mi) in enumerate(tiles):
                        E_ = Es[ti]
                        first, last = ti == 0, ti == len(tiles) - 1
                        mm(P, pN, pN[0:64, 0:512], V_, V_[:, kt, :], E_, E_[:], start=first, stop=last)
                        mm(P, pD, pD[0:64, 0:512], ones, ones[:], E_, E_[:], start=first, stop=last)
                    s = ii % 2
                    ii += 1
                    P.op("dve", "tensor_tensor", out=den[s][:], in0=pD[0:64, 0:512].rearrange("p (g t) -> p g t", g=4),
                         in1=es[:, 4 * hk:4 * hk + 4].unsqueeze(2).broadcast_to([64, 4, 128]), op=ALU.add,
                         reads=[pD, es], writes=[den[s]])
                    P.op("dve", "reciprocal", out=den[s][:], in_=den[s][:], reads=[den[s]], writes=[den[s]])
                    P.op("dve", "tensor_tensor", out=ob[s][:], in0=pN[0:64, 0:512].rearrange("p (g t) -> p g t", g=4),
                         in1=den[s][:], op=ALU.mult, reads=[pN, den[s]], writes=[ob[s]])
                    P.dma("pool", G.oT1[b, 2 * hk:2 * hk + 2, :, i * 128:(i + 1) * 128].rearrange("c (hh k) t -> k (c hh) t", hh=2),
                          ob[s][:], ob[s], reads=[ob[s]], writes=[G.oT1])


def stage_router(P, G):
    bank = G.bank
    with P.scope():
        rw = P.sb("rw", [128, 8, 8])
        P.dma("sp", rw[:], G.moe_router[:].rearrange("(k p) e -> p k e", p=128), rw, reads=[G.moe_router], writes=[rw])
        rb = P.sb("rb", [128, 8])
        P.dma("sp", rb[:], G.moe_router_b[0:1, :].partition_broadcast(128), rb, reads=[G.moe_router_b], writes=[rb])
        ff = [P.sb(f"ff{i}", [128, 8, 128]) for i in range(2)]
        lg = [P.sb(f"lg{i}", [128, 8]) for i in range(2)]
        tm = [P.sb(f"tm{i}", [128, 4, 8]) for i in range(2)]
        sc = [P.sb(f"rsc{i}", [128, 4]) for i in range(2)]
        it = 0
        for b in range(NB):
            for tt in range(16):
                s = it % 2
                tix = b * 16 + tt
                it += 1
                P.dma("sp", ff[s][:], G.fT1f[b, :, :, tt * 128:(tt + 1) * 128], ff[s], reads=[G.fT1f], writes=[ff[s]])
                ps = bank[it % 2]
                for k in range(8):
                    mm(P, ps, ps[:, 0:8], ff[s], ff[s][:, k, :], rw, rw[:, k, :], start=(k == 0), stop=(k == 7))
                L_, T_, S_ = lg[s], tm[s], sc[s]
                P.op("dve", "tensor_tensor", out=L_[:], in0=ps[:, 0:8], in1=rb[:], op=ALU.add, reads=[ps, rb], writes=[L_])
                P.op("dve", "tensor_reduce", out=S_[:, 0:1], in_=L_[:], axis=AX.X, op=ALU.max, reads=[L_], writes=[S_])
                P.op("dve", "tensor_scalar", out=T_[:, 0, :], in0=L_[:], scalar1=S_[:, 0:1], scalar2=-1e30, op0=ALU.is_equal,
                     op1=ALU.mult, reads=[L_, S_], writes=[T_])
                P.op("dve", "tensor_tensor", out=T_[:, 0, :], in0=T_[:, 0, :], in1=L_[:], op=ALU.add, reads=[T_, L_], writes=[T_])
                P.op("dve", "tensor_reduce", out=S_[:, 1:2], in_=T_[:, 0, :], axis=AX.X, op=ALU.max, reads=[T_], writes=[S_])
                P.op("dve", "tensor_scalar", out=T_[:, 1, :], in0=L_[:], scalar1=S_[:, 1:2], scalar2=None, op0=ALU.is_ge,
                     reads=[L_, S_], writes=[T_])
                P.op("dve", "tensor_scalar", out=S_[:, 2:3], in0=S_[:, 0:1], scalar1=-1.0, scalar2=None, op0=ALU.mult,
                     reads=[S_], writes=[S_])
                P.op("act", "activation", out=T_[:, 2, :], in_=L_[:], func=AF.Exp, bias=S_[:, 2:3], reads=[L_, S_], writes=[T_])
                P.op("dve", "tensor_tensor", out=T_[:, 2, :], in0=T_[:, 2, :], in1=T_[:, 1, :], op=ALU.mult, reads=[T_], writes=[T_])
                P.op("dve", "tensor_reduce", out=S_[:, 3:4], in_=T_[:, 2, :], axis=AX.X, op=ALU.add, reads=[T_], writes=[S_])
                P.op("dve", "reciprocal", out=S_[:, 3:4], in_=S_[:, 3:4], reads=[S_], writes=[S_])
                P.op("dve", "tensor_scalar", out=G.WTm[:, tix, :], in0=T_[:, 2, :], scalar1=S_[:, 3:4], scalar2=None, op0=ALU.mult,
                     reads=[T_, S_], writes=[G.WTm])


def stage_moe(P, G, experts=range(8)):
    with P.scope():
        gts = load_gates(P, G, 1, 5)
        first = True
        blocks = [(i * 512, (i + 1) * 512) for i in range(4)]
        for e in experts:
            for hf in range(2):
                cs = slice(hf * 1408, (hf + 1) * 1408)
                xin_t = G.x3 if first else G.x4
                first = False
                swiglu_pass(P, G, G.fT1, blocks, G.moe_wg[e, :, cs], G.moe_wu[e, :, cs], G.moe_wd[e, cs, :], 11, gts, 0,
                            (lambda b, tt, X=xin_t: X[b, tt * 128:(tt + 1) * 128, :]),
                            (lambda b, tt: G.x4[b, tt * 128:(tt + 1) * 128, :]), xin_t, G.x4,
                            [G.moe_wg, G.moe_wu, G.moe_wd], tokw=G.WTm, tokw_col=e, tiles_per_b=16)


def stage_final(P, G, src):
    with P.scope():
        gf = P.sb("gfin", [128, D])
        P.dma("sp", gf[:], G.norm_final[0:1, :].partition_broadcast(128), gf, reads=[G.norm_final], writes=[gf])
        xt = [P.sb(f"zxt{i}", [128, D]) for i in range(2)]
        sq = P.sb("zsq", [128, D], BF16)
        ss = [P.sb(f"zss{i}", [128, 1]) for i in range(2)]
        yo = [P.sb(f"zyo{i}", [128, D]) for i in range(2)]
        it = 0
        for b in range(NB):
            for tt in range(16):
                s = it % 2
                it += 1
                P.dma("sp", xt[s][:], src[b, tt * 128:(tt + 1) * 128, :], xt[s], reads=[src], writes=[xt[s]])
                P.op("act", "activation", out=sq[:], in_=xt[s][:], func=AF.Square, accum_out=ss[s][:], reads=[xt[s]], writes=[sq, ss[s]])
                P.op("act", "activation", out=ss[s][:], in_=ss[s][:], func=AF.Sqrt, scale=1.0 / D, bias=G.eps[:, 0:1],
                     reads=[ss[s], G.eps], writes=[ss[s]])
                P.op("dve", "reciprocal", out=ss[s][:], in_=ss[s][:], reads=[ss[s]], writes=[ss[s]])
                P.op("dve", "scalar_tensor_tensor", out=yo[s][:], in0=xt[s][:], scalar=ss[s][:, 0:1], in1=gf[:], op0=ALU.mult,
                     op1=ALU.mult, reads=[xt[s], ss[s], gf], writes=[yo[s]])
                P.dma("pool", G.out[b, tt * 128:(tt + 1) * 128, :], yo[s][:], yo[s], reads=[yo[s]], writes=[G.out])


I32 = mybir.dt.int32
BS = 512
SUBS = BS // 512
NBLK = (NB * TL * 2) // BS + 8
NSLOT = NBLK * BS
DUMMY = NB * TL


def stage_moe_sparse(P, G, nblk=NBLK):
    bank = G.bank
    IOA = bass.IndirectOffsetOnAxis
    with P.scope():
        WAB = P.sb("WAB", [128, 32, 2])
        SAB = P.sb("SAB", [128, 2, 32], I32)
        IDXG = P.sb("IDXG", [128, NBLK, 2], I32)
        IDXD = P.sb("IDXD", [128, NBLK, 2], I32)
        _phaseA = P.scope()
        _phaseA.__enter__()
        LG = G.LG
        T3 = lambda nm: P.sb(nm, [128, 32, 8])
        T2 = lambda nm: P.sb(nm, [128, 32])
        bc3 = lambda t2: t2[:].unsqueeze(2).broadcast_to([128, 32, 8])
        M1, M2, NM1, DEN = T2("M1"), T2("M2"), T2("NM1"), T2("DEN")
        TMP, SEL, WT = T3("TMP"), T3("SEL"), T3("WT")
        P.op("dve", "tensor_reduce", out=M1[:], in_=LG[:], axis=AX.X, op=ALU.max, reads=[LG], writes=[M1])
        P.op("dve", "tensor_tensor", out=TMP[:], in0=LG[:], in1=bc3(M1), op=ALU.is_equal, reads=[LG, M1], writes=[TMP])
        P.op("dve", "scalar_tensor_tensor", out=TMP[:], in0=TMP[:], scalar=-1e30, in1=LG[:], op0=ALU.mult, op1=ALU.add,
             reads=[TMP, LG], writes=[TMP])
        P.op("dve", "tensor_reduce", out=M2[:], in_=TMP[:], axis=AX.X, op=ALU.max, reads=[TMP], writes=[M2])
        P.op("dve", "tensor_tensor", out=SEL[:], in0=LG[:], in1=bc3(M2), op=ALU.is_ge, reads=[LG, M2], writes=[SEL])
        P.op("dve", "tensor_tensor", out=TMP[:], in0=LG[:], in1=bc3(M1), op=ALU.subtract, reads=[LG, M1], writes=[TMP])
        P.op("act", "activation", out=TMP[:], in_=TMP[:], func=AF.Exp, reads=[TMP], writes=[TMP])
        P.op("dve", "tensor_tensor", out=TMP[:], in0=TMP[:], in1=SEL[:], op=ALU.mult, reads=[TMP, SEL], writes=[TMP])
        P.op("dve", "tensor_reduce", out=DEN[:], in_=TMP[:], axis=AX.X, op=ALU.add, reads=[TMP], writes=[DEN])
        P.op("dve", "reciprocal", out=DEN[:], in_=DEN[:], reads=[DEN], writes=[DEN])
        P.op("dve", "tensor_tensor", out=WT[:], in0=TMP[:], in1=bc3(DEN), op=ALU.mult, reads=[TMP, DEN], writes=[WT])
        cst = P.sb("mcst", [128, 512])
        P.dma("sp", cst[:], G.moe_consts[:], cst, reads=[G.moe_consts], writes=[cst])
        TH = cst[:, 0:64].rearrange("p (e m) -> p e m", m=8)
        JJ = cst[:, 64:64 + NBLK]
        CG = cst[:, 88:90]
        CD = cst[:, 104:106]
        TIDc = cst[:, 128:160]
        RM = cst[:, 256:512]
        selb = P.sb("selb", [128, 256], BF16)
        P.op("dve", "tensor_copy", out=selb[:], in_=SEL[:].rearrange("p i e -> p (i e)"), reads=[SEL], writes=[selb])
        mb_ = P.sb("mskb2", [128, 4, 128], BF16)
        P.dma("pool", mb_[:], G.masks[:], mb_, reads=[G.masks], writes=[mb_])
        onesb = P.sb("onesb", [128, 128], BF16)
        P.op("dve", "memset", ap=onesb[:], constant=1.0, writes=[onesb])
        mm(P, bank[2], bank[2][:, 0:256], mb_, mb_[:, 0, :], selb, selb[:])
        mm(P, bank[3], bank[3][:, 0:256], onesb, onesb[:], selb, selb[:])
        SLOT = T3("SLOT")
        P.op("dve", "tensor_copy", out=SLOT[:], in_=bank[2][:, 0:256].rearrange("p (i e) -> p i e", e=8), reads=[bank[2]], writes=[SLOT])
        TOTp = P.sb("TOTp", [128, 8, 32])
        P.op("dve", "tensor_copy", out=TOTp[:], in_=bank[3][:, 0:256].rearrange("p (i e) -> p e i", e=8), reads=[bank[3]], writes=[TOTp])
        INC = P.sb("INC", [128, 8, 32])
        P.op("dve", "tensor_tensor_scan", out=INC[:].rearrange("p e i -> p (e i)"), data0=RM, data1=TOTp[:].rearrange("p e i -> p (e i)"),
             initial=0.0, op0=ALU.mult, op1=ALU.add, reads=[cst, TOTp], writes=[INC])
        OFFp = P.sb("OFFp", [128, 8, 32])
        P.op("dve", "tensor_tensor", out=OFFp[:], in0=INC[:], in1=TOTp[:], op=ALU.subtract, reads=[INC, TOTp], writes=[OFFp])
        CMP = P.sb("CMP", [128, 8, 8])
        P.op("dve", "tensor_tensor", out=CMP[:], in0=INC[:, :, 31:32].broadcast_to([128, 8, 8]), in1=TH, op=ALU.is_gt,
             reads=[INC, cst], writes=[CMP])
        nbk = P.sb("nbk", [128, 8])
        P.op("dve", "tensor_reduce", out=nbk[:], in_=CMP[:], axis=AX.X, op=ALU.add, reads=[CMP], writes=[nbk])
        PEI = P.sb("PEI", [128, 8])
        P.op("dve", "tensor_tensor_scan", out=PEI[:], data0=onesb[:, 0:8], data1=nbk[:], initial=0.0, op0=ALU.mult, op1=ALU.add,
             reads=[onesb, nbk], writes=[PEI])
        PST = P.sb("PST", [128, 8])
        P.op("dve", "tensor_tensor", out=PST[:], in0=PEI[:], in1=nbk[:], op=ALU.subtract, reads=[PEI, nbk], writes=[PST])
        P.op("dve", "tensor_scalar", out=PST[:], in0=PST[:], scalar1=float(BS), scalar2=None, op0=ALU.mult, reads=[PST], writes=[PST])
        P.op("dve", "tensor_tensor", out=SLOT[:], in0=SLOT[:], in1=OFFp[:].rearrange("p e i -> p i e"), op=ALU.add,
             reads=[SLOT, OFFp], writes=[SLOT])
        P.op("dve", "tensor_tensor", out=SLOT[:], in0=SLOT[:], in1=PST[:].unsqueeze(1).broadcast_to([128, 32, 8]), op=ALU.add,
             reads=[SLOT, PST], writes=[SLOT])
        V = T3("V")
        P.op("dve", "scalar_tensor_tensor", out=V[:], in0=SLOT[:], scalar=1.0, in1=SEL[:], op0=ALU.add, op1=ALU.mult,
             reads=[SLOT, SEL], writes=[V])
        MA, MB = T2("MA"), T2("MB")
        P.op("dve", "tensor_reduce", out=MA[:], in_=V[:], axis=AX.X, op=ALU.max, reads=[V], writes=[MA])
        P.op("dve", "tensor_tensor", out=TMP[:], in0=V[:], in1=bc3(MA), op=ALU.is_equal, reads=[V, MA], writes=[TMP])
        IS2 = T3("IS2")
        P.op("dve", "tensor_tensor", out=IS2[:], in0=TMP[:], in1=WT[:], op=ALU.mult, reads=[TMP, WT], writes=[IS2])
        P.op("dve", "tensor_reduce", out=WAB[:, :, 0], in_=IS2[:], axis=AX.X, op=ALU.add, reads=[IS2], writes=[WAB])
        P.op("dve", "tensor_tensor", out=TMP[:], in0=TMP[:], in1=V[:], op=ALU.mult, reads=[TMP, V], writes=[TMP])
        P.op("dve", "tensor_tensor", out=V[:], in0=V[:], in1=TMP[:], op=ALU.subtract, reads=[V, TMP], writes=[V])
        P.op("dve", "tensor_reduce", out=MB[:], in_=V[:], axis=AX.X, op=ALU.max, reads=[V], writes=[MB])
        P.op("dve", "tensor_tensor", out=TMP[:], in0=V[:], in1=bc3(MB), op=ALU.is_equal, reads=[V, MB], writes=[TMP])
        P.op("dve", "tensor_tensor", out=IS2[:], in0=TMP[:], in1=WT[:], op=ALU.mult, reads=[TMP, WT], writes=[IS2])
        P.op("dve", "tensor_reduce", out=WAB[:, :, 1], in_=IS2[:], axis=AX.X, op=ALU.add, reads=[IS2], writes=[WAB])
        P.op("dve", "tensor_scalar", out=MA[:], in0=MA[:], scalar1=-1.0, scalar2=None, op0=ALU.add, reads=[MA], writes=[MA])
        P.op("dve", "tensor_scalar", out=MB[:], in0=MB[:], scalar1=-1.0, scalar2=None, op0=ALU.add, reads=[MB], writes=[MB])
        P.op("dve", "tensor_copy", out=SAB[:, 0, :], in_=MA[:], reads=[MA], writes=[SAB])
        P.op("dve", "tensor_copy", out=SAB[:, 1, :], in_=MB[:], reads=[MB], writes=[SAB])
        CJ = P.sb("CJ", [128, NBLK, 8])
        P.op("dve", "tensor_tensor", out=CJ[:], in0=PEI[:].unsqueeze(1).broadcast_to([128, NBLK, 8]),
             in1=JJ.unsqueeze(2).broadcast_to([128, NBLK, 8]), op=ALU.is_le, reads=[PEI, cst], writes=[CJ])
        EJ = P.sb("EJ", [128, NBLK])
        P.op("dve", "tensor_reduce", out=EJ[:], in_=CJ[:], axis=AX.X, op=ALU.add, reads=[CJ], writes=[EJ])
        P.op("dve", "tensor_scalar", out=EJ[:], in0=EJ[:], scalar1=7.0, scalar2=None, op0=ALU.min, reads=[EJ], writes=[EJ])
        IGf = P.sb("IGf", [128, NBLK, 2])
        P.op("dve", "scalar_tensor_tensor", out=IGf[:], in0=EJ[:].unsqueeze(2).broadcast_to([128, NBLK, 2]), scalar=256.0,
             in1=CG.unsqueeze(1).broadcast_to([128, NBLK, 2]), op0=ALU.mult, op1=ALU.add, reads=[EJ, cst], writes=[IGf])
        P.op("dve", "tensor_copy", out=IDXG[:], in_=IGf[:], reads=[IGf], writes=[IDXG])
        IDf = P.sb("IDf", [128, NBLK, 2])
        P.op("dve", "scalar_tensor_tensor", out=IDf[:], in0=EJ[:].unsqueeze(2).broadcast_to([128, NBLK, 2]), scalar=256.0,
             in1=CD.unsqueeze(1).broadcast_to([128, NBLK, 2]), op0=ALU.mult, op1=ALU.add, reads=[EJ, cst], writes=[IDf])
        P.op("dve", "tensor_copy", out=IDXD[:], in_=IDf[:], reads=[IDf], writes=[IDXD])
        ini = P.sb("ini", [128, NSLOT // 128], I32)
        P.op("dve", "memset", ap=ini[:], constant=DUMMY, writes=[ini])
        P.dma("sp", G.tokidx[:, 0].rearrange("(p c) -> p c", c=NSLOT // 128), ini[:], ini, reads=[ini], writes=[G.tokidx])
        tid = P.sb("tid", [128, 32], I32)
        P.op("dve", "tensor_copy", out=tid[:], in_=TIDc, reads=[cst], writes=[tid])
        for i in range(32):
            for ab in range(2):
                P.dma("pool", None, None, tid, reads=[tid, SAB], writes=[G.tokidx], meth="indirect_dma_start",
                      out=G.tokidx[:, :], out_offset=IOA(ap=SAB[:, ab, i:i + 1], axis=0), in_=tid[:, i:i + 1], in_offset=None)
        _phaseA.__exit__(None, None, None)
        WG2 = G.moe_wg[:, :]
        WU2 = G.moe_wu[:, :]
        WD2 = G.moe_wd[:, :]
        with P.scope():
            wg = [P.sb(f"swg{i}", [128, 8, 1408], BF16) for i in range(2)]
            wu = [P.sb(f"swu{i}", [128, 8, 1408], BF16) for i in range(2)]
            wd = [P.sb(f"swd{i}", [128, 11, D], BF16) for i in range(2)]
            idx = [P.sb(f"sidx{i}", [128, 1], I32) for i in range(8)]
            xg = [P.sb(f"sxg{i}", [128, D]) for i in range(4)]
            xTs = [P.sb(f"sxT{i}", [128, SUBS, 8, 512], BF16) for i in range(2)]
            sg = [P.sb(f"ssg{i}", [128, 512]) for i in range(2)]
            act = P.sb("sact", [128, 11, 512], BF16)
            yt = [P.sb(f"syt{i}", [128, D]) for i in range(3)]
            yi = 0
            gstate = {"gi": 0}

            def emit_gather(j):
                xT = xTs[j % 2]
                gi = gstate["gi"]
                for sub in range(SUBS):
                    for c in range(4):
                        ix = idx[gi % 8]
                        X_ = xg[gi % 4]
                        gi += 1
                        r0 = j * BS + sub * 512 + c * 128
                        P.dma("sp", ix[:], G.tokidx[r0:r0 + 128, :], ix, reads=[G.tokidx], writes=[ix])
                        P.dma("pool", None, None, X_, reads=[ix, G.f_tok], writes=[X_], meth="indirect_dma_start",
                              out=X_[:], out_offset=None, in_=G.f_tok[:, :], in_offset=IOA(ap=ix[:, 0:1], axis=0))
                        for k in range(8):
                            ps = bank[4 + (gi % 2) * 2 + k // 4]
                            P.op("pe", "transpose", out=ps[:, (k % 4) * 128:(k % 4 + 1) * 128], in_=X_[:, k * 128:(k + 1) * 128],
                                 identity=G.identf[:], reads=[X_, G.identf], writes=[ps])
                        for kh in range(2):
                            ps = bank[4 + (gi % 2) * 2 + kh]
                            P.op("act" if kh else "dve", "activation" if kh else "tensor_copy",
                                 out=xT[:, sub, kh * 4:(kh + 1) * 4, c * 128:(c + 1) * 128],
                                 in_=ps[:, 0:512].rearrange("p (k t) -> p k t", k=4), reads=[ps], writes=[xT],
                                 **({"func": AF.Copy} if kh else {}))
                gstate["gi"] = gi

            emit_gather(0)
            for j in range(nblk):
                xT = xTs[j % 2]
                for hf in range(2):
                    s = (2 * j + hf) % 2
                    if not (os.environ.get("NO_WGATHER") and j > 0):
                      P.dma("pool", None, None, wg[s], reads=[IDXG, G.moe_wg], writes=[wg[s]], meth="indirect_dma_start",
                          out=wg[s][:].rearrange("p k c -> p (k c)"), out_offset=None, in_=WG2, in_offset=IOA(ap=IDXG[:, j, hf:hf + 1], axis=0))
                    if not (os.environ.get("NO_WGATHER") and j > 0):
                      P.dma("pool", None, None, wu[s], reads=[IDXG, G.moe_wu], writes=[wu[s]], meth="indirect_dma_start",
                          out=wu[s][:].rearrange("p k c -> p (k c)"), out_offset=None, in_=WU2, in_offset=IOA(ap=IDXG[:, j, hf:hf + 1], axis=0))
                    if not (os.environ.get("NO_WGATHER") and j > 0):
                      P.dma("pool", None, None, wd[s], reads=[IDXD, G.moe_wd], writes=[wd[s]], meth="indirect_dma_start",
                          out=wd[s][:].rearrange("p c n -> p (c n)"), out_offset=None, in_=WD2, in_offset=IOA(ap=IDXD[:, j, hf:hf + 1], axis=0))
                    for sub in range(SUBS):
                        for jc in range(11):
                            pg, pu = bank[jc % 2], bank[2 + jc % 2]
                            cs = slice(jc * 128, (jc + 1) * 128)
                            for k in range(8):
                                mm(P, pg, pg[:, 0:512], wg[s], wg[s][:, k, cs], xT, xT[:, sub, k, :], start=(k == 0), stop=(k == 7))
                            for k in range(8):
                                mm(P, pu, pu[:, 0:512], wu[s], wu[s][:, k, cs], xT, xT[:, sub, k, :], start=(k == 0), stop=(k == 7))
                            S_ = sg[jc % 2]
                            P.op("act", "activation", out=S_[:], in_=pg[:, 0:512], func=AF.Silu, reads=[pg], writes=[S_])
                            P.op("dve", "tensor_tensor", out=act[:, jc, :], in0=S_[:], in1=pu[:, 0:512], op=ALU.mult,
                                 reads=[S_, pu], writes=[act])
                        if hf == 0 and sub == SUBS - 1 and j + 1 < nblk:
                            emit_gather(j + 1)
                        for tt in range(4):
                            Y_ = yt[yi % 3]
                            yi += 1
                            for half in range(2):
                                po = bank[4 + (tt * 2 + half) % 4]
                                for jc in range(11):
                                    mm(P, po, po[:, 0:512], act, act[:, jc, tt * 128:(tt + 1) * 128], wd[s],
                                       wd[s][:, jc, half * 512:(half + 1) * 512], start=(jc == 0), stop=(jc == 10))
                                hs = slice(half * 512, (half + 1) * 512)
                                if half == 0:
                                    P.op("act", "activation", out=Y_[:, hs], in_=po[:, 0:512], func=AF.Copy, reads=[po], writes=[Y_])
                                else:
                                    P.op("dve", "tensor_copy", out=Y_[:, hs], in_=po[:, 0:512], reads=[po], writes=[Y_])
                            r0 = j * BS + sub * 512 + tt * 128
                            P.dma("sp", G.Yb[r0:r0 + 128, hf, :], Y_[:], Y_, reads=[Y_], writes=[G.Yb])
        with P.scope():
            gts = load_gates(P, G, 1, 5)
            gf = P.sb("gfin", [128, D])
            P.dma("sp", gf[:], G.norm_final[0:1, :].partition_broadcast(128), gf, reads=[G.norm_final], writes=[gf])
            ya = [P.sb(f"cya{i}", [128, D]) for i in range(2)]
            yb = [P.sb(f"cyb{i}", [128, D]) for i in range(2)]
            yy = [P.sb(f"cyy{i}", [128, 2, D]) for i in range(2)]
            zz = [P.sb(f"czz{i}", [128, 2, D]) for i in range(2)]
            xt = [P.sb(f"cxt{i}", [128, D]) for i in range(2)]
            sq = P.sb("csq", [128, D], BF16)
            ss = [P.sb(f"css{i}", [128, 1]) for i in range(2)]
            for b in range(NB):
                for tt in range(16):
                    i = b * 16 + tt
                    s = i % 2
                    for (dst_, ab) in ((yy[s], 0), (zz[s], 1)):
                        P.dma("pool", None, None, dst_, reads=[SAB, G.Yb], writes=[dst_], meth="indirect_dma_start",
                              out=dst_[:].rearrange("p h d -> p (h d)"), out_offset=None, in_=G.Yb[:].rearrange("n h d -> n (h d)"),
                              in_offset=IOA(ap=SAB[:, ab, i:i + 1], axis=0))
                    P.op("dve", "tensor_tensor", out=ya[s][:], in0=yy[s][:, 0, :], in1=yy[s][:, 1, :], op=ALU.add, reads=[yy[s]], writes=[ya[s]])
                    P.op("dve", "tensor_tensor", out=yb[s][:], in0=zz[s][:, 0, :], in1=zz[s][:, 1, :], op=ALU.add, reads=[zz[s]], writes=[yb[s]])
                    P.dma("sp", xt[s][:], G.x3[b, tt * 128:(tt + 1) * 128, :], xt[s], reads=[G.x3], writes=[xt[s]])
                    P.op("dve", "tensor_scalar", out=ya[s][:], in0=ya[s][:], scalar1=WAB[:, i, 0:1], scalar2=None, op0=ALU.mult,
                         reads=[ya[s], WAB], writes=[ya[s]])
                    P.op("dve", "scalar_tensor_tensor", out=ya[s][:], in0=yb[s][:], scalar=WAB[:, i, 1:2], in1=ya[s][:],
                         op0=ALU.mult, op1=ALU.add, reads=[ya[s], yb[s], WAB], writes=[ya[s]])
                    P.op("dve", "tensor_tensor", out=ya[s][:], in0=ya[s][:], in1=gts[b][:], op=ALU.mult, reads=[ya[s], gts[b]], writes=[ya[s]])
                    P.op("dve", "tensor_tensor", out=xt[s][:], in0=xt[s][:], in1=ya[s][:], op=ALU.add, reads=[xt[s], ya[s]], writes=[xt[s]])
                    P.op("act", "activation", out=sq[:], in_=xt[s][:], func=AF.Square, accum_out=ss[s][:], reads=[xt[s]], writes=[sq, ss[s]])
                    P.op("act", "activation", out=ss[s][:], in_=ss[s][:], func=AF.Sqrt, scale=1.0 / D, bias=G.eps[:, 0:1],
                         reads=[ss[s], G.eps], writes=[ss[s]])
                    P.op("dve", "reciprocal", out=ss[s][:], in_=ss[s][:], reads=[ss[s]], writes=[ss[s]])
                    P.op("dve", "scalar_tensor_tensor", out=yb[s][:], in0=xt[s][:], scalar=ss[s][:, 0:1], in1=gf[:], op0=ALU.mult,
                         op1=ALU.mult, reads=[xt[s], ss[s], gf], writes=[yb[s]])
                    P.dma("sp", G.out[b, tt * 128:(tt + 1) * 128, :], yb[s][:], yb[s], reads=[yb[s]], writes=[G.out])


def stage_norm_tok(P, G):
    with P.scope():
        grow = P.sb("grow", [128, D])
        P.dma("sp", grow[:], G.norm_ffn_row[1:2, :].partition_broadcast(128), grow, reads=[G.norm_ffn_row], writes=[grow])
        GSb, SHb = [], []
        for j in range(2):
            g_ = P.sb(f"GSb{j}", [128, D])
            P.dma("sp", g_[:], G.modD[1, j:j + 1, 4 * D:5 * D].partition_broadcast(128), g_, reads=[G.modD], writes=[g_])
            P.op("dve", "scalar_tensor_tensor", out=g_[:], in0=g_[:], scalar=1.0, in1=grow[:], op0=ALU.add, op1=ALU.mult,
                 reads=[g_, grow], writes=[g_])
            GSb.append(g_)
            h_ = P.sb(f"SHb{j}", [128, D])
            P.dma("sp", h_[:], G.modD[1, j:j + 1, 3 * D:4 * D].partition_broadcast(128), h_, reads=[G.modD], writes=[h_])
            SHb.append(h_)
        z = P.sb("zrow", [128, D])
        P.op("dve", "memset", ap=z[:], constant=0.0, writes=[z])
        P.dma("sp", G.f_tok[NB * TL:NB * TL + 128, :], z[:], z, reads=[z], writes=[G.f_tok])
        rw = P.sb("rw", [128, 8, 8])
        P.dma("sp", rw[:], G.moe_router[:].rearrange("(k p) e -> p k e", p=128), rw, reads=[G.moe_router], writes=[rw])
        rb = P.sb("rb", [128, 8])
        P.dma("sp", rb[:], G.moe_router_b[0:1, :].partition_broadcast(128), rb, reads=[G.moe_router_b], writes=[rb])
        fT = [P.sb(f"nfT{i}", [128, 8, 128]) for i in range(2)]
        xt = [P.sb(f"nxt{i}", [128, D]) for i in range(2)]
        sq = P.sb("nsq", [128, D], BF16)
        ss = [P.sb(f"nss{i}", [128, 1]) for i in range(2)]
        fo = [P.sb(f"nfo{i}", [128, D]) for i in range(2)]
        it = 0
        for b in range(NB):
            for tt in range(16):
                s = it % 2
                it += 1
                P.dma("sp", xt[s][:], G.x3[b, tt * 128:(tt + 1) * 128, :], xt[s], reads=[G.x3], writes=[xt[s]])
                P.op("act", "activation", out=sq[:], in_=xt[s][:], func=AF.Square, accum_out=ss[s][:], reads=[xt[s]], writes=[sq, ss[s]])
                P.op("act", "activation", out=ss[s][:], in_=ss[s][:], func=AF.Sqrt, scale=1.0 / D, bias=G.eps[:, 0:1],
                     reads=[ss[s], G.eps], writes=[ss[s]])
                P.op("dve", "reciprocal", out=ss[s][:], in_=ss[s][:], reads=[ss[s]], writes=[ss[s]])
                P.op("dve", "scalar_tensor_tensor", out=fo[s][:], in0=xt[s][:], scalar=ss[s][:, 0:1], in1=GSb[b][:], op0=ALU.mult,
                     op1=ALU.mult, reads=[xt[s], ss[s], GSb[b]], writes=[fo[s]])
                P.op("dve", "tensor_tensor", out=fo[s][:], in0=fo[s][:], in1=SHb[b][:], op=ALU.add, reads=[fo[s], SHb[b]], writes=[fo[s]])
                P.dma("pool", G.f_tok[b * TL + tt * 128:b * TL + (tt + 1) * 128, :], fo[s][:], fo[s], reads=[fo[s]], writes=[G.f_tok])
                tix = b * 16 + tt
                for k in range(8):
                    ps = G.bank[(it % 2) * 2 + k // 4]
                    P.op("pe", "transpose", out=ps[:, (k % 4) * 128:(k % 4 + 1) * 128], in_=fo[s][:, k * 128:(k + 1) * 128],
                         identity=G.identf[:], reads=[fo[s], G.identf], writes=[ps])
                for kh in range(2):
                    ps = G.bank[(it % 2) * 2 + kh]
                    if kh:
                        P.op("act", "activation", out=fT[s][:, 4:8, :], in_=ps[:, 0:512].rearrange("p (k t) -> p k t", k=4),
                             func=AF.Copy, reads=[ps], writes=[fT[s]])
                    else:
                        P.op("dve", "tensor_copy", out=fT[s][:, 0:4, :], in_=ps[:, 0:512].rearrange("p (k t) -> p k t", k=4),
                             reads=[ps], writes=[fT[s]])
                pl = G.bank[4 + it % 2]
                for k in range(8):
                    mm(P, pl, pl[:, 0:8], fT[s], fT[s][:, k, :], rw, rw[:, k, :], start=(k == 0), stop=(k == 7))
                P.op("dve", "tensor_tensor", out=G.LG[:, tix, :], in0=pl[:, 0:8], in1=rb[:], op=ALU.add, reads=[pl, rb], writes=[G.LG])


def build(upto="all", dbg=()):
    nc = bass.Bass("TRN2", target_bir_lowering=False)
    P = Prog(nc)
    G = Ctx(P)
    nc.G = G
    kinds = lambda n: "ExternalOutput" if n in dbg else "Internal"
    S = lambda n, s, dt=F32: P.dram(n, s, dt, kind=kinds(n))
    G.out = P.dram("out", [NB, TL, D], F32, kind="ExternalOutput")
    G.hT0 = S("hT0", [NB, 128, 8, TT], BF16)
    G.RS = [S(f"RS{d}", [NB, 8, 64, 2, W], BF16) for d in range(2)]
    G.BK = [S(f"BK{d}", [NB, 8, 64, 2, W], BF16) for d in range(2)]
    G.BKt = [S(f"BKt{d}", [NB, 18, 128, 2, 512], BF16) for d in range(2)]
    G.GL = [S(f"GL{d}", [NB, 8, 64, 18]) for d in range(2)]
    G.Vt = S("Vt", [NB, 18, 128, 512], BF16)
    G.gT = S("gT", [NB, 4, 128, W])
    G.vT = S("vT", [NB, 4, 128, W])
    G.bcT = S("bcT", [NB, 4, 128, W])
    G.oA = [S(f"oA{d}", [NB, 18, 128, 512]) for d in range(2)]
    G.yT = S("yT", [NB, 8, 128, TT], BF16)
    G.x1 = S("x1", [NB, TT, D])
    G.x2 = S("x2", [NB, TT, D])
    G.fT0 = S("fT0", [NB, 128, 8, TT], BF16)
    G.hT1 = S("hT1", [NB, 128, 8, TT], BF16)
    G.qT = S("qT", [NB, 16, 64, TL], BF16)
    G.kT = S("kT", [NB, 4, 64, TT], BF16)
    G.Vt1 = S("Vt1", [NB, 18, 128, 256], BF16)
    G.oT1 = S("oT1", [NB, 8, 128, TL], BF16)
    G.x3 = S("x3", [NB, TL, D])
    G.x4 = S("x4", [NB, TL, D])
    G.fT1 = S("fT1", [NB, 128, 8, TL], BF16)
    G.fT1f = S("fT1f", [NB, 128, 8, TL], F32)
    G.f_tok = S("f_tok", [NB * TL + 128, D])
    G.Yb = S("Yb", [NSLOT, 2, D])
    G.tokidx = P.dram("tokidx", [NSLOT, 1], I32, kind=kinds("tokidx"))
    G.HQK = [S(f"HQK{d}", [NB, 4, 128, 2, TT], BF16) for d in range(2)]
    G.HKt = [S(f"HKt{d}", [NB, 18, 128, 512], BF16) for d in range(2)]
    G.HGL = [S(f"HGL{d}", [NB, 4, 128, 36]) for d in range(2)]
    G.HVt = S("HVt", [NB, 18, 128, 512], BF16)
    G.HGt = S("HGt", [NB, 18, 128, 512], BF16)
    G.oH = [S(f"oH{d}", [NB, 18, 128, 512]) for d in range(2)]
    setup_globals(P, G)
    G.modD = S("modD", [2, 3, 6 * D])
    outs = [G.out]

    def done():
        pass
        with P.scope():
            pass
        P.emit()
        return nc

    stage_adaln(P, G)
    if upto == "adaln":
        return done()
    if upto.startswith("l1"):
        G.x2 = P.dram("x2in", [NB, TT, D], F32, kind="ExternalInput")
        G.used_inputs.append("x2in")
        stage_norm(P, G, G.x2, TT, 1, 0, G.hT1, TC)
        if upto == "l1n":
            return done()
        for b in range(NB):
            stage_att_inproj(P, G, b)
        if upto == "l1i":
            return done()
        stage_attention(P, G)
        if upto == "l1t":
            return done()
        stage_outproj(P, G, G.oT1, G.att_w_out, 1, TL, 0, lambda b, tt: G.x2[b, TC + tt * 128:TC + (tt + 1) * 128, :],
                      lambda b, tt: G.x3[b, tt * 128:(tt + 1) * 128, :], G.x2, G.x3)
        if upto == "l1a":
            return done()
        if upto == "l1d":
            stage_norm(P, G, G.x3, TL, 1, 1, G.fT1, 0, dst32=G.fT1f)
            stage_router(P, G)
            stage_moe(P, G)
            stage_final(P, G, G.x4)
            return done()
        stage_norm_tok(P, G)
        stage_moe_sparse(P, G, nblk=(2 if upto == "l1s2" else NBLK))
        return done()
    stage_norm(P, G, G.xin, TT, 0, 0, G.hT0, TC)
    if upto == "norm0":
        return done()
    for b in range(NB if upto != "rwf1" else 1):
        stage_rwkv_feat(P, G, b)
    if upto in ("rwf", "rwf1"):
        return done()
    if upto == "hg1":
        stage_hgrn_feat(P, G, 0)
        stage_hgrn_scan(P, G, bs=(0,))
        return done()
    if upto == "rws1":
        stage_rwkv_scan(P, G, bs=(0,), ds=(0, 1), nmax=18)
        return done()
    stage_rwkv_scan(P, G)
    if upto == "rws":
        return done()
    stage_rwkv_post(P, G)
    if upto == "rwp":
        return done()
    for b in range(NB):
        stage_hgrn_feat(P, G, b)
    stage_hgrn_scan(P, G)
    stage_hgrn_post(P, G)
    if upto == "hgp":
        return done()
    stage_outproj(P, G, G.yT, G.rec_w_out, 0, TT, TC, lambda b, tt: G.xin[b, tt * 128:(tt + 1) * 128, :],
                  lambda b, tt: G.x1[b, tt * 128:(tt + 1) * 128, :], G.xin, G.x1)
    if upto == "x1":
        return done()
    stage_norm(P, G, G.x1, TT, 0, 1, G.fT0, TC)
    stage_ffn0(P, G)
    if upto == "x2":
        return done()
    stage_norm(P, G, G.x2, TT, 1, 0, G.hT1, TC)
    for b in range(NB):
        stage_att_inproj(P, G, b)
    stage_attention(P, G)
    stage_outproj(P, G, G.oT1, G.att_w_out, 1, TL, 0, lambda b, tt: G.x2[b, TC + tt * 128:TC + (tt + 1) * 128, :],
                  lambda b, tt: G.x3[b, tt * 128:(tt + 1) * 128, :], G.x2, G.x3)
    if upto == "x3":
        return done()
    if upto == "dense":
        stage_norm(P, G, G.x3, TL, 1, 1, G.fT1, 0, dst32=G.fT1f)
        stage_router(P, G)
        stage_moe(P, G)
        stage_final(P, G, G.x4)
        return done()
    stage_norm_tok(P, G)
    stage_moe_sparse(P, G)
    return done()


def host_prep(inp, core):
    f = lambda a: np.ascontiguousarray(a, dtype=np.float32)
    b0 = core * NB
    m = {}
    m["xin"] = f(np.concatenate([inp["ctx"][b0:b0 + NB], inp["x"][b0:b0 + NB]], axis=1))
    cv = np.stack([inp["c"][b0], inp["c"][b0 + 1], inp["c_ctx"]], axis=-1)
    m["cT"] = f(cv.reshape(8, 128, 3).transpose(1, 0, 2))
    m["mod_w"] = f(inp["mod_w"])
    m["mod_b"] = f(inp["mod_b"].reshape(2, 48, 128).transpose(0, 2, 1))
    m["norm_mix"] = f(inp["norm_mix"].reshape(2, 8, 128).transpose(0, 2, 1))
    m["norm_ffn"] = f(inp["norm_ffn"].reshape(2, 8, 128).transpose(0, 2, 1))
    m["norm_final"] = f(inp["norm_final"].reshape(1, D))
    m["ident"] = np.eye(128, dtype=np.float32)
    m["rec_w_in"] = f(inp["rec_w_in"][0])
    m["rec_w_out"] = f(inp["rec_w_out"][0])
    fm = lambda v: np.asarray(v, np.float32).reshape(4, 128).T
    rv = np.zeros((128, 4, 12), np.float32)
    for i, v in enumerate([inp["rwkv_k_k"][0], inp["rwkv_k_a"][0], None, inp["rwkv_a0"][0],
                           inp["rwkv_r_k"][0].reshape(-1), inp["rwkv_ln_w"][0], inp["rwkv_ln_b"][0],
                           inp["rwkv_w0"][0, 0], inp["rwkv_w0"][0, 1]]):
        if v is not None:
            rv[:, :, i] = fm(v)
    m["rw_vec"] = rv
    m["rw_mu"] = f(inp["rwkv_mu"][0].reshape(14, 128).T)
    m["rw_w_up"] = f(inp["rwkv_w_up"][0])
    m["rw_a_up"] = f(inp["rwkv_a_up"][0])
    m["rw_g_up"] = f(inp["rwkv_g_up"][0])
    m["ffn_wg"] = f(inp["ffn_w_gate"][0])
    m["ffn_wu"] = f(inp["ffn_w_up"][0])
    m["ffn_wd"] = f(inp["ffn_w_down"][0])
    m["att_w_in"] = f(inp["att_w_in"][0])
    m["att_w_out"] = f(inp["att_w_out"][0])
    m["att_sink"] = f(inp["att_sink"][0].reshape(1, 16))
    m["moe_router"] = f(inp["moe_router"][0])
    m["moe_router_b"] = f(inp["moe_router_b"][0].reshape(1, 8))
    m["moe_wg"], m["moe_wu"], m["moe_wd"] = _moe_layout(inp)
    m["norm_ffn_row"] = f(inp["norm_ffn"])
    m["hg_lb"] = f(inp["hgrn_lb"].reshape(2, 2, 4, 128).transpose(0, 1, 3, 2))
    m["hg_norm"] = f(inp["hgrn_norm"][0].reshape(1, 128))
    m.update(CONSTS)
    return m


def _consts():
    c = {}
    blk = np.zeros((128, 128), np.float32)
    blk[:64, :64] = 1
    blk[64:, 64:] = 1
    c["blk64"] = blk
    s_ = np.arange(128)[:, None]
    t_ = np.arange(128)[None, :]
    c["masks"] = np.stack([s_ < t_, s_ <= t_, s_ > t_, s_ >= t_], 1).astype(np.float32)
    sm = np.ones((128, W), np.float32)
    for n in range(18):
        sm[:, pcol(n * 128)] = 0.0
    c["scanmask"] = sm
    t = np.arange(TL)
    row = (t // 64).astype(np.float32)
    col = (t % 64).astype(np.float32)
    inv = (10000.0 ** (-np.arange(0, 32, 2, dtype=np.float32) / 32)).astype(np.float32)
    rope = np.zeros((128, 2, TL), np.float32)
    perm = np.zeros((128, 128), np.float32)
    for p in range(128):
        dd = p % 64
        axis, half, jj = dd // 32, (dd % 32) // 16, dd % 16
        ang = ((row if axis == 0 else col) * inv[jj]).astype(np.float32)
        rope[p, 0] = np.cos(ang)
        rope[p, 1] = np.sin(ang) * (-1.0 if half == 0 else 1.0)
        sw = p + 16 if half == 0 else p - 16
        perm[sw, p] = 1.0
    c["rope"] = rope
    c["perm"] = perm
    mc = np.zeros((128, 512), np.float32)
    p_ = np.arange(128)[:, None]
    mc[:, 0:64] = np.tile(np.arange(8) * float(BS), 8)[None, :]
    mc[:, 64:88] = np.arange(24)[None, :] + 0.0
    mc[:, 88:90] = 2 * p_ + np.arange(2)[None, :]
    mc[:, 104:106] = np.arange(2)[None, :] * 128 + p_
    mc[:, 128:160] = np.arange(32)[None, :] * 128 + p_
    rm = np.ones((8, 32), np.float32)
    rm[:, 0] = 0.0
    mc[:, 256:512] = rm.reshape(1, 256)
    c["moe_consts"] = mc
    return c


CONSTS = _consts()


_MOE_CACHE = {}


def _moe_layout(inp):
    key = id(inp["moe_w_gate"])
    if key not in _MOE_CACHE:
        _MOE_CACHE.clear()
        def gu(w):
            w = np.asarray(w[0], np.float32).reshape(8, 8, 128, 2, 1408)
            return np.ascontiguousarray(w.transpose(0, 2, 3, 1, 4)).reshape(2048, 11264)
        wd = np.asarray(inp["moe_w_down"][0], np.float32).reshape(8, 2, 11, 128, 1024)
        wd = np.ascontiguousarray(wd.transpose(0, 1, 3, 2, 4)).reshape(2048, 11264)
        _MOE_CACHE[key] = (gu(inp["moe_w_gate"]), gu(inp["moe_w_up"]), wd)
    return _MOE_CACHE[key]


_NC_CACHE = {}


def kernel(**inp):
    inp = {k: np.asarray(v) for k, v in inp.items()}
    if "nc" not in _NC_CACHE:
        _NC_CACHE["nc"] = build()
    nc = _NC_CACHE["nc"]
    used = nc.G.used_inputs
    in_maps = []
    for c in range(8):
        m = host_prep(inp, c)
        in_maps.append({k: m[k] for k in used})
    res = run_bass_kernel_spmd(nc, in_maps, core_ids=list(range(8)))
    out = np.concatenate([r["out"] for r in res.results], axis=0)
    return out.astype(np.float32)
```
